# Optimizing a Trainium2 kernel written in Bass

```python
import math
import jax, jax.numpy as jnp
from jax import lax
import numpy as np

D_MODEL = 2048
BATCH = 16
SEQ = 2048
DEPTH = 4

MIX_GROUP = D_MODEL // 4
NSA_HEAD_DIM = 64
NSA_HEADS = MIX_GROUP // NSA_HEAD_DIM
NSA_KV_GROUPS = max(1, NSA_HEADS // 4)
NSA_KV = NSA_KV_GROUPS * NSA_HEAD_DIM
CMP_BLOCK = 32
CMP_STRIDE = 16
SLC_BLOCK = 64
SLC_TOP_N = 8
WINDOW = 512
Q_BLOCK = 128
REL_BUCKETS = 32
REL_MAX_DIST = 128
SSD_HEAD_DIM = 64
SSD_HEADS = MIX_GROUP // SSD_HEAD_DIM
SSD_INNER = SSD_HEADS * SSD_HEAD_DIM
SSD_GROUPS = 2
SSD_STATE = 128
SSD_CONV = 4
SSD_XBC = SSD_INNER + 2 * SSD_GROUPS * SSD_STATE
GDN_HEAD_DIM = 128
GDN_HEADS = MIX_GROUP // GDN_HEAD_DIM
GDN_WIDTH = GDN_HEADS * GDN_HEAD_DIM
GDN_CONV = 4
GLA_DV = 128
GLA_HEADS = MIX_GROUP // GLA_DV
GLA_DK = GLA_DV // 2
GLA_KEY = GLA_HEADS * GLA_DK
GLA_VAL = GLA_HEADS * GLA_DV
GLA_GATE_RANK = 16
GLA_GATE_NORM = 16.0
CHUNK = 64
MLP_HIDDEN = 4 * D_MODEL
EPS = 1e-6
NEG_INF = -1e30
FORCE_SCORE = 1e9
IN_SPLITS = (NSA_HEADS * NSA_HEAD_DIM, NSA_KV, NSA_KV, NSA_KV, NSA_KV, NSA_KV, NSA_KV, NSA_HEADS * 3,
             SSD_INNER, SSD_XBC, SSD_HEADS,
             GDN_WIDTH, GDN_WIDTH, GDN_WIDTH, GDN_WIDTH, GDN_HEADS, GDN_HEADS,
             GLA_KEY, GLA_KEY, GLA_VAL, GLA_VAL, GLA_GATE_RANK)
IN_COLS = sum(IN_SPLITS)
MIX_OUT = NSA_HEADS * NSA_HEAD_DIM + SSD_INNER + GDN_WIDTH + GLA_VAL

kernel_name = 'hymba_nsa_ssd_gdn_gla_trunk'


def _split(t, sizes):
    cuts, acc = [], 0
    for s in sizes[:-1]:
        acc += s
        cuts.append(acc)
    return jnp.split(t, cuts, axis=-1)


def _rmsnorm(x, w):
    xf = x.astype(jnp.float32)
    y = xf * lax.rsqrt(jnp.mean(xf * xf, axis=-1, keepdims=True) + EPS)
    return (y * w.astype(jnp.float32)).astype(x.dtype)


def _l2norm(x):
    return x * lax.rsqrt(jnp.sum(x * x, axis=-1, keepdims=True) + EPS)


def _causal_conv(x, w):
    K, C = w.shape
    return lax.conv_general_dilated(x, w.astype(x.dtype)[:, None, :], window_strides=(1,), padding=((K - 1, 0),),
                                    dimension_numbers=('NWC', 'WIO', 'NWC'), feature_group_count=C)


def _masked_softmax(logits, mask):
    p = jax.nn.softmax(jnp.where(mask, logits.astype(jnp.float32), NEG_INF), axis=-1)
    return p * mask


def _t5_bucket(rel):
    n = jnp.maximum(rel, 0)
    exact = REL_BUCKETS // 2
    large = exact + (jnp.log(jnp.maximum(n, 1).astype(jnp.float32) / exact)
                     / math.log(REL_MAX_DIST / exact) * (REL_BUCKETS - exact)).astype(jnp.int32)
    return jnp.where(n < exact, n, jnp.minimum(large, REL_BUCKETS - 1))


def _rel_bias(table, rel, G):
    b = jnp.moveaxis(table[_t5_bucket(rel)], -1, 0)
    return b.reshape(G, table.shape[1] // G, *rel.shape)


def _chunk_state_scan(local, decay):
    def step(state, inp):
        u, a = inp
        return a * state + u, state
    lt = jnp.moveaxis(local, 1, 0)
    dt = jnp.moveaxis(decay, 1, 0)
    _, prev = lax.scan(step, jnp.zeros_like(lt[0]), (lt, dt))
    return jnp.moveaxis(prev, 0, 1)


def _nsa_compress(t, pos, w1, w2):
    B, S, G, hd = t.shape
    r = CMP_BLOCK // CMP_STRIDE
    ch = t.reshape(B, S // CMP_STRIDE, CMP_STRIDE, G, hd)
    n = S // CMP_STRIDE - r + 1
    blk = jnp.concatenate([ch[:, i:i + n] for i in range(r)], axis=2) + pos[:, None, :]
    blk = jnp.swapaxes(blk, 2, 3).reshape(B, n, G, CMP_BLOCK * hd)
    return jax.nn.silu(blk @ w1) @ w2


def _nsa(q, k_cmp, v_cmp, k_slc, v_slc, k_win, v_win, gates, cmp_pos, cmp_w1, cmp_w2, rel_bias):
    B, S, H, hd = q.shape
    G = NSA_KV_GROUPS
    R = H // G
    dt = q.dtype
    scale = hd ** -0.5
    kc = _nsa_compress(k_cmp, cmp_pos[0], cmp_w1[0], cmp_w2[0])
    vc = _nsa_compress(v_cmp, cmp_pos[1], cmp_w1[1], cmp_w2[1])
    nc = kc.shape[1]
    cmp_start = jnp.arange(nc) * CMP_STRIDE
    cmp_end = cmp_start + CMP_BLOCK - 1
    nsb = S // SLC_BLOCK
    top_n = min(SLC_TOP_N, nsb)
    sel_start = jnp.arange(nsb) * SLC_BLOCK
    overlap = ((cmp_start[:, None] <= sel_start[None, :] + SLC_BLOCK - 1)
               & (cmp_end[:, None] >= sel_start[None, :])).astype(jnp.float32)
    ks = k_slc.reshape(B, nsb, SLC_BLOCK, G, hd).transpose(0, 3, 1, 2, 4)
    vs = v_slc.reshape(B, nsb, SLC_BLOCK, G, hd).transpose(0, 3, 1, 2, 4)
    kw = jnp.pad(k_win, ((0, 0), (WINDOW, 0), (0, 0), (0, 0)))
    vw = jnp.pad(v_win, ((0, 0), (WINDOW, 0), (0, 0), (0, 0)))
    kw_len = Q_BLOCK + WINDOW
    win_rel = jnp.arange(Q_BLOCK)[:, None] + WINDOW - jnp.arange(kw_len)[None, :]
    win_mask = (win_rel >= 0) & (win_rel < WINDOW)
    win_bias = _rel_bias(rel_bias, win_rel, G)
    table = rel_bias.reshape(REL_BUCKETS, G, R)
    bi = jnp.arange(B)[:, None, None, None]
    gi = jnp.arange(G)[None, :, None, None]

    def block(args):
        qb, gb, qs = args
        t = qs + jnp.arange(Q_BLOCK)
        qg = (qb * scale).reshape(B, Q_BLOCK, G, R, hd)
        rel_c = t[:, None] - cmp_end[None, :]
        s_c = jnp.einsum('bqgrd,bkgd->bgrqk', qg, kc) + _rel_bias(rel_bias, rel_c, G)
        p_c = _masked_softmax(s_c, rel_c >= 0)
        o_c = jnp.einsum('bgrqk,bkgd->bqgrd', p_c.astype(dt), vc)
        imp = jnp.einsum('bgrqk,kj->bgqj', p_c, overlap)
        blk = jnp.arange(nsb)[None, :]
        cur = (t // SLC_BLOCK)[:, None]
        forced = (blk == 0) | (blk == cur) | (blk == cur - 1)
        imp = jnp.where(forced, FORCE_SCORE, imp)
        imp = jnp.where(blk <= cur, imp, NEG_INF)
        _, idx = lax.top_k(imp, top_n)
        k_sel = ks[bi, gi, idx].reshape(B, G, Q_BLOCK, top_n * SLC_BLOCK, hd)
        v_sel = vs[bi, gi, idx].reshape(B, G, Q_BLOCK, top_n * SLC_BLOCK, hd)
        pos = (idx[..., None] * SLC_BLOCK + jnp.arange(SLC_BLOCK)).reshape(B, G, Q_BLOCK, top_n * SLC_BLOCK)
        rel_s = t[:, None] - pos
        bias_s = jnp.moveaxis(table[_t5_bucket(rel_s), gi], -1, 2)
        s_s = jnp.einsum('bqgrd,bgqkd->bgrqk', qg, k_sel) + bias_s
        p_s = _masked_softmax(s_s, (rel_s >= 0)[:, :, None])
        o_s = jnp.einsum('bgrqk,bgqkd->bqgrd', p_s.astype(dt), v_sel)
        k_w = lax.dynamic_slice_in_dim(kw, qs, kw_len, axis=1)
        v_w = lax.dynamic_slice_in_dim(vw, qs, kw_len, axis=1)
        valid = win_mask & ((qs - WINDOW + jnp.arange(kw_len)) >= 0)[None, :]
        s_w = jnp.einsum('bqgrd,bkgd->bgrqk', qg, k_w) + win_bias
        p_w = _masked_softmax(s_w, valid)
        o_w = jnp.einsum('bgrqk,bkgd->bqgrd', p_w.astype(dt), v_w)
        gb = gb.reshape(B, Q_BLOCK, G, R, 3)
        o = gb[..., 0:1] * o_c + gb[..., 1:2] * o_s + gb[..., 2:3] * o_w
        return o.reshape(B, Q_BLOCK, H * hd)

    nqb = S // Q_BLOCK
    qbs = q.reshape(B, nqb, Q_BLOCK, H, hd).transpose(1, 0, 2, 3, 4)
    gbs = gates.reshape(B, nqb, Q_BLOCK, H, 3).transpose(1, 0, 2, 3, 4)
    starts = jnp.arange(nqb, dtype=jnp.int32) * Q_BLOCK
    out = lax.map(block, (qbs, gbs, starts))
    return out.transpose(1, 0, 2, 3).reshape(B, S, H * hd)


def _ssd(z, xbc, dt_raw, conv_w, conv_b, dt_bias, a_log, d_skip, norm_w):
    B, S, _ = z.shape
    f32 = jnp.float32
    G, R, P, N, L = SSD_GROUPS, SSD_HEADS // SSD_GROUPS, SSD_HEAD_DIM, SSD_STATE, CHUNK
    nc = S // L
    xbc = jax.nn.silu(_causal_conv(xbc, conv_w) + conv_b).astype(f32)
    xs, bm, cm = _split(xbc, (SSD_INNER, G * N, G * N))
    x = xs.reshape(B, nc, L, G, R, P)
    bm = bm.reshape(B, nc, L, G, N)
    cm = cm.reshape(B, nc, L, G, N)
    dt = jax.nn.softplus(dt_raw.astype(f32) + dt_bias.astype(f32)).reshape(B, nc, L, G, R)
    a_cum = jnp.cumsum(dt * (-jnp.exp(a_log.astype(f32))).reshape(G, R), axis=2)
    tril = (jnp.arange(L)[:, None] >= jnp.arange(L)[None, :])[:, :, None, None]
    seg = jnp.exp(jnp.where(tril, a_cum[:, :, :, None] - a_cum[:, :, None, :], -jnp.inf))
    xdt = x * dt[..., None]
    cb = jnp.einsum('bcign,bcjgn->bcijg', cm, bm)
    y = jnp.einsum('bcijg,bcijgr,bcjgrp->bcigrp', cb, seg, xdt)
    states = jnp.einsum('bcjgn,bcjgr,bcjgrp->bcgrpn', bm, jnp.exp(a_cum[:, :, -1:] - a_cum), xdt)
    prev = _chunk_state_scan(states, jnp.exp(a_cum[:, :, -1])[..., None, None])
    y = y + jnp.einsum('bcign,bcgrpn,bcigr->bcigrp', cm, prev, jnp.exp(a_cum))
    y = y + x * d_skip.astype(f32).reshape(G, R, 1)
    y = y.reshape(B, S, SSD_INNER) * jax.nn.silu(z.astype(f32))
    y = _rmsnorm(y.reshape(B, S, G, SSD_INNER // G), norm_w.reshape(G, SSD_INNER // G))
    return y.reshape(B, S, SSD_INNER).astype(z.dtype)


def _gdn(q, k, v, z, beta_raw, a_raw, conv_w, dt_bias, a_log, norm_w):
    B, S, _ = q.shape
    f32 = jnp.float32
    H, Dh, L = GDN_HEADS, GDN_HEAD_DIM, CHUNK
    nc = S // L
    qkv = jax.nn.silu(_causal_conv(jnp.concatenate([q, k, v], axis=-1), conv_w)).astype(f32)
    qq, kk, vv = _split(qkv, (GDN_WIDTH, GDN_WIDTH, GDN_WIDTH))
    qq = _l2norm(qq.reshape(B, S, H, Dh)) * Dh ** -0.5
    kk = _l2norm(kk.reshape(B, S, H, Dh))
    vv = vv.reshape(B, S, H, Dh)
    beta = jax.nn.sigmoid(beta_raw.astype(f32))
    g = -jnp.exp(a_log.astype(f32)) * jax.nn.softplus(a_raw.astype(f32) + dt_bias.astype(f32))
    chunk = lambda t: jnp.moveaxis(t.reshape(B, nc, L, H, *t.shape[3:]), 3, 2)
    qc, kc, vc, bc, gc = chunk(qq), chunk(kk), chunk(vv), chunk(beta), chunk(g)
    gcum = jnp.cumsum(gc, axis=-1)
    i = jnp.arange(L)
    tril = i[:, None] >= i[None, :]
    strict = i[:, None] > i[None, :]
    decay = jnp.exp(jnp.where(tril, gcum[..., :, None] - gcum[..., None, :], -jnp.inf))
    kb = kc * bc[..., None]
    a_mat = jnp.where(strict, jnp.einsum('bnhid,bnhjd->bnhij', kb, kc) * decay, 0.0)
    rhs = jnp.concatenate([vc * bc[..., None], kb * jnp.exp(gcum)[..., None]], axis=-1)
    sol = lax.linalg.triangular_solve(a_mat + jnp.eye(L, dtype=f32), rhs, left_side=True, lower=True,
                                      unit_diagonal=True)
    u, w = sol[..., :Dh], sol[..., Dh:]
    aqk = jnp.einsum('bnhid,bnhjd->bnhij', qc, kc) * decay
    q_dec = qc * jnp.exp(gcum)[..., None]
    k_end = kc * jnp.exp(gcum[..., -1:] - gcum)[..., None]
    d_last = jnp.exp(gcum[..., -1])

    def step(state, inp):
        qd, ke, uu, ww, aa, dl = inp
        v_new = uu - jnp.einsum('bhid,bhde->bhie', ww, state)
        o = jnp.einsum('bhid,bhde->bhie', qd, state) + jnp.einsum('bhij,bhje->bhie', aa, v_new)
        state = state * dl[..., None, None] + jnp.einsum('bhjd,bhje->bhde', ke, v_new)
        return state, o

    xs = tuple(jnp.moveaxis(t, 1, 0) for t in (q_dec, k_end, u, w, aqk, d_last))
    _, o = lax.scan(step, jnp.zeros((B, H, Dh, Dh), f32), xs)
    o = jnp.moveaxis(jnp.moveaxis(o, 0, 1), 3, 2).reshape(B, S, H, Dh)
    o = _rmsnorm(o, norm_w) * jax.nn.silu(z.astype(f32).reshape(B, S, H, Dh))
    return o.reshape(B, S, GDN_WIDTH).astype(q.dtype)


def _gla(q, k, v, g_out, g_lr, gate_w2, gate_b, norm_w):
    B, S, _ = q.shape
    f32 = jnp.float32
    H, Dk, Dv, L = GLA_HEADS, GLA_DK, GLA_DV, CHUNK
    nc = S // L
    gk = jax.nn.log_sigmoid((g_lr @ gate_w2 + gate_b).astype(f32)) / GLA_GATE_NORM
    chunk = lambda t, d: jnp.moveaxis(t.astype(f32).reshape(B, nc, L, H, d), 3, 2)
    qc = chunk(q, Dk) * Dk ** -0.5
    kc = chunk(k, Dk)
    vc = chunk(v, Dv)
    bcum = jnp.cumsum(chunk(gk, Dk), axis=3)
    q_dec = qc * jnp.exp(bcum)
    k_inv = kc * jnp.exp(-bcum)
    tril = jnp.arange(L)[:, None] >= jnp.arange(L)[None, :]
    attn = jnp.where(tril, jnp.einsum('bnhid,bnhjd->bnhij', q_dec, k_inv), 0.0)
    o = jnp.einsum('bnhij,bnhje->bnhie', attn, vc)
    k_end = kc * jnp.exp(bcum[..., -1:, :] - bcum)
    local = jnp.einsum('bnhjd,bnhje->bnhde', k_end, vc)
    prev = _chunk_state_scan(local, jnp.exp(bcum[..., -1, :])[..., None])
    o = o + jnp.einsum('bnhid,bnhde->bnhie', q_dec, prev)
    o = jnp.moveaxis(o, 2, 3).reshape(B, S, H, Dv)
    o = _rmsnorm(o, norm_w) * jax.nn.silu(g_out.astype(f32).reshape(B, S, H, Dv))
    return o.reshape(B, S, GLA_VAL).astype(q.dtype)


def _token_mixers(h, w_in, w_out, rel_bias, cmp_pos, cmp_w1, cmp_w2,
                  ssd_conv_w, ssd_conv_b, ssd_dt_bias, ssd_a_log, ssd_d, ssd_norm_w,
                  gdn_conv_w, gdn_dt_bias, gdn_a_log, gdn_norm_w, gla_gate_w2, gla_gate_b, gla_norm_w):
    B, S, _ = h.shape
    (nq, nkc, nvc, nks, nvs, nkw, nvw, ngate,
     sz, sxbc, sdt,
     gq, gk, gv, gz, gbeta, ga,
     lq, lk, lv, lg, llr) = _split(h @ w_in, IN_SPLITS)
    kv = lambda t: t.reshape(B, S, NSA_KV_GROUPS, NSA_HEAD_DIM)
    y_nsa = _nsa(nq.reshape(B, S, NSA_HEADS, NSA_HEAD_DIM), kv(nkc), kv(nvc), kv(nks), kv(nvs), kv(nkw), kv(nvw),
                 jax.nn.sigmoid(ngate).reshape(B, S, NSA_HEADS, 3), cmp_pos, cmp_w1, cmp_w2, rel_bias)
    y_ssd = _ssd(sz, sxbc, sdt, ssd_conv_w, ssd_conv_b, ssd_dt_bias, ssd_a_log, ssd_d, ssd_norm_w)
    y_gdn = _gdn(gq, gk, gv, gz, gbeta, ga, gdn_conv_w, gdn_dt_bias, gdn_a_log, gdn_norm_w)
    y_gla = _gla(lq, lk, lv, lg, llr, gla_gate_w2, gla_gate_b, gla_norm_w)
    return jnp.concatenate([y_nsa, y_ssd, y_gdn, y_gla], axis=-1) @ w_out


def setup_inputs(seed: int = 0) -> dict:
    key = jax.random.key(seed)
    ks = iter(jax.random.split(key, 40))
    f32 = jnp.float32
    D = D_MODEL

    def nrm(shape, s):
        return jax.random.normal(next(ks), shape, f32) * s

    def gain(shape):
        return 1.0 + nrm(shape, 0.02)

    def dt_bias(shape):
        dt = jnp.exp(jax.random.uniform(next(ks), shape, f32, math.log(1e-3), math.log(1e-1)))
        return dt + jnp.log(-jnp.expm1(-dt))

    def a_log(shape):
        return jnp.log(jax.random.uniform(next(ks), shape, f32, 1.0, 16.0))

    return {
        'x': nrm((BATCH, SEQ, D), 1.0),
        'c': nrm((BATCH, D), 1.0),
        'rel_bias': nrm((REL_BUCKETS, NSA_HEADS), 0.2),
        'norm1_w': gain((DEPTH, D)),
        'norm2_w': gain((DEPTH, D)),
        'ada_w': nrm((DEPTH, D, 6 * D), 0.3 * D ** -0.5),
        'ada_b': nrm((DEPTH, 6 * D), 0.01),
        'w_in': nrm((DEPTH, D, IN_COLS), D ** -0.5),
        'w_out': nrm((DEPTH, MIX_OUT, D), MIX_OUT ** -0.5),
        'nsa_cmp_pos': nrm((DEPTH, 2, CMP_BLOCK, NSA_HEAD_DIM), 0.1),
        'nsa_cmp_w1': nrm((DEPTH, 2, CMP_BLOCK * NSA_HEAD_DIM, NSA_HEAD_DIM), (CMP_BLOCK * NSA_HEAD_DIM) ** -0.5),
        'nsa_cmp_w2': nrm((DEPTH, 2, NSA_HEAD_DIM, NSA_HEAD_DIM), NSA_HEAD_DIM ** -0.5),
        'ssd_conv_w': nrm((DEPTH, SSD_CONV, SSD_XBC), SSD_CONV ** -0.5),
        'ssd_conv_b': nrm((DEPTH, SSD_XBC), 0.01),
        'ssd_dt_bias': dt_bias((DEPTH, SSD_HEADS)),
        'ssd_a_log': a_log((DEPTH, SSD_HEADS)),
        'ssd_d': gain((DEPTH, SSD_HEADS)),
        'ssd_norm_w': gain((DEPTH, SSD_INNER)),
        'gdn_conv_w': nrm((DEPTH, GDN_CONV, 3 * GDN_WIDTH), GDN_CONV ** -0.5),
        'gdn_dt_bias': dt_bias((DEPTH, GDN_HEADS)),
        'gdn_a_log': a_log((DEPTH, GDN_HEADS)),
        'gdn_norm_w': gain((DEPTH, GDN_HEAD_DIM)),
        'gla_gate_w2': nrm((DEPTH, GLA_GATE_RANK, GLA_KEY), GLA_GATE_RANK ** -0.5),
        'gla_gate_b': nrm((DEPTH, GLA_KEY), 0.01),
        'gla_norm_w': gain((DEPTH, GLA_DV)),
        'mlp_w1': nrm((DEPTH, D, MLP_HIDDEN), D ** -0.5),
        'mlp_w2': nrm((DEPTH, MLP_HIDDEN, D), MLP_HIDDEN ** -0.5),
        'final_norm_w': gain((D,)),
    }


def reference(x, c, rel_bias, norm1_w, norm2_w, ada_w, ada_b, w_in, w_out, nsa_cmp_pos, nsa_cmp_w1, nsa_cmp_w2,
              ssd_conv_w, ssd_conv_b, ssd_dt_bias, ssd_a_log, ssd_d, ssd_norm_w,
              gdn_conv_w, gdn_dt_bias, gdn_a_log, gdn_norm_w, gla_gate_w2, gla_gate_b, gla_norm_w,
              mlp_w1, mlp_w2, final_norm_w):
    B = x.shape[0]
    c_act = jax.nn.silu(c)
    for l in range(DEPTH):
        mod = (c_act @ ada_w[l] + ada_b[l]).reshape(B, 6, 1, D_MODEL)
        sh1, sc1, g1, sh2, sc2, g2 = (mod[:, i] for i in range(6))
        h = _rmsnorm(x, norm1_w[l]) * (1.0 + sc1) + sh1
        y = _token_mixers(h, w_in[l], w_out[l], rel_bias, nsa_cmp_pos[l], nsa_cmp_w1[l], nsa_cmp_w2[l],
                          ssd_conv_w[l], ssd_conv_b[l], ssd_dt_bias[l], ssd_a_log[l], ssd_d[l], ssd_norm_w[l],
                          gdn_conv_w[l], gdn_dt_bias[l], gdn_a_log[l], gdn_norm_w[l],
                          gla_gate_w2[l], gla_gate_b[l], gla_norm_w[l])
        x = x + g1 * y
        h = _rmsnorm(x, norm2_w[l]) * (1.0 + sc2) + sh2
        x = x + g2 * (jnp.square(jax.nn.relu(h @ mlp_w1[l])) @ mlp_w2[l])
    return _rmsnorm(x, final_norm_w)
```

```python
import math
from contextlib import ExitStack, contextmanager
import numpy as np
import concourse.bass as bass
import concourse.mybir as mybir
from concourse.bass_utils import run_bass_kernel_spmd

F32 = mybir.dt.float32
BF16 = mybir.dt.bfloat16
AF = mybir.ActivationFunctionType
ALU = mybir.AluOpType
AX = mybir.AxisListType

EPOCH = 12000
SAME_ENGINE_SYNC = True

D_MODEL = 2048
SEQ = 2048
DEPTH = 4
NCORES = 8
BPC = 2
IN_COLS = 6456
EPS = 1e-6
NT = SEQ // 128


class StopStage(Exception):
    pass


DBG = {"stop": 99}


def dbg(k):
    if DBG["stop"] <= k:
        DBG["P"].dead = True


class Buf:
    __slots__ = ("t", "name", "lw", "rd", "excl")

    def __init__(self, t, name="", excl=False):
        self.t = t
        self.name = name
        self.lw = None
        self.rd = {}
        self.excl = excl

    def __getitem__(self, k):
        return self.t[k]


class Prog:
    ENGS = ("pe", "act", "dve", "pool", "sp")

    def __init__(self, nc, stack):
        self.nc = nc
        self.stack = stack
        self.cnt = {e: 0 for e in ("pe", "act", "dve", "pool")}
        self.sems = {}
        self.seen = {e: {} for e in self.ENGS}
        self.dslots = {}
        self.dnext = {}
        self.E = dict(pe=nc.tensor, act=nc.scalar, dve=nc.vector, pool=nc.gpsimd, sp=nc.sync)
        self.ninstr = 0
        self.base_stack = stack
        self.uid = 0
        self.dead = False
        DBG["P"] = self

    def sem(self, name):
        return self.base_stack.enter_context(self.nc.semaphore(name))

    def sbuf(self, name, shape, dt=F32):
        self.uid += 1
        t = self.stack.enter_context(self.nc.sbuf_tensor(f"{name}_{self.uid}", list(shape), dt))
        return Buf(t, name)

    def psum(self, name, shape, dt=F32):
        self.uid += 1
        t = self.stack.enter_context(self.nc.psum_tensor(f"{name}_{self.uid}", list(shape), dt))
        return Buf(t, name, excl=True)

    def dram(self, name, shape, dt=F32, kind="Internal"):
        t = self.nc.dram_tensor(name, list(shape), dt, kind=kind)
        return Buf(t.ap(), name)

    def _esem(self, eng, idx):
        ep = idx // EPOCH
        k = (eng, ep)
        if k not in self.sems:
            self.sems[k] = self.sem(f"s_{eng}_{ep}")
        return self.sems[k], (idx % EPOCH) + 1

    def _wait(self, eng, ev):
        q, idx = ev
        if isinstance(q, str):
            if q == eng and (eng == "pe" or not SAME_ENGINE_SYNC):
                return
            if self.seen[eng].get(q, -1) >= idx:
                return
            self.seen[eng][q] = idx
            s, v = self._esem(q, idx)
        else:
            if self.seen[eng].get(q, -1) >= idx:
                return
            self.seen[eng][q] = idx
            s = self.dslots[q[0]][q[1]][0]
            v = idx
        self.E[eng].wait_ge(s, v)

    def _deps(self, eng, reads, writes):
        for b in reads:
            if b.lw is not None:
                self._wait(eng, b.lw)
            if b.excl:
                for q, i in list(b.rd.items()):
                    if q != eng:
                        self._wait(eng, (q, i))
        for b in writes:
            if b.lw is not None:
                self._wait(eng, b.lw)
            for q, i in list(b.rd.items()):
                self._wait(eng, (q, i))

    def _mark(self, ev, reads, writes):
        q, idx = ev
        for b in reads:
            if b.rd.get(q, -1) < idx:
                b.rd[q] = idx
        for b in writes:
            b.lw = ev
            b.rd = {}

    def op(self, eng, fn, reads=(), writes=()):
        if self.dead:
            return
        self._deps(eng, reads, writes)
        idx = self.cnt[eng]
        self.cnt[eng] += 1
        s, v = self._esem(eng, idx)
        fn(self.E[eng]).then_inc(s, 1)
        self._mark((eng, idx), reads, writes)
        self.ninstr += 1

    def pe(self, fn, reads=(), writes=()):
        self.op("pe", fn, reads, writes)

    def act(self, fn, reads=(), writes=()):
        self.op("act", fn, reads, writes)

    def dve(self, fn, reads=(), writes=()):
        self.op("dve", fn, reads, writes)

    def pool(self, fn, reads=(), writes=()):
        self.op("pool", fn, reads, writes)

    def dma(self, eng, out_ap, in_ap, reads=(), writes=(), nslots=8, **kw):
        if self.dead:
            return
        if eng not in self.dslots:
            self.dslots[eng] = [[self.sem(f"d_{eng}_{i}"), 0] for i in range(nslots)]
            self.dnext[eng] = 0
        si = self.dnext[eng]
        self.dnext[eng] = (si + 1) % len(self.dslots[eng])
        slot = self.dslots[eng][si]
        q = (eng, si)
        if slot[1] > 0:
            self._wait(eng, (q, slot[1]))
        self._deps(eng, reads, writes)
        slot[1] += 16
        self.E[eng].dma_start(out=out_ap, in_=in_ap, **kw).then_inc(slot[0], 16)
        self._mark((q, slot[1]), reads, writes)
        self.ninstr += 1

    def all_events(self):
        evs = []
        for e in ("pe", "act", "dve", "pool"):
            if self.cnt[e] > 0:
                evs.append((e, self.cnt[e] - 1))
        for eng, slots in self.dslots.items():
            for si, (s, c) in enumerate(slots):
                if c > 0:
                    evs.append(((eng, si), c))
        return evs

    def barrier(self, engs=None):
        evs = self.all_events()
        for e in (engs or self.ENGS):
            for ev in evs:
                if ev[0] == e and e == "pe":
                    continue
                self._wait(e, ev)

    @contextmanager
    def scope(self):
        old = self.stack
        try:
            with ExitStack() as st:
                self.stack = st
                try:
                    yield
                finally:
                    self.barrier()
        finally:
            self.stack = old

    def finish(self):
        self.barrier(["sp"])


def make_consts():
    p = np.arange(128)[:, None]
    f = np.arange(128)[None, :]
    same = (p // 64) == (f // 64)
    cols = {}
    parts = []

    def add(name, arr):
        cols[name] = (sum(a.shape[1] for a in parts), arr.shape[1])
        parts.append(arr.astype(np.float32))

    add("ident", (p == f))
    add("tri01", same & (p <= f))
    add("stri01", same & (p < f))
    add("su01", same & (p > f))
    add("sl01", same & (p >= f))
    add("bones", same)
    add("ones", np.ones((128, 128)))
    add("chunkind", (p // 64) == np.arange(2)[None, :])
    add("tri16", (same & (p <= f)) * (-1.0 / 16.0))
    add("bones16", same * (-1.0 / 16.0))
    add("chunkind16", ((p // 64) == np.arange(2)[None, :]) * (-1.0 / 16.0))
    return np.concatenate(parts, axis=1), cols


CST_NP, CST_COLS = make_consts()


class Consts:
    def __init__(self, P, cst_dram):
        self.P = P
        n = CST_NP.shape[1]
        self.f = P.sbuf("cst_f", [128, n])
        P.dma("sp", self.f[:], cst_dram.t[:, :], reads=[cst_dram], writes=[self.f])
        self.identb = P.sbuf("identb", [128, 128], BF16)
        P.dve(lambda e: e.tensor_copy(out=self.identb[:], in_=self.c("ident")), reads=[self.f], writes=[self.identb])

    def c(self, name, rows=128):
        o, w = CST_COLS[name]
        return self.f[0:rows, o:o + w]


def norm_gate(P, src_ap, src_bufs, z_ap, z_bufs, nw_ap, nw_bufs, G, gsz, out, tmp, gate_first):
    a, b, ss, sg = tmp["a"], tmp["b"], tmp["ss"], tmp["sg"]
    n = G * gsz
    P.act(lambda e: e.activation(out=sg[:, 0:n], in_=z_ap, func=AF.Silu), reads=z_bufs, writes=[sg])
    if gate_first:
        P.dve(lambda e: e.tensor_tensor(out=a[:, 0:n], in0=src_ap, in1=sg[:, 0:n], op=ALU.mult),
              reads=list(src_bufs) + [sg], writes=[a])
    else:
        P.dve(lambda e: e.tensor_copy(out=a[:, 0:n], in_=src_ap), reads=list(src_bufs), writes=[a])
    P.act(lambda e: e.activation(out=b[:, 0:n], in_=a[:, 0:n], func=AF.Square), reads=[a], writes=[b])
    P.dve(lambda e: e.tensor_reduce(out=ss[:, 0:G], in_=b[:, 0:n].rearrange("p (g e) -> p g e", g=G), axis=AX.X, op=ALU.add),
          reads=[b], writes=[ss])
    P.act(lambda e: e.activation(out=ss[:, 0:G], in_=ss[:, 0:G], func=AF.Sqrt, scale=1.0 / gsz, bias=tmp["eps"][:, 0:1]),
          reads=[ss, tmp["eps"]], writes=[ss])
    P.dve(lambda e: e.reciprocal(out=ss[:, 0:G], in_=ss[:, 0:G]), reads=[ss], writes=[ss])
    P.dve(lambda e: e.tensor_tensor(out=b[:, 0:n].rearrange("p (g e) -> p g e", g=G),
                                    in0=a[:, 0:n].rearrange("p (g e) -> p g e", g=G),
                                    in1=ss[:, 0:G].unsqueeze(2).to_broadcast([128, G, gsz]), op=ALU.mult),
          reads=[a, ss], writes=[b])
    if gate_first:
        P.pool(lambda e: e.tensor_tensor(out=out[:, 0:n].rearrange("p (g e) -> p g e", g=G),
                                         in0=b[:, 0:n].rearrange("p (g e) -> p g e", g=G), in1=nw_ap, op=ALU.mult),
               reads=[b] + list(nw_bufs), writes=[out])
    else:
        P.pool(lambda e: e.tensor_tensor(out=a[:, 0:n].rearrange("p (g e) -> p g e", g=G),
                                         in0=b[:, 0:n].rearrange("p (g e) -> p g e", g=G), in1=nw_ap, op=ALU.mult),
               reads=[b] + list(nw_bufs), writes=[a])
        P.pool(lambda e: e.tensor_tensor(out=out[:, 0:n], in0=a[:, 0:n], in1=sg[:, 0:n], op=ALU.mult),
               reads=[a, sg], writes=[out])


def ng_tmp(P):
    t = dict(a=P.sbuf("ng_a", [128, 512]), b=P.sbuf("ng_b", [128, 512]), ss=P.sbuf("ng_ss", [128, 8]),
             sg=P.sbuf("ng_sg", [128, 512]), eps=P.sbuf("ng_eps", [128, 1]), one=P.sbuf("ng_one", [128, 1]))
    P.pool(lambda e: e.memset(t["eps"][:], EPS), writes=[t["eps"]])
    P.pool(lambda e: e.memset(t["one"][:], 1.0), writes=[t["one"]])
    return t


PT_NQ, PT_NKC, PT_NVC, PT_NKS, PT_NKW = 0, 512, 640, 768, 896
PT_SXBC = 1024
PT_GQKV = 2048
PT_LQ, PT_LK, PT_LLR = 3584, 3840, 4096
PT_ROWS = 4112
PN_NVS, PN_NVW, PN_NGATE, PN_SZ, PN_SDT = 0, 128, 256, 280, 792
PN_GZ, PN_GBETA, PN_GA, PN_LK, PN_LV, PN_LG = 800, 1312, 1316, 1320, 1576, 2088
PN_COLS = 2600
PT_GROUPS = ([(0 + 128 * i, 128, PT_NQ + 128 * i) for i in range(4)] +
             [(512, 128, PT_NKC), (640, 128, PT_NVC), (768, 128, PT_NKS), (1024, 128, PT_NKW)] +
             [(1816 + 128 * i, 128, PT_SXBC + 128 * i) for i in range(8)] +
             [(2848 + 128 * i, 128, PT_GQKV + 128 * i) for i in range(12)] +
             [(4904 + 128 * i, 128, PT_LQ + 128 * i) for i in range(2)] +
             [(5160 + 128 * i, 128, PT_LK + 128 * i) for i in range(2)] +
             [(6440, 16, PT_LLR)])
PN_GROUPS = [(896, 128, PN_NVS), (1152, 512, PN_NVW), (1664, 152, PN_NVW + 512), (2840, 8, PN_SDT),
             (4384, 512, PN_GZ), (4896, 8, PN_GBETA), (5160, 256, PN_LK), (5416, 512, PN_LV), (5928, 512, PN_LG)]


def stage_gla(P, C, projT, projN, ymix, prm, l):
    with P.scope():
        w2 = P.sbuf("gla_w2", [16, 256])
        gb = P.sbuf("gla_gb", [1, 256])
        nwb = P.sbuf("gla_nwb", [128, 128])
        P.dma("sp", w2[:], prm["gla_gate_w2"].t[l], reads=[prm["gla_gate_w2"]], writes=[w2])
        P.dma("sp", gb[:], prm["gla_gate_b"].t[l:l + 1, :], reads=[prm["gla_gate_b"]], writes=[gb])
        P.dma("sp", nwb[:], prm["gla_norm_w"].t[l:l + 1, :].partition_broadcast(128), reads=[prm["gla_norm_w"]], writes=[nwb])
        S = P.sbuf("gla_S", [64, 4, 128])
        Sb = [P.sbuf(f"gla_Sb{i}", [64, 4, 128], BF16) for i in range(2)]
        P.dve(lambda e: e.memset(S[:], 0.0), writes=[S])
        P.dve(lambda e: e.memset(Sb[0][:], 0.0), writes=[Sb[0]])
        tmp = ng_tmp(P)
        NB = 2
        qT = [P.sbuf(f"gla_qT{i}", [64, 4, 128]) for i in range(NB)]
        kT = [P.sbuf(f"gla_kT{i}", [64, 4, 128]) for i in range(NB)]
        lrT = [P.sbuf(f"gla_lrT{i}", [16, 128]) for i in range(NB)]
        tokN = [P.sbuf(f"gla_tokN{i}", [128, 1280]) for i in range(NB)]
        lsp = P.sbuf("gla_lsp", [128, 256])
        ex = P.sbuf("gla_ex", [128, 256])
        kend = P.sbuf("gla_kend", [128, 256], BF16)
        vb = P.sbuf("gla_vb", [128, 512], BF16)
        ebT = P.sbuf("gla_ebT", [64, 512])
        qdT = P.sbuf("gla_qdT", [64, 4, 128], BF16)
        kiT = P.sbuf("gla_kiT", [64, 4, 128], BF16)
        dec = P.sbuf("gla_dec", [64, 8])
        AT = P.sbuf("gla_AT", [128, 4, 128], BF16)
        yo = [P.sbuf(f"gla_yo{i}", [128, 512]) for i in range(2)]
        ps_gk = P.psum("gla_ps_gk", [128, 512])
        ps_bl = P.psum("gla_ps_bl", [128, 512])
        ps_bT = P.psum("gla_ps_bT", [64, 512])
        ps_blT = P.psum("gla_ps_blT", [64, 8])
        ps_at = P.psum("gla_ps_at", [128, 512])
        ps_o = P.psum("gla_ps_o", [128, 512])
        ps_loc = [P.psum(f"gla_ps_loc{i}", [64, 512]) for i in range(2)]
        cf = [C.f]

        def load(t):
            i = t % NB
            tok = slice(t * 128, (t + 1) * 128)
            P.dma("sp", qT[i][:], projT.t[PT_LQ:PT_LQ + 256, tok].rearrange("(h d) t -> d h t", d=64), reads=[projT], writes=[qT[i]])
            P.dma("sp", kT[i][:], projT.t[PT_LK:PT_LK + 256, tok].rearrange("(h d) t -> d h t", d=64), reads=[projT], writes=[kT[i]])
            P.dma("sp", lrT[i][:], projT.t[PT_LLR:PT_LLR + 16, tok], reads=[projT], writes=[lrT[i]])
            P.dma("sp", tokN[i][:], projN.t[tok, PN_LK:PN_LK + 1280], reads=[projN], writes=[tokN[i]])

        load(0)
        for t in range(NT):
            if t + 1 < NT:
                load(t + 1)
            i = t % NB
            tok = slice(t * 128, (t + 1) * 128)
            kN = tokN[i][:, 0:256]
            vN = tokN[i][:, 256:768]
            gN = tokN[i][:, 768:1280]
            P.pe(lambda e: e.matmul(ps_gk[:, 0:256], lhsT=lrT[i][:], rhs=w2[:], start=True, stop=False), reads=[lrT[i], w2], writes=[ps_gk])
            P.pe(lambda e: e.matmul(ps_gk[:, 0:256], lhsT=C.c("ones", 1), rhs=gb[:], start=False, stop=True), reads=[gb] + cf, writes=[ps_gk])
            P.act(lambda e: e.activation(out=ex[:], in_=ps_gk[:, 0:256], func=AF.Exp, scale=-1.0), reads=[ps_gk], writes=[ex])
            P.act(lambda e: e.activation(out=lsp[:], in_=ex[:], func=AF.Ln, bias=tmp["one"][:, 0:1]), reads=[ex, tmp["one"]], writes=[lsp])
            P.pe(lambda e: e.matmul(ps_gk[:, 256:512], lhsT=C.c("tri16"), rhs=lsp[:], start=True, stop=True), reads=[lsp] + cf, writes=[ps_gk])
            P.pe(lambda e: e.matmul(ps_bl[:, 0:256], lhsT=C.c("bones16"), rhs=lsp[:], start=True, stop=True), reads=[lsp] + cf, writes=[ps_bl])
            for h in range(4):
                P.pe(lambda e, h=h: e.matmul(ps_bT[:, h * 128:(h + 1) * 128], lhsT=lsp[:, h * 64:(h + 1) * 64], rhs=C.c("tri16"), start=True, stop=True),
                     reads=[lsp] + cf, writes=[ps_bT])
            for h in range(4):
                P.pe(lambda e, h=h: e.matmul(ps_blT[:, h * 2:(h + 1) * 2], lhsT=lsp[:, h * 64:(h + 1) * 64], rhs=C.c("chunkind16"), start=True, stop=True),
                     reads=[lsp] + cf, writes=[ps_blT])
            P.dve(lambda e: e.tensor_copy(out=ex[:], in_=ps_gk[:, 256:512]), reads=[ps_gk], writes=[ex])
            P.dve(lambda e: e.tensor_tensor(out=ex[:], in0=ps_bl[:, 0:256], in1=ex[:], op=ALU.subtract), reads=[ps_bl, ex], writes=[ex])
            P.act(lambda e: e.activation(out=ex[:], in_=ex[:], func=AF.Exp), reads=[ex], writes=[ex])
            P.dve(lambda e: e.tensor_tensor(out=kend[:], in0=kN, in1=ex[:], op=ALU.mult), reads=[tokN[i], ex], writes=[kend])
            P.pool(lambda e: e.tensor_copy(out=vb[:], in_=vN), reads=[tokN[i]], writes=[vb])
            P.act(lambda e: e.activation(out=ebT[:], in_=ps_bT[:], func=AF.Exp), reads=[ps_bT], writes=[ebT])
            P.dve(lambda e: e.scalar_tensor_tensor(out=qdT[:].rearrange("d h t -> d (h t)"), in0=qT[i][:].rearrange("d h t -> d (h t)"), scalar=0.125,
                                                   in1=ebT[:], op0=ALU.mult, op1=ALU.mult), reads=[qT[i], ebT], writes=[qdT])
            P.act(lambda e: e.activation(out=ebT[:], in_=ps_bT[:], func=AF.Exp, scale=-1.0), reads=[ps_bT], writes=[ebT])
            P.dve(lambda e: e.tensor_tensor(out=kiT[:].rearrange("d h t -> d (h t)"), in0=kT[i][:].rearrange("d h t -> d (h t)"), in1=ebT[:], op=ALU.mult),
                  reads=[kT[i], ebT], writes=[kiT])
            P.act(lambda e: e.activation(out=dec[:], in_=ps_blT[:], func=AF.Exp), reads=[ps_blT], writes=[dec])
            for h in range(4):
                P.pe(lambda e, h=h: e.matmul(ps_at[:, h * 128:(h + 1) * 128], lhsT=kiT[:, h, :], rhs=qdT[:, h, :], start=True, stop=True),
                     reads=[kiT, qdT], writes=[ps_at])
            P.dve(lambda e: e.tensor_tensor(out=AT[:], in0=ps_at[:].rearrange("p (h t) -> p h t", h=4),
                                            in1=C.c("tri01").unsqueeze(1).to_broadcast([128, 4, 128]), op=ALU.mult), reads=[ps_at] + cf, writes=[AT])
            for c in range(2):
                rows = slice(c * 64, (c + 1) * 64)
                for h in range(4):
                    P.pe(lambda e, h=h, rows=rows, c=c: e.matmul(ps_loc[c][:, h * 128:(h + 1) * 128], lhsT=kend[rows, h * 64:(h + 1) * 64],
                                                                 rhs=vb[rows, h * 128:(h + 1) * 128], start=True, stop=True),
                         reads=[kend, vb], writes=[ps_loc[c]])
            for c in range(2):
                P.dve(lambda e, c=c: e.tensor_tensor(out=S[:], in0=S[:], in1=dec[:].rearrange("d (h c) -> d h c", c=2)[:, :, c:c + 1].to_broadcast([64, 4, 128]),
                                                     op=ALU.mult), reads=[S, dec], writes=[S])
                P.dve(lambda e, c=c: e.tensor_tensor(out=S[:].rearrange("d h e -> d (h e)"), in0=S[:].rearrange("d h e -> d (h e)"), in1=ps_loc[c][:], op=ALU.add),
                      reads=[S, ps_loc[c]], writes=[S])
                if c == 0:
                    P.act(lambda e: e.copy(out=Sb[1][:], in_=S[:]), reads=[S], writes=[Sb[1]])
            for h in range(4):
                cols = slice(h * 128, (h + 1) * 128)
                P.pe(lambda e, h=h, cols=cols: e.matmul(ps_o[:, cols], lhsT=AT[:, h, :], rhs=vb[:, cols], start=True, stop=False),
                     reads=[AT, vb], writes=[ps_o])
                for c in range(2):
                    rows = slice(c * 64, (c + 1) * 64)
                    P.pe(lambda e, h=h, cols=cols, rows=rows, c=c: e.matmul(ps_o[rows, cols], lhsT=qdT[:, h, rows], rhs=Sb[c][:, h, :], start=False, stop=(c == 1)),
                         reads=[qdT, Sb[c]], writes=[ps_o])
            P.act(lambda e: e.copy(out=Sb[0][:], in_=S[:]), reads=[S], writes=[Sb[0]])
            y = yo[t % 2]
            norm_gate(P, ps_o[:], [ps_o], gN, [tokN[i]], nwb[:].unsqueeze(1).to_broadcast([128, 4, 128]), [nwb], 4, 128, y, tmp, False)
            P.dma("sp", ymix.t[tok, 1536:2048], y[:], reads=[y], writes=[ymix])


def host_param(name, arr):
    a = np.asarray(arr, np.float32)
    if name in ("ssd_conv_w", "gdn_conv_w"):
        L, K, CH = a.shape
        a = a.reshape(L, K, CH // 128, 128).transpose(0, 3, 2, 1)
    elif name in ("norm1_w", "norm2_w"):
        L = a.shape[0]
        a = a.reshape(L, 16, 128).transpose(2, 0, 1)
    elif name == "final_norm_w":
        a = a.reshape(1, -1)
    elif name == "nsa_cmp_pos":
        a = a.transpose(0, 1, 3, 2)
    elif name == "ssd_conv_b":
        L, CH = a.shape
        a = a.reshape(L, CH // 128, 128).transpose(0, 2, 1)
    return np.ascontiguousarray(a)


def bc(ap, shape):
    return ap.to_broadcast(list(shape))


def causal_conv_silu(P, projT, row0, ntiles, cw, cb, dst, dst_off, name, bias=True):
    xpad = [P.sbuf(f"{name}_xpad{i}", [128, SEQ + 3]) for i in range(2)]
    acc = [P.sbuf(f"{name}_acc{i}", [128, SEQ]) for i in range(2)]
    for i in range(2):
        P.pool(lambda e, i=i: e.memset(xpad[i][:, 0:3], 0.0), writes=[xpad[i]])
    for ct in range(ntiles):
        xp = xpad[ct % 2]
        ac = acc[ct % 2]
        P.dma("sp", xp[:, 3:SEQ + 3], projT.t[row0 + ct * 128:row0 + (ct + 1) * 128, :], reads=[projT], writes=[xp])
        eng = P.dve
        eng(lambda e, ct=ct, xp=xp, ac=ac: e.tensor_scalar(out=ac[:], in0=xp[:, 0:SEQ], scalar1=cw[:, ct, 0:1], scalar2=None, op0=ALU.mult),
            reads=[xp, cw], writes=[ac])
        for k in range(1, 4):
            eng(lambda e, ct=ct, xp=xp, ac=ac, k=k: e.scalar_tensor_tensor(out=ac[:], in0=xp[:, k:SEQ + k], scalar=cw[:, ct, k:k + 1], in1=ac[:],
                                                                            op0=ALU.mult, op1=ALU.add), reads=[xp, cw, ac], writes=[ac])
        if bias:
            P.act(lambda e, ct=ct, ac=ac: e.activation(out=dst[:, dst_off + ct, :], in_=ac[:], func=AF.Silu, bias=cb[:, ct:ct + 1]),
                  reads=[ac, cb], writes=[dst])
        else:
            P.act(lambda e, ct=ct, ac=ac: e.activation(out=dst[:, dst_off + ct, :], in_=ac[:], func=AF.Silu), reads=[ac], writes=[dst])


def softplus_small(P, x_ap, xbuf, tmpb, one):
    P.act(lambda e: e.activation(out=x_ap, in_=x_ap, func=AF.Exp), reads=[xbuf], writes=[xbuf])
    P.act(lambda e: e.activation(out=x_ap, in_=x_ap, func=AF.Ln, bias=one[:, 0:1]), reads=[xbuf, one], writes=[xbuf])


def stage_ssd(P, C, projT, projN, ymix, prm, l):
    with P.scope():
        cf = [C.f]
        cw = P.sbuf("ssd_cw", [128, 8, 4])
        cb = P.sbuf("ssd_cb", [128, 8])
        dtb = P.sbuf("ssd_dtb", [128, 8])
        aneg = P.sbuf("ssd_aneg", [128, 8])
        dsk = P.sbuf("ssd_dsk", [128, 8])
        nwb = P.sbuf("ssd_nwb", [128, 512])
        P.dma("sp", cw[:], prm["ssd_conv_w"].t[l], reads=[prm["ssd_conv_w"]], writes=[cw])
        P.dma("sp", cb[:], prm["ssd_conv_b"].t[l], reads=[prm["ssd_conv_b"]], writes=[cb])
        P.dma("sp", dtb[:], prm["ssd_dt_bias"].t[l:l + 1, :].partition_broadcast(128), reads=[prm["ssd_dt_bias"]], writes=[dtb])
        P.dma("sp", aneg[:], prm["ssd_a_log"].t[l:l + 1, :].partition_broadcast(128), reads=[prm["ssd_a_log"]], writes=[aneg])
        P.dma("sp", dsk[:], prm["ssd_d"].t[l:l + 1, :].partition_broadcast(128), reads=[prm["ssd_d"]], writes=[dsk])
        P.dma("sp", nwb[:], prm["ssd_norm_w"].t[l:l + 1, :].partition_broadcast(128), reads=[prm["ssd_norm_w"]], writes=[nwb])
        P.act(lambda e: e.activation(out=aneg[:], in_=aneg[:], func=AF.Exp), reads=[aneg], writes=[aneg])
        P.dve(lambda e: e.tensor_scalar(out=aneg[:], in0=aneg[:], scalar1=-1.0, scalar2=None, op0=ALU.mult), reads=[aneg], writes=[aneg])
        act = P.sbuf("ssd_act", [128, 8, SEQ])
        with P.scope():
            causal_conv_silu(P, projT, PT_SXBC, 8, cw, cb, act, 0, "ssd")
        BCb = P.sbuf("ssd_BCb", [128, 4, SEQ], BF16)
        for k in range(4):
            (P.dve if k % 2 == 0 else P.pool)(lambda e, k=k: e.tensor_copy(out=BCb[:, k, :], in_=act[:, 4 + k, :]), reads=[act], writes=[BCb])
        tmp = ng_tmp(P)
        S = P.sbuf("ssd_S", [128, 8, 64])
        Sb = [P.sbuf(f"ssd_Sb{i}", [128, 8, 64], BF16) for i in range(2)]
        P.dve(lambda e: e.memset(S[:], 0.0), writes=[S])
        P.dve(lambda e: e.memset(Sb[0][:], 0.0), writes=[Sb[0]])
        tokN = [P.sbuf(f"ssd_tokN{i}", [128, 520]) for i in range(2)]
        xN = P.sbuf("ssd_xN", [128, 512])
        BNb = P.sbuf("ssd_BNb", [128, 256], BF16)
        dt8 = P.sbuf("ssd_dt8", [128, 8])
        a8 = P.sbuf("ssd_a8", [128, 8])
        dw8 = P.sbuf("ssd_dw8", [128, 8])
        ac16 = P.sbuf("ssd_ac16", [128, 2, 8])
        e32 = P.sbuf("ssd_e32", [128, 32])
        Aexp = P.sbuf("ssd_Aexp", [128, 8, 128])
        seg = P.sbuf("ssd_seg", [128, 8, 128])
        CBm = P.sbuf("ssd_CBm", [128, 2, 128])
        MT = P.sbuf("ssd_MT", [128, 8, 128], BF16)
        xdt = P.sbuf("ssd_xdt", [128, 8, 64], BF16)
        xw = P.sbuf("ssd_xw", [128, 8, 64], BF16)
        y1 = P.sbuf("ssd_y1", [128, 512])
        y2 = P.sbuf("ssd_y2", [128, 512])
        yo = [P.sbuf(f"ssd_yo{i}", [128, 512]) for i in range(2)]
        psA = P.psum("ssd_psA", [128, 512])
        psB = P.psum("ssd_psB", [128, 512])
        psC = P.psum("ssd_psC", [128, 512])
        psD = P.psum("ssd_psD", [128, 512])
        psE = P.psum("ssd_psE", [128, 512])
        psF = P.psum("ssd_psF", [128, 512])
        psG = [P.psum(f"ssd_psG{i}", [128, 512]) for i in range(2)]

        def load(t):
            P.dma("sp", tokN[t % 2][:], projN.t[t * 128:(t + 1) * 128, PN_SZ:PN_SZ + 520], reads=[projN], writes=[tokN[t % 2]])

        load(0)
        for t in range(NT):
            if t + 1 < NT:
                load(t + 1)
            tk = tokN[t % 2]
            tok = slice(t * 128, (t + 1) * 128)
            for k in range(4):
                P.pe(lambda e, k=k: e.transpose(out=psA[:, k * 128:(k + 1) * 128], in_=act[:, k, tok], identity=C.c("ident")), reads=[act] + cf, writes=[psA])
            for k in range(2):
                P.pe(lambda e, k=k: e.transpose(out=psB[:, k * 128:(k + 1) * 128], in_=act[:, 4 + k, tok], identity=C.c("ident")), reads=[act] + cf, writes=[psB])
            P.act(lambda e: e.copy(out=xN[:], in_=psA[:]), reads=[psA], writes=[xN])
            P.dve(lambda e: e.tensor_copy(out=BNb[:], in_=psB[:, 0:256]), reads=[psB], writes=[BNb])
            P.dve(lambda e: e.tensor_tensor(out=dt8[:], in0=tk[:, 512:520], in1=dtb[:], op=ALU.add), reads=[tk, dtb], writes=[dt8])
            softplus_small(P, dt8[:], dt8, None, tmp["one"])
            P.dve(lambda e: e.tensor_tensor(out=a8[:], in0=dt8[:], in1=aneg[:], op=ALU.mult), reads=[dt8, aneg], writes=[a8])
            P.dve(lambda e: e.tensor_tensor(out=Aexp[:], in0=bc(a8[:].unsqueeze(2), [128, 8, 128]), in1=bc(C.c("tri01").unsqueeze(1), [128, 8, 128]), op=ALU.mult),
                  reads=[a8] + cf, writes=[Aexp])
            P.dve(lambda e: e.tensor_tensor(out=ac16[:], in0=bc(a8[:].unsqueeze(1), [128, 2, 8]), in1=bc(C.c("chunkind").unsqueeze(2), [128, 2, 8]), op=ALU.mult),
                  reads=[a8] + cf, writes=[ac16])
            P.pe(lambda e: e.matmul(psC[:], lhsT=C.c("su01"), rhs=Aexp[:, 0:4, :].rearrange("p h i -> p (h i)"), start=True, stop=True), reads=[Aexp] + cf, writes=[psC])
            P.pe(lambda e: e.matmul(psD[:], lhsT=C.c("su01"), rhs=Aexp[:, 4:8, :].rearrange("p h i -> p (h i)"), start=True, stop=True), reads=[Aexp] + cf, writes=[psD])
            P.act(lambda e: e.activation(out=seg[:, 0:4, :].rearrange("p h i -> p (h i)"), in_=psC[:], func=AF.Exp), reads=[psC], writes=[seg])
            P.act(lambda e: e.activation(out=seg[:, 4:8, :].rearrange("p h i -> p (h i)"), in_=psD[:], func=AF.Exp), reads=[psD], writes=[seg])
            P.pe(lambda e: e.matmul(psE[:, 0:8], lhsT=C.c("tri01"), rhs=a8[:], start=True, stop=True), reads=[a8] + cf, writes=[psE])
            P.pe(lambda e: e.matmul(psE[:, 8:16], lhsT=C.c("su01"), rhs=a8[:], start=True, stop=True), reads=[a8] + cf, writes=[psE])
            P.pe(lambda e: e.matmul(psE[:, 16:32], lhsT=C.c("ones"), rhs=ac16[:].rearrange("p c h -> p (c h)"), start=True, stop=True), reads=[ac16] + cf, writes=[psE])
            P.act(lambda e: e.activation(out=e32[:], in_=psE[:, 0:32], func=AF.Exp), reads=[psE], writes=[e32])
            ea = e32[:, 0:8]
            w8 = e32[:, 8:16]
            for g in range(2):
                P.pe(lambda e, g=g: e.matmul(psB[:, 256 + g * 128:256 + (g + 1) * 128], lhsT=BCb[:, g, tok], rhs=BCb[:, 2 + g, tok], start=True, stop=True),
                     reads=[BCb], writes=[psB])
            P.dve(lambda e: e.tensor_tensor(out=CBm[:], in0=psB[:, 256:512].rearrange("p (g i) -> p g i", g=2), in1=bc(C.c("tri01").unsqueeze(1), [128, 2, 128]), op=ALU.mult),
                  reads=[psB] + cf, writes=[CBm])
            P.dve(lambda e: e.tensor_tensor(out=MT[:].rearrange("p (g r) i -> p g r i", g=2), in0=seg[:].rearrange("p (g r) i -> p g r i", g=2),
                                            in1=bc(CBm[:].unsqueeze(2), [128, 2, 4, 128]), op=ALU.mult), reads=[seg, CBm], writes=[MT])
            P.dve(lambda e: e.tensor_tensor(out=dw8[:], in0=dt8[:], in1=w8, op=ALU.mult), reads=[dt8, e32], writes=[dw8])
            P.pool(lambda e: e.tensor_tensor(out=xdt[:], in0=xN[:].rearrange("p (h q) -> p h q", h=8), in1=bc(dt8[:].unsqueeze(2), [128, 8, 64]), op=ALU.mult),
                   reads=[xN, dt8], writes=[xdt])
            P.pool(lambda e: e.tensor_tensor(out=xw[:], in0=xN[:].rearrange("p (h q) -> p h q", h=8), in1=bc(dw8[:].unsqueeze(2), [128, 8, 64]), op=ALU.mult),
                   reads=[xN, dw8], writes=[xw])
            for h in range(8):
                P.pe(lambda e, h=h: e.matmul(psF[:, h * 64:(h + 1) * 64], lhsT=MT[:, h, :], rhs=xdt[:, h, :], start=True, stop=True), reads=[MT, xdt], writes=[psF])
            for c in range(2):
                rows = slice(c * 64, (c + 1) * 64)
                for g in range(2):
                    P.pe(lambda e, c=c, g=g, rows=rows: e.matmul(psG[c][:, g * 256:(g + 1) * 256], lhsT=BNb[rows, g * 128:(g + 1) * 128],
                                                                 rhs=xw[rows, 4 * g:4 * g + 4, :].rearrange("p h q -> p (h q)"), start=True, stop=True),
                         reads=[BNb, xw], writes=[psG[c]])
            for c in range(2):
                P.dve(lambda e, c=c: e.tensor_tensor(out=S[:], in0=S[:], in1=bc(e32[:, 16 + 8 * c:24 + 8 * c].unsqueeze(2), [128, 8, 64]), op=ALU.mult),
                      reads=[S, e32], writes=[S])
                P.dve(lambda e, c=c: e.tensor_tensor(out=S[:].rearrange("p h q -> p (h q)"), in0=S[:].rearrange("p h q -> p (h q)"), in1=psG[c][:], op=ALU.add),
                      reads=[S, psG[c]], writes=[S])
                if c == 0:
                    P.act(lambda e: e.copy(out=Sb[1][:], in_=S[:]), reads=[S], writes=[Sb[1]])
            for c in range(2):
                rows = slice(c * 64, (c + 1) * 64)
                for g in range(2):
                    P.pe(lambda e, c=c, g=g, rows=rows: e.matmul(psC[rows, g * 256:(g + 1) * 256], lhsT=BCb[:, 2 + g, t * 128 + c * 64:t * 128 + (c + 1) * 64],
                                                                 rhs=Sb[c][:, 4 * g:4 * g + 4, :].rearrange("p h q -> p (h q)"), start=True, stop=True),
                         reads=[BCb, Sb[c]], writes=[psC])
            P.act(lambda e: e.copy(out=Sb[0][:], in_=S[:]), reads=[S], writes=[Sb[0]])
            P.dve(lambda e: e.tensor_tensor(out=y1[:].rearrange("p (h q) -> p h q", h=8), in0=psC[:].rearrange("p (h q) -> p h q", h=8),
                                            in1=bc(ea.unsqueeze(2), [128, 8, 64]), op=ALU.mult), reads=[psC, e32], writes=[y1])
            P.dve(lambda e: e.tensor_tensor(out=y1[:], in0=y1[:], in1=psF[:], op=ALU.add), reads=[y1, psF], writes=[y1])
            P.pool(lambda e: e.tensor_tensor(out=y2[:].rearrange("p (h q) -> p h q", h=8), in0=xN[:].rearrange("p (h q) -> p h q", h=8),
                                             in1=bc(dsk[:].unsqueeze(2), [128, 8, 64]), op=ALU.mult), reads=[xN, dsk], writes=[y2])
            P.pool(lambda e: e.tensor_tensor(out=y1[:], in0=y1[:], in1=y2[:], op=ALU.add), reads=[y1, y2], writes=[y1])
            y = yo[t % 2]
            norm_gate(P, y1[:], [y1], tk[:, 0:512], [tk], nwb[:].rearrange("p (g e) -> p g e", g=2), [nwb], 2, 256, y, tmp, True)
            P.dma("sp", ymix.t[tok, 512:1024], y[:], reads=[y], writes=[ymix])


def stage_gdn(P, C, projT, projN, ymix, prm, l):
    H = 4
    with P.scope():
        cf = [C.f]
        cw = P.sbuf("gdn_cw", [128, 12, 4])
        dtb = P.sbuf("gdn_dtb", [128, 4])
        aneg = P.sbuf("gdn_aneg", [128, 4])
        nwb = P.sbuf("gdn_nwb", [128, 128])
        P.dma("sp", cw[:], prm["gdn_conv_w"].t[l], reads=[prm["gdn_conv_w"]], writes=[cw])
        P.dma("sp", dtb[:], prm["gdn_dt_bias"].t[l:l + 1, :].partition_broadcast(128), reads=[prm["gdn_dt_bias"]], writes=[dtb])
        P.dma("sp", aneg[:], prm["gdn_a_log"].t[l:l + 1, :].partition_broadcast(128), reads=[prm["gdn_a_log"]], writes=[aneg])
        P.dma("sp", nwb[:], prm["gdn_norm_w"].t[l:l + 1, :].partition_broadcast(128), reads=[prm["gdn_norm_w"]], writes=[nwb])
        P.act(lambda e: e.activation(out=aneg[:], in_=aneg[:], func=AF.Exp), reads=[aneg], writes=[aneg])
        P.dve(lambda e: e.tensor_scalar(out=aneg[:], in0=aneg[:], scalar1=-1.0, scalar2=None, op0=ALU.mult), reads=[aneg], writes=[aneg])
        tmp = ng_tmp(P)
        qkvb = P.sbuf("gdn_qkvb", [128, 12, SEQ], BF16)
        with P.scope():
            cvt = P.sbuf("gdn_cvt", [128, 1, SEQ])
            sq = P.sbuf("gdn_sq", [128, SEQ])
            rinv = P.sbuf("gdn_rinv", [128, SEQ])
            pss = [P.psum(f"gdn_pss{i}", [128, 512]) for i in range(4)]
            xpad = [P.sbuf(f"gdn_xpad{i}", [128, SEQ + 3]) for i in range(2)]
            acc = P.sbuf("gdn_acc", [128, SEQ])
            for i in range(2):
                P.pool(lambda e, i=i: e.memset(xpad[i][:, 0:3], 0.0), writes=[xpad[i]])
            for ct in range(12):
                xp = xpad[ct % 2]
                P.dma("sp", xp[:, 3:SEQ + 3], projT.t[PT_GQKV + ct * 128:PT_GQKV + (ct + 1) * 128, :], reads=[projT], writes=[xp])
                P.dve(lambda e, ct=ct, xp=xp: e.tensor_scalar(out=acc[:], in0=xp[:, 0:SEQ], scalar1=cw[:, ct, 0:1], scalar2=None, op0=ALU.mult),
                      reads=[xp, cw], writes=[acc])
                for k in range(1, 4):
                    P.dve(lambda e, ct=ct, xp=xp, k=k: e.scalar_tensor_tensor(out=acc[:], in0=xp[:, k:SEQ + k], scalar=cw[:, ct, k:k + 1], in1=acc[:],
                                                                               op0=ALU.mult, op1=ALU.add), reads=[xp, cw, acc], writes=[acc])
                if ct >= 8:
                    P.act(lambda e, ct=ct: e.activation(out=qkvb[:, ct, :], in_=acc[:], func=AF.Silu), reads=[acc], writes=[qkvb])
                    continue
                P.act(lambda e: e.activation(out=cvt[:, 0, :], in_=acc[:], func=AF.Silu), reads=[acc], writes=[cvt])
                P.act(lambda e: e.activation(out=sq[:], in_=cvt[:, 0, :], func=AF.Square), reads=[cvt], writes=[sq])
                for n in range(4):
                    P.pe(lambda e, n=n: e.matmul(pss[n][:], lhsT=C.c("ones"), rhs=sq[:, n * 512:(n + 1) * 512], start=True, stop=True), reads=[sq] + cf, writes=[pss[n]])
                    P.act(lambda e, n=n: e.activation(out=rinv[:, n * 512:(n + 1) * 512], in_=pss[n][:], func=AF.Sqrt, bias=tmp["eps"][:, 0:1]),
                          reads=[pss[n], tmp["eps"]], writes=[rinv])
                P.dve(lambda e: e.reciprocal(out=rinv[:], in_=rinv[:]), reads=[rinv], writes=[rinv])
                scl = 128.0 ** -0.5 if ct < 4 else 1.0
                P.dve(lambda e, ct=ct, scl=scl: e.scalar_tensor_tensor(out=qkvb[:, ct, :], in0=cvt[:, 0, :], scalar=scl, in1=rinv[:], op0=ALU.mult, op1=ALU.mult),
                      reads=[cvt, rinv], writes=[qkvb])
        dbg(1)
        S = P.sbuf("gdn_S", [128, H, 128])
        Sb = P.sbuf("gdn_Sb", [128, H, 128], BF16)
        P.dve(lambda e: e.memset(S[:], 0.0), writes=[S])
        P.dve(lambda e: e.memset(Sb[:], 0.0), writes=[Sb])
        tokN = [P.sbuf(f"gdn_tokN{i}", [128, 520]) for i in range(2)]

        def f4(name, dt=F32):
            return P.sbuf("gdn_" + name, [128, H, 128], dt)

        b4 = P.sbuf("gdn_b4", [128, 4])
        g4 = P.sbuf("gdn_g4", [128, 4])
        gc8 = P.sbuf("gdn_gc8", [128, 2, 4])
        e16 = P.sbuf("gdn_e16", [128, 16])
        bg4 = P.sbuf("gdn_bg4", [128, 4])
        Gt, Gs, DTm, Dm, egb = f4("Gt"), f4("Gs"), f4("DTm"), f4("Dm"), f4("egb")
        A, AT, TT, vb, Kg, u = f4("A"), f4("AT"), f4("TT"), f4("vb"), f4("Kg"), f4("u")
        X = [f4("X0"), f4("X1")]
        XT = [f4("XT0"), f4("XT1")]
        aqkT, wT, qdT, kend, vnew = f4("aqkT", BF16), f4("wT", BF16), f4("qdT", BF16), f4("kend", BF16), f4("vnew", BF16)
        yo = [P.sbuf(f"gdn_yo{i}", [128, 512]) for i in range(2)]
        B0 = P.psum("gdn_B0", [128, 512])
        B1 = P.psum("gdn_B1", [128, 512])
        B2 = P.psum("gdn_B2", [128, 512])
        B3 = P.psum("gdn_B3", [128, 512])
        B4 = P.psum("gdn_B4", [128, 1024], BF16)
        B5 = P.psum("gdn_B5", [128, 512])
        B6 = P.psum("gdn_B6", [128, 512])
        B7 = P.psum("gdn_B7", [128, 512])

        def v4(ap):
            return ap.rearrange("p (h i) -> p h i", h=H)

        def fl(ap):
            return ap.rearrange("p h i -> p (h i)")

        def load(t):
            P.dma("sp", tokN[t % 2][:], projN.t[t * 128:(t + 1) * 128, PN_GZ:PN_GZ + 520], reads=[projN], writes=[tokN[t % 2]])

        tri = C.c("tri01")
        su = C.c("su01")
        load(0)
        for t in range(NT):
            if t + 1 < NT:
                load(t + 1)
            tk = tokN[t % 2]
            tok = slice(t * 128, (t + 1) * 128)
            P.act(lambda e: e.activation(out=b4[:], in_=tk[:, 512:516], func=AF.Sigmoid), reads=[tk], writes=[b4])
            P.dve(lambda e: e.tensor_tensor(out=g4[:], in0=tk[:, 516:520], in1=dtb[:], op=ALU.add), reads=[tk, dtb], writes=[g4])
            softplus_small(P, g4[:], g4, None, tmp["one"])
            P.dve(lambda e: e.tensor_tensor(out=g4[:], in0=g4[:], in1=aneg[:], op=ALU.mult), reads=[g4, aneg], writes=[g4])
            P.dve(lambda e: e.tensor_tensor(out=Gt[:], in0=bc(g4[:].unsqueeze(2), [128, H, 128]), in1=bc(tri.unsqueeze(1), [128, H, 128]), op=ALU.mult),
                  reads=[g4] + cf, writes=[Gt])
            P.pool(lambda e: e.tensor_tensor(out=Gs[:], in0=bc(g4[:].unsqueeze(2), [128, H, 128]), in1=bc(su.unsqueeze(1), [128, H, 128]), op=ALU.mult),
                   reads=[g4] + cf, writes=[Gs])
            P.dve(lambda e: e.tensor_tensor(out=gc8[:], in0=bc(g4[:].unsqueeze(1), [128, 2, 4]), in1=bc(C.c("chunkind").unsqueeze(2), [128, 2, 4]), op=ALU.mult),
                  reads=[g4] + cf, writes=[gc8])
            P.pe(lambda e: e.matmul(B0[:], lhsT=su, rhs=fl(Gt[:]), start=True, stop=True), reads=[Gt] + cf, writes=[B0])
            P.pe(lambda e: e.matmul(B1[:], lhsT=tri, rhs=fl(Gs[:]), start=True, stop=True), reads=[Gs] + cf, writes=[B1])
            P.pe(lambda e: e.matmul(B2[:], lhsT=C.c("ones"), rhs=fl(Gt[:]), start=True, stop=True), reads=[Gt] + cf, writes=[B2])
            P.pe(lambda e: e.matmul(B3[:, 0:4], lhsT=tri, rhs=g4[:], start=True, stop=True), reads=[g4] + cf, writes=[B3])
            P.pe(lambda e: e.matmul(B3[:, 4:8], lhsT=su, rhs=g4[:], start=True, stop=True), reads=[g4] + cf, writes=[B3])
            P.pe(lambda e: e.matmul(B3[:, 8:16], lhsT=C.c("ones"), rhs=gc8[:].rearrange("p c h -> p (c h)"), start=True, stop=True), reads=[gc8] + cf, writes=[B3])
            P.act(lambda e: e.activation(out=fl(DTm[:]), in_=B0[:], func=AF.Exp), reads=[B0], writes=[DTm])
            P.act(lambda e: e.activation(out=fl(Dm[:]), in_=B1[:], func=AF.Exp), reads=[B1], writes=[Dm])
            P.act(lambda e: e.activation(out=fl(egb[:]), in_=B2[:], func=AF.Exp), reads=[B2], writes=[egb])
            P.act(lambda e: e.activation(out=e16[:], in_=B3[:, 0:16], func=AF.Exp), reads=[B3], writes=[e16])
            P.pool(lambda e: e.tensor_tensor(out=DTm[:], in0=DTm[:], in1=bc(tri.unsqueeze(1), [128, H, 128]), op=ALU.mult), reads=[DTm] + cf, writes=[DTm])
            P.pool(lambda e: e.tensor_tensor(out=Dm[:], in0=Dm[:], in1=bc(su.unsqueeze(1), [128, H, 128]), op=ALU.mult), reads=[Dm] + cf, writes=[Dm])
            P.dve(lambda e: e.tensor_tensor(out=bg4[:], in0=b4[:], in1=e16[:, 0:4], op=ALU.mult), reads=[b4, e16], writes=[bg4])
            dbg(2)
            for h in range(H):
                P.pe(lambda e, h=h: e.transpose(out=B4[:, h * 128:(h + 1) * 128], in_=qkvb[:, 4 + h, tok], identity=C.identb[:]), reads=[qkvb, C.identb], writes=[B4])
            for h in range(H):
                P.pe(lambda e, h=h: e.transpose(out=B4[:, 512 + h * 128:512 + (h + 1) * 128], in_=qkvb[:, 8 + h, tok], identity=C.identb[:]), reads=[qkvb, C.identb], writes=[B4])
            P.dve(lambda e: e.tensor_tensor(out=vb[:], in0=v4(B4[:, 512:1024]), in1=bc(b4[:].unsqueeze(2), [128, H, 128]), op=ALU.mult), reads=[B4, b4], writes=[vb])
            P.dve(lambda e: e.tensor_tensor(out=Kg[:], in0=v4(B4[:, 0:512]), in1=bc(bg4[:].unsqueeze(2), [128, H, 128]), op=ALU.mult), reads=[B4, bg4], writes=[Kg])
            P.dve(lambda e: e.tensor_tensor(out=kend[:], in0=v4(B4[:, 0:512]), in1=bc(e16[:, 4:8].unsqueeze(2), [128, H, 128]), op=ALU.mult), reads=[B4, e16], writes=[kend])
            dbg(3)
            for h in range(H):
                P.pe(lambda e, h=h: e.matmul(B5[:, h * 128:(h + 1) * 128], lhsT=qkvb[:, 4 + h, tok], rhs=qkvb[:, 4 + h, tok], start=True, stop=True), reads=[qkvb], writes=[B5])
            for h in range(H):
                P.pe(lambda e, h=h: e.matmul(B6[:, h * 128:(h + 1) * 128], lhsT=qkvb[:, 4 + h, tok], rhs=qkvb[:, h, tok], start=True, stop=True), reads=[qkvb], writes=[B6])
            P.dve(lambda e: e.tensor_tensor(out=A[:], in0=v4(B5[:]), in1=Dm[:], op=ALU.mult), reads=[B5, Dm], writes=[A])
            P.dve(lambda e: e.tensor_tensor(out=A[:], in0=A[:], in1=bc(b4[:].unsqueeze(2), [128, H, 128]), op=ALU.mult), reads=[A, b4], writes=[A])
            P.dve(lambda e: e.tensor_tensor(out=aqkT[:], in0=v4(B6[:]), in1=DTm[:], op=ALU.mult), reads=[B6, DTm], writes=[aqkT])
            P.pool(lambda e: e.tensor_tensor(out=qdT[:], in0=qkvb[:, 0:4, tok], in1=egb[:], op=ALU.mult), reads=[qkvb, egb], writes=[qdT])
            dbg(4)
            for h in range(H):
                P.pe(lambda e, h=h: e.transpose(out=B0[:, h * 128:(h + 1) * 128], in_=A[:, h, :], identity=C.c("ident")), reads=[A] + cf, writes=[B0])
            dbg(4.3)
            P.act(lambda e: e.copy(out=fl(AT[:]), in_=B0[:]), reads=[B0], writes=[AT])
            dbg(4.6)
            P.dve(lambda e: e.scalar_tensor_tensor(out=TT[:], in0=v4(B0[:]), scalar=-1.0, in1=bc(C.c("ident").unsqueeze(1), [128, H, 128]), op0=ALU.mult, op1=ALU.add),
                  reads=[B0] + cf, writes=[TT])
            dbg(5)
            Xc, XTc = A, AT
            for k in range(1, 6):
                Xn, XTn = X[k % 2], XT[k % 2]
                for h in range(H):
                    P.pe(lambda e, h=h, Xc=Xc, XTc=XTc: e.matmul(B1[:, h * 128:(h + 1) * 128], lhsT=XTc[:, h, :], rhs=Xc[:, h, :], start=True, stop=True),
                         reads=[Xc, XTc], writes=[B1])
                if k < 5:
                    for h in range(H):
                        P.pe(lambda e, h=h, Xc=Xc, XTc=XTc: e.matmul(B2[:, h * 128:(h + 1) * 128], lhsT=Xc[:, h, :], rhs=XTc[:, h, :], start=True, stop=True),
                             reads=[Xc, XTc], writes=[B2])
                P.act(lambda e, Xn=Xn: e.copy(out=fl(Xn[:]), in_=B1[:]), reads=[B1], writes=[Xn])
                if k < 5:
                    P.dve(lambda e, XTn=XTn: e.tensor_copy(out=fl(XTn[:]), in_=B2[:]), reads=[B2], writes=[XTn])
                for h in range(H):
                    P.pe(lambda e, h=h, Xn=Xn: e.matmul(B5[:, h * 128:(h + 1) * 128], lhsT=Xn[:, h, :], rhs=TT[:, h, :], start=True, stop=True),
                         reads=[Xn, TT], writes=[B5])
                P.dve(lambda e: e.tensor_tensor(out=fl(TT[:]), in0=fl(TT[:]), in1=B5[:], op=ALU.add), reads=[TT, B5], writes=[TT])
                Xc, XTc = Xn, XTn
            dbg(6)
            for h in range(H):
                P.pe(lambda e, h=h: e.matmul(B0[:, h * 128:(h + 1) * 128], lhsT=TT[:, h, :], rhs=vb[:, h, :], start=True, stop=True), reads=[TT, vb], writes=[B0])
            for h in range(H):
                P.pe(lambda e, h=h: e.matmul(B5[:, h * 128:(h + 1) * 128], lhsT=Kg[:, h, :], rhs=TT[:, h, :], start=True, stop=True), reads=[TT, Kg], writes=[B5])
            P.act(lambda e: e.copy(out=fl(u[:]), in_=B0[:]), reads=[B0], writes=[u])
            P.dve(lambda e: e.tensor_copy(out=fl(wT[:]), in_=B5[:]), reads=[B5], writes=[wT])
            dbg(7)
            for c in range(2):
                rows = slice(c * 64, (c + 1) * 64)
                for h in range(H):
                    P.pe(lambda e, h=h, rows=rows: e.matmul(B6[rows, h * 128:(h + 1) * 128], lhsT=wT[:, h, rows], rhs=Sb[:, h, :], start=True, stop=True),
                         reads=[wT, Sb], writes=[B6])
                P.dve(lambda e, rows=rows: e.tensor_tensor(out=fl(vnew[rows]), in0=fl(u[rows]), in1=B6[rows, :], op=ALU.subtract), reads=[u, B6], writes=[vnew])
                for h in range(H):
                    P.pe(lambda e, h=h, rows=rows: e.matmul(B7[rows, h * 128:(h + 1) * 128], lhsT=qdT[:, h, rows], rhs=Sb[:, h, :], start=True, stop=False),
                         reads=[qdT, Sb], writes=[B7])
                    P.pe(lambda e, h=h, rows=rows: e.matmul(B7[rows, h * 128:(h + 1) * 128], lhsT=aqkT[rows, h, rows], rhs=vnew[rows, h, :], start=False, stop=True),
                         reads=[aqkT, vnew], writes=[B7])
                for h in range(H):
                    P.pe(lambda e, h=h, rows=rows: e.matmul(B1[:, h * 128:(h + 1) * 128], lhsT=kend[rows, h, :], rhs=vnew[rows, h, :], start=True, stop=True),
                         reads=[kend, vnew], writes=[B1])
                P.dve(lambda e, c=c: e.tensor_tensor(out=S[:], in0=S[:], in1=bc(e16[:, 8 + 4 * c:12 + 4 * c].unsqueeze(2), [128, H, 128]), op=ALU.mult),
                      reads=[S, e16], writes=[S])
                P.dve(lambda e: e.tensor_tensor(out=fl(S[:]), in0=fl(S[:]), in1=B1[:], op=ALU.add), reads=[S, B1], writes=[S])
                P.act(lambda e: e.copy(out=Sb[:], in_=S[:]), reads=[S], writes=[Sb])
            dbg(8)
            y = yo[t % 2]
            norm_gate(P, B7[:], [B7], tk[:, 0:512], [tk], bc(nwb[:].unsqueeze(1), [128, 4, 128]), [nwb], 4, 128, y, tmp, False)
            P.dma("sp", ymix.t[tok, 1024:1536], y[:], reads=[y], writes=[ymix])


NBIG = 30000.0


def t5_bucket_np(rel):
    n = np.maximum(rel, 0)
    exact = 16
    large = exact + (np.log(np.maximum(n, 1).astype(np.float32) / np.float32(exact)) / np.float32(math.log(128 / 16)) * np.float32(32 - exact)).astype(np.int32)
    return np.where(n < exact, n, np.minimum(large, 31)).astype(np.int64)


def nsa_host_tables(rel_bias):
    rb = np.asarray(rel_bias, np.float32)
    ki = np.arange(128)[:, None]
    qi = np.arange(128)[None, :]
    r0 = qi - ki
    r128 = 128 + qi - ki
    qq = np.arange(128)[:, None]
    mm = np.arange(248)[None, :]
    rc = qq - 16 * (mm - 120) - 31
    tab = np.concatenate([
        rb[t5_bucket_np(r0)].transpose(0, 2, 1).reshape(128, 8 * 128),
        rb[t5_bucket_np(r128)].transpose(0, 2, 1).reshape(128, 8 * 128),
        rb[t5_bucket_np(rc)].transpose(0, 2, 1).reshape(128, 8 * 248)], axis=1)
    t31 = np.broadcast_to(rb[31][None, :], (128, 8)).copy()
    return np.ascontiguousarray(tab, np.float32), np.ascontiguousarray(t31, np.float32)


def nsa_host_consts():
    ki = np.arange(128)[:, None]
    qi = np.arange(128)[None, :]
    qq = np.arange(128)[:, None]
    mm = np.arange(248)[None, :]
    rc = qq - 16 * (mm - 120) - 31
    m0 = np.where(qi - ki >= 0, 0.0, -NBIG)
    msk = np.concatenate([
        np.broadcast_to(m0[:, None, :], (128, 8, 128)).reshape(128, -1),
        np.zeros((128, 8 * 128)),
        np.broadcast_to(np.where(rc >= 0, 0.0, -NBIG)[:, None, :], (128, 8, 248)).reshape(128, -1)], axis=1)
    mtri = np.where(ki > qi, 0.0, -NBIG)
    k = np.arange(128)[:, None]
    j = np.arange(32)[None, :]
    ov = ((16 * k <= 64 * j + 63) & (16 * k + 31 >= 64 * j) & (k < 127)).astype(np.float32)
    keep = np.zeros((128, 16, 32)); addc = np.zeros((128, 16, 32))
    for qb in range(16):
        cur = (2 * qb + (np.arange(128) >= 64))[:, None]
        blk = np.arange(32)[None, :]
        forced = (blk == 0) | (blk == cur) | (blk == cur - 1)
        fut = blk > cur
        keep[:, qb, :] = (~forced & ~fut)
        addc[:, qb, :] = np.where(fut, -1e30, np.where(forced, 1e9, 0.0))
    E = np.zeros((128, 2048))
    E[:32] = (np.arange(2048)[None, :] // 64) == np.arange(32)[:, None]
    parts = dict(msk=msk, mtri=mtri, ov=ov, keep=keep.reshape(128, -1), addc=addc.reshape(128, -1), E=E)
    cols = {}
    o = 0
    arrs = []
    for n, a in parts.items():
        cols[n] = (o, a.shape[1]); o += a.shape[1]; arrs.append(a.astype(np.float32))
    return np.concatenate(arrs, axis=1), cols


NSA_CST_NP, NSA_CST_COLS = nsa_host_consts()


def stage_nsa(P, C, projT, projN, ymix, prm, l):
    G, R = 2, 4
    with P.scope():
        cf = [C.f]
        tmp = ng_tmp(P)
        one = tmp["one"]
        Bn0 = P.sbuf("nsa_Bn0", [128, 8, 128], BF16)
        Bn1 = P.sbuf("nsa_Bn1", [128, 8, 128], BF16)
        Mtri = P.sbuf("nsa_Mtri", [128, 4, 128], BF16)
        FT = P.sbuf("nsa_FT", [128, 8, 248])
        ovb = P.sbuf("nsa_ovb", [128, 32], BF16)
        keep = P.sbuf("nsa_keep", [128, 16, 32])
        addc = P.sbuf("nsa_addc", [128, 16, 32])
        Eb = P.sbuf("nsa_Eb", [32, 2048], BF16)
        ncst = prm["nsa_cst"]
        cc = NSA_CST_COLS
        P.dma("sp", keep[:].rearrange("p a b -> p (a b)"), ncst.t[:, cc["keep"][0]:cc["keep"][0] + 512], reads=[ncst], writes=[keep])
        P.dma("sp", addc[:].rearrange("p a b -> p (a b)"), ncst.t[:, cc["addc"][0]:cc["addc"][0] + 512], reads=[ncst], writes=[addc])
        with P.scope():
            tb = P.sbuf("nsa_tb", [128, 4032])
            mk = P.sbuf("nsa_mk", [128, 4032])
            t31 = P.sbuf("nsa_t31", [128, 8])
            st = P.sbuf("nsa_st", [128, 2048])
            P.dma("sp", tb[:], prm["nsa_tab"].t[:, :], reads=[prm["nsa_tab"]], writes=[tb])
            P.dma("sp", mk[:], ncst.t[:, cc["msk"][0]:cc["msk"][0] + 4032], reads=[ncst], writes=[mk])
            P.dma("sp", t31[:], prm["nsa_t31"].t[:, :], reads=[prm["nsa_t31"]], writes=[t31])
            for (o, w, dst) in ((0, 128, Bn0), (1024, 128, Bn1), (2048, 248, FT)):
                v = tb[:, o:o + 8 * w].rearrange("p (h x) -> p h x", h=8)
                P.dve(lambda e, v=v, w=w: e.tensor_tensor(out=v, in0=v, in1=bc(t31[:].unsqueeze(2), [128, 8, w]), op=ALU.subtract), reads=[tb, t31], writes=[tb])
                P.dve(lambda e, v=v, w=w, o=o, dst=dst: e.tensor_tensor(out=dst[:], in0=v, in1=mk[:, o:o + 8 * w].rearrange("p (h x) -> p h x", h=8), op=ALU.add),
                      reads=[tb, mk], writes=[dst])
            P.dma("sp", st[:, 0:128], ncst.t[:, cc["mtri"][0]:cc["mtri"][0] + 128], reads=[ncst], writes=[st])
            P.dve(lambda e: e.tensor_copy(out=Mtri[:], in_=bc(st[:, 0:128].unsqueeze(1), [128, 4, 128])), reads=[st], writes=[Mtri])
            P.dma("sp", st[:, 128:160], ncst.t[:, cc["ov"][0]:cc["ov"][0] + 32], reads=[ncst], writes=[st])
            P.dve(lambda e: e.tensor_copy(out=ovb[:], in_=st[:, 128:160]), reads=[st], writes=[ovb])
            P.dma("sp", st[0:32, :], ncst.t[0:32, cc["E"][0]:cc["E"][0] + 2048], reads=[ncst], writes=[st])
            P.dve(lambda e: e.tensor_copy(out=Eb[:], in_=st[0:32, :]), reads=[st], writes=[Eb])
        dbg(0.1)
        qTb = P.sbuf("nsa_qTb", [64, 8, SEQ], BF16)
        ksT = P.sbuf("nsa_ksT", [64, 2, SEQ], BF16)
        kwT = P.sbuf("nsa_kwT", [64, 2, SEQ], BF16)
        vsb = P.sbuf("nsa_vsb", [128, 16, 2, 65], BF16)
        vwb = P.sbuf("nsa_vwb", [128, 16, 2, 65], BF16)
        gts = P.sbuf("nsa_gts", [128, 16, 24])
        kcT = P.sbuf("nsa_kcT", [64, 2, 128], BF16)
        vcx = P.sbuf("nsa_vcx", [128, 2, 96], BF16)
        with P.scope():
            stg = [P.sbuf(f"nsa_stg{i}", [64, SEQ]) for i in range(2)]
            n = 0
            for h in range(8):
                s_ = stg[n % 2]; n += 1
                P.dma("sp", s_[:], projT.t[PT_NQ + h * 64:PT_NQ + (h + 1) * 64, :], reads=[projT], writes=[s_])
                P.act(lambda e, h=h, s_=s_: e.activation(out=qTb[:, h, :], in_=s_[:], func=AF.Copy, scale=0.125), reads=[s_], writes=[qTb])
            for (r0, dst) in ((PT_NKS, ksT), (PT_NKW, kwT)):
                for g in range(2):
                    s_ = stg[n % 2]; n += 1
                    P.dma("sp", s_[:], projT.t[r0 + g * 64:r0 + (g + 1) * 64, :], reads=[projT], writes=[s_])
                    P.dve(lambda e, g=g, s_=s_, dst=dst: e.tensor_copy(out=dst[:, g, :], in_=s_[:]), reads=[s_], writes=[dst])
            tn = P.sbuf("nsa_tn", [128, 16, 280])
            P.dma("sp", tn[:], projN.t[:, 0:280].rearrange("(t p) c -> p t c", p=128), reads=[projN], writes=[tn])
            P.pool(lambda e: e.memset(vsb[:], 1.0), writes=[vsb])
            P.pool(lambda e: e.memset(vwb[:], 1.0), writes=[vwb])
            P.dve(lambda e: e.tensor_copy(out=vsb[:, :, :, 0:64], in_=tn[:, :, 0:128].rearrange("p t (g d) -> p t g d", g=2)), reads=[tn], writes=[vsb])
            P.dve(lambda e: e.tensor_copy(out=vwb[:, :, :, 0:64], in_=tn[:, :, 128:256].rearrange("p t (g d) -> p t g d", g=2)), reads=[tn], writes=[vwb])
            P.act(lambda e: e.activation(out=gts[:], in_=tn[:, :, 256:280], func=AF.Sigmoid), reads=[tn], writes=[gts])
            dbg(0.2)
            tT = P.sbuf("nsa_tT", [64, 2, SEQ])
            tG = P.sbuf("nsa_tG", [64, 2, 16, 129], BF16)
            P.pool(lambda e: e.memset(tG[:], 0.0), writes=[tG])
            w1f = P.sbuf("nsa_w1f", [64, 32, 64])
            w1b = P.sbuf("nsa_w1b", [64, 32, 64], BF16)
            w2f = P.sbuf("nsa_w2f", [64, 64])
            w2b = P.sbuf("nsa_w2b", [64, 64], BF16)
            posT = P.sbuf("nsa_posT", [64, 32])
            posb = P.sbuf("nsa_posb", [64, 32], BF16)
            cvec = P.sbuf("nsa_cvec", [64, 1])
            hid = P.sbuf("nsa_hid", [64, 2, 128], BF16)
            ps_h = P.psum("nsa_ps_h", [64, 512])
            ps_c = P.psum("nsa_ps_c", [64, 8])
            ps_o = P.psum("nsa_ps_o", [128, 512])
            P.dve(lambda e: e.memset(hid[:], 0.0), writes=[hid])
            for kv in range(2):
                r0 = PT_NKC if kv == 0 else PT_NVC
                for g in range(2):
                    P.dma("sp", tT[:, g, :], projT.t[r0 + g * 64:r0 + (g + 1) * 64, :], reads=[projT], writes=[tT])
                    P.dve(lambda e, g=g: e.tensor_copy(out=tG[:, g, :, 0:128], in_=tT[:, g, :].rearrange("p (n s) -> p s n", s=16)), reads=[tT], writes=[tG])
                w1src = prm["nsa_cmp_w1"].t[l, kv].rearrange("(j d) o -> d j o", d=64)
                P.dma("sp", w1f[:], w1src, reads=[prm["nsa_cmp_w1"]], writes=[w1f])
                P.pool(lambda e: e.tensor_copy(out=w1b[:], in_=w1f[:]), reads=[w1f], writes=[w1b])
                P.dma("sp", w2f[:], prm["nsa_cmp_w2"].t[l, kv], reads=[prm["nsa_cmp_w2"]], writes=[w2f])
                P.dve(lambda e: e.tensor_copy(out=w2b[:], in_=w2f[:]), reads=[w2f], writes=[w2b])
                P.dma("sp", posT[:], prm["nsa_cmp_pos"].t[l, kv], reads=[prm["nsa_cmp_pos"]], writes=[posT])
                P.dve(lambda e: e.tensor_copy(out=posb[:], in_=posT[:]), reads=[posT], writes=[posb])
                for j in range(32):
                    P.pe(lambda e, j=j: e.matmul(ps_c[:, 0:1], lhsT=w1b[:, j, :], rhs=posb[:, j:j + 1], start=(j == 0), stop=(j == 31)), reads=[w1b, posb], writes=[ps_c])
                P.dve(lambda e: e.tensor_copy(out=cvec[:], in_=ps_c[:, 0:1]), reads=[ps_c], writes=[cvec])
                dbg(0.3 + 0.3 * kv)
                for g in range(2):
                    rows = slice(g * 64, (g + 1) * 64)
                    dbg(0.32 + 0.03 * g + 0.3 * kv)
                    for j in range(32):
                        P.pe(lambda e, j=j, g=g, rows=rows: e.matmul(ps_h[:, g * 128:(g + 1) * 128], lhsT=w1b[:, j, :], rhs=tG[:, g, j % 16, (j // 16):(j // 16) + 128],
                                                                    start=(j == 0), stop=(j == 31)), reads=[w1b, tG], writes=[ps_h])
                for g in range(2):
                    P.act(lambda e, g=g: e.activation(out=hid[:, g, :], in_=ps_h[:, g * 128:(g + 1) * 128], func=AF.Silu, bias=cvec[:, 0:1]),
                          reads=[ps_h, cvec], writes=[hid])
                dbg(0.4 + 0.3 * kv)
                if kv == 0:
                    P.pe(lambda e: e.matmul(ps_h[:, 256:512], lhsT=w2b[:], rhs=hid[:].rearrange("o g n -> o (g n)"), start=True, stop=True), reads=[w2b, hid], writes=[ps_h])
                    P.dve(lambda e: e.tensor_copy(out=kcT[:].rearrange("o g n -> o (g n)"), in_=ps_h[:, 256:512]), reads=[ps_h], writes=[kcT])
                else:
                    for g in range(2):
                        P.pe(lambda e, g=g: e.matmul(ps_o[:, g * 64:(g + 1) * 64], lhsT=hid[:, g, :], rhs=w2b[:], start=True, stop=True), reads=[w2b, hid], writes=[ps_o])
                    P.dve(lambda e: e.tensor_copy(out=vcx[:, :, 0:64], in_=ps_o[:, 0:128].rearrange("p (g d) -> p g d", g=2)), reads=[ps_o], writes=[vcx])
                    P.dve(lambda e: e.tensor_copy(out=vcx[:, :, 64:96], in_=bc(ovb[:].unsqueeze(1), [128, 2, 32])), reads=[ovb], writes=[vcx])
        dbg(1)
        ps_sc = [P.psum(f"nsa_ps_sc{i}", [128, 512]) for i in range(2)]
        ps_os = P.psum("nsa_ps_os", [128, 512])
        ps_ow = P.psum("nsa_ps_ow", [128, 512])
        ps_oc = P.psum("nsa_ps_oc", [128, 512])
        ps_cs = P.psum("nsa_ps_cs", [128, 512])
        ps_tr = P.psum("nsa_ps_tr", [128, 1024], BF16)
        sc = P.sbuf("nsa_sc", [128, 4, 128])
        ssum = P.sbuf("nsa_ssum", [128, 4])
        pnb = P.sbuf("nsa_pnb", [128, 4, 128], BF16)
        pT = P.sbuf("nsa_pT", [128, 4, 128], BF16)
        imp = P.sbuf("nsa_imp", [128, 32])
        mx8 = P.sbuf("nsa_mx8", [128, 8])
        negm = P.sbuf("nsa_negm", [128, 32], BF16)
        negT = P.sbuf("nsa_negT", [32, 4, 128], BF16)
        eT = [P.sbuf(f"nsa_eT{i}", [128, 4, 128], BF16) for i in range(2)]
        cs = P.sbuf("nsa_cs", [128, 3, 4])
        ya = P.sbuf("nsa_ya", [128, 4, 64])
        yb = P.sbuf("nsa_yb", [128, 4, 64])
        yt = [P.sbuf(f"nsa_yt{i}", [128, 512]) for i in range(2)]
        nsc = 0

        def v3(ap, r=4):
            return ap.rearrange("p (r q) -> p r q", r=r)

        for qb in range(NT):
            qtok = slice(qb * 128, (qb + 1) * 128)
            y = yt[qb % 2]
            for g in range(G):
                hs = slice(4 * g, 4 * g + 4)
                for r in range(R):
                    P.pe(lambda e, r=r: e.matmul(ps_cs[:, r * 128:(r + 1) * 128], lhsT=qTb[:, 4 * g + r, qtok], rhs=kcT[:, g, :], start=True, stop=True),
                         reads=[qTb, kcT], writes=[ps_cs])
                m0 = 120 - 8 * qb
                P.dve(lambda e: e.tensor_tensor(out=sc[:], in0=v3(ps_cs[:]), in1=FT[:, hs, m0:m0 + 128], op=ALU.add), reads=[ps_cs, FT], writes=[sc])
                P.act(lambda e: e.activation(out=sc[:], in_=sc[:], func=AF.Exp), reads=[sc], writes=[sc])
                P.dve(lambda e: e.tensor_reduce(out=ssum[:], in_=sc[:], axis=AX.X, op=ALU.add), reads=[sc], writes=[ssum])
                P.dve(lambda e: e.tensor_scalar(out=ssum[:], in0=ssum[:], scalar1=1e-30, scalar2=None, op0=ALU.max), reads=[ssum], writes=[ssum])
                P.dve(lambda e: e.reciprocal(out=ssum[:], in_=ssum[:]), reads=[ssum], writes=[ssum])
                P.dve(lambda e: e.tensor_tensor(out=pnb[:], in0=sc[:], in1=bc(ssum[:].unsqueeze(2), [128, 4, 128]), op=ALU.mult), reads=[sc, ssum], writes=[pnb])
                for r in range(R):
                    P.pe(lambda e, r=r: e.transpose(out=ps_tr[:, r * 128:(r + 1) * 128], in_=pnb[:, r, :], identity=C.identb[:]), reads=[pnb, C.identb], writes=[ps_tr])
                P.act(lambda e: e.copy(out=pT[:].rearrange("p r q -> p (r q)"), in_=ps_tr[:, 0:512]), reads=[ps_tr], writes=[pT])
                for r in range(R):
                    P.pe(lambda e, r=r: e.matmul(ps_oc[:, r * 96:(r + 1) * 96], lhsT=pT[:, r, :], rhs=vcx[:, g, :], start=True, stop=True), reads=[pT, vcx], writes=[ps_oc])
                oc4 = ps_oc[:, 0:384].rearrange("p (r x) -> p r x", r=4)
                P.dve(lambda e: e.tensor_reduce(out=imp[:], in_=oc4[:, :, 64:96].rearrange("p r j -> p j r"), axis=AX.X, op=ALU.add), reads=[ps_oc], writes=[imp])
                P.dve(lambda e: e.tensor_tensor(out=imp[:], in0=imp[:], in1=keep[:, qb, :], op=ALU.mult), reads=[imp, keep], writes=[imp])
                P.dve(lambda e: e.tensor_tensor(out=imp[:], in0=imp[:], in1=addc[:, qb, :], op=ALU.add), reads=[imp, addc], writes=[imp])
                P.dve(lambda e: e.max(out=mx8[:], in_=imp[:]), reads=[imp], writes=[mx8])
                P.dve(lambda e: e.tensor_scalar(out=imp[:], in0=imp[:], scalar1=mx8[:, 7:8], scalar2=None, op0=ALU.is_ge), reads=[imp, mx8], writes=[imp])
                P.dve(lambda e: e.tensor_scalar(out=negm[:], in0=imp[:], scalar1=-1.0, scalar2=NBIG, op0=ALU.add, op1=ALU.mult), reads=[imp], writes=[negm])
                P.pe(lambda e: e.transpose(out=ps_tr[0:32, 512:640], in_=negm[:], identity=C.identb[:]), reads=[negm, C.identb], writes=[ps_tr])
                P.dve(lambda e: e.tensor_copy(out=negT[:], in_=bc(ps_tr[0:32, 512:640].unsqueeze(1), [32, 4, 128])), reads=[ps_tr], writes=[negT])
                for kt in range(qb + 1):
                    ps = ps_sc[nsc % 2]; et = eT[nsc % 2]; nsc += 1
                    ktok = slice(kt * 128, (kt + 1) * 128)
                    near = kt >= qb - 1
                    P.pe(lambda e, ps=ps, ktok=ktok: e.matmul(v3(ps[:]), lhsT=ksT[:, g, ktok], rhs=qTb[:, hs, qtok], start=True, stop=False), reads=[ksT, qTb], writes=[ps])
                    P.pe(lambda e, ps=ps, ktok=ktok, near=near: e.matmul(v3(ps[:]), lhsT=Eb[:, ktok], rhs=negT[:], start=False, stop=not near), reads=[Eb, negT], writes=[ps])
                    if near:
                        Bn = Bn0 if kt == qb else Bn1
                        P.pe(lambda e, ps=ps, Bn=Bn: e.matmul(v3(ps[:]), lhsT=C.identb[:], rhs=Bn[:, hs, :], start=False, stop=True), reads=[Bn, C.identb], writes=[ps])
                    P.act(lambda e, ps=ps, et=et: e.activation(out=et[:].rearrange("p r q -> p (r q)"), in_=ps[:], func=AF.Exp), reads=[ps], writes=[et])
                    for r in range(R):
                        P.pe(lambda e, r=r, et=et, kt=kt: e.matmul(ps_os[:, r * 65:(r + 1) * 65], lhsT=et[:, r, :], rhs=vsb[:, kt, g, :], start=(kt == 0 and r == 0), stop=(kt == qb), skip_group_check=True),
                             reads=[et, vsb], writes=[ps_os])
                kt0 = max(0, qb - 4)
                for kt in range(kt0, qb + 1):
                    ps = ps_sc[nsc % 2]; et = eT[nsc % 2]; nsc += 1
                    ktok = slice(kt * 128, (kt + 1) * 128)
                    dl = qb - kt
                    extra = {0: Bn0[:, hs, :], 1: Bn1[:, hs, :], 4: Mtri[:]}.get(dl)
                    P.pe(lambda e, ps=ps, ktok=ktok, extra=extra: e.matmul(v3(ps[:]), lhsT=kwT[:, g, ktok], rhs=qTb[:, hs, qtok], start=True, stop=extra is None),
                         reads=[kwT, qTb], writes=[ps])
                    if extra is not None:
                        P.pe(lambda e, ps=ps, extra=extra: e.matmul(v3(ps[:]), lhsT=C.identb[:], rhs=extra, start=False, stop=True), reads=[Bn0, Bn1, Mtri, C.identb], writes=[ps])
                    P.act(lambda e, ps=ps, et=et: e.activation(out=et[:].rearrange("p r q -> p (r q)"), in_=ps[:], func=AF.Exp), reads=[ps], writes=[et])
                    for r in range(R):
                        P.pe(lambda e, r=r, et=et, kt=kt: e.matmul(ps_ow[:, r * 65:(r + 1) * 65], lhsT=et[:, r, :], rhs=vwb[:, kt, g, :], start=(kt == kt0 and r == 0), stop=(kt == qb), skip_group_check=True),
                             reads=[et, vwb], writes=[ps_ow])
                os4 = ps_os[:, 0:260].rearrange("p (r x) -> p r x", r=4)
                ow4 = ps_ow[:, 0:260].rearrange("p (r x) -> p r x", r=4)
                g3 = gts[:, qb, 12 * g:12 * g + 12].rearrange("p (r b) -> p b r", b=3)
                P.dve(lambda e: e.reciprocal(out=cs[:, 1, :], in_=os4[:, :, 64]), reads=[ps_os], writes=[cs])
                P.dve(lambda e: e.reciprocal(out=cs[:, 2, :], in_=ow4[:, :, 64]), reads=[ps_ow], writes=[cs])
                P.dve(lambda e: e.memset(cs[:, 0, :], 1.0), writes=[cs])
                P.dve(lambda e: e.tensor_tensor(out=cs[:], in0=cs[:], in1=g3, op=ALU.mult), reads=[cs, gts], writes=[cs])
                P.dve(lambda e: e.tensor_tensor(out=ya[:], in0=oc4[:, :, 0:64], in1=bc(cs[:, 0, :].unsqueeze(2), [128, 4, 64]), op=ALU.mult), reads=[ps_oc, cs], writes=[ya])
                P.dve(lambda e: e.tensor_tensor(out=yb[:], in0=os4[:, :, 0:64], in1=bc(cs[:, 1, :].unsqueeze(2), [128, 4, 64]), op=ALU.mult), reads=[ps_os, cs], writes=[yb])
                P.pool(lambda e: e.tensor_tensor(out=ya[:], in0=ya[:], in1=yb[:], op=ALU.add), reads=[ya, yb], writes=[ya])
                P.dve(lambda e: e.tensor_tensor(out=yb[:], in0=ow4[:, :, 0:64], in1=bc(cs[:, 2, :].unsqueeze(2), [128, 4, 64]), op=ALU.mult), reads=[ps_ow, cs], writes=[yb])
                P.pool(lambda e: e.tensor_tensor(out=y[:, g * 256:(g + 1) * 256].rearrange("p (r d) -> p r d", r=4), in0=ya[:], in1=yb[:], op=ALU.add), reads=[ya, yb], writes=[y])
            P.dma("sp", ymix.t[qtok, 0:512], y[:], reads=[y], writes=[ymix])
            dbg(2 + qb)


def stage_mod(P, C, cT, ada_w, ada_b, modT, gsc, nlayers):
    with P.scope():
        cf = [C.f]
        ca = P.sbuf("mod_ca", [128, 16, BPC])
        P.dma("sp", ca[:], cT.t[:, :, :], reads=[cT], writes=[ca])
        P.act(lambda e: e.activation(out=ca[:], in_=ca[:], func=AF.Silu), reads=[ca], writes=[ca])
        wst = [P.sbuf(f"mod_w{i}", [128, 16, 512]) for i in range(2)]
        brow = [P.sbuf(f"mod_b{i}", [1, 512]) for i in range(2)]
        grow = [P.sbuf(f"mod_g{i}", [BPC, 512]) for i in range(2)]
        ps_f = P.psum("mod_psf", [128, 512])
        ps_g = [P.psum(f"mod_psg{i}", [BPC, 512]) for i in range(2)]
        n = 0
        for l in range(nlayers):
            wv = ada_w.t[l].rearrange("(k p) n -> p k n", p=128)
            for seg in range(6):
                for ct in range(4):
                    w = wst[n % 2]; br = brow[n % 2]
                    c0 = seg * 2048 + ct * 512
                    P.dma("sp", w[:], wv[:, :, c0:c0 + 512], reads=[ada_w], writes=[w])
                    P.dma("sp", br[:], ada_b.t[l:l + 1, c0:c0 + 512], reads=[ada_b], writes=[br])
                    if seg in (2, 5):
                        pg = ps_g[n % 2]; gr = grow[n % 2]
                        for k in range(16):
                            P.pe(lambda e, k=k, w=w, pg=pg: e.matmul(pg[:], lhsT=ca[:, k, :], rhs=w[:, k, :], start=(k == 0), stop=False), reads=[ca, w], writes=[pg])
                        P.pe(lambda e, br=br, pg=pg: e.matmul(pg[:], lhsT=C.c("ones", 1)[:, 0:BPC], rhs=br[:], start=False, stop=True), reads=[br] + cf, writes=[pg])
                        P.act(lambda e, pg=pg, gr=gr: e.copy(out=gr[:], in_=pg[:]), reads=[pg], writes=[gr])
                        P.dma("sp", gsc.t[l, 0 if seg == 2 else 1, :, ct * 512:(ct + 1) * 512], gr[:], reads=[gr], writes=[gsc])
                    else:
                        si = {0: 0, 1: 1, 3: 2, 4: 3}[seg]
                        for cc in range(4):
                            col = ((si * 16) + ct * 4 + cc) * BPC
                            for k in range(16):
                                P.pe(lambda e, k=k, w=w, cc=cc, col=col: e.matmul(ps_f[:, col:col + BPC], lhsT=w[:, k, cc * 128:(cc + 1) * 128], rhs=ca[:, k, :],
                                                                                 start=(k == 0), stop=False), reads=[ca, w], writes=[ps_f])
                            P.pe(lambda e, br=br, cc=cc, col=col: e.matmul(ps_f[:, col:col + BPC], lhsT=br[:, cc * 128:(cc + 1) * 128], rhs=C.c("ones", 1)[:, 0:BPC],
                                                                          start=False, stop=True), reads=[br] + cf, writes=[ps_f])
                    n += 1
            P.dve(lambda e, l=l: e.tensor_copy(out=modT[:, l].rearrange("p s k b -> p (s k b)"), in_=ps_f[:, 0:4 * 16 * BPC]), reads=[ps_f], writes=[modT])


def to_featmajor(P, C, src, src_ap_fn, ntt, hT, norm, scl=None, shf=None, pools=None):
    xt, xb, ss, ps_tr, eps = pools["xt"], pools["xb"], pools["ss"], pools["ps_tr"], pools["eps"]
    for tt in range(ntt):
        x = xt[tt % 2]; xn = xb[tt % 2]; s1 = ss[tt % 2]
        P.dma("sp", x[:], src_ap_fn(tt), reads=[src], writes=[x])
        if norm:
            P.pool(lambda e, s1=s1: e.memset(s1[:], 0.0), writes=[s1])
            P.act(lambda e, x=x, xn=xn, s1=s1: e.activation(out=xn[:], in_=x[:], func=AF.Square, accum_out=s1[:, 0:1]), reads=[x, s1], writes=[xn, s1])
            P.act(lambda e, s1=s1: e.activation(out=s1[:, 0:1], in_=s1[:, 0:1], func=AF.Sqrt, scale=1.0 / D_MODEL, bias=eps[:, 0:1]), reads=[s1, eps], writes=[s1])
            P.dve(lambda e, s1=s1: e.reciprocal(out=s1[:, 0:1], in_=s1[:, 0:1]), reads=[s1], writes=[s1])
            P.dve(lambda e, x=x, xn=xn, s1=s1: e.tensor_scalar(out=xn[:], in0=x[:], scalar1=s1[:, 0:1], scalar2=None, op0=ALU.mult), reads=[x, s1], writes=[xn])
        else:
            P.pool(lambda e, x=x, xn=xn: e.tensor_copy(out=xn[:], in_=x[:]), reads=[x], writes=[xn])
        for half in range(2):
            pt = ps_tr[(2 * tt + half) % len(ps_tr)]
            for kk in range(8):
                k = half * 8 + kk
                P.pe(lambda e, k=k, kk=kk, xn=xn, pt=pt: e.transpose(out=pt[:, kk * 128:(kk + 1) * 128], in_=xn[:, k * 128:(k + 1) * 128], identity=C.identb[:]),
                     reads=[xn, C.identb], writes=[pt])
            dst = hT[:, half * 8:half * 8 + 8, tt * 128:(tt + 1) * 128]
            src3 = pt[:].rearrange("p (k t) -> p k t", k=8)
            if scl is None:
                P.act(lambda e, dst=dst, src3=src3: e.copy(out=dst, in_=src3), reads=[pt], writes=[hT])
            else:
                tm = pools["tm"][(2 * tt + half) % 2]
                P.dve(lambda e, src3=src3, tm=tm, half=half: e.tensor_tensor(out=tm[:], in0=src3, in1=bc(scl[:, half * 8:half * 8 + 8].unsqueeze(2), [128, 8, 128]), op=ALU.mult),
                      reads=[pt] + pools["affb"], writes=[tm])
                P.pool(lambda e, dst=dst, tm=tm, half=half: e.tensor_tensor(out=dst, in0=tm[:], in1=bc(shf[:, half * 8:half * 8 + 8].unsqueeze(2), [128, 8, 128]), op=ALU.add),
                       reads=[tm] + pools["affb"], writes=[hT])


def fm_pools(P, affine):
    d = dict(xt=[P.sbuf(f"fm_xt{i}", [128, D_MODEL]) for i in range(2)],
             xb=[P.sbuf(f"fm_xb{i}", [128, D_MODEL], BF16) for i in range(2)],
             ss=[P.sbuf(f"fm_ss{i}", [128, 1]) for i in range(2)],
             ps_tr=[P.psum(f"fm_pst{i}", [128, 1024], BF16) for i in range(2)],
             eps=P.sbuf("fm_eps", [128, 1]))
    P.pool(lambda e: e.memset(d["eps"][:], EPS), writes=[d["eps"]])
    if affine:
        d["tm"] = [P.sbuf(f"fm_tm{i}", [128, 8, 128]) for i in range(2)]
    return d


def affine_vecs(P, modT, l, b, which, nw, scl, shf):
    P.dve(lambda e: e.tensor_scalar(out=scl[:], in0=modT[:, l, 2 * which + 1, :, b], scalar1=1.0, scalar2=None, op0=ALU.add), reads=[modT], writes=[scl])
    P.dve(lambda e: e.tensor_tensor(out=scl[:], in0=scl[:], in1=nw, op=ALU.mult), reads=[scl], writes=[scl])
    P.dve(lambda e: e.tensor_copy(out=shf[:], in_=modT[:, l, 2 * which, :, b]), reads=[modT], writes=[shf])


def stage_inproj(P, C, xsrc, xsrc_fn, modT, nw1T, w_in, projT, projN, l, b):
    with P.scope():
        hT = P.sbuf("ip_hT", [128, 16, SEQ], BF16)
        scl = P.sbuf("ip_scl", [128, 16]); shf = P.sbuf("ip_shf", [128, 16])
        affine_vecs(P, modT, l, b, 0, nw1T[:, l, :], scl, shf)
        with P.scope():
            pools = fm_pools(P, True)
            pools["affb"] = [scl, shf]
            to_featmajor(P, C, xsrc, xsrc_fn, NT, hT, True, scl[:], shf[:], pools)
        wst = [P.sbuf(f"ip_wst{i}", [128, 16, 512]) for i in range(2)]
        wb = [P.sbuf(f"ip_wb{i}", [128, 16, 512], BF16) for i in range(2)]
        ev = [P.sbuf(f"ip_ev{i}", [128, 2048]) for i in range(2)]
        ps = [P.psum(f"ip_ps{i}", [128, 512]) for i in range(4)]
        wv = w_in.t[l].rearrange("(k p) n -> p k n", p=128)
        n = 0
        npp = 0
        nev = 0
        for (c0, nc_, r0) in PT_GROUPS:
            w = wst[n % 2]; wbb = wb[n % 2]; n += 1
            P.dma("sp", w[:, :, 0:nc_], wv[:, :, c0:c0 + nc_], reads=[w_in], writes=[w])
            P.pool(lambda e, w=w, wbb=wbb, nc_=nc_: e.tensor_copy(out=wbb[:, :, 0:nc_], in_=w[:, :, 0:nc_]), reads=[w], writes=[wbb])
            e_ = ev[nev % 2]; nev += 1
            for tq in range(4):
                p_ = ps[npp % 4]; npp += 1
                for k in range(16):
                    P.pe(lambda e, k=k, p_=p_, wbb=wbb, nc_=nc_, tq=tq: e.matmul(p_[0:nc_, :], lhsT=wbb[:, k, 0:nc_], rhs=hT[:, k, tq * 512:(tq + 1) * 512], start=(k == 0), stop=(k == 15)),
                         reads=[wbb, hT], writes=[p_])
                (P.act if tq % 2 == 0 else P.dve)(
                    (lambda e, p_=p_, e_=e_, nc_=nc_, tq=tq: e.copy(out=e_[0:nc_, tq * 512:(tq + 1) * 512], in_=p_[0:nc_, :])) if tq % 2 == 0 else
                    (lambda e, p_=p_, e_=e_, nc_=nc_, tq=tq: e.tensor_copy(out=e_[0:nc_, tq * 512:(tq + 1) * 512], in_=p_[0:nc_, :])), reads=[p_], writes=[e_])
            P.dma("sp", projT.t[r0:r0 + nc_, :], e_[0:nc_, :], reads=[e_], writes=[projT])
        for (c0, nc_, o0) in PN_GROUPS:
            w = wst[n % 2]; wbb = wb[n % 2]; n += 1
            P.dma("sp", w[:, :, 0:nc_], wv[:, :, c0:c0 + nc_], reads=[w_in], writes=[w])
            P.pool(lambda e, w=w, wbb=wbb, nc_=nc_: e.tensor_copy(out=wbb[:, :, 0:nc_], in_=w[:, :, 0:nc_]), reads=[w], writes=[wbb])
            for t4 in range(4):
                e_ = ev[nev % 2]; nev += 1
                for ti in range(4):
                    tt = t4 * 4 + ti
                    p_ = ps[npp % 4]; npp += 1
                    for k in range(16):
                        P.pe(lambda e, k=k, p_=p_, wbb=wbb, nc_=nc_, tt=tt: e.matmul(p_[:, 0:nc_], lhsT=hT[:, k, tt * 128:(tt + 1) * 128], rhs=wbb[:, k, 0:nc_], start=(k == 0), stop=(k == 15)),
                             reads=[wbb, hT], writes=[p_])
                    (P.act if ti % 2 == 0 else P.dve)(
                        (lambda e, p_=p_, e_=e_, nc_=nc_, ti=ti: e.copy(out=e_[:, ti * 512:ti * 512 + nc_], in_=p_[:, 0:nc_])) if ti % 2 == 0 else
                        (lambda e, p_=p_, e_=e_, nc_=nc_, ti=ti: e.tensor_copy(out=e_[:, ti * 512:ti * 512 + nc_], in_=p_[:, 0:nc_])), reads=[p_], writes=[e_])
                P.dma("sp", projN.t[t4 * 512:(t4 + 1) * 512, o0:o0 + nc_].rearrange("(i p) c -> p i c", p=128),
                      e_[:].rearrange("p (i c) -> p i c", i=4)[:, :, 0:nc_], reads=[e_], writes=[projN])


def stage_outproj(P, C, ymix, xsrc, xsrc_fn, xdst, xdst_fn, gsc, w_out, l, b):
    with P.scope():
        yT = P.sbuf("op_yT", [128, 16, SEQ], BF16)
        with P.scope():
            pools = fm_pools(P, False)
            to_featmajor(P, C, ymix, lambda tt: ymix.t[tt * 128:(tt + 1) * 128, :], NT, yT, False, None, None, pools)
        wob = P.sbuf("op_wob", [128, 16, D_MODEL], BF16)
        gb = P.sbuf("op_gb", [128, D_MODEL])
        P.dma("sp", gb[:], gsc.t[l, 0, b:b + 1, :].partition_broadcast(128), reads=[gsc], writes=[gb])
        wv = w_out.t[l].rearrange("(k p) n -> p k n", p=128)
        with P.scope():
            wst = [P.sbuf(f"op_wst{i}", [128, 16, 512]) for i in range(2)]
            for ct in range(4):
                w = wst[ct % 2]
                P.dma("sp", w[:], wv[:, :, ct * 512:(ct + 1) * 512], reads=[w_out], writes=[w])
                P.pool(lambda e, w=w, ct=ct: e.tensor_copy(out=wob[:, :, ct * 512:(ct + 1) * 512], in_=w[:]), reads=[w], writes=[wob])
        xt = [P.sbuf(f"op_xt{i}", [128, D_MODEL]) for i in range(2)]
        xo = [P.sbuf(f"op_xo{i}", [128, D_MODEL]) for i in range(2)]
        ps = [P.psum(f"op_ps{i}", [128, 512]) for i in range(8)]
        for tt in range(NT):
            x = xt[tt % 2]; o = xo[tt % 2]
            P.dma("sp", x[:], xsrc_fn(tt), reads=[xsrc], writes=[x])
            for ct in range(4):
                p_ = ps[(tt * 4 + ct) % 8]
                for k in range(16):
                    P.pe(lambda e, k=k, p_=p_, ct=ct, tt=tt: e.matmul(p_[:], lhsT=yT[:, k, tt * 128:(tt + 1) * 128], rhs=wob[:, k, ct * 512:(ct + 1) * 512], start=(k == 0), stop=(k == 15)),
                         reads=[yT, wob], writes=[p_])
                cs = slice(ct * 512, (ct + 1) * 512)
                P.dve(lambda e, p_=p_, o=o, cs=cs: e.tensor_tensor(out=o[:, cs], in0=p_[:], in1=gb[:, cs], op=ALU.mult), reads=[p_, gb], writes=[o])
                P.pool(lambda e, o=o, x=x, cs=cs: e.tensor_tensor(out=o[:, cs], in0=o[:, cs], in1=x[:, cs], op=ALU.add), reads=[o, x], writes=[o])
            P.dma("sp", xdst_fn(tt), o[:], reads=[o], writes=[xdst])


def stage_mlp(P, C, xsrc, xsrc_fn, xdst, xdst_fn, modT, nw2T, gsc, w1, w2, l, b):
    HC = 64
    with P.scope():
        scl = P.sbuf("ml_scl", [128, 16]); shf = P.sbuf("ml_shf", [128, 16])
        affine_vecs(P, modT, l, b, 1, nw2T[:, l, :], scl, shf)
        gb = P.sbuf("ml_gb", [128, D_MODEL])
        P.dma("sp", gb[:], gsc.t[l, 1, b:b + 1, :].partition_broadcast(128), reads=[gsc], writes=[gb])
        hT = P.sbuf("ml_hT", [128, 16, 512], BF16)
        uT = P.sbuf("ml_uT", [128, HC, 512], BF16)
        pools = fm_pools(P, True)
        pools["affb"] = [scl, shf]
        w1st = [P.sbuf(f"ml_w1st{i}", [128, 16, 256]) for i in range(2)]
        w1b = [P.sbuf(f"ml_w1b{i}", [128, 16, 256], BF16) for i in range(2)]
        w2st = [P.sbuf(f"ml_w2st{i}", [128, 1024]) for i in range(2)]
        w2b = [P.sbuf(f"ml_w2b{i}", [128, 1024], BF16) for i in range(2)]
        rl = [P.sbuf(f"ml_rl{i}", [128, 512]) for i in range(2)]
        xo = [P.sbuf(f"ml_xo{i}", [128, 1024]) for i in range(2)]
        ps = [P.psum(f"ml_ps{i}", [128, 512]) for i in range(6)]
        w1v = w1.t[l].rearrange("(k p) n -> p k n", p=128)
        w2v = w2.t[l].rearrange("(k p) n -> p k n", p=128)
        n1 = 0
        n2 = 0
        nps = 0
        for t5 in range(SEQ // 512):
            to_featmajor(P, C, xsrc, lambda tt, t5=t5: xsrc_fn(t5 * 4 + tt), 4, hT, True, scl[:], shf[:], pools)
            for hp in range(HC // 2):
                w = w1st[n1 % 2]; wbb = w1b[n1 % 2]; n1 += 1
                P.dma("sp", w[:], w1v[:, :, hp * 256:(hp + 1) * 256], reads=[w1], writes=[w])
                P.pool(lambda e, w=w, wbb=wbb: e.tensor_copy(out=wbb[:], in_=w[:]), reads=[w], writes=[wbb])
                for cc in range(2):
                    hc = hp * 2 + cc
                    p_ = ps[nps % 6]; nps += 1
                    r_ = rl[hc % 2]
                    for k in range(16):
                        P.pe(lambda e, k=k, p_=p_, wbb=wbb, cc=cc: e.matmul(p_[:], lhsT=wbb[:, k, cc * 128:(cc + 1) * 128], rhs=hT[:, k, :], start=(k == 0), stop=(k == 15)),
                             reads=[wbb, hT], writes=[p_])
                    P.act(lambda e, p_=p_, r_=r_: e.activation(out=r_[:], in_=p_[:], func=AF.Relu), reads=[p_], writes=[r_])
                    P.dve(lambda e, r_=r_, hc=hc: e.tensor_tensor(out=uT[:, hc, :], in0=r_[:], in1=r_[:], op=ALU.mult), reads=[r_], writes=[uT])
            for ct in range(4):
                pa = [ps[(nps + i) % 6] for i in range(4)]
                nps += 4
                for k in range(HC):
                    w = w2st[n2 % 2]; wbb = w2b[n2 % 2]; n2 += 1
                    P.dma("sp", w[:, 0:512], w2v[:, k, ct * 512:(ct + 1) * 512], reads=[w2], writes=[w])
                    P.pool(lambda e, w=w, wbb=wbb: e.tensor_copy(out=wbb[:, 0:512], in_=w[:, 0:512]), reads=[w], writes=[wbb])
                    for ti in range(4):
                        P.pe(lambda e, k=k, ti=ti, wbb=wbb, pa=pa: e.matmul(pa[ti][:], lhsT=uT[:, k, ti * 128:(ti + 1) * 128], rhs=wbb[:, 0:512], start=(k == 0), stop=(k == HC - 1)),
                             reads=[uT, wbb], writes=[pa[ti]])
                for ti in range(4):
                    tt = t5 * 4 + ti
                    o = xo[(ct * 4 + ti) % 2]
                    P.dma("sp", o[:, 512:1024], xsrc_fn(tt)[:, ct * 512:(ct + 1) * 512], reads=[xsrc], writes=[o])
                    P.dve(lambda e, o=o, ti=ti, pa=pa, ct=ct: e.tensor_tensor(out=o[:, 0:512], in0=pa[ti][:], in1=gb[:, ct * 512:(ct + 1) * 512], op=ALU.mult),
                          reads=[pa[ti], gb], writes=[o])
                    P.pool(lambda e, o=o: e.tensor_tensor(out=o[:, 0:512], in0=o[:, 0:512], in1=o[:, 512:1024], op=ALU.add), reads=[o], writes=[o])
                    P.dma("sp", xdst_fn(tt)[:, ct * 512:(ct + 1) * 512], o[:, 0:512], reads=[o], writes=[xdst])


def stage_final(P, C, xsrc, xsrc_fn, fnw, out, out_fn, nseq):
    with P.scope():
        nwb = P.sbuf("fn_nwb", [128, D_MODEL])
        P.dma("sp", nwb[:], fnw.t[0:1, :].partition_broadcast(128), reads=[fnw], writes=[nwb])
        eps = P.sbuf("fn_eps", [128, 1])
        P.pool(lambda e: e.memset(eps[:], EPS), writes=[eps])
        xt = [P.sbuf(f"fn_xt{i}", [128, D_MODEL]) for i in range(2)]
        sq = [P.sbuf(f"fn_sq{i}", [128, D_MODEL]) for i in range(2)]
        ss = [P.sbuf(f"fn_ss{i}", [128, 1]) for i in range(2)]
        for i in range(nseq * NT):
            x = xt[i % 2]; q = sq[i % 2]; s1 = ss[i % 2]
            P.dma("sp", x[:], xsrc_fn(i), reads=[xsrc], writes=[x])
            P.pool(lambda e, s1=s1: e.memset(s1[:], 0.0), writes=[s1])
            P.act(lambda e, x=x, q=q, s1=s1: e.activation(out=q[:], in_=x[:], func=AF.Square, accum_out=s1[:, 0:1]), reads=[x, s1], writes=[q, s1])
            P.act(lambda e, s1=s1: e.activation(out=s1[:, 0:1], in_=s1[:, 0:1], func=AF.Sqrt, scale=1.0 / D_MODEL, bias=eps[:, 0:1]), reads=[s1, eps], writes=[s1])
            P.dve(lambda e, s1=s1: e.reciprocal(out=s1[:, 0:1], in_=s1[:, 0:1]), reads=[s1], writes=[s1])
            P.dve(lambda e, x=x, q=q, s1=s1: e.scalar_tensor_tensor(out=q[:], in0=x[:], scalar=s1[:, 0:1], in1=nwb[:], op0=ALU.mult, op1=ALU.mult), reads=[x, s1, nwb], writes=[q])
            P.dma("sp", out_fn(i), q[:], reads=[q], writes=[out])


SMALL_PARAMS = ["gla_gate_w2", "gla_gate_b", "gla_norm_w", "ssd_conv_w", "ssd_conv_b", "ssd_dt_bias", "ssd_a_log", "ssd_d", "ssd_norm_w",
                "gdn_conv_w", "gdn_dt_bias", "gdn_a_log", "gdn_norm_w", "nsa_cmp_pos", "nsa_cmp_w1", "nsa_cmp_w2"]
BIG_PARAMS = ["ada_w", "ada_b", "w_in", "w_out", "mlp_w1", "mlp_w2"]


def build(nlayers=DEPTH, nseq=BPC, shapes=None):
    nc = bass.Bass("TRN2", target_bir_lowering=False)
    st = ExitStack()
    with st:
        P = Prog(nc, st)

        def ext(name, shape):
            return Buf(nc.dram_tensor(name, list(shape), F32, kind="ExternalInput").ap(), name)

        x = ext("x", [nseq, SEQ, D_MODEL])
        cT = ext("cT", [128, 16, BPC])
        prm = {k: ext(k, shapes[k]) for k in SMALL_PARAMS + BIG_PARAMS + ["nw1T", "nw2T", "fnw", "nsa_tab", "nsa_t31", "nsa_cst", "cst"]}
        out = Buf(nc.dram_tensor("out", [nseq, SEQ, D_MODEL], F32, kind="ExternalOutput").ap(), "out")
        xres = P.dram("xres", [nseq, SEQ, D_MODEL])
        projT = P.dram("projT", [PT_ROWS, SEQ])
        projN = P.dram("projN", [SEQ, PN_COLS])
        ymix = P.dram("ymix", [SEQ, D_MODEL])
        gsc = P.dram("gsc", [nlayers, 2, BPC, D_MODEL])
        C = Consts(P, prm["cst"])
        modT = P.sbuf("modT", [128, nlayers, 4, 16, BPC])
        nw1T = P.sbuf("nw1T", [128, shapes["nw1T"][1], 16])
        nw2T = P.sbuf("nw2T", [128, shapes["nw2T"][1], 16])
        P.dma("sp", nw1T[:], prm["nw1T"].t[:, :, :], reads=[prm["nw1T"]], writes=[nw1T])
        P.dma("sp", nw2T[:], prm["nw2T"].t[:, :, :], reads=[prm["nw2T"]], writes=[nw2T])
        stage_mod(P, C, cT, prm["ada_w"], prm["ada_b"], modT, gsc, nlayers)
        for l in range(nlayers):
            for b in range(nseq):
                if l == 0:
                    xs, xs_fn = x, (lambda tt, b=b: x.t[b, tt * 128:(tt + 1) * 128, :])
                else:
                    xs, xs_fn = xres, (lambda tt, b=b: xres.t[b, tt * 128:(tt + 1) * 128, :])
                xr_fn = (lambda tt, b=b: xres.t[b, tt * 128:(tt + 1) * 128, :])
                stage_inproj(P, C, xs, xs_fn, modT, nw1T, prm["w_in"], projT, projN, l, b)
                stage_nsa(P, C, projT, projN, ymix, prm, l)
                stage_ssd(P, C, projT, projN, ymix, prm, l)
                stage_gdn(P, C, projT, projN, ymix, prm, l)
                stage_gla(P, C, projT, projN, ymix, prm, l)
                stage_outproj(P, C, ymix, xs, xs_fn, xres, xr_fn, gsc, prm["w_out"], l, b)
                stage_mlp(P, C, xres, xr_fn, xres, xr_fn, modT, nw2T, gsc, prm["mlp_w1"], prm["mlp_w2"], l, b)
        stage_final(P, C, xres, lambda i: xres.t[i // NT, (i % NT) * 128:(i % NT + 1) * 128, :], prm["fnw"],
                    out, lambda i: out.t[i // NT, (i % NT) * 128:(i % NT + 1) * 128, :], nseq)
        P.finish()
        ninstr = P.ninstr
    return nc, ninstr


def host_inputs(inputs, nlayers=DEPTH):
    d = {}
    for k in SMALL_PARAMS:
        d[k] = host_param(k, inputs[k][:nlayers])
    for k in BIG_PARAMS:
        d[k] = np.ascontiguousarray(np.asarray(inputs[k][:nlayers], np.float32))
    d["nw1T"] = host_param("norm1_w", inputs["norm1_w"][:nlayers])
    d["nw2T"] = host_param("norm2_w", inputs["norm2_w"][:nlayers])
    d["fnw"] = host_param("final_norm_w", inputs["final_norm_w"])
    d["nsa_tab"], d["nsa_t31"] = nsa_host_tables(inputs["rel_bias"])
    d["nsa_cst"] = NSA_CST_NP
    d["cst"] = CST_NP
    return d


def core_inputs(inputs, shared, core, nseq=BPC):
    xs = np.ascontiguousarray(np.asarray(inputs["x"][core * BPC:core * BPC + nseq], np.float32))
    c = np.asarray(inputs["c"][core * BPC:(core + 1) * BPC], np.float32)
    cT = np.ascontiguousarray(c.T.reshape(16, 128, BPC).transpose(1, 0, 2))
    m = dict(shared)
    m["x"] = xs
    m["cT"] = cT
    return m


_CACHE = {}


def kernel(**inputs):
    shared = host_inputs(inputs)
    shapes = {k: v.shape for k, v in shared.items()}
    if "nc" not in _CACHE:
        _CACHE["nc"] = build(DEPTH, BPC, shapes)[0]
    nc = _CACHE["nc"]
    in_maps = [core_inputs(inputs, shared, c) for c in range(NCORES)]
    res = run_bass_kernel_spmd(nc, in_maps, core_ids=list(range(NCORES)))
    out = np.concatenate([r["out"] for r in res.results], axis=0)
    return out.astype(np.float32)
```

```python
import math
from contextlib import ExitStack, contextmanager
import numpy as np
import concourse.bass as bass
import concourse.mybir as mybir
from concourse.bass_utils import run_bass_kernel_spmd

F32 = mybir.dt.float32
BF16 = mybir.dt.bfloat16
AF = mybir.ActivationFunctionType
ALU = mybir.AluOpType
AX = mybir.AxisListType

EPOCH = 12000
SAME_ENGINE_SYNC = True

D_MODEL = 2048
SEQ = 2048
DEPTH = 4
NCORES = 8
BPC = 2
IN_COLS = 6456
EPS = 1e-6
NT = SEQ // 128


class StopStage(Exception):
    pass


DBG = {"stop": 99, "skip": set()}


def dbg(k):
    if DBG["stop"] <= k:
        DBG["P"].dead = True


class Buf:
    __slots__ = ("t", "name", "lw", "rd", "excl")

    def __init__(self, t, name="", excl=False):
        self.t = t
        self.name = name
        self.lw = None
        self.rd = {}
        self.excl = excl

    def __getitem__(self, k):
        return self.t[k]


class Prog:
    ENGS = ("pe", "act", "dve", "pool", "sp")

    def __init__(self, nc, stack):
        self.nc = nc
        self.stack = stack
        self.cnt = {e: 0 for e in ("pe", "act", "dve", "pool")}
        self.sems = {}
        self.seen = {e: {} for e in self.ENGS}
        self.dslots = {}
        self.dnext = {}
        self.E = dict(pe=nc.tensor, act=nc.scalar, dve=nc.vector, pool=nc.gpsimd, sp=nc.sync)
        self.ninstr = 0
        self.base_stack = stack
        self.uid = 0
        self.dead = False
        DBG["P"] = self

    def sem(self, name):
        return self.base_stack.enter_context(self.nc.semaphore(name))

    def sbuf(self, name, shape, dt=F32):
        self.uid += 1
        t = self.stack.enter_context(self.nc.sbuf_tensor(f"{name}_{self.uid}", list(shape), dt))
        return Buf(t, name)

    def psum(self, name, shape, dt=F32):
        self.uid += 1
        t = self.stack.enter_context(self.nc.psum_tensor(f"{name}_{self.uid}", list(shape), dt))
        return Buf(t, name, excl=True)

    def dram(self, name, shape, dt=F32, kind="Internal"):
        t = self.nc.dram_tensor(name, list(shape), dt, kind=kind)
        return Buf(t.ap(), name)

    def _esem(self, eng, idx):
        ep = idx // EPOCH
        k = (eng, ep)
        if k not in self.sems:
            self.sems[k] = self.sem(f"s_{eng}_{ep}")
        return self.sems[k], (idx % EPOCH) + 1

    def _wait(self, eng, ev):
        q, idx = ev
        if isinstance(q, str):
            if q == eng and (eng == "pe" or not SAME_ENGINE_SYNC):
                return
            if self.seen[eng].get(q, -1) >= idx:
                return
            self.seen[eng][q] = idx
            s, v = self._esem(q, idx)
        else:
            if self.seen[eng].get(q, -1) >= idx:
                return
            self.seen[eng][q] = idx
            s = self.dslots[q[0]][q[1]][0]
            v = idx
        self.E[eng].wait_ge(s, v)

    def _deps(self, eng, reads, writes):
        for b in reads:
            if b.lw is not None:
                self._wait(eng, b.lw)
            if b.excl:
                for q, i in list(b.rd.items()):
                    if q != eng:
                        self._wait(eng, (q, i))
        for b in writes:
            if b.lw is not None:
                self._wait(eng, b.lw)
            for q, i in list(b.rd.items()):
                self._wait(eng, (q, i))

    def _mark(self, ev, reads, writes):
        q, idx = ev
        for b in reads:
            if b.rd.get(q, -1) < idx:
                b.rd[q] = idx
        for b in writes:
            b.lw = ev
            b.rd = {}

    def op(self, eng, fn, reads=(), writes=()):
        if self.dead:
            return
        self._deps(eng, reads, writes)
        idx = self.cnt[eng]
        self.cnt[eng] += 1
        s, v = self._esem(eng, idx)
        fn(self.E[eng]).then_inc(s, 1)
        self._mark((eng, idx), reads, writes)
        self.ninstr += 1

    def pe(self, fn, reads=(), writes=()):
        self.op("pe", fn, reads, writes)

    def act(self, fn, reads=(), writes=()):
        self.op("act", fn, reads, writes)

    def dve(self, fn, reads=(), writes=()):
        self.op("dve", fn, reads, writes)

    def pool(self, fn, reads=(), writes=()):
        self.op("pool", fn, reads, writes)

    def dma(self, eng, out_ap, in_ap, reads=(), writes=(), nslots=8, **kw):
        if self.dead:
            return
        if eng not in self.dslots:
            self.dslots[eng] = [[self.sem(f"d_{eng}_{i}"), 0] for i in range(nslots)]
            self.dnext[eng] = 0
        si = self.dnext[eng]
        self.dnext[eng] = (si + 1) % len(self.dslots[eng])
        slot = self.dslots[eng][si]
        q = (eng, si)
        if slot[1] > 0:
            self._wait(eng, (q, slot[1]))
        self._deps(eng, reads, writes)
        slot[1] += 16
        self.E[eng].dma_start(out=out_ap, in_=in_ap, **kw).then_inc(slot[0], 16)
        self._mark((q, slot[1]), reads, writes)
        self.ninstr += 1

    def all_events(self):
        evs = []
        for e in ("pe", "act", "dve", "pool"):
            if self.cnt[e] > 0:
                evs.append((e, self.cnt[e] - 1))
        for eng, slots in self.dslots.items():
            for si, (s, c) in enumerate(slots):
                if c > 0:
                    evs.append(((eng, si), c))
        return evs

    def barrier(self, engs=None):
        evs = self.all_events()
        for e in (engs or self.ENGS):
            for ev in evs:
                if ev[0] == e and e == "pe":
                    continue
                self._wait(e, ev)

    @contextmanager
    def scope(self):
        old = self.stack
        try:
            with ExitStack() as st:
                self.stack = st
                try:
                    yield
                finally:
                    self.barrier()
        finally:
            self.stack = old

    def finish(self):
        self.barrier(["sp"])


def make_consts():
    p = np.arange(128)[:, None]
    f = np.arange(128)[None, :]
    same = (p // 64) == (f // 64)
    cols = {}
    parts = []

    def add(name, arr):
        cols[name] = (sum(a.shape[1] for a in parts), arr.shape[1])
        parts.append(arr.astype(np.float32))

    add("ident", (p == f))
    add("tri01", same & (p <= f))
    add("stri01", same & (p < f))
    add("su01", same & (p > f))
    add("sl01", same & (p >= f))
    add("bones", same)
    add("ones", np.ones((128, 128)))
    add("chunkind", (p // 64) == np.arange(2)[None, :])
    add("tri16", (same & (p <= f)) * (-1.0 / 16.0))
    add("bones16", same * (-1.0 / 16.0))
    add("chunkind16", ((p // 64) == np.arange(2)[None, :]) * (-1.0 / 16.0))
    return np.concatenate(parts, axis=1), cols


CST_NP, CST_COLS = make_consts()


class Consts:
    def __init__(self, P, cst_dram):
        self.P = P
        n = CST_NP.shape[1]
        self.f = P.sbuf("cst_f", [128, n])
        P.dma("sp", self.f[:], cst_dram.t[:, :], reads=[cst_dram], writes=[self.f])
        self.identb = P.sbuf("identb", [128, 128], BF16)
        P.dve(lambda e: e.tensor_copy(out=self.identb[:], in_=self.c("ident")), reads=[self.f], writes=[self.identb])

    def c(self, name, rows=128):
        o, w = CST_COLS[name]
        return self.f[0:rows, o:o + w]


def norm_gate(P, src_ap, src_bufs, z_ap, z_bufs, nw_ap, nw_bufs, G, gsz, out, tmp, gate_first):
    a, b, ss, sg = tmp["a"], tmp["b"], tmp["ss"], tmp["sg"]
    n = G * gsz
    P.act(lambda e: e.activation(out=sg[:, 0:n], in_=z_ap, func=AF.Silu), reads=z_bufs, writes=[sg])
    if gate_first:
        P.dve(lambda e: e.tensor_tensor(out=a[:, 0:n], in0=src_ap, in1=sg[:, 0:n], op=ALU.mult),
              reads=list(src_bufs) + [sg], writes=[a])
    else:
        P.dve(lambda e: e.tensor_copy(out=a[:, 0:n], in_=src_ap), reads=list(src_bufs), writes=[a])
    P.act(lambda e: e.activation(out=b[:, 0:n], in_=a[:, 0:n], func=AF.Square), reads=[a], writes=[b])
    P.dve(lambda e: e.tensor_reduce(out=ss[:, 0:G], in_=b[:, 0:n].rearrange("p (g e) -> p g e", g=G), axis=AX.X, op=ALU.add),
          reads=[b], writes=[ss])
    P.act(lambda e: e.activation(out=ss[:, 0:G], in_=ss[:, 0:G], func=AF.Sqrt, scale=1.0 / gsz, bias=tmp["eps"][:, 0:1]),
          reads=[ss, tmp["eps"]], writes=[ss])
    P.dve(lambda e: e.reciprocal(out=ss[:, 0:G], in_=ss[:, 0:G]), reads=[ss], writes=[ss])
    P.dve(lambda e: e.tensor_tensor(out=b[:, 0:n].rearrange("p (g e) -> p g e", g=G),
                                    in0=a[:, 0:n].rearrange("p (g e) -> p g e", g=G),
                                    in1=ss[:, 0:G].unsqueeze(2).to_broadcast([128, G, gsz]), op=ALU.mult),
          reads=[a, ss], writes=[b])
    if gate_first:
        P.pool(lambda e: e.tensor_tensor(out=out[:, 0:n].rearrange("p (g e) -> p g e", g=G),
                                         in0=b[:, 0:n].rearrange("p (g e) -> p g e", g=G), in1=nw_ap, op=ALU.mult),
               reads=[b] + list(nw_bufs), writes=[out])
    else:
        P.pool(lambda e: e.tensor_tensor(out=a[:, 0:n].rearrange("p (g e) -> p g e", g=G),
                                         in0=b[:, 0:n].rearrange("p (g e) -> p g e", g=G), in1=nw_ap, op=ALU.mult),
               reads=[b] + list(nw_bufs), writes=[a])
        P.pool(lambda e: e.tensor_tensor(out=out[:, 0:n], in0=a[:, 0:n], in1=sg[:, 0:n], op=ALU.mult),
               reads=[a, sg], writes=[out])


def ng_tmp(P):
    t = dict(a=P.sbuf("ng_a", [128, 512]), b=P.sbuf("ng_b", [128, 512]), ss=P.sbuf("ng_ss", [128, 8]),
             sg=P.sbuf("ng_sg", [128, 512]), eps=P.sbuf("ng_eps", [128, 1]), one=P.sbuf("ng_one", [128, 1]))
    P.pool(lambda e: e.memset(t["eps"][:], EPS), writes=[t["eps"]])
    P.pool(lambda e: e.memset(t["one"][:], 1.0), writes=[t["one"]])
    return t


PT_NQ, PT_NKC, PT_NVC, PT_NKS, PT_NKW = 0, 512, 640, 768, 896
PT_SXBC = 1024
PT_GQKV = 2048
PT_LQ, PT_LK, PT_LLR = 3584, 3840, 4096
PT_ROWS = 4112
PN_NVS, PN_NVW, PN_NGATE, PN_SZ, PN_SDT = 0, 128, 256, 280, 792
PN_GZ, PN_GBETA, PN_GA, PN_LK, PN_LV, PN_LG = 800, 1312, 1316, 1320, 1576, 2088
PN_COLS = 2600
PT_GROUPS = ([(0 + 128 * i, 128, PT_NQ + 128 * i) for i in range(4)] +
             [(512, 128, PT_NKC), (640, 128, PT_NVC), (768, 128, PT_NKS), (1024, 128, PT_NKW)] +
             [(1816 + 128 * i, 128, PT_SXBC + 128 * i) for i in range(8)] +
             [(2848 + 128 * i, 128, PT_GQKV + 128 * i) for i in range(12)] +
             [(4904 + 128 * i, 128, PT_LQ + 128 * i) for i in range(2)] +
             [(5160 + 128 * i, 128, PT_LK + 128 * i) for i in range(2)] +
             [(6440, 16, PT_LLR)])
PN_GROUPS = [(896, 128, PN_NVS), (1152, 512, PN_NVW), (1664, 152, PN_NVW + 512), (2840, 8, PN_SDT),
             (4384, 512, PN_GZ), (4896, 8, PN_GBETA), (5160, 256, PN_LK), (5416, 512, PN_LV), (5928, 512, PN_LG)]


def stage_gla(P, C, projT, projN, ymix, prm, l):
    with P.scope():
        w2 = P.sbuf("gla_w2", [16, 256])
        gb = P.sbuf("gla_gb", [1, 256])
        nwb = P.sbuf("gla_nwb", [128, 128])
        P.dma("sp", w2[:], prm["gla_gate_w2"].t[l], reads=[prm["gla_gate_w2"]], writes=[w2])
        P.dma("sp", gb[:], prm["gla_gate_b"].t[l:l + 1, :], reads=[prm["gla_gate_b"]], writes=[gb])
        P.dma("sp", nwb[:], prm["gla_norm_w"].t[l:l + 1, :].partition_broadcast(128), reads=[prm["gla_norm_w"]], writes=[nwb])
        S = P.sbuf("gla_S", [64, 4, 128])
        Sb = [P.sbuf(f"gla_Sb{i}", [64, 4, 128], BF16) for i in range(2)]
        P.dve(lambda e: e.memset(S[:], 0.0), writes=[S])
        P.dve(lambda e: e.memset(Sb[0][:], 0.0), writes=[Sb[0]])
        tmp = ng_tmp(P)
        NB = 2
        qT = [P.sbuf(f"gla_qT{i}", [64, 4, 128]) for i in range(NB)]
        kT = [P.sbuf(f"gla_kT{i}", [64, 4, 128]) for i in range(NB)]
        lrT = [P.sbuf(f"gla_lrT{i}", [16, 128]) for i in range(NB)]
        tokN = [P.sbuf(f"gla_tokN{i}", [128, 1280]) for i in range(NB)]
        lsp = P.sbuf("gla_lsp", [128, 256])
        ex = P.sbuf("gla_ex", [128, 256])
        kend = P.sbuf("gla_kend", [128, 256], BF16)
        vb = P.sbuf("gla_vb", [128, 512], BF16)
        ebT = P.sbuf("gla_ebT", [64, 512])
        qdT = P.sbuf("gla_qdT", [64, 4, 128], BF16)
        kiT = P.sbuf("gla_kiT", [64, 4, 128], BF16)
        dec = P.sbuf("gla_dec", [64, 8])
        AT = P.sbuf("gla_AT", [128, 4, 128], BF16)
        yo = [P.sbuf(f"gla_yo{i}", [128, 512]) for i in range(2)]
        ps_gk = P.psum("gla_ps_gk", [128, 512])
        ps_bl = P.psum("gla_ps_bl", [128, 512])
        ps_bT = P.psum("gla_ps_bT", [64, 512])
        ps_blT = P.psum("gla_ps_blT", [64, 8])
        ps_at = P.psum("gla_ps_at", [128, 512])
        ps_o = P.psum("gla_ps_o", [128, 512])
        ps_loc = [P.psum(f"gla_ps_loc{i}", [64, 512]) for i in range(2)]
        cf = [C.f]

        def load(t):
            i = t % NB
            tok = slice(t * 128, (t + 1) * 128)
            P.dma("sp", qT[i][:], projT.t[PT_LQ:PT_LQ + 256, tok].rearrange("(h d) t -> d h t", d=64), reads=[projT], writes=[qT[i]])
            P.dma("sp", kT[i][:], projT.t[PT_LK:PT_LK + 256, tok].rearrange("(h d) t -> d h t", d=64), reads=[projT], writes=[kT[i]])
            P.dma("sp", lrT[i][:], projT.t[PT_LLR:PT_LLR + 16, tok], reads=[projT], writes=[lrT[i]])
            P.dma("sp", tokN[i][:], projN.t[tok, PN_LK:PN_LK + 1280], reads=[projN], writes=[tokN[i]])

        load(0)
        for t in range(NT):
            if t + 1 < NT:
                load(t + 1)
            i = t % NB
            tok = slice(t * 128, (t + 1) * 128)
            kN = tokN[i][:, 0:256]
            vN = tokN[i][:, 256:768]
            gN = tokN[i][:, 768:1280]
            P.pe(lambda e: e.matmul(ps_gk[:, 0:256], lhsT=lrT[i][:], rhs=w2[:], start=True, stop=False), reads=[lrT[i], w2], writes=[ps_gk])
            P.pe(lambda e: e.matmul(ps_gk[:, 0:256], lhsT=C.c("ones", 1), rhs=gb[:], start=False, stop=True), reads=[gb] + cf, writes=[ps_gk])
            P.act(lambda e: e.activation(out=ex[:], in_=ps_gk[:, 0:256], func=AF.Exp, scale=-1.0), reads=[ps_gk], writes=[ex])
            P.act(lambda e: e.activation(out=lsp[:], in_=ex[:], func=AF.Ln, bias=tmp["one"][:, 0:1]), reads=[ex, tmp["one"]], writes=[lsp])
            P.pe(lambda e: e.matmul(ps_gk[:, 256:512], lhsT=C.c("tri16"), rhs=lsp[:], start=True, stop=True), reads=[lsp] + cf, writes=[ps_gk])
            P.pe(lambda e: e.matmul(ps_bl[:, 0:256], lhsT=C.c("bones16"), rhs=lsp[:], start=True, stop=True), reads=[lsp] + cf, writes=[ps_bl])
            for h in range(4):
                P.pe(lambda e, h=h: e.matmul(ps_bT[:, h * 128:(h + 1) * 128], lhsT=lsp[:, h * 64:(h + 1) * 64], rhs=C.c("tri16"), start=True, stop=True),
                     reads=[lsp] + cf, writes=[ps_bT])
            for h in range(4):
                P.pe(lambda e, h=h: e.matmul(ps_blT[:, h * 2:(h + 1) * 2], lhsT=lsp[:, h * 64:(h + 1) * 64], rhs=C.c("chunkind16"), start=True, stop=True),
                     reads=[lsp] + cf, writes=[ps_blT])
            P.dve(lambda e: e.tensor_copy(out=ex[:], in_=ps_gk[:, 256:512]), reads=[ps_gk], writes=[ex])
            P.dve(lambda e: e.tensor_tensor(out=ex[:], in0=ps_bl[:, 0:256], in1=ex[:], op=ALU.subtract), reads=[ps_bl, ex], writes=[ex])
            P.act(lambda e: e.activation(out=ex[:], in_=ex[:], func=AF.Exp), reads=[ex], writes=[ex])
            P.dve(lambda e: e.tensor_tensor(out=kend[:], in0=kN, in1=ex[:], op=ALU.mult), reads=[tokN[i], ex], writes=[kend])
            P.pool(lambda e: e.tensor_copy(out=vb[:], in_=vN), reads=[tokN[i]], writes=[vb])
            P.act(lambda e: e.activation(out=ebT[:], in_=ps_bT[:], func=AF.Exp), reads=[ps_bT], writes=[ebT])
            P.dve(lambda e: e.scalar_tensor_tensor(out=qdT[:].rearrange("d h t -> d (h t)"), in0=qT[i][:].rearrange("d h t -> d (h t)"), scalar=0.125,
                                                   in1=ebT[:], op0=ALU.mult, op1=ALU.mult), reads=[qT[i], ebT], writes=[qdT])
            P.act(lambda e: e.activation(out=ebT[:], in_=ps_bT[:], func=AF.Exp, scale=-1.0), reads=[ps_bT], writes=[ebT])
            P.dve(lambda e: e.tensor_tensor(out=kiT[:].rearrange("d h t -> d (h t)"), in0=kT[i][:].rearrange("d h t -> d (h t)"), in1=ebT[:], op=ALU.mult),
                  reads=[kT[i], ebT], writes=[kiT])
            P.act(lambda e: e.activation(out=dec[:], in_=ps_blT[:], func=AF.Exp), reads=[ps_blT], writes=[dec])
            for h in range(4):
                P.pe(lambda e, h=h: e.matmul(ps_at[:, h * 128:(h + 1) * 128], lhsT=kiT[:, h, :], rhs=qdT[:, h, :], start=True, stop=True),
                     reads=[kiT, qdT], writes=[ps_at])
            P.dve(lambda e: e.tensor_tensor(out=AT[:], in0=ps_at[:].rearrange("p (h t) -> p h t", h=4),
                                            in1=C.c("tri01").unsqueeze(1).to_broadcast([128, 4, 128]), op=ALU.mult), reads=[ps_at] + cf, writes=[AT])
            for c in range(2):
                rows = slice(c * 64, (c + 1) * 64)
                for h in range(4):
                    P.pe(lambda e, h=h, rows=rows, c=c: e.matmul(ps_loc[c][:, h * 128:(h + 1) * 128], lhsT=kend[rows, h * 64:(h + 1) * 64],
                                                                 rhs=vb[rows, h * 128:(h + 1) * 128], start=True, stop=True),
                         reads=[kend, vb], writes=[ps_loc[c]])
            for c in range(2):
                P.dve(lambda e, c=c: e.tensor_tensor(out=S[:], in0=S[:], in1=dec[:].rearrange("d (h c) -> d h c", c=2)[:, :, c:c + 1].to_broadcast([64, 4, 128]),
                                                     op=ALU.mult), reads=[S, dec], writes=[S])
                P.dve(lambda e, c=c: e.tensor_tensor(out=S[:].rearrange("d h e -> d (h e)"), in0=S[:].rearrange("d h e -> d (h e)"), in1=ps_loc[c][:], op=ALU.add),
                      reads=[S, ps_loc[c]], writes=[S])
                if c == 0:
                    P.act(lambda e: e.copy(out=Sb[1][:], in_=S[:]), reads=[S], writes=[Sb[1]])
            for h in range(4):
                cols = slice(h * 128, (h + 1) * 128)
                P.pe(lambda e, h=h, cols=cols: e.matmul(ps_o[:, cols], lhsT=AT[:, h, :], rhs=vb[:, cols], start=True, stop=False),
                     reads=[AT, vb], writes=[ps_o])
                for c in range(2):
                    rows = slice(c * 64, (c + 1) * 64)
                    P.pe(lambda e, h=h, cols=cols, rows=rows, c=c: e.matmul(ps_o[rows, cols], lhsT=qdT[:, h, rows], rhs=Sb[c][:, h, :], start=False, stop=(c == 1)),
                         reads=[qdT, Sb[c]], writes=[ps_o])
            P.act(lambda e: e.copy(out=Sb[0][:], in_=S[:]), reads=[S], writes=[Sb[0]])
            y = yo[t % 2]
            norm_gate(P, ps_o[:], [ps_o], gN, [tokN[i]], nwb[:].unsqueeze(1).to_broadcast([128, 4, 128]), [nwb], 4, 128, y, tmp, False)
            P.dma("sp", ymix.t[tok, 1536:2048], y[:], reads=[y], writes=[ymix])


def host_param(name, arr):
    a = np.asarray(arr, np.float32)
    if name in ("ssd_conv_w", "gdn_conv_w"):
        L, K, CH = a.shape
        a = a.reshape(L, K, CH // 128, 128).transpose(0, 3, 2, 1)
    elif name in ("norm1_w", "norm2_w"):
        L = a.shape[0]
        a = a.reshape(L, 16, 128).transpose(2, 0, 1)
    elif name == "final_norm_w":
        a = a.reshape(1, -1)
    elif name == "nsa_cmp_pos":
        a = a.transpose(0, 1, 3, 2)
    elif name == "ssd_conv_b":
        L, CH = a.shape
        a = a.reshape(L, CH // 128, 128).transpose(0, 2, 1)
    return np.ascontiguousarray(a)


def bc(ap, shape):
    return ap.to_broadcast(list(shape))


def causal_conv_silu(P, projT, row0, ntiles, cw, cb, dst, dst_off, name, bias=True):
    xpad = [P.sbuf(f"{name}_xpad{i}", [128, SEQ + 3]) for i in range(2)]
    acc = [P.sbuf(f"{name}_acc{i}", [128, SEQ]) for i in range(2)]
    for i in range(2):
        P.pool(lambda e, i=i: e.memset(xpad[i][:, 0:3], 0.0), writes=[xpad[i]])
    for ct in range(ntiles):
        xp = xpad[ct % 2]
        ac = acc[ct % 2]
        P.dma("sp", xp[:, 3:SEQ + 3], projT.t[row0 + ct * 128:row0 + (ct + 1) * 128, :], reads=[projT], writes=[xp])
        eng = P.dve
        eng(lambda e, ct=ct, xp=xp, ac=ac: e.tensor_scalar(out=ac[:], in0=xp[:, 0:SEQ], scalar1=cw[:, ct, 0:1], scalar2=None, op0=ALU.mult),
            reads=[xp, cw], writes=[ac])
        for k in range(1, 4):
            eng(lambda e, ct=ct, xp=xp, ac=ac, k=k: e.scalar_tensor_tensor(out=ac[:], in0=xp[:, k:SEQ + k], scalar=cw[:, ct, k:k + 1], in1=ac[:],
                                                                            op0=ALU.mult, op1=ALU.add), reads=[xp, cw, ac], writes=[ac])
        if bias:
            P.act(lambda e, ct=ct, ac=ac: e.activation(out=dst[:, dst_off + ct, :], in_=ac[:], func=AF.Silu, bias=cb[:, ct:ct + 1]),
                  reads=[ac, cb], writes=[dst])
        else:
            P.act(lambda e, ct=ct, ac=ac: e.activation(out=dst[:, dst_off + ct, :], in_=ac[:], func=AF.Silu), reads=[ac], writes=[dst])


def softplus_small(P, x_ap, xbuf, tmpb, one):
    P.act(lambda e: e.activation(out=x_ap, in_=x_ap, func=AF.Exp), reads=[xbuf], writes=[xbuf])
    P.act(lambda e: e.activation(out=x_ap, in_=x_ap, func=AF.Ln, bias=one[:, 0:1]), reads=[xbuf, one], writes=[xbuf])


def stage_ssd(P, C, projT, projN, ymix, prm, l):
    with P.scope():
        cf = [C.f]
        cw = P.sbuf("ssd_cw", [128, 8, 4])
        cb = P.sbuf("ssd_cb", [128, 8])
        dtb = P.sbuf("ssd_dtb", [128, 8])
        aneg = P.sbuf("ssd_aneg", [128, 8])
        dsk = P.sbuf("ssd_dsk", [128, 8])
        nwb = P.sbuf("ssd_nwb", [128, 512])
        P.dma("sp", cw[:], prm["ssd_conv_w"].t[l], reads=[prm["ssd_conv_w"]], writes=[cw])
        P.dma("sp", cb[:], prm["ssd_conv_b"].t[l], reads=[prm["ssd_conv_b"]], writes=[cb])
        P.dma("sp", dtb[:], prm["ssd_dt_bias"].t[l:l + 1, :].partition_broadcast(128), reads=[prm["ssd_dt_bias"]], writes=[dtb])
        P.dma("sp", aneg[:], prm["ssd_a_log"].t[l:l + 1, :].partition_broadcast(128), reads=[prm["ssd_a_log"]], writes=[aneg])
        P.dma("sp", dsk[:], prm["ssd_d"].t[l:l + 1, :].partition_broadcast(128), reads=[prm["ssd_d"]], writes=[dsk])
        P.dma("sp", nwb[:], prm["ssd_norm_w"].t[l:l + 1, :].partition_broadcast(128), reads=[prm["ssd_norm_w"]], writes=[nwb])
        P.act(lambda e: e.activation(out=aneg[:], in_=aneg[:], func=AF.Exp), reads=[aneg], writes=[aneg])
        P.dve(lambda e: e.tensor_scalar(out=aneg[:], in0=aneg[:], scalar1=-1.0, scalar2=None, op0=ALU.mult), reads=[aneg], writes=[aneg])
        act = P.sbuf("ssd_act", [128, 8, SEQ])
        with P.scope():
            causal_conv_silu(P, projT, PT_SXBC, 8, cw, cb, act, 0, "ssd")
        BCb = P.sbuf("ssd_BCb", [128, 4, SEQ], BF16)
        for k in range(4):
            (P.dve if k % 2 == 0 else P.pool)(lambda e, k=k: e.tensor_copy(out=BCb[:, k, :], in_=act[:, 4 + k, :]), reads=[act], writes=[BCb])
        tmp = ng_tmp(P)
        S = P.sbuf("ssd_S", [128, 8, 64])
        Sb = [P.sbuf(f"ssd_Sb{i}", [128, 8, 64], BF16) for i in range(2)]
        P.dve(lambda e: e.memset(S[:], 0.0), writes=[S])
        P.dve(lambda e: e.memset(Sb[0][:], 0.0), writes=[Sb[0]])
        tokN = [P.sbuf(f"ssd_tokN{i}", [128, 520]) for i in range(2)]
        xN = P.sbuf("ssd_xN", [128, 512])
        BNb = P.sbuf("ssd_BNb", [128, 256], BF16)
        dt8 = P.sbuf("ssd_dt8", [128, 8])
        a8 = P.sbuf("ssd_a8", [128, 8])
        dw8 = P.sbuf("ssd_dw8", [128, 8])
        ac16 = P.sbuf("ssd_ac16", [128, 2, 8])
        e32 = P.sbuf("ssd_e32", [128, 32])
        Aexp = P.sbuf("ssd_Aexp", [128, 8, 128])
        seg = P.sbuf("ssd_seg", [128, 8, 128])
        CBm = P.sbuf("ssd_CBm", [128, 2, 128])
        MT = P.sbuf("ssd_MT", [128, 8, 128], BF16)
        xdt = P.sbuf("ssd_xdt", [128, 8, 64], BF16)
        xw = P.sbuf("ssd_xw", [128, 8, 64], BF16)
        y1 = P.sbuf("ssd_y1", [128, 512])
        y2 = P.sbuf("ssd_y2", [128, 512])
        yo = [P.sbuf(f"ssd_yo{i}", [128, 512]) for i in range(2)]
        psA = P.psum("ssd_psA", [128, 512])
        psB = P.psum("ssd_psB", [128, 512])
        psC = P.psum("ssd_psC", [128, 512])
        psD = P.psum("ssd_psD", [128, 512])
        psE = P.psum("ssd_psE", [128, 512])
        psF = P.psum("ssd_psF", [128, 512])
        psG = [P.psum(f"ssd_psG{i}", [128, 512]) for i in range(2)]

        def load(t):
            P.dma("sp", tokN[t % 2][:], projN.t[t * 128:(t + 1) * 128, PN_SZ:PN_SZ + 520], reads=[projN], writes=[tokN[t % 2]])

        load(0)
        for t in range(NT):
            if t + 1 < NT:
                load(t + 1)
            tk = tokN[t % 2]
            tok = slice(t * 128, (t + 1) * 128)
            for k in range(4):
                P.pe(lambda e, k=k: e.transpose(out=psA[:, k * 128:(k + 1) * 128], in_=act[:, k, tok], identity=C.c("ident")), reads=[act] + cf, writes=[psA])
            for k in range(2):
                P.pe(lambda e, k=k: e.transpose(out=psB[:, k * 128:(k + 1) * 128], in_=act[:, 4 + k, tok], identity=C.c("ident")), reads=[act] + cf, writes=[psB])
            P.act(lambda e: e.copy(out=xN[:], in_=psA[:]), reads=[psA], writes=[xN])
            P.dve(lambda e: e.tensor_copy(out=BNb[:], in_=psB[:, 0:256]), reads=[psB], writes=[BNb])
            P.dve(lambda e: e.tensor_tensor(out=dt8[:], in0=tk[:, 512:520], in1=dtb[:], op=ALU.add), reads=[tk, dtb], writes=[dt8])
            softplus_small(P, dt8[:], dt8, None, tmp["one"])
            P.dve(lambda e: e.tensor_tensor(out=a8[:], in0=dt8[:], in1=aneg[:], op=ALU.mult), reads=[dt8, aneg], writes=[a8])
            P.dve(lambda e: e.tensor_tensor(out=Aexp[:], in0=bc(a8[:].unsqueeze(2), [128, 8, 128]), in1=bc(C.c("tri01").unsqueeze(1), [128, 8, 128]), op=ALU.mult),
                  reads=[a8] + cf, writes=[Aexp])
            P.dve(lambda e: e.tensor_tensor(out=ac16[:], in0=bc(a8[:].unsqueeze(1), [128, 2, 8]), in1=bc(C.c("chunkind").unsqueeze(2), [128, 2, 8]), op=ALU.mult),
                  reads=[a8] + cf, writes=[ac16])
            P.pe(lambda e: e.matmul(psC[:], lhsT=C.c("su01"), rhs=Aexp[:, 0:4, :].rearrange("p h i -> p (h i)"), start=True, stop=True), reads=[Aexp] + cf, writes=[psC])
            P.pe(lambda e: e.matmul(psD[:], lhsT=C.c("su01"), rhs=Aexp[:, 4:8, :].rearrange("p h i -> p (h i)"), start=True, stop=True), reads=[Aexp] + cf, writes=[psD])
            P.act(lambda e: e.activation(out=seg[:, 0:4, :].rearrange("p h i -> p (h i)"), in_=psC[:], func=AF.Exp), reads=[psC], writes=[seg])
            P.act(lambda e: e.activation(out=seg[:, 4:8, :].rearrange("p h i -> p (h i)"), in_=psD[:], func=AF.Exp), reads=[psD], writes=[seg])
            P.pe(lambda e: e.matmul(psE[:, 0:8], lhsT=C.c("tri01"), rhs=a8[:], start=True, stop=True), reads=[a8] + cf, writes=[psE])
            P.pe(lambda e: e.matmul(psE[:, 8:16], lhsT=C.c("su01"), rhs=a8[:], start=True, stop=True), reads=[a8] + cf, writes=[psE])
            P.pe(lambda e: e.matmul(psE[:, 16:32], lhsT=C.c("ones"), rhs=ac16[:].rearrange("p c h -> p (c h)"), start=True, stop=True), reads=[ac16] + cf, writes=[psE])
            P.act(lambda e: e.activation(out=e32[:], in_=psE[:, 0:32], func=AF.Exp), reads=[psE], writes=[e32])
            ea = e32[:, 0:8]
            w8 = e32[:, 8:16]
            for g in range(2):
                P.pe(lambda e, g=g: e.matmul(psB[:, 256 + g * 128:256 + (g + 1) * 128], lhsT=BCb[:, g, tok], rhs=BCb[:, 2 + g, tok], start=True, stop=True),
                     reads=[BCb], writes=[psB])
            P.dve(lambda e: e.tensor_tensor(out=CBm[:], in0=psB[:, 256:512].rearrange("p (g i) -> p g i", g=2), in1=bc(C.c("tri01").unsqueeze(1), [128, 2, 128]), op=ALU.mult),
                  reads=[psB] + cf, writes=[CBm])
            P.dve(lambda e: e.tensor_tensor(out=MT[:].rearrange("p (g r) i -> p g r i", g=2), in0=seg[:].rearrange("p (g r) i -> p g r i", g=2),
                                            in1=bc(CBm[:].unsqueeze(2), [128, 2, 4, 128]), op=ALU.mult), reads=[seg, CBm], writes=[MT])
            P.dve(lambda e: e.tensor_tensor(out=dw8[:], in0=dt8[:], in1=w8, op=ALU.mult), reads=[dt8, e32], writes=[dw8])
            P.pool(lambda e: e.tensor_tensor(out=xdt[:], in0=xN[:].rearrange("p (h q) -> p h q", h=8), in1=bc(dt8[:].unsqueeze(2), [128, 8, 64]), op=ALU.mult),
                   reads=[xN, dt8], writes=[xdt])
            P.pool(lambda e: e.tensor_tensor(out=xw[:], in0=xN[:].rearrange("p (h q) -> p h q", h=8), in1=bc(dw8[:].unsqueeze(2), [128, 8, 64]), op=ALU.mult),
                   reads=[xN, dw8], writes=[xw])
            for h in range(8):
                P.pe(lambda e, h=h: e.matmul(psF[:, h * 64:(h + 1) * 64], lhsT=MT[:, h, :], rhs=xdt[:, h, :], start=True, stop=True), reads=[MT, xdt], writes=[psF])
            for c in range(2):
                rows = slice(c * 64, (c + 1) * 64)
                for g in range(2):
                    P.pe(lambda e, c=c, g=g, rows=rows: e.matmul(psG[c][:, g * 256:(g + 1) * 256], lhsT=BNb[rows, g * 128:(g + 1) * 128],
                                                                 rhs=xw[rows, 4 * g:4 * g + 4, :].rearrange("p h q -> p (h q)"), start=True, stop=True),
                         reads=[BNb, xw], writes=[psG[c]])
            for c in range(2):
                P.dve(lambda e, c=c: e.tensor_tensor(out=S[:], in0=S[:], in1=bc(e32[:, 16 + 8 * c:24 + 8 * c].unsqueeze(2), [128, 8, 64]), op=ALU.mult),
                      reads=[S, e32], writes=[S])
                P.dve(lambda e, c=c: e.tensor_tensor(out=S[:].rearrange("p h q -> p (h q)"), in0=S[:].rearrange("p h q -> p (h q)"), in1=psG[c][:], op=ALU.add),
                      reads=[S, psG[c]], writes=[S])
                if c == 0:
                    P.act(lambda e: e.copy(out=Sb[1][:], in_=S[:]), reads=[S], writes=[Sb[1]])
            for c in range(2):
                rows = slice(c * 64, (c + 1) * 64)
                for g in range(2):
                    P.pe(lambda e, c=c, g=g, rows=rows: e.matmul(psC[rows, g * 256:(g + 1) * 256], lhsT=BCb[:, 2 + g, t * 128 + c * 64:t * 128 + (c + 1) * 64],
                                                                 rhs=Sb[c][:, 4 * g:4 * g + 4, :].rearrange("p h q -> p (h q)"), start=True, stop=True),
                         reads=[BCb, Sb[c]], writes=[psC])
            P.act(lambda e: e.copy(out=Sb[0][:], in_=S[:]), reads=[S], writes=[Sb[0]])
            P.dve(lambda e: e.tensor_tensor(out=y1[:].rearrange("p (h q) -> p h q", h=8), in0=psC[:].rearrange("p (h q) -> p h q", h=8),
                                            in1=bc(ea.unsqueeze(2), [128, 8, 64]), op=ALU.mult), reads=[psC, e32], writes=[y1])
            P.dve(lambda e: e.tensor_tensor(out=y1[:], in0=y1[:], in1=psF[:], op=ALU.add), reads=[y1, psF], writes=[y1])
            P.pool(lambda e: e.tensor_tensor(out=y2[:].rearrange("p (h q) -> p h q", h=8), in0=xN[:].rearrange("p (h q) -> p h q", h=8),
                                             in1=bc(dsk[:].unsqueeze(2), [128, 8, 64]), op=ALU.mult), reads=[xN, dsk], writes=[y2])
            P.pool(lambda e: e.tensor_tensor(out=y1[:], in0=y1[:], in1=y2[:], op=ALU.add), reads=[y1, y2], writes=[y1])
            y = yo[t % 2]
            norm_gate(P, y1[:], [y1], tk[:, 0:512], [tk], nwb[:].rearrange("p (g e) -> p g e", g=2), [nwb], 2, 256, y, tmp, True)
            P.dma("sp", ymix.t[tok, 512:1024], y[:], reads=[y], writes=[ymix])


def stage_gdn(P, C, projT, projN, ymix, prm, l):
    H = 4
    with P.scope():
        cf = [C.f]
        cw = P.sbuf("gdn_cw", [128, 12, 4])
        dtb = P.sbuf("gdn_dtb", [128, 4])
        aneg = P.sbuf("gdn_aneg", [128, 4])
        nwb = P.sbuf("gdn_nwb", [128, 128])
        P.dma("sp", cw[:], prm["gdn_conv_w"].t[l], reads=[prm["gdn_conv_w"]], writes=[cw])
        P.dma("sp", dtb[:], prm["gdn_dt_bias"].t[l:l + 1, :].partition_broadcast(128), reads=[prm["gdn_dt_bias"]], writes=[dtb])
        P.dma("sp", aneg[:], prm["gdn_a_log"].t[l:l + 1, :].partition_broadcast(128), reads=[prm["gdn_a_log"]], writes=[aneg])
        P.dma("sp", nwb[:], prm["gdn_norm_w"].t[l:l + 1, :].partition_broadcast(128), reads=[prm["gdn_norm_w"]], writes=[nwb])
        P.act(lambda e: e.activation(out=aneg[:], in_=aneg[:], func=AF.Exp), reads=[aneg], writes=[aneg])
        P.dve(lambda e: e.tensor_scalar(out=aneg[:], in0=aneg[:], scalar1=-1.0, scalar2=None, op0=ALU.mult), reads=[aneg], writes=[aneg])
        tmp = ng_tmp(P)
        qkvb = P.sbuf("gdn_qkvb", [128, 12, SEQ], BF16)
        with P.scope():
            cvt = P.sbuf("gdn_cvt", [128, 1, SEQ])
            sq = P.sbuf("gdn_sq", [128, SEQ])
            rinv = P.sbuf("gdn_rinv", [128, SEQ])
            pss = [P.psum(f"gdn_pss{i}", [128, 512]) for i in range(4)]
            xpad = [P.sbuf(f"gdn_xpad{i}", [128, SEQ + 3]) for i in range(2)]
            acc = P.sbuf("gdn_acc", [128, SEQ])
            for i in range(2):
                P.pool(lambda e, i=i: e.memset(xpad[i][:, 0:3], 0.0), writes=[xpad[i]])
            for ct in range(12):
                xp = xpad[ct % 2]
                P.dma("sp", xp[:, 3:SEQ + 3], projT.t[PT_GQKV + ct * 128:PT_GQKV + (ct + 1) * 128, :], reads=[projT], writes=[xp])
                P.dve(lambda e, ct=ct, xp=xp: e.tensor_scalar(out=acc[:], in0=xp[:, 0:SEQ], scalar1=cw[:, ct, 0:1], scalar2=None, op0=ALU.mult),
                      reads=[xp, cw], writes=[acc])
                for k in range(1, 4):
                    P.dve(lambda e, ct=ct, xp=xp, k=k: e.scalar_tensor_tensor(out=acc[:], in0=xp[:, k:SEQ + k], scalar=cw[:, ct, k:k + 1], in1=acc[:],
                                                                               op0=ALU.mult, op1=ALU.add), reads=[xp, cw, acc], writes=[acc])
                if ct >= 8:
                    P.act(lambda e, ct=ct: e.activation(out=qkvb[:, ct, :], in_=acc[:], func=AF.Silu), reads=[acc], writes=[qkvb])
                    continue
                P.act(lambda e: e.activation(out=cvt[:, 0, :], in_=acc[:], func=AF.Silu), reads=[acc], writes=[cvt])
                P.act(lambda e: e.activation(out=sq[:], in_=cvt[:, 0, :], func=AF.Square), reads=[cvt], writes=[sq])
                for n in range(4):
                    P.pe(lambda e, n=n: e.matmul(pss[n][:], lhsT=C.c("ones"), rhs=sq[:, n * 512:(n + 1) * 512], start=True, stop=True), reads=[sq] + cf, writes=[pss[n]])
                    P.act(lambda e, n=n: e.activation(out=rinv[:, n * 512:(n + 1) * 512], in_=pss[n][:], func=AF.Sqrt, bias=tmp["eps"][:, 0:1]),
                          reads=[pss[n], tmp["eps"]], writes=[rinv])
                P.dve(lambda e: e.reciprocal(out=rinv[:], in_=rinv[:]), reads=[rinv], writes=[rinv])
                scl = 128.0 ** -0.5 if ct < 4 else 1.0
                P.dve(lambda e, ct=ct, scl=scl: e.scalar_tensor_tensor(out=qkvb[:, ct, :], in0=cvt[:, 0, :], scalar=scl, in1=rinv[:], op0=ALU.mult, op1=ALU.mult),
                      reads=[cvt, rinv], writes=[qkvb])
        dbg(1)
        S = P.sbuf("gdn_S", [128, H, 128])
        Sb = P.sbuf("gdn_Sb", [128, H, 128], BF16)
        P.dve(lambda e: e.memset(S[:], 0.0), writes=[S])
        P.dve(lambda e: e.memset(Sb[:], 0.0), writes=[Sb])
        tokN = [P.sbuf(f"gdn_tokN{i}", [128, 520]) for i in range(2)]

        def f4(name, dt=F32):
            return P.sbuf("gdn_" + name, [128, H, 128], dt)

        b4 = P.sbuf("gdn_b4", [128, 4])
        g4 = P.sbuf("gdn_g4", [128, 4])
        gc8 = P.sbuf("gdn_gc8", [128, 2, 4])
        e16 = P.sbuf("gdn_e16", [128, 16])
        bg4 = P.sbuf("gdn_bg4", [128, 4])
        Gt, Gs, DTm, Dm, egb = f4("Gt"), f4("Gs"), f4("DTm"), f4("Dm"), f4("egb")
        A, AT, TT, vb, Kg, u = f4("A"), f4("AT"), f4("TT"), f4("vb"), f4("Kg"), f4("u")
        X = [f4("X0"), f4("X1")]
        XT = [f4("XT0"), f4("XT1")]
        aqkT, wT, qdT, kend, vnew = f4("aqkT", BF16), f4("wT", BF16), f4("qdT", BF16), f4("kend", BF16), f4("vnew", BF16)
        yo = [P.sbuf(f"gdn_yo{i}", [128, 512]) for i in range(2)]
        B0 = P.psum("gdn_B0", [128, 512])
        B1 = P.psum("gdn_B1", [128, 512])
        B2 = P.psum("gdn_B2", [128, 512])
        B3 = P.psum("gdn_B3", [128, 512])
        B4 = P.psum("gdn_B4", [128, 1024], BF16)
        B5 = P.psum("gdn_B5", [128, 512])
        B6 = P.psum("gdn_B6", [128, 512])
        B7 = P.psum("gdn_B7", [128, 512])

        def v4(ap):
            return ap.rearrange("p (h i) -> p h i", h=H)

        def fl(ap):
            return ap.rearrange("p h i -> p (h i)")

        def load(t):
            P.dma("sp", tokN[t % 2][:], projN.t[t * 128:(t + 1) * 128, PN_GZ:PN_GZ + 520], reads=[projN], writes=[tokN[t % 2]])

        tri = C.c("tri01")
        su = C.c("su01")
        load(0)
        for t in range(NT):
            if t + 1 < NT:
                load(t + 1)
            tk = tokN[t % 2]
            tok = slice(t * 128, (t + 1) * 128)
            P.act(lambda e: e.activation(out=b4[:], in_=tk[:, 512:516], func=AF.Sigmoid), reads=[tk], writes=[b4])
            P.dve(lambda e: e.tensor_tensor(out=g4[:], in0=tk[:, 516:520], in1=dtb[:], op=ALU.add), reads=[tk, dtb], writes=[g4])
            softplus_small(P, g4[:], g4, None, tmp["one"])
            P.dve(lambda e: e.tensor_tensor(out=g4[:], in0=g4[:], in1=aneg[:], op=ALU.mult), reads=[g4, aneg], writes=[g4])
            P.dve(lambda e: e.tensor_tensor(out=Gt[:], in0=bc(g4[:].unsqueeze(2), [128, H, 128]), in1=bc(tri.unsqueeze(1), [128, H, 128]), op=ALU.mult),
                  reads=[g4] + cf, writes=[Gt])
            P.pool(lambda e: e.tensor_tensor(out=Gs[:], in0=bc(g4[:].unsqueeze(2), [128, H, 128]), in1=bc(su.unsqueeze(1), [128, H, 128]), op=ALU.mult),
                   reads=[g4] + cf, writes=[Gs])
            P.dve(lambda e: e.tensor_tensor(out=gc8[:], in0=bc(g4[:].unsqueeze(1), [128, 2, 4]), in1=bc(C.c("chunkind").unsqueeze(2), [128, 2, 4]), op=ALU.mult),
                  reads=[g4] + cf, writes=[gc8])
            P.pe(lambda e: e.matmul(B0[:], lhsT=su, rhs=fl(Gt[:]), start=True, stop=True), reads=[Gt] + cf, writes=[B0])
            P.pe(lambda e: e.matmul(B1[:], lhsT=tri, rhs=fl(Gs[:]), start=True, stop=True), reads=[Gs] + cf, writes=[B1])
            P.pe(lambda e: e.matmul(B2[:], lhsT=C.c("ones"), rhs=fl(Gt[:]), start=True, stop=True), reads=[Gt] + cf, writes=[B2])
            P.pe(lambda e: e.matmul(B3[:, 0:4], lhsT=tri, rhs=g4[:], start=True, stop=True), reads=[g4] + cf, writes=[B3])
            P.pe(lambda e: e.matmul(B3[:, 4:8], lhsT=su, rhs=g4[:], start=True, stop=True), reads=[g4] + cf, writes=[B3])
            P.pe(lambda e: e.matmul(B3[:, 8:16], lhsT=C.c("ones"), rhs=gc8[:].rearrange("p c h -> p (c h)"), start=True, stop=True), reads=[gc8] + cf, writes=[B3])
            P.act(lambda e: e.activation(out=fl(DTm[:]), in_=B0[:], func=AF.Exp), reads=[B0], writes=[DTm])
            P.act(lambda e: e.activation(out=fl(Dm[:]), in_=B1[:], func=AF.Exp), reads=[B1], writes=[Dm])
            P.act(lambda e: e.activation(out=fl(egb[:]), in_=B2[:], func=AF.Exp), reads=[B2], writes=[egb])
            P.act(lambda e: e.activation(out=e16[:], in_=B3[:, 0:16], func=AF.Exp), reads=[B3], writes=[e16])
            P.pool(lambda e: e.tensor_tensor(out=DTm[:], in0=DTm[:], in1=bc(tri.unsqueeze(1), [128, H, 128]), op=ALU.mult), reads=[DTm] + cf, writes=[DTm])
            P.pool(lambda e: e.tensor_tensor(out=Dm[:], in0=Dm[:], in1=bc(su.unsqueeze(1), [128, H, 128]), op=ALU.mult), reads=[Dm] + cf, writes=[Dm])
            P.dve(lambda e: e.tensor_tensor(out=bg4[:], in0=b4[:], in1=e16[:, 0:4], op=ALU.mult), reads=[b4, e16], writes=[bg4])
            dbg(2)
            for h in range(H):
                P.pe(lambda e, h=h: e.transpose(out=B4[:, h * 128:(h + 1) * 128], in_=qkvb[:, 4 + h, tok], identity=C.identb[:]), reads=[qkvb, C.identb], writes=[B4])
            for h in range(H):
                P.pe(lambda e, h=h: e.transpose(out=B4[:, 512 + h * 128:512 + (h + 1) * 128], in_=qkvb[:, 8 + h, tok], identity=C.identb[:]), reads=[qkvb, C.identb], writes=[B4])
            P.dve(lambda e: e.tensor_tensor(out=vb[:], in0=v4(B4[:, 512:1024]), in1=bc(b4[:].unsqueeze(2), [128, H, 128]), op=ALU.mult), reads=[B4, b4], writes=[vb])
            P.dve(lambda e: e.tensor_tensor(out=Kg[:], in0=v4(B4[:, 0:512]), in1=bc(bg4[:].unsqueeze(2), [128, H, 128]), op=ALU.mult), reads=[B4, bg4], writes=[Kg])
            P.dve(lambda e: e.tensor_tensor(out=kend[:], in0=v4(B4[:, 0:512]), in1=bc(e16[:, 4:8].unsqueeze(2), [128, H, 128]), op=ALU.mult), reads=[B4, e16], writes=[kend])
            dbg(3)
            for h in range(H):
                P.pe(lambda e, h=h: e.matmul(B5[:, h * 128:(h + 1) * 128], lhsT=qkvb[:, 4 + h, tok], rhs=qkvb[:, 4 + h, tok], start=True, stop=True), reads=[qkvb], writes=[B5])
            for h in range(H):
                P.pe(lambda e, h=h: e.matmul(B6[:, h * 128:(h + 1) * 128], lhsT=qkvb[:, 4 + h, tok], rhs=qkvb[:, h, tok], start=True, stop=True), reads=[qkvb], writes=[B6])
            P.dve(lambda e: e.tensor_tensor(out=A[:], in0=v4(B5[:]), in1=Dm[:], op=ALU.mult), reads=[B5, Dm], writes=[A])
            P.dve(lambda e: e.tensor_tensor(out=A[:], in0=A[:], in1=bc(b4[:].unsqueeze(2), [128, H, 128]), op=ALU.mult), reads=[A, b4], writes=[A])
            P.dve(lambda e: e.tensor_tensor(out=aqkT[:], in0=v4(B6[:]), in1=DTm[:], op=ALU.mult), reads=[B6, DTm], writes=[aqkT])
            P.pool(lambda e: e.tensor_tensor(out=qdT[:], in0=qkvb[:, 0:4, tok], in1=egb[:], op=ALU.mult), reads=[qkvb, egb], writes=[qdT])
            dbg(4)
            for h in range(H):
                P.pe(lambda e, h=h: e.transpose(out=B0[:, h * 128:(h + 1) * 128], in_=A[:, h, :], identity=C.c("ident")), reads=[A] + cf, writes=[B0])
            dbg(4.3)
            P.act(lambda e: e.copy(out=fl(AT[:]), in_=B0[:]), reads=[B0], writes=[AT])
            dbg(4.6)
            P.dve(lambda e: e.scalar_tensor_tensor(out=TT[:], in0=v4(B0[:]), scalar=-1.0, in1=bc(C.c("ident").unsqueeze(1), [128, H, 128]), op0=ALU.mult, op1=ALU.add),
                  reads=[B0] + cf, writes=[TT])
            dbg(5)
            Xc, XTc = A, AT
            for k in range(1, 6):
                Xn, XTn = X[k % 2], XT[k % 2]
                for h in range(H):
                    P.pe(lambda e, h=h, Xc=Xc, XTc=XTc: e.matmul(B1[:, h * 128:(h + 1) * 128], lhsT=XTc[:, h, :], rhs=Xc[:, h, :], start=True, stop=True),
                         reads=[Xc, XTc], writes=[B1])
                if k < 5:
                    for h in range(H):
                        P.pe(lambda e, h=h, Xc=Xc, XTc=XTc: e.matmul(B2[:, h * 128:(h + 1) * 128], lhsT=Xc[:, h, :], rhs=XTc[:, h, :], start=True, stop=True),
                             reads=[Xc, XTc], writes=[B2])
                P.act(lambda e, Xn=Xn: e.copy(out=fl(Xn[:]), in_=B1[:]), reads=[B1], writes=[Xn])
                if k < 5:
                    P.dve(lambda e, XTn=XTn: e.tensor_copy(out=fl(XTn[:]), in_=B2[:]), reads=[B2], writes=[XTn])
                for h in range(H):
                    P.pe(lambda e, h=h, Xn=Xn: e.matmul(B5[:, h * 128:(h + 1) * 128], lhsT=Xn[:, h, :], rhs=TT[:, h, :], start=True, stop=True),
                         reads=[Xn, TT], writes=[B5])
                P.dve(lambda e: e.tensor_tensor(out=fl(TT[:]), in0=fl(TT[:]), in1=B5[:], op=ALU.add), reads=[TT, B5], writes=[TT])
                Xc, XTc = Xn, XTn
            dbg(6)
            for h in range(H):
                P.pe(lambda e, h=h: e.matmul(B0[:, h * 128:(h + 1) * 128], lhsT=TT[:, h, :], rhs=vb[:, h, :], start=True, stop=True), reads=[TT, vb], writes=[B0])
            for h in range(H):
                P.pe(lambda e, h=h: e.matmul(B5[:, h * 128:(h + 1) * 128], lhsT=Kg[:, h, :], rhs=TT[:, h, :], start=True, stop=True), reads=[TT, Kg], writes=[B5])
            P.act(lambda e: e.copy(out=fl(u[:]), in_=B0[:]), reads=[B0], writes=[u])
            P.dve(lambda e: e.tensor_copy(out=fl(wT[:]), in_=B5[:]), reads=[B5], writes=[wT])
            dbg(7)
            for c in range(2):
                rows = slice(c * 64, (c + 1) * 64)
                for h in range(H):
                    P.pe(lambda e, h=h, rows=rows: e.matmul(B6[rows, h * 128:(h + 1) * 128], lhsT=wT[:, h, rows], rhs=Sb[:, h, :], start=True, stop=True),
                         reads=[wT, Sb], writes=[B6])
                P.dve(lambda e, rows=rows: e.tensor_tensor(out=fl(vnew[rows]), in0=fl(u[rows]), in1=B6[rows, :], op=ALU.subtract), reads=[u, B6], writes=[vnew])
                for h in range(H):
                    P.pe(lambda e, h=h, rows=rows: e.matmul(B7[rows, h * 128:(h + 1) * 128], lhsT=qdT[:, h, rows], rhs=Sb[:, h, :], start=True, stop=False),
                         reads=[qdT, Sb], writes=[B7])
                    P.pe(lambda e, h=h, rows=rows: e.matmul(B7[rows, h * 128:(h + 1) * 128], lhsT=aqkT[rows, h, rows], rhs=vnew[rows, h, :], start=False, stop=True),
                         reads=[aqkT, vnew], writes=[B7])
                for h in range(H):
                    P.pe(lambda e, h=h, rows=rows: e.matmul(B1[:, h * 128:(h + 1) * 128], lhsT=kend[rows, h, :], rhs=vnew[rows, h, :], start=True, stop=True),
                         reads=[kend, vnew], writes=[B1])
                P.dve(lambda e, c=c: e.tensor_tensor(out=S[:], in0=S[:], in1=bc(e16[:, 8 + 4 * c:12 + 4 * c].unsqueeze(2), [128, H, 128]), op=ALU.mult),
                      reads=[S, e16], writes=[S])
                P.dve(lambda e: e.tensor_tensor(out=fl(S[:]), in0=fl(S[:]), in1=B1[:], op=ALU.add), reads=[S, B1], writes=[S])
                P.act(lambda e: e.copy(out=Sb[:], in_=S[:]), reads=[S], writes=[Sb])
            dbg(8)
            y = yo[t % 2]
            norm_gate(P, B7[:], [B7], tk[:, 0:512], [tk], bc(nwb[:].unsqueeze(1), [128, 4, 128]), [nwb], 4, 128, y, tmp, False)
            P.dma("sp", ymix.t[tok, 1024:1536], y[:], reads=[y], writes=[ymix])


NBIG = 30000.0


def t5_bucket_np(rel):
    n = np.maximum(rel, 0)
    exact = 16
    large = exact + (np.log(np.maximum(n, 1).astype(np.float32) / np.float32(exact)) / np.float32(math.log(128 / 16)) * np.float32(32 - exact)).astype(np.int32)
    return np.where(n < exact, n, np.minimum(large, 31)).astype(np.int64)


def nsa_host_tables(rel_bias):
    rb = np.asarray(rel_bias, np.float32)
    ki = np.arange(128)[:, None]
    qi = np.arange(128)[None, :]
    r0 = qi - ki
    r128 = 128 + qi - ki
    qq = np.arange(128)[:, None]
    mm = np.arange(248)[None, :]
    rc = qq - 16 * (mm - 120) - 31
    tab = np.concatenate([
        rb[t5_bucket_np(r0)].transpose(0, 2, 1).reshape(128, 8 * 128),
        rb[t5_bucket_np(r128)].transpose(0, 2, 1).reshape(128, 8 * 128),
        rb[t5_bucket_np(rc)].transpose(0, 2, 1).reshape(128, 8 * 248)], axis=1)
    t31 = np.broadcast_to(rb[31][None, :], (128, 8)).copy()
    return np.ascontiguousarray(tab, np.float32), np.ascontiguousarray(t31, np.float32)


def nsa_host_consts():
    ki = np.arange(128)[:, None]
    qi = np.arange(128)[None, :]
    qq = np.arange(128)[:, None]
    mm = np.arange(248)[None, :]
    rc = qq - 16 * (mm - 120) - 31
    m0 = np.where(qi - ki >= 0, 0.0, -NBIG)
    msk = np.concatenate([
        np.broadcast_to(m0[:, None, :], (128, 8, 128)).reshape(128, -1),
        np.zeros((128, 8 * 128)),
        np.broadcast_to(np.where(rc >= 0, 0.0, -NBIG)[:, None, :], (128, 8, 248)).reshape(128, -1)], axis=1)
    mtri = np.where(ki > qi, 0.0, -NBIG)
    k = np.arange(128)[:, None]
    j = np.arange(32)[None, :]
    ov = ((16 * k <= 64 * j + 63) & (16 * k + 31 >= 64 * j) & (k < 127)).astype(np.float32)
    keep = np.zeros((128, 16, 32)); addc = np.zeros((128, 16, 32))
    for qb in range(16):
        cur = (2 * qb + (np.arange(128) >= 64))[:, None]
        blk = np.arange(32)[None, :]
        forced = (blk == 0) | (blk == cur) | (blk == cur - 1)
        fut = blk > cur
        keep[:, qb, :] = (~forced & ~fut)
        addc[:, qb, :] = np.where(fut, -1e30, np.where(forced, 1e9, 0.0))
    E = np.zeros((128, 2048))
    E[:32] = (np.arange(2048)[None, :] // 64) == np.arange(32)[:, None]
    parts = dict(msk=msk, mtri=mtri, ov=ov, keep=keep.reshape(128, -1), addc=addc.reshape(128, -1), E=E)
    cols = {}
    o = 0
    arrs = []
    for n, a in parts.items():
        cols[n] = (o, a.shape[1]); o += a.shape[1]; arrs.append(a.astype(np.float32))
    return np.concatenate(arrs, axis=1), cols


NSA_CST_NP, NSA_CST_COLS = nsa_host_consts()


def stage_nsa(P, C, projT, projN, ymix, prm, l):
    G, R = 2, 4
    with P.scope():
        cf = [C.f]
        tmp = ng_tmp(P)
        one = tmp["one"]
        Bn0 = P.sbuf("nsa_Bn0", [128, 8, 128], BF16)
        Bn1 = P.sbuf("nsa_Bn1", [128, 8, 128], BF16)
        Mtri = P.sbuf("nsa_Mtri", [128, 4, 128], BF16)
        FT = P.sbuf("nsa_FT", [128, 8, 248])
        ovb = P.sbuf("nsa_ovb", [128, 32], BF16)
        keep = P.sbuf("nsa_keep", [128, 16, 32])
        addc = P.sbuf("nsa_addc", [128, 16, 32])
        Eb = P.sbuf("nsa_Eb", [32, 2048], BF16)
        ncst = prm["nsa_cst"]
        cc = NSA_CST_COLS
        P.dma("sp", keep[:].rearrange("p a b -> p (a b)"), ncst.t[:, cc["keep"][0]:cc["keep"][0] + 512], reads=[ncst], writes=[keep])
        P.dma("sp", addc[:].rearrange("p a b -> p (a b)"), ncst.t[:, cc["addc"][0]:cc["addc"][0] + 512], reads=[ncst], writes=[addc])
        with P.scope():
            tb = P.sbuf("nsa_tb", [128, 4032])
            mk = P.sbuf("nsa_mk", [128, 4032])
            t31 = P.sbuf("nsa_t31", [128, 8])
            st = P.sbuf("nsa_st", [128, 2048])
            P.dma("sp", tb[:], prm["nsa_tab"].t[:, :], reads=[prm["nsa_tab"]], writes=[tb])
            P.dma("sp", mk[:], ncst.t[:, cc["msk"][0]:cc["msk"][0] + 4032], reads=[ncst], writes=[mk])
            P.dma("sp", t31[:], prm["nsa_t31"].t[:, :], reads=[prm["nsa_t31"]], writes=[t31])
            for (o, w, dst) in ((0, 128, Bn0), (1024, 128, Bn1), (2048, 248, FT)):
                v = tb[:, o:o + 8 * w].rearrange("p (h x) -> p h x", h=8)
                P.dve(lambda e, v=v, w=w: e.tensor_tensor(out=v, in0=v, in1=bc(t31[:].unsqueeze(2), [128, 8, w]), op=ALU.subtract), reads=[tb, t31], writes=[tb])
                P.dve(lambda e, v=v, w=w, o=o, dst=dst: e.tensor_tensor(out=dst[:], in0=v, in1=mk[:, o:o + 8 * w].rearrange("p (h x) -> p h x", h=8), op=ALU.add),
                      reads=[tb, mk], writes=[dst])
            P.dma("sp", st[:, 0:128], ncst.t[:, cc["mtri"][0]:cc["mtri"][0] + 128], reads=[ncst], writes=[st])
            P.dve(lambda e: e.tensor_copy(out=Mtri[:], in_=bc(st[:, 0:128].unsqueeze(1), [128, 4, 128])), reads=[st], writes=[Mtri])
            P.dma("sp", st[:, 128:160], ncst.t[:, cc["ov"][0]:cc["ov"][0] + 32], reads=[ncst], writes=[st])
            P.dve(lambda e: e.tensor_copy(out=ovb[:], in_=st[:, 128:160]), reads=[st], writes=[ovb])
            P.dma("sp", st[0:32, :], ncst.t[0:32, cc["E"][0]:cc["E"][0] + 2048], reads=[ncst], writes=[st])
            P.dve(lambda e: e.tensor_copy(out=Eb[:], in_=st[0:32, :]), reads=[st], writes=[Eb])
        dbg(0.1)
        qTb = P.sbuf("nsa_qTb", [64, 8, SEQ], BF16)
        ksT = P.sbuf("nsa_ksT", [64, 2, SEQ], BF16)
        kwT = P.sbuf("nsa_kwT", [64, 2, SEQ], BF16)
        vsb = P.sbuf("nsa_vsb", [128, 16, 2, 65], BF16)
        vwb = P.sbuf("nsa_vwb", [128, 16, 2, 65], BF16)
        gts = P.sbuf("nsa_gts", [128, 16, 24])
        kcT = P.sbuf("nsa_kcT", [64, 2, 128], BF16)
        vcx = P.sbuf("nsa_vcx", [128, 2, 96], BF16)
        with P.scope():
            stg = [P.sbuf(f"nsa_stg{i}", [64, SEQ]) for i in range(2)]
            n = 0
            for h in range(8):
                s_ = stg[n % 2]; n += 1
                P.dma("sp", s_[:], projT.t[PT_NQ + h * 64:PT_NQ + (h + 1) * 64, :], reads=[projT], writes=[s_])
                P.act(lambda e, h=h, s_=s_: e.activation(out=qTb[:, h, :], in_=s_[:], func=AF.Copy, scale=0.125), reads=[s_], writes=[qTb])
            for (r0, dst) in ((PT_NKS, ksT), (PT_NKW, kwT)):
                for g in range(2):
                    s_ = stg[n % 2]; n += 1
                    P.dma("sp", s_[:], projT.t[r0 + g * 64:r0 + (g + 1) * 64, :], reads=[projT], writes=[s_])
                    P.dve(lambda e, g=g, s_=s_, dst=dst: e.tensor_copy(out=dst[:, g, :], in_=s_[:]), reads=[s_], writes=[dst])
            tn = P.sbuf("nsa_tn", [128, 16, 280])
            P.dma("sp", tn[:], projN.t[:, 0:280].rearrange("(t p) c -> p t c", p=128), reads=[projN], writes=[tn])
            P.pool(lambda e: e.memset(vsb[:], 1.0), writes=[vsb])
            P.pool(lambda e: e.memset(vwb[:], 1.0), writes=[vwb])
            P.dve(lambda e: e.tensor_copy(out=vsb[:, :, :, 0:64], in_=tn[:, :, 0:128].rearrange("p t (g d) -> p t g d", g=2)), reads=[tn], writes=[vsb])
            P.dve(lambda e: e.tensor_copy(out=vwb[:, :, :, 0:64], in_=tn[:, :, 128:256].rearrange("p t (g d) -> p t g d", g=2)), reads=[tn], writes=[vwb])
            P.act(lambda e: e.activation(out=gts[:], in_=tn[:, :, 256:280], func=AF.Sigmoid), reads=[tn], writes=[gts])
            dbg(0.2)
            tT = P.sbuf("nsa_tT", [64, 2, SEQ])
            tG = P.sbuf("nsa_tG", [64, 2, 16, 129], BF16)
            P.pool(lambda e: e.memset(tG[:], 0.0), writes=[tG])
            w1f = P.sbuf("nsa_w1f", [64, 32, 64])
            w1b = P.sbuf("nsa_w1b", [64, 32, 64], BF16)
            w2f = P.sbuf("nsa_w2f", [64, 64])
            w2b = P.sbuf("nsa_w2b", [64, 64], BF16)
            posT = P.sbuf("nsa_posT", [64, 32])
            posb = P.sbuf("nsa_posb", [64, 32], BF16)
            cvec = P.sbuf("nsa_cvec", [64, 1])
            hid = P.sbuf("nsa_hid", [64, 2, 128], BF16)
            ps_h = P.psum("nsa_ps_h", [64, 512])
            ps_c = P.psum("nsa_ps_c", [64, 8])
            ps_o = P.psum("nsa_ps_o", [128, 512])
            P.dve(lambda e: e.memset(hid[:], 0.0), writes=[hid])
            for kv in range(2):
                r0 = PT_NKC if kv == 0 else PT_NVC
                for g in range(2):
                    P.dma("sp", tT[:, g, :], projT.t[r0 + g * 64:r0 + (g + 1) * 64, :], reads=[projT], writes=[tT])
                    P.dve(lambda e, g=g: e.tensor_copy(out=tG[:, g, :, 0:128], in_=tT[:, g, :].rearrange("p (n s) -> p s n", s=16)), reads=[tT], writes=[tG])
                w1src = prm["nsa_cmp_w1"].t[l, kv].rearrange("(j d) o -> d j o", d=64)
                P.dma("sp", w1f[:], w1src, reads=[prm["nsa_cmp_w1"]], writes=[w1f])
                P.pool(lambda e: e.tensor_copy(out=w1b[:], in_=w1f[:]), reads=[w1f], writes=[w1b])
                P.dma("sp", w2f[:], prm["nsa_cmp_w2"].t[l, kv], reads=[prm["nsa_cmp_w2"]], writes=[w2f])
                P.dve(lambda e: e.tensor_copy(out=w2b[:], in_=w2f[:]), reads=[w2f], writes=[w2b])
                P.dma("sp", posT[:], prm["nsa_cmp_pos"].t[l, kv], reads=[prm["nsa_cmp_pos"]], writes=[posT])
                P.dve(lambda e: e.tensor_copy(out=posb[:], in_=posT[:]), reads=[posT], writes=[posb])
                for j in range(32):
                    P.pe(lambda e, j=j: e.matmul(ps_c[:, 0:1], lhsT=w1b[:, j, :], rhs=posb[:, j:j + 1], start=(j == 0), stop=(j == 31)), reads=[w1b, posb], writes=[ps_c])
                P.dve(lambda e: e.tensor_copy(out=cvec[:], in_=ps_c[:, 0:1]), reads=[ps_c], writes=[cvec])
                dbg(0.3 + 0.3 * kv)
                for g in range(2):
                    rows = slice(g * 64, (g + 1) * 64)
                    dbg(0.32 + 0.03 * g + 0.3 * kv)
                    for j in range(32):
                        P.pe(lambda e, j=j, g=g, rows=rows: e.matmul(ps_h[:, g * 128:(g + 1) * 128], lhsT=w1b[:, j, :], rhs=tG[:, g, j % 16, (j // 16):(j // 16) + 128],
                                                                    start=(j == 0), stop=(j == 31)), reads=[w1b, tG], writes=[ps_h])
                for g in range(2):
                    P.act(lambda e, g=g: e.activation(out=hid[:, g, :], in_=ps_h[:, g * 128:(g + 1) * 128], func=AF.Silu, bias=cvec[:, 0:1]),
                          reads=[ps_h, cvec], writes=[hid])
                dbg(0.4 + 0.3 * kv)
                if kv == 0:
                    P.pe(lambda e: e.matmul(ps_h[:, 256:512], lhsT=w2b[:], rhs=hid[:].rearrange("o g n -> o (g n)"), start=True, stop=True), reads=[w2b, hid], writes=[ps_h])
                    P.dve(lambda e: e.tensor_copy(out=kcT[:].rearrange("o g n -> o (g n)"), in_=ps_h[:, 256:512]), reads=[ps_h], writes=[kcT])
                else:
                    for g in range(2):
                        P.pe(lambda e, g=g: e.matmul(ps_o[:, g * 64:(g + 1) * 64], lhsT=hid[:, g, :], rhs=w2b[:], start=True, stop=True), reads=[w2b, hid], writes=[ps_o])
                    P.dve(lambda e: e.tensor_copy(out=vcx[:, :, 0:64], in_=ps_o[:, 0:128].rearrange("p (g d) -> p g d", g=2)), reads=[ps_o], writes=[vcx])
                    P.dve(lambda e: e.tensor_copy(out=vcx[:, :, 64:96], in_=bc(ovb[:].unsqueeze(1), [128, 2, 32])), reads=[ovb], writes=[vcx])
        dbg(1)
        ps_sc = [P.psum(f"nsa_ps_sc{i}", [128, 512]) for i in range(2)]
        ps_os = P.psum("nsa_ps_os", [128, 512])
        ps_ow = P.psum("nsa_ps_ow", [128, 512])
        ps_oc = P.psum("nsa_ps_oc", [128, 512])
        ps_cs = P.psum("nsa_ps_cs", [128, 512])
        ps_tr = P.psum("nsa_ps_tr", [128, 1024], BF16)
        sc = P.sbuf("nsa_sc", [128, 4, 128])
        ssum = P.sbuf("nsa_ssum", [128, 4])
        pnb = P.sbuf("nsa_pnb", [128, 4, 128], BF16)
        pT = P.sbuf("nsa_pT", [128, 4, 128], BF16)
        imp = P.sbuf("nsa_imp", [128, 32])
        mx8 = P.sbuf("nsa_mx8", [128, 8])
        negm = P.sbuf("nsa_negm", [128, 32], BF16)
        negT = P.sbuf("nsa_negT", [32, 4, 128], BF16)
        eT = [P.sbuf(f"nsa_eT{i}", [128, 4, 128], BF16) for i in range(2)]
        cs = P.sbuf("nsa_cs", [128, 3, 4])
        ya = P.sbuf("nsa_ya", [128, 4, 64])
        yb = P.sbuf("nsa_yb", [128, 4, 64])
        yt = [P.sbuf(f"nsa_yt{i}", [128, 512]) for i in range(2)]
        nsc = 0

        def v3(ap, r=4):
            return ap.rearrange("p (r q) -> p r q", r=r)

        for qb in range(NT):
            qtok = slice(qb * 128, (qb + 1) * 128)
            y = yt[qb % 2]
            for g in range(G):
                hs = slice(4 * g, 4 * g + 4)
                for r in range(R):
                    P.pe(lambda e, r=r: e.matmul(ps_cs[:, r * 128:(r + 1) * 128], lhsT=qTb[:, 4 * g + r, qtok], rhs=kcT[:, g, :], start=True, stop=True),
                         reads=[qTb, kcT], writes=[ps_cs])
                m0 = 120 - 8 * qb
                P.dve(lambda e: e.tensor_tensor(out=sc[:], in0=v3(ps_cs[:]), in1=FT[:, hs, m0:m0 + 128], op=ALU.add), reads=[ps_cs, FT], writes=[sc])
                P.act(lambda e: e.activation(out=sc[:], in_=sc[:], func=AF.Exp), reads=[sc], writes=[sc])
                P.dve(lambda e: e.tensor_reduce(out=ssum[:], in_=sc[:], axis=AX.X, op=ALU.add), reads=[sc], writes=[ssum])
                P.dve(lambda e: e.tensor_scalar(out=ssum[:], in0=ssum[:], scalar1=1e-30, scalar2=None, op0=ALU.max), reads=[ssum], writes=[ssum])
                P.dve(lambda e: e.reciprocal(out=ssum[:], in_=ssum[:]), reads=[ssum], writes=[ssum])
                P.dve(lambda e: e.tensor_tensor(out=pnb[:], in0=sc[:], in1=bc(ssum[:].unsqueeze(2), [128, 4, 128]), op=ALU.mult), reads=[sc, ssum], writes=[pnb])
                for r in range(R):
                    P.pe(lambda e, r=r: e.transpose(out=ps_tr[:, r * 128:(r + 1) * 128], in_=pnb[:, r, :], identity=C.identb[:]), reads=[pnb, C.identb], writes=[ps_tr])
                P.act(lambda e: e.copy(out=pT[:].rearrange("p r q -> p (r q)"), in_=ps_tr[:, 0:512]), reads=[ps_tr], writes=[pT])
                for r in range(R):
                    P.pe(lambda e, r=r: e.matmul(ps_oc[:, r * 96:(r + 1) * 96], lhsT=pT[:, r, :], rhs=vcx[:, g, :], start=True, stop=True), reads=[pT, vcx], writes=[ps_oc])
                oc4 = ps_oc[:, 0:384].rearrange("p (r x) -> p r x", r=4)
                P.dve(lambda e: e.tensor_reduce(out=imp[:], in_=oc4[:, :, 64:96].rearrange("p r j -> p j r"), axis=AX.X, op=ALU.add), reads=[ps_oc], writes=[imp])
                P.dve(lambda e: e.tensor_tensor(out=imp[:], in0=imp[:], in1=keep[:, qb, :], op=ALU.mult), reads=[imp, keep], writes=[imp])
                P.dve(lambda e: e.tensor_tensor(out=imp[:], in0=imp[:], in1=addc[:, qb, :], op=ALU.add), reads=[imp, addc], writes=[imp])
                P.dve(lambda e: e.max(out=mx8[:], in_=imp[:]), reads=[imp], writes=[mx8])
                P.dve(lambda e: e.tensor_scalar(out=imp[:], in0=imp[:], scalar1=mx8[:, 7:8], scalar2=None, op0=ALU.is_ge), reads=[imp, mx8], writes=[imp])
                P.dve(lambda e: e.tensor_scalar(out=negm[:], in0=imp[:], scalar1=-1.0, scalar2=NBIG, op0=ALU.add, op1=ALU.mult), reads=[imp], writes=[negm])
                P.pe(lambda e: e.transpose(out=ps_tr[0:32, 512:640], in_=negm[:], identity=C.identb[:]), reads=[negm, C.identb], writes=[ps_tr])
                P.dve(lambda e: e.tensor_copy(out=negT[:], in_=bc(ps_tr[0:32, 512:640].unsqueeze(1), [32, 4, 128])), reads=[ps_tr], writes=[negT])
                for kt in range(qb + 1):
                    ps = ps_sc[nsc % 2]; et = eT[nsc % 2]; nsc += 1
                    ktok = slice(kt * 128, (kt + 1) * 128)
                    near = kt >= qb - 1
                    P.pe(lambda e, ps=ps, ktok=ktok: e.matmul(v3(ps[:]), lhsT=ksT[:, g, ktok], rhs=qTb[:, hs, qtok], start=True, stop=False), reads=[ksT, qTb], writes=[ps])
                    P.pe(lambda e, ps=ps, ktok=ktok, near=near: e.matmul(v3(ps[:]), lhsT=Eb[:, ktok], rhs=negT[:], start=False, stop=not near), reads=[Eb, negT], writes=[ps])
                    if near:
                        Bn = Bn0 if kt == qb else Bn1
                        P.pe(lambda e, ps=ps, Bn=Bn: e.matmul(v3(ps[:]), lhsT=C.identb[:], rhs=Bn[:, hs, :], start=False, stop=True), reads=[Bn, C.identb], writes=[ps])
                    P.act(lambda e, ps=ps, et=et: e.activation(out=et[:].rearrange("p r q -> p (r q)"), in_=ps[:], func=AF.Exp), reads=[ps], writes=[et])
                    for r in range(R):
                        P.pe(lambda e, r=r, et=et, kt=kt: e.matmul(ps_os[:, r * 65:(r + 1) * 65], lhsT=et[:, r, :], rhs=vsb[:, kt, g, :], start=(kt == 0 and r == 0), stop=(kt == qb), skip_group_check=True),
                             reads=[et, vsb], writes=[ps_os])
                kt0 = max(0, qb - 4)
                for kt in range(kt0, qb + 1):
                    ps = ps_sc[nsc % 2]; et = eT[nsc % 2]; nsc += 1
                    ktok = slice(kt * 128, (kt + 1) * 128)
                    dl = qb - kt
                    extra = {0: Bn0[:, hs, :], 1: Bn1[:, hs, :], 4: Mtri[:]}.get(dl)
                    P.pe(lambda e, ps=ps, ktok=ktok, extra=extra: e.matmul(v3(ps[:]), lhsT=kwT[:, g, ktok], rhs=qTb[:, hs, qtok], start=True, stop=extra is None),
                         reads=[kwT, qTb], writes=[ps])
                    if extra is not None:
                        P.pe(lambda e, ps=ps, extra=extra: e.matmul(v3(ps[:]), lhsT=C.identb[:], rhs=extra, start=False, stop=True), reads=[Bn0, Bn1, Mtri, C.identb], writes=[ps])
                    P.act(lambda e, ps=ps, et=et: e.activation(out=et[:].rearrange("p r q -> p (r q)"), in_=ps[:], func=AF.Exp), reads=[ps], writes=[et])
                    for r in range(R):
                        P.pe(lambda e, r=r, et=et, kt=kt: e.matmul(ps_ow[:, r * 65:(r + 1) * 65], lhsT=et[:, r, :], rhs=vwb[:, kt, g, :], start=(kt == kt0 and r == 0), stop=(kt == qb), skip_group_check=True),
                             reads=[et, vwb], writes=[ps_ow])
                os4 = ps_os[:, 0:260].rearrange("p (r x) -> p r x", r=4)
                ow4 = ps_ow[:, 0:260].rearrange("p (r x) -> p r x", r=4)
                g3 = gts[:, qb, 12 * g:12 * g + 12].rearrange("p (r b) -> p b r", b=3)
                P.dve(lambda e: e.reciprocal(out=cs[:, 1, :], in_=os4[:, :, 64]), reads=[ps_os], writes=[cs])
                P.dve(lambda e: e.reciprocal(out=cs[:, 2, :], in_=ow4[:, :, 64]), reads=[ps_ow], writes=[cs])
                P.dve(lambda e: e.memset(cs[:, 0, :], 1.0), writes=[cs])
                P.dve(lambda e: e.tensor_tensor(out=cs[:], in0=cs[:], in1=g3, op=ALU.mult), reads=[cs, gts], writes=[cs])
                P.dve(lambda e: e.tensor_tensor(out=ya[:], in0=oc4[:, :, 0:64], in1=bc(cs[:, 0, :].unsqueeze(2), [128, 4, 64]), op=ALU.mult), reads=[ps_oc, cs], writes=[ya])
                P.dve(lambda e: e.tensor_tensor(out=yb[:], in0=os4[:, :, 0:64], in1=bc(cs[:, 1, :].unsqueeze(2), [128, 4, 64]), op=ALU.mult), reads=[ps_os, cs], writes=[yb])
                P.pool(lambda e: e.tensor_tensor(out=ya[:], in0=ya[:], in1=yb[:], op=ALU.add), reads=[ya, yb], writes=[ya])
                P.dve(lambda e: e.tensor_tensor(out=yb[:], in0=ow4[:, :, 0:64], in1=bc(cs[:, 2, :].unsqueeze(2), [128, 4, 64]), op=ALU.mult), reads=[ps_ow, cs], writes=[yb])
                P.pool(lambda e: e.tensor_tensor(out=y[:, g * 256:(g + 1) * 256].rearrange("p (r d) -> p r d", r=4), in0=ya[:], in1=yb[:], op=ALU.add), reads=[ya, yb], writes=[y])
            P.dma("sp", ymix.t[qtok, 0:512], y[:], reads=[y], writes=[ymix])
            dbg(2 + qb)


WIN_OFF = {}
_o = 0
for (_c0, _n, _r0) in PT_GROUPS:
    WIN_OFF[("T", _c0)] = _o; _o += 16 * _n
for (_c0, _n, _r0) in PN_GROUPS:
    WIN_OFF[("N", _c0)] = _o; _o += 16 * _n
WIN_TOTAL = _o
WOUT_TOTAL = 4 * 16 * 512
W1_TOTAL = 32 * 16 * 256
W2_TOTAL = 4 * 64 * 512


def stage_convert(P, prm, l, wsc):
    with P.scope():
        st = [P.sbuf(f"cv_st{i}", [128, 8192]) for i in range(3)]
        sb = [P.sbuf(f"cv_sb{i}", [128, 8192], BF16) for i in range(3)]
        n = [0]

        def job(src_ap3, nk, ncols, dst_buf, off, split=False):
            i = n[0] % 3
            n[0] += 1
            f, b_ = st[i], sb[i]
            tot = nk * ncols
            P.dma("sp", f[:, 0:tot].rearrange("p (k c) -> p k c", k=nk), src_ap3[0], reads=[src_ap3[1]], writes=[f])
            ei = n[0] % 3
            if ei == 2:
                P.act(lambda e: e.copy(out=b_[:, 0:tot], in_=f[:, 0:tot]), reads=[f], writes=[b_])
            else:
                (P.dve, P.pool)[ei](lambda e: e.tensor_copy(out=b_[:, 0:tot], in_=f[:, 0:tot]), reads=[f], writes=[b_])
            if split:
                for hh in range(2):
                    P.dma("sp", dst_buf.t[:, off + hh * 4096:off + (hh + 1) * 4096].rearrange("p (k c) -> p k c", k=16),
                          b_[:, 0:tot].rearrange("p (k c) -> p k c", k=16)[:, :, hh * 256:(hh + 1) * 256], reads=[b_], writes=[dst_buf])
            else:
                P.dma("sp", dst_buf.t[:, off:off + tot], b_[:, 0:tot], reads=[b_], writes=[dst_buf])

        wv = prm["w_in"].t[l].rearrange("(k p) n -> p k n", p=128)
        for (c0, nc_, r0) in PT_GROUPS:
            job((wv[:, :, c0:c0 + nc_], prm["w_in"]), 16, nc_, wsc["win"], WIN_OFF[("T", c0)])
        for (c0, nc_, o0) in PN_GROUPS:
            job((wv[:, :, c0:c0 + nc_], prm["w_in"]), 16, nc_, wsc["win"], WIN_OFF[("N", c0)])
        wv = prm["w_out"].t[l].rearrange("(k p) n -> p k n", p=128)
        for ct in range(4):
            job((wv[:, :, ct * 512:(ct + 1) * 512], prm["w_out"]), 16, 512, wsc["wout"], ct * 8192)
        wv = prm["mlp_w1"].t[l].rearrange("(k p) n -> p k n", p=128)
        for hp in range(16):
            job((wv[:, :, hp * 512:(hp + 1) * 512], prm["mlp_w1"]), 16, 512, wsc["w1"], hp * 8192, split=True)
        wv = prm["mlp_w2"].t[l].rearrange("(k p) n -> p k n", p=128)
        for ct in range(4):
            for kg in range(4):
                job((wv[:, kg * 16:(kg + 1) * 16, ct * 512:(ct + 1) * 512], prm["mlp_w2"]), 16, 512, wsc["w2"], (ct * 4 + kg) * 8192)

def stage_mod(P, C, cT, ada_w, ada_b, modT, gsc, nlayers):
    with P.scope():
        cf = [C.f]
        ca = P.sbuf("mod_ca", [128, 16, BPC])
        P.dma("sp", ca[:], cT.t[:, :, :], reads=[cT], writes=[ca])
        P.act(lambda e: e.activation(out=ca[:], in_=ca[:], func=AF.Silu), reads=[ca], writes=[ca])
        wst = [P.sbuf(f"mod_w{i}", [128, 16, 512]) for i in range(2)]
        brow = [P.sbuf(f"mod_b{i}", [1, 512]) for i in range(2)]
        grow = [P.sbuf(f"mod_g{i}", [BPC, 512]) for i in range(2)]
        ps_f = P.psum("mod_psf", [128, 512])
        ps_g = [P.psum(f"mod_psg{i}", [BPC, 512]) for i in range(2)]
        n = 0
        for l in range(nlayers):
            wv = ada_w.t[l].rearrange("(k p) n -> p k n", p=128)
            for seg in range(6):
                for ct in range(4):
                    w = wst[n % 2]; br = brow[n % 2]
                    c0 = seg * 2048 + ct * 512
                    P.dma("sp", w[:], wv[:, :, c0:c0 + 512], reads=[ada_w], writes=[w])
                    P.dma("sp", br[:], ada_b.t[l:l + 1, c0:c0 + 512], reads=[ada_b], writes=[br])
                    if seg in (2, 5):
                        pg = ps_g[n % 2]; gr = grow[n % 2]
                        for k in range(16):
                            P.pe(lambda e, k=k, w=w, pg=pg: e.matmul(pg[:], lhsT=ca[:, k, :], rhs=w[:, k, :], start=(k == 0), stop=False), reads=[ca, w], writes=[pg])
                        P.pe(lambda e, br=br, pg=pg: e.matmul(pg[:], lhsT=C.c("ones", 1)[:, 0:BPC], rhs=br[:], start=False, stop=True), reads=[br] + cf, writes=[pg])
                        P.act(lambda e, pg=pg, gr=gr: e.copy(out=gr[:], in_=pg[:]), reads=[pg], writes=[gr])
                        P.dma("sp", gsc.t[l, 0 if seg == 2 else 1, :, ct * 512:(ct + 1) * 512], gr[:], reads=[gr], writes=[gsc])
                    else:
                        si = {0: 0, 1: 1, 3: 2, 4: 3}[seg]
                        for cc in range(4):
                            col = ((si * 16) + ct * 4 + cc) * BPC
                            for k in range(16):
                                P.pe(lambda e, k=k, w=w, cc=cc, col=col: e.matmul(ps_f[:, col:col + BPC], lhsT=w[:, k, cc * 128:(cc + 1) * 128], rhs=ca[:, k, :],
                                                                                 start=(k == 0), stop=False), reads=[ca, w], writes=[ps_f])
                            P.pe(lambda e, br=br, cc=cc, col=col: e.matmul(ps_f[:, col:col + BPC], lhsT=br[:, cc * 128:(cc + 1) * 128], rhs=C.c("ones", 1)[:, 0:BPC],
                                                                          start=False, stop=True), reads=[br] + cf, writes=[ps_f])
                    n += 1
            P.dve(lambda e, l=l: e.tensor_copy(out=modT[:, l].rearrange("p s k b -> p (s k b)"), in_=ps_f[:, 0:4 * 16 * BPC]), reads=[ps_f], writes=[modT])


def to_featmajor(P, C, src, src_ap_fn, ntt, hT, norm, scl=None, shf=None, pools=None):
    xt, xb, ss, ps_tr, eps = pools["xt"], pools["xb"], pools["ss"], pools["ps_tr"], pools["eps"]
    for tt in range(ntt):
        x = xt[tt % 2]; xn = xb[tt % 2]; s1 = ss[tt % 2]
        P.dma("sp", x[:], src_ap_fn(tt), reads=[src], writes=[x])
        if norm:
            P.pool(lambda e, s1=s1: e.memset(s1[:], 0.0), writes=[s1])
            P.act(lambda e, x=x, xn=xn, s1=s1: e.activation(out=xn[:], in_=x[:], func=AF.Square, accum_out=s1[:, 0:1]), reads=[x, s1], writes=[xn, s1])
            P.act(lambda e, s1=s1: e.activation(out=s1[:, 0:1], in_=s1[:, 0:1], func=AF.Sqrt, scale=1.0 / D_MODEL, bias=eps[:, 0:1]), reads=[s1, eps], writes=[s1])
            P.dve(lambda e, s1=s1: e.reciprocal(out=s1[:, 0:1], in_=s1[:, 0:1]), reads=[s1], writes=[s1])
            P.dve(lambda e, x=x, xn=xn, s1=s1: e.tensor_scalar(out=xn[:], in0=x[:], scalar1=s1[:, 0:1], scalar2=None, op0=ALU.mult), reads=[x, s1], writes=[xn])
        else:
            P.pool(lambda e, x=x, xn=xn: e.tensor_copy(out=xn[:], in_=x[:]), reads=[x], writes=[xn])
        for half in range(2):
            pt = ps_tr[(2 * tt + half) % len(ps_tr)]
            for kk in range(8):
                k = half * 8 + kk
                P.pe(lambda e, k=k, kk=kk, xn=xn, pt=pt: e.transpose(out=pt[:, kk * 128:(kk + 1) * 128], in_=xn[:, k * 128:(k + 1) * 128], identity=C.identb[:]),
                     reads=[xn, C.identb], writes=[pt])
            dst = hT[:, half * 8:half * 8 + 8, tt * 128:(tt + 1) * 128]
            src3 = pt[:].rearrange("p (k t) -> p k t", k=8)
            if scl is None:
                P.act(lambda e, dst=dst, src3=src3: e.copy(out=dst, in_=src3), reads=[pt], writes=[hT])
            else:
                tm = pools["tm"][(2 * tt + half) % 2]
                P.dve(lambda e, src3=src3, tm=tm, half=half: e.tensor_tensor(out=tm[:], in0=src3, in1=bc(scl[:, half * 8:half * 8 + 8].unsqueeze(2), [128, 8, 128]), op=ALU.mult),
                      reads=[pt] + pools["affb"], writes=[tm])
                P.pool(lambda e, dst=dst, tm=tm, half=half: e.tensor_tensor(out=dst, in0=tm[:], in1=bc(shf[:, half * 8:half * 8 + 8].unsqueeze(2), [128, 8, 128]), op=ALU.add),
                       reads=[tm] + pools["affb"], writes=[hT])


def fm_pools(P, affine):
    d = dict(xt=[P.sbuf(f"fm_xt{i}", [128, D_MODEL]) for i in range(2)],
             xb=[P.sbuf(f"fm_xb{i}", [128, D_MODEL], BF16) for i in range(2)],
             ss=[P.sbuf(f"fm_ss{i}", [128, 1]) for i in range(2)],
             ps_tr=[P.psum(f"fm_pst{i}", [128, 1024], BF16) for i in range(2)],
             eps=P.sbuf("fm_eps", [128, 1]))
    P.pool(lambda e: e.memset(d["eps"][:], EPS), writes=[d["eps"]])
    if affine:
        d["tm"] = [P.sbuf(f"fm_tm{i}", [128, 8, 128]) for i in range(2)]
    return d


def affine_vecs(P, modT, l, b, which, nw, scl, shf):
    P.dve(lambda e: e.tensor_scalar(out=scl[:], in0=modT[:, l, 2 * which + 1, :, b], scalar1=1.0, scalar2=None, op0=ALU.add), reads=[modT], writes=[scl])
    P.dve(lambda e: e.tensor_tensor(out=scl[:], in0=scl[:], in1=nw, op=ALU.mult), reads=[scl], writes=[scl])
    P.dve(lambda e: e.tensor_copy(out=shf[:], in_=modT[:, l, 2 * which, :, b]), reads=[modT], writes=[shf])


def stage_inproj(P, C, xsrc, xsrc_fn, modT, nw1T, win, projT, projN, l, b):
    with P.scope():
        hT = P.sbuf("ip_hT", [128, 16, SEQ], BF16)
        scl = P.sbuf("ip_scl", [128, 16]); shf = P.sbuf("ip_shf", [128, 16])
        affine_vecs(P, modT, l, b, 0, nw1T[:, l, :], scl, shf)
        with P.scope():
            pools = fm_pools(P, True)
            pools["affb"] = [scl, shf]
            to_featmajor(P, C, xsrc, xsrc_fn, NT, hT, True, scl[:], shf[:], pools)
        NWB = 3
        wb = [P.sbuf(f"ip_wb{i}", [128, 16, 512], BF16) for i in range(NWB)]
        ev = [P.sbuf(f"ip_ev{i}", [128, 2048]) for i in range(2)]
        ps = [P.psum(f"ip_ps{i}", [128, 512]) for i in range(4)]
        groups = [("T",) + g for g in PT_GROUPS] + [("N",) + g for g in PN_GROUPS]

        def wload(i):
            kind, c0, nc_, _ = groups[i]
            off = WIN_OFF[(kind, c0)]
            wbb = wb[i % NWB]
            P.dma("sp", wbb[:, :, 0:nc_], win.t[:, off:off + 16 * nc_].rearrange("p (k c) -> p k c", k=16), reads=[win], writes=[wbb])

        npp = 0
        nev = 0
        wload(0)
        wload(1)
        for gi, (kind, c0, nc_, dst0) in enumerate(groups):
            if gi + 2 < len(groups):
                wload(gi + 2)
            wbb = wb[gi % NWB]
            if kind == "T":
                e_ = ev[nev % 2]; nev += 1
                for tq in range(4):
                    p_ = ps[npp % 4]; npp += 1
                    for k in range(16):
                        P.pe(lambda e, k=k, p_=p_, wbb=wbb, nc_=nc_, tq=tq: e.matmul(p_[0:nc_, :], lhsT=wbb[:, k, 0:nc_], rhs=hT[:, k, tq * 512:(tq + 1) * 512], start=(k == 0), stop=(k == 15)),
                             reads=[wbb, hT], writes=[p_])
                    if tq % 2 == 0:
                        P.act(lambda e, p_=p_, e_=e_, nc_=nc_, tq=tq: e.copy(out=e_[0:nc_, tq * 512:(tq + 1) * 512], in_=p_[0:nc_, :]), reads=[p_], writes=[e_])
                    else:
                        P.dve(lambda e, p_=p_, e_=e_, nc_=nc_, tq=tq: e.tensor_copy(out=e_[0:nc_, tq * 512:(tq + 1) * 512], in_=p_[0:nc_, :]), reads=[p_], writes=[e_])
                P.dma("sp", projT.t[dst0:dst0 + nc_, :], e_[0:nc_, :], reads=[e_], writes=[projT])
            else:
                for t4 in range(4):
                    e_ = ev[nev % 2]; nev += 1
                    for ti in range(4):
                        tt = t4 * 4 + ti
                        p_ = ps[npp % 4]; npp += 1
                        for k in range(16):
                            P.pe(lambda e, k=k, p_=p_, wbb=wbb, nc_=nc_, tt=tt: e.matmul(p_[:, 0:nc_], lhsT=hT[:, k, tt * 128:(tt + 1) * 128], rhs=wbb[:, k, 0:nc_], start=(k == 0), stop=(k == 15)),
                                 reads=[wbb, hT], writes=[p_])
                        if ti % 2 == 0:
                            P.act(lambda e, p_=p_, e_=e_, nc_=nc_, ti=ti: e.copy(out=e_[:, ti * 512:ti * 512 + nc_], in_=p_[:, 0:nc_]), reads=[p_], writes=[e_])
                        else:
                            P.dve(lambda e, p_=p_, e_=e_, nc_=nc_, ti=ti: e.tensor_copy(out=e_[:, ti * 512:ti * 512 + nc_], in_=p_[:, 0:nc_]), reads=[p_], writes=[e_])
                    P.dma("sp", projN.t[t4 * 512:(t4 + 1) * 512, dst0:dst0 + nc_].rearrange("(i p) c -> p i c", p=128),
                          e_[:].rearrange("p (i c) -> p i c", i=4)[:, :, 0:nc_], reads=[e_], writes=[projN])


def stage_outproj(P, C, ymix, xsrc, xsrc_fn, xdst, xdst_fn, gsc, wout, l, b):
    with P.scope():
        yT = P.sbuf("op_yT", [128, 16, SEQ], BF16)
        with P.scope():
            pools = fm_pools(P, False)
            to_featmajor(P, C, ymix, lambda tt: ymix.t[tt * 128:(tt + 1) * 128, :], NT, yT, False, None, None, pools)
        wob = P.sbuf("op_wob", [128, 16, D_MODEL], BF16)
        gb = P.sbuf("op_gb", [128, D_MODEL])
        P.dma("sp", gb[:], gsc.t[l, 0, b:b + 1, :].partition_broadcast(128), reads=[gsc], writes=[gb])
        for ct in range(4):
            P.dma("sp", wob[:, :, ct * 512:(ct + 1) * 512], wout.t[:, ct * 8192:(ct + 1) * 8192].rearrange("p (k c) -> p k c", k=16), reads=[wout], writes=[wob])
        xt = [P.sbuf(f"op_xt{i}", [128, D_MODEL]) for i in range(2)]
        xo = [P.sbuf(f"op_xo{i}", [128, D_MODEL]) for i in range(2)]
        ps = [P.psum(f"op_ps{i}", [128, 512]) for i in range(8)]
        for tt in range(NT):
            x = xt[tt % 2]; o = xo[tt % 2]
            P.dma("sp", x[:], xsrc_fn(tt), reads=[xsrc], writes=[x])
            for ct in range(4):
                p_ = ps[(tt * 4 + ct) % 8]
                for k in range(16):
                    P.pe(lambda e, k=k, p_=p_, ct=ct, tt=tt: e.matmul(p_[:], lhsT=yT[:, k, tt * 128:(tt + 1) * 128], rhs=wob[:, k, ct * 512:(ct + 1) * 512], start=(k == 0), stop=(k == 15)),
                         reads=[yT, wob], writes=[p_])
                cs = slice(ct * 512, (ct + 1) * 512)
                P.dve(lambda e, p_=p_, o=o, cs=cs: e.tensor_tensor(out=o[:, cs], in0=p_[:], in1=gb[:, cs], op=ALU.mult), reads=[p_, gb], writes=[o])
                P.pool(lambda e, o=o, x=x, cs=cs: e.tensor_tensor(out=o[:, cs], in0=o[:, cs], in1=x[:, cs], op=ALU.add), reads=[o, x], writes=[o])
            P.dma("sp", xdst_fn(tt), o[:], reads=[o], writes=[xdst])


def stage_mlp(P, C, xsrc, xsrc_fn, xdst, xdst_fn, modT, nw2T, gsc, w1s, w2s, l, b):
    HC = 64
    with P.scope():
        scl = P.sbuf("ml_scl", [128, 16]); shf = P.sbuf("ml_shf", [128, 16])
        affine_vecs(P, modT, l, b, 1, nw2T[:, l, :], scl, shf)
        gb = P.sbuf("ml_gb", [128, D_MODEL])
        P.dma("sp", gb[:], gsc.t[l, 1, b:b + 1, :].partition_broadcast(128), reads=[gsc], writes=[gb])
        hT = P.sbuf("ml_hT", [128, 16, 512], BF16)
        uT = P.sbuf("ml_uT", [128, HC, 512], BF16)
        pools = fm_pools(P, True)
        pools["affb"] = [scl, shf]
        NWB = 4
        wbuf = [P.sbuf(f"ml_wb{i}", [128, 4096], BF16) for i in range(NWB)]
        rl = [P.sbuf(f"ml_rl{i}", [128, 512]) for i in range(2)]
        xo = [P.sbuf(f"ml_xo{i}", [128, 1024]) for i in range(2)]
        ps = [P.psum(f"ml_ps{i}", [128, 512]) for i in range(6)]
        NTILE = 64
        total = (SEQ // 512) * NTILE
        state = {"n": 0}

        def wload(j):
            jj = j % NTILE
            wbb = wbuf[j % NWB]
            if jj < 32:
                P.dma("sp", wbb[:], w1s.t[:, jj * 4096:(jj + 1) * 4096], reads=[w1s], writes=[wbb])
            else:
                P.dma("sp", wbb[:], w2s.t[:, (jj - 32) * 4096:(jj - 31) * 4096], reads=[w2s], writes=[wbb])

        PRE = 3
        for j in range(PRE):
            wload(j)
        nps = 0
        j = 0
        for t5 in range(SEQ // 512):
            to_featmajor(P, C, xsrc, lambda tt, t5=t5: xsrc_fn(t5 * 4 + tt), 4, hT, True, scl[:], shf[:], pools)
            for hp in range(32):
                if j + PRE < total:
                    wload(j + PRE)
                wbb = wbuf[j % NWB]; j += 1
                w3 = wbb[:].rearrange("p (k c) -> p k c", k=16)
                for cc in range(2):
                    hc = hp * 2 + cc
                    p_ = ps[nps % 6]; nps += 1
                    r_ = rl[hc % 2]
                    for k in range(16):
                        P.pe(lambda e, k=k, p_=p_, w3=w3, cc=cc: e.matmul(p_[:], lhsT=w3[:, k, cc * 128:(cc + 1) * 128], rhs=hT[:, k, :], start=(k == 0), stop=(k == 15)),
                             reads=[wbb, hT], writes=[p_])
                    P.act(lambda e, p_=p_, r_=r_: e.activation(out=r_[:], in_=p_[:], func=AF.Relu), reads=[p_], writes=[r_])
                    P.dve(lambda e, r_=r_, hc=hc: e.tensor_tensor(out=uT[:, hc, :], in0=r_[:], in1=r_[:], op=ALU.mult), reads=[r_], writes=[uT])
            for ct in range(4):
                pa = [ps[(nps + i) % 6] for i in range(4)]
                nps += 4
                for kg in range(8):
                    if j + PRE < total:
                        wload(j + PRE)
                    wbb = wbuf[j % NWB]; j += 1
                    w3 = wbb[:].rearrange("p (k c) -> p k c", k=8)
                    for kk in range(8):
                        k = kg * 8 + kk
                        for ti in range(4):
                            P.pe(lambda e, k=k, kk=kk, ti=ti, w3=w3, pa=pa: e.matmul(pa[ti][:], lhsT=uT[:, k, ti * 128:(ti + 1) * 128], rhs=w3[:, kk, :], start=(k == 0), stop=(k == HC - 1)),
                                 reads=[uT, wbb], writes=[pa[ti]])
                for ti in range(4):
                    tt = t5 * 4 + ti
                    o = xo[(ct * 4 + ti) % 2]
                    P.dma("sp", o[:, 512:1024], xsrc_fn(tt)[:, ct * 512:(ct + 1) * 512], reads=[xsrc], writes=[o])
                    P.dve(lambda e, o=o, ti=ti, pa=pa, ct=ct: e.tensor_tensor(out=o[:, 0:512], in0=pa[ti][:], in1=gb[:, ct * 512:(ct + 1) * 512], op=ALU.mult),
                          reads=[pa[ti], gb], writes=[o])
                    P.pool(lambda e, o=o: e.tensor_tensor(out=o[:, 0:512], in0=o[:, 0:512], in1=o[:, 512:1024], op=ALU.add), reads=[o], writes=[o])
                    P.dma("sp", xdst_fn(tt)[:, ct * 512:(ct + 1) * 512], o[:, 0:512], reads=[o], writes=[xdst])


def stage_final(P, C, xsrc, xsrc_fn, fnw, out, out_fn, nseq):
    with P.scope():
        nwb = P.sbuf("fn_nwb", [128, D_MODEL])
        P.dma("sp", nwb[:], fnw.t[0:1, :].partition_broadcast(128), reads=[fnw], writes=[nwb])
        eps = P.sbuf("fn_eps", [128, 1])
        P.pool(lambda e: e.memset(eps[:], EPS), writes=[eps])
        xt = [P.sbuf(f"fn_xt{i}", [128, D_MODEL]) for i in range(2)]
        sq = [P.sbuf(f"fn_sq{i}", [128, D_MODEL]) for i in range(2)]
        ss = [P.sbuf(f"fn_ss{i}", [128, 1]) for i in range(2)]
        for i in range(nseq * NT):
            x = xt[i % 2]; q = sq[i % 2]; s1 = ss[i % 2]
            P.dma("sp", x[:], xsrc_fn(i), reads=[xsrc], writes=[x])
            P.pool(lambda e, s1=s1: e.memset(s1[:], 0.0), writes=[s1])
            P.act(lambda e, x=x, q=q, s1=s1: e.activation(out=q[:], in_=x[:], func=AF.Square, accum_out=s1[:, 0:1]), reads=[x, s1], writes=[q, s1])
            P.act(lambda e, s1=s1: e.activation(out=s1[:, 0:1], in_=s1[:, 0:1], func=AF.Sqrt, scale=1.0 / D_MODEL, bias=eps[:, 0:1]), reads=[s1, eps], writes=[s1])
            P.dve(lambda e, s1=s1: e.reciprocal(out=s1[:, 0:1], in_=s1[:, 0:1]), reads=[s1], writes=[s1])
            P.dve(lambda e, x=x, q=q, s1=s1: e.scalar_tensor_tensor(out=q[:], in0=x[:], scalar=s1[:, 0:1], in1=nwb[:], op0=ALU.mult, op1=ALU.mult), reads=[x, s1, nwb], writes=[q])
            P.dma("sp", out_fn(i), q[:], reads=[q], writes=[out])


SMALL_PARAMS = ["gla_gate_w2", "gla_gate_b", "gla_norm_w", "ssd_conv_w", "ssd_conv_b", "ssd_dt_bias", "ssd_a_log", "ssd_d", "ssd_norm_w",
                "gdn_conv_w", "gdn_dt_bias", "gdn_a_log", "gdn_norm_w", "nsa_cmp_pos", "nsa_cmp_w1", "nsa_cmp_w2"]
BIG_PARAMS = ["ada_w", "ada_b", "w_in", "w_out", "mlp_w1", "mlp_w2"]


def build(nlayers=DEPTH, nseq=BPC, shapes=None):
    nc = bass.Bass("TRN2", target_bir_lowering=False)
    st = ExitStack()
    with st:
        P = Prog(nc, st)

        def ext(name, shape):
            return Buf(nc.dram_tensor(name, list(shape), F32, kind="ExternalInput").ap(), name)

        x = ext("x", [nseq, SEQ, D_MODEL])
        cT = ext("cT", [128, 16, BPC])
        prm = {k: ext(k, shapes[k]) for k in SMALL_PARAMS + BIG_PARAMS + ["nw1T", "nw2T", "fnw", "nsa_tab", "nsa_t31", "nsa_cst", "cst"]}
        out = Buf(nc.dram_tensor("out", [nseq, SEQ, D_MODEL], F32, kind="ExternalOutput").ap(), "out")
        xres = P.dram("xres", [nseq, SEQ, D_MODEL])
        projT = P.dram("projT", [PT_ROWS, SEQ])
        projN = P.dram("projN", [SEQ, PN_COLS])
        ymix = P.dram("ymix", [SEQ, D_MODEL])
        gsc = P.dram("gsc", [nlayers, 2, BPC, D_MODEL])
        C = Consts(P, prm["cst"])
        modT = P.sbuf("modT", [128, nlayers, 4, 16, BPC])
        nw1T = P.sbuf("nw1T", [128, shapes["nw1T"][1], 16])
        nw2T = P.sbuf("nw2T", [128, shapes["nw2T"][1], 16])
        P.dma("sp", nw1T[:], prm["nw1T"].t[:, :, :], reads=[prm["nw1T"]], writes=[nw1T])
        P.dma("sp", nw2T[:], prm["nw2T"].t[:, :, :], reads=[prm["nw2T"]], writes=[nw2T])
        if "mod" not in DBG["skip"]:
            stage_mod(P, C, cT, prm["ada_w"], prm["ada_b"], modT, gsc, nlayers)
        wsc = dict(win=P.dram("wsc_win", [128, WIN_TOTAL], BF16), wout=P.dram("wsc_wout", [128, WOUT_TOTAL], BF16),
                   w1=P.dram("wsc_w1", [128, W1_TOTAL], BF16), w2=P.dram("wsc_w2", [128, W2_TOTAL], BF16))
        for l in range(nlayers):
            if "convert" not in DBG["skip"]:
                stage_convert(P, prm, l, wsc)
            for b in range(nseq):
                if l == 0:
                    xs, xs_fn = x, (lambda tt, b=b: x.t[b, tt * 128:(tt + 1) * 128, :])
                else:
                    xs, xs_fn = xres, (lambda tt, b=b: xres.t[b, tt * 128:(tt + 1) * 128, :])
                xr_fn = (lambda tt, b=b: xres.t[b, tt * 128:(tt + 1) * 128, :])
                if "inproj" not in DBG["skip"]:
                    stage_inproj(P, C, xs, xs_fn, modT, nw1T, wsc["win"], projT, projN, l, b)
                if "nsa" not in DBG["skip"]:
                    stage_nsa(P, C, projT, projN, ymix, prm, l)
                if "ssd" not in DBG["skip"]:
                    stage_ssd(P, C, projT, projN, ymix, prm, l)
                if "gdn" not in DBG["skip"]:
                    stage_gdn(P, C, projT, projN, ymix, prm, l)
                if "gla" not in DBG["skip"]:
                    stage_gla(P, C, projT, projN, ymix, prm, l)
                if "outproj" not in DBG["skip"]:
                    stage_outproj(P, C, ymix, xs, xs_fn, xres, xr_fn, gsc, wsc["wout"], l, b)
                if "mlp" not in DBG["skip"]:
                    stage_mlp(P, C, xres, xr_fn, xres, xr_fn, modT, nw2T, gsc, wsc["w1"], wsc["w2"], l, b)
        stage_final(P, C, xres, lambda i: xres.t[i // NT, (i % NT) * 128:(i % NT + 1) * 128, :], prm["fnw"],
                    out, lambda i: out.t[i // NT, (i % NT) * 128:(i % NT + 1) * 128, :], nseq)
        P.finish()
        ninstr = P.ninstr
    return nc, ninstr


def host_inputs(inputs, nlayers=DEPTH):
    d = {}
    for k in SMALL_PARAMS:
        d[k] = host_param(k, inputs[k][:nlayers])
    for k in BIG_PARAMS:
        d[k] = np.ascontiguousarray(np.asarray(inputs[k][:nlayers], np.float32))
    d["nw1T"] = host_param("norm1_w", inputs["norm1_w"][:nlayers])
    d["nw2T"] = host_param("norm2_w", inputs["norm2_w"][:nlayers])
    d["fnw"] = host_param("final_norm_w", inputs["final_norm_w"])
    d["nsa_tab"], d["nsa_t31"] = nsa_host_tables(inputs["rel_bias"])
    d["nsa_cst"] = NSA_CST_NP
    d["cst"] = CST_NP
    return d


def core_inputs(inputs, shared, core, nseq=BPC):
    xs = np.ascontiguousarray(np.asarray(inputs["x"][core * BPC:core * BPC + nseq], np.float32))
    c = np.asarray(inputs["c"][core * BPC:(core + 1) * BPC], np.float32)
    cT = np.ascontiguousarray(c.T.reshape(16, 128, BPC).transpose(1, 0, 2))
    m = dict(shared)
    m["x"] = xs
    m["cT"] = cT
    return m


_CACHE = {}


def kernel(**inputs):
    shared = host_inputs(inputs)
    shapes = {k: v.shape for k, v in shared.items()}
    if "nc" not in _CACHE:
        _CACHE["nc"] = build(DEPTH, BPC, shapes)[0]
    nc = _CACHE["nc"]
    in_maps = [core_inputs(inputs, shared, c) for c in range(NCORES)]
    res = run_bass_kernel_spmd(nc, in_maps, core_ids=list(range(NCORES)))
    out = np.concatenate([r["out"] for r in res.results], axis=0)
    return out.astype(np.float32)
```

```python
import math
from contextlib import ExitStack, contextmanager
import numpy as np
import concourse.bass as bass
import concourse.mybir as mybir
from concourse.bass_utils import run_bass_kernel_spmd

F32 = mybir.dt.float32
BF16 = mybir.dt.bfloat16
AF = mybir.ActivationFunctionType
ALU = mybir.AluOpType
AX = mybir.AxisListType

EPOCH = 12000
SAME_ENGINE_SYNC = True

D_MODEL = 2048
SEQ = 2048
DEPTH = 4
NCORES = 8
BPC = 2
IN_COLS = 6456
EPS = 1e-6
NT = SEQ // 128


class StopStage(Exception):
    pass


DBG = {"stop": 99, "skip": set()}


def dbg(k):
    if DBG["stop"] <= k:
        DBG["P"].dead = True


class Buf:
    __slots__ = ("t", "name", "lw", "rd", "excl")

    def __init__(self, t, name="", excl=False):
        self.t = t
        self.name = name
        self.lw = None
        self.rd = {}
        self.excl = excl

    def __getitem__(self, k):
        return self.t[k]


class Prog:
    ENGS = ("pe", "act", "dve", "pool", "sp")

    def __init__(self, nc, stack):
        self.nc = nc
        self.stack = stack
        self.cnt = {e: 0 for e in ("pe", "act", "dve", "pool")}
        self.sems = {}
        self.seen = {e: {} for e in self.ENGS}
        self.dslots = {}
        self.dnext = {}
        self.E = dict(pe=nc.tensor, act=nc.scalar, dve=nc.vector, pool=nc.gpsimd, sp=nc.sync)
        self.ninstr = 0
        self.base_stack = stack
        self.uid = 0
        self.dead = False
        DBG["P"] = self

    def sem(self, name):
        return self.base_stack.enter_context(self.nc.semaphore(name))

    def sbuf(self, name, shape, dt=F32):
        self.uid += 1
        t = self.stack.enter_context(self.nc.sbuf_tensor(f"{name}_{self.uid}", list(shape), dt))
        return Buf(t, name)

    def psum(self, name, shape, dt=F32):
        self.uid += 1
        t = self.stack.enter_context(self.nc.psum_tensor(f"{name}_{self.uid}", list(shape), dt))
        return Buf(t, name, excl=True)

    def dram(self, name, shape, dt=F32, kind="Internal"):
        t = self.nc.dram_tensor(name, list(shape), dt, kind=kind)
        return Buf(t.ap(), name)

    def _esem(self, eng, idx):
        ep = idx // EPOCH
        k = (eng, ep)
        if k not in self.sems:
            self.sems[k] = self.sem(f"s_{eng}_{ep}")
        return self.sems[k], (idx % EPOCH) + 1

    def _wait(self, eng, ev):
        q, idx = ev
        if isinstance(q, str):
            if q == eng and (eng == "pe" or not SAME_ENGINE_SYNC):
                return
            if self.seen[eng].get(q, -1) >= idx:
                return
            self.seen[eng][q] = idx
            s, v = self._esem(q, idx)
        else:
            if self.seen[eng].get(q, -1) >= idx:
                return
            self.seen[eng][q] = idx
            s = self.dslots[q[0]][q[1]][0]
            v = idx
        self.E[eng].wait_ge(s, v)

    def _deps(self, eng, reads, writes):
        for b in reads:
            if b.lw is not None:
                self._wait(eng, b.lw)
            if b.excl:
                for q, i in list(b.rd.items()):
                    if q != eng:
                        self._wait(eng, (q, i))
        for b in writes:
            if b.lw is not None:
                self._wait(eng, b.lw)
            for q, i in list(b.rd.items()):
                self._wait(eng, (q, i))

    def _mark(self, ev, reads, writes):
        q, idx = ev
        for b in reads:
            if b.rd.get(q, -1) < idx:
                b.rd[q] = idx
        for b in writes:
            b.lw = ev
            b.rd = {}

    def op(self, eng, fn, reads=(), writes=()):
        if self.dead:
            return
        self._deps(eng, reads, writes)
        idx = self.cnt[eng]
        self.cnt[eng] += 1
        s, v = self._esem(eng, idx)
        fn(self.E[eng]).then_inc(s, 1)
        self._mark((eng, idx), reads, writes)
        self.ninstr += 1

    def pe(self, fn, reads=(), writes=()):
        self.op("pe", fn, reads, writes)

    def act(self, fn, reads=(), writes=()):
        self.op("act", fn, reads, writes)

    def dve(self, fn, reads=(), writes=()):
        self.op("dve", fn, reads, writes)

    def pool(self, fn, reads=(), writes=()):
        self.op("pool", fn, reads, writes)

    def dma(self, eng, out_ap, in_ap, reads=(), writes=(), nslots=8, **kw):
        if self.dead:
            return
        if eng not in self.dslots:
            self.dslots[eng] = [[self.sem(f"d_{eng}_{i}"), 0] for i in range(nslots)]
            self.dnext[eng] = 0
        si = self.dnext[eng]
        self.dnext[eng] = (si + 1) % len(self.dslots[eng])
        slot = self.dslots[eng][si]
        q = (eng, si)
        if slot[1] > 0:
            self._wait(eng, (q, slot[1]))
        self._deps(eng, reads, writes)
        slot[1] += 16
        self.E[eng].dma_start(out=out_ap, in_=in_ap, **kw).then_inc(slot[0], 16)
        self._mark((q, slot[1]), reads, writes)
        self.ninstr += 1

    def all_events(self):
        evs = []
        for e in ("pe", "act", "dve", "pool"):
            if self.cnt[e] > 0:
                evs.append((e, self.cnt[e] - 1))
        for eng, slots in self.dslots.items():
            for si, (s, c) in enumerate(slots):
                if c > 0:
                    evs.append(((eng, si), c))
        return evs

    def barrier(self, engs=None):
        evs = self.all_events()
        for e in (engs or self.ENGS):
            for ev in evs:
                if ev[0] == e and e == "pe":
                    continue
                self._wait(e, ev)

    @contextmanager
    def scope(self):
        old = self.stack
        try:
            with ExitStack() as st:
                self.stack = st
                try:
                    yield
                finally:
                    self.barrier()
        finally:
            self.stack = old

    def finish(self):
        self.barrier(["sp"])


def make_consts():
    p = np.arange(128)[:, None]
    f = np.arange(128)[None, :]
    same = (p // 64) == (f // 64)
    cols = {}
    parts = []

    def add(name, arr):
        cols[name] = (sum(a.shape[1] for a in parts), arr.shape[1])
        parts.append(arr.astype(np.float32))

    add("ident", (p == f))
    add("tri01", same & (p <= f))
    add("stri01", same & (p < f))
    add("su01", same & (p > f))
    add("sl01", same & (p >= f))
    add("bones", same)
    add("ones", np.ones((128, 128)))
    add("chunkind", (p // 64) == np.arange(2)[None, :])
    add("tri16", (same & (p <= f)) * (-1.0 / 16.0))
    add("bones16", same * (-1.0 / 16.0))
    add("chunkind16", ((p // 64) == np.arange(2)[None, :]) * (-1.0 / 16.0))
    return np.concatenate(parts, axis=1), cols


CST_NP, CST_COLS = make_consts()


class Consts:
    def __init__(self, P, cst_dram):
        self.P = P
        n = CST_NP.shape[1]
        self.f = P.sbuf("cst_f", [128, n])
        P.dma("sp", self.f[:], cst_dram.t[:, :], reads=[cst_dram], writes=[self.f])
        self.identb = P.sbuf("identb", [128, 128], BF16)
        P.dve(lambda e: e.tensor_copy(out=self.identb[:], in_=self.c("ident")), reads=[self.f], writes=[self.identb])

    def c(self, name, rows=128):
        o, w = CST_COLS[name]
        return self.f[0:rows, o:o + w]


def norm_gate(P, src_ap, src_bufs, z_ap, z_bufs, nw_ap, nw_bufs, G, gsz, out, tmp, gate_first):
    a, b, ss, sg = tmp["a"], tmp["b"], tmp["ss"], tmp["sg"]
    n = G * gsz
    P.act(lambda e: e.activation(out=sg[:, 0:n], in_=z_ap, func=AF.Silu), reads=z_bufs, writes=[sg])
    if gate_first:
        P.dve(lambda e: e.tensor_tensor(out=a[:, 0:n], in0=src_ap, in1=sg[:, 0:n], op=ALU.mult),
              reads=list(src_bufs) + [sg], writes=[a])
    else:
        P.dve(lambda e: e.tensor_copy(out=a[:, 0:n], in_=src_ap), reads=list(src_bufs), writes=[a])
    P.act(lambda e: e.activation(out=b[:, 0:n], in_=a[:, 0:n], func=AF.Square), reads=[a], writes=[b])
    P.dve(lambda e: e.tensor_reduce(out=ss[:, 0:G], in_=b[:, 0:n].rearrange("p (g e) -> p g e", g=G), axis=AX.X, op=ALU.add),
          reads=[b], writes=[ss])
    P.act(lambda e: e.activation(out=ss[:, 0:G], in_=ss[:, 0:G], func=AF.Sqrt, scale=1.0 / gsz, bias=tmp["eps"][:, 0:1]),
          reads=[ss, tmp["eps"]], writes=[ss])
    P.dve(lambda e: e.reciprocal(out=ss[:, 0:G], in_=ss[:, 0:G]), reads=[ss], writes=[ss])
    P.dve(lambda e: e.tensor_tensor(out=b[:, 0:n].rearrange("p (g e) -> p g e", g=G),
                                    in0=a[:, 0:n].rearrange("p (g e) -> p g e", g=G),
                                    in1=ss[:, 0:G].unsqueeze(2).to_broadcast([128, G, gsz]), op=ALU.mult),
          reads=[a, ss], writes=[b])
    if gate_first:
        P.pool(lambda e: e.tensor_tensor(out=out[:, 0:n].rearrange("p (g e) -> p g e", g=G),
                                         in0=b[:, 0:n].rearrange("p (g e) -> p g e", g=G), in1=nw_ap, op=ALU.mult),
               reads=[b] + list(nw_bufs), writes=[out])
    else:
        P.pool(lambda e: e.tensor_tensor(out=a[:, 0:n].rearrange("p (g e) -> p g e", g=G),
                                         in0=b[:, 0:n].rearrange("p (g e) -> p g e", g=G), in1=nw_ap, op=ALU.mult),
               reads=[b] + list(nw_bufs), writes=[a])
        P.pool(lambda e: e.tensor_tensor(out=out[:, 0:n], in0=a[:, 0:n], in1=sg[:, 0:n], op=ALU.mult),
               reads=[a, sg], writes=[out])


def ng_tmp(P):
    t = dict(a=P.sbuf("ng_a", [128, 512]), b=P.sbuf("ng_b", [128, 512]), ss=P.sbuf("ng_ss", [128, 8]),
             sg=P.sbuf("ng_sg", [128, 512]), eps=P.sbuf("ng_eps", [128, 1]), one=P.sbuf("ng_one", [128, 1]))
    P.pool(lambda e: e.memset(t["eps"][:], EPS), writes=[t["eps"]])
    P.pool(lambda e: e.memset(t["one"][:], 1.0), writes=[t["one"]])
    return t


PT_NQ, PT_NKC, PT_NVC, PT_NKS, PT_NKW = 0, 512, 640, 768, 896
PT_SXBC = 1024
PT_GQKV = 2048
PT_LQ, PT_LK, PT_LLR = 3584, 3840, 4096
PT_ROWS = 4112
PN_NVS, PN_NVW, PN_NGATE, PN_SZ, PN_SDT = 0, 128, 256, 280, 792
PN_GZ, PN_GBETA, PN_GA, PN_LK, PN_LV, PN_LG = 800, 1312, 1316, 1320, 1576, 2088
PN_COLS = 2600
PT_GROUPS = ([(0 + 128 * i, 128, PT_NQ + 128 * i) for i in range(4)] +
             [(512, 128, PT_NKC), (640, 128, PT_NVC), (768, 128, PT_NKS), (1024, 128, PT_NKW)] +
             [(1816 + 128 * i, 128, PT_SXBC + 128 * i) for i in range(8)] +
             [(2848 + 128 * i, 128, PT_GQKV + 128 * i) for i in range(12)] +
             [(4904 + 128 * i, 128, PT_LQ + 128 * i) for i in range(2)] +
             [(5160 + 128 * i, 128, PT_LK + 128 * i) for i in range(2)] +
             [(6440, 16, PT_LLR)])
PN_GROUPS = [(896, 128, PN_NVS), (1152, 512, PN_NVW), (1664, 152, PN_NVW + 512), (2840, 8, PN_SDT),
             (4384, 512, PN_GZ), (4896, 8, PN_GBETA), (5160, 256, PN_LK), (5416, 512, PN_LV), (5928, 512, PN_LG)]


def stage_gla(P, C, projT, projN, ymix, prm, l):
    with P.scope():
        w2 = P.sbuf("gla_w2", [16, 256])
        gb = P.sbuf("gla_gb", [1, 256])
        nwb = P.sbuf("gla_nwb", [128, 128])
        P.dma("sp", w2[:], prm["gla_gate_w2"].t[l], reads=[prm["gla_gate_w2"]], writes=[w2])
        P.dma("sp", gb[:], prm["gla_gate_b"].t[l:l + 1, :], reads=[prm["gla_gate_b"]], writes=[gb])
        P.dma("sp", nwb[:], prm["gla_norm_w"].t[l:l + 1, :].partition_broadcast(128), reads=[prm["gla_norm_w"]], writes=[nwb])
        S = P.sbuf("gla_S", [64, 4, 128])
        Sb = [P.sbuf(f"gla_Sb{i}", [64, 4, 128], BF16) for i in range(2)]
        P.dve(lambda e: e.memset(S[:], 0.0), writes=[S])
        P.dve(lambda e: e.memset(Sb[0][:], 0.0), writes=[Sb[0]])
        tmp = ng_tmp(P)
        NB = 2
        qT = [P.sbuf(f"gla_qT{i}", [64, 4, 128]) for i in range(NB)]
        kT = [P.sbuf(f"gla_kT{i}", [64, 4, 128]) for i in range(NB)]
        lrT = [P.sbuf(f"gla_lrT{i}", [16, 128]) for i in range(NB)]
        tokN = [P.sbuf(f"gla_tokN{i}", [128, 1280]) for i in range(NB)]
        lsp = P.sbuf("gla_lsp", [128, 256])
        ex = P.sbuf("gla_ex", [128, 256])
        kend = P.sbuf("gla_kend", [128, 256], BF16)
        vb = P.sbuf("gla_vb", [128, 512], BF16)
        ebT = P.sbuf("gla_ebT", [64, 512])
        qdT = P.sbuf("gla_qdT", [64, 4, 128], BF16)
        kiT = P.sbuf("gla_kiT", [64, 4, 128], BF16)
        dec = P.sbuf("gla_dec", [64, 8])
        AT = P.sbuf("gla_AT", [128, 4, 128], BF16)
        yo = [P.sbuf(f"gla_yo{i}", [128, 512]) for i in range(2)]
        ps_gk = P.psum("gla_ps_gk", [128, 512])
        ps_bl = P.psum("gla_ps_bl", [128, 512])
        ps_bT = P.psum("gla_ps_bT", [64, 512])
        ps_blT = P.psum("gla_ps_blT", [64, 8])
        ps_at = P.psum("gla_ps_at", [128, 512])
        ps_o = P.psum("gla_ps_o", [128, 512])
        ps_loc = [P.psum(f"gla_ps_loc{i}", [64, 512]) for i in range(2)]
        cf = [C.f]
        bg_begin(P)

        def load(t):
            i = t % NB
            tok = slice(t * 128, (t + 1) * 128)
            P.dma("sp", qT[i][:], projT.t[PT_LQ:PT_LQ + 256, tok].rearrange("(h d) t -> d h t", d=64), reads=[projT], writes=[qT[i]])
            P.dma("sp", kT[i][:], projT.t[PT_LK:PT_LK + 256, tok].rearrange("(h d) t -> d h t", d=64), reads=[projT], writes=[kT[i]])
            P.dma("sp", lrT[i][:], projT.t[PT_LLR:PT_LLR + 16, tok], reads=[projT], writes=[lrT[i]])
            P.dma("sp", tokN[i][:], projN.t[tok, PN_LK:PN_LK + 1280], reads=[projN], writes=[tokN[i]])

        load(0)
        for t in range(NT):
            if t + 1 < NT:
                load(t + 1)
            i = t % NB
            tok = slice(t * 128, (t + 1) * 128)
            kN = tokN[i][:, 0:256]
            vN = tokN[i][:, 256:768]
            gN = tokN[i][:, 768:1280]
            P.pe(lambda e: e.matmul(ps_gk[:, 0:256], lhsT=lrT[i][:], rhs=w2[:], start=True, stop=False), reads=[lrT[i], w2], writes=[ps_gk])
            P.pe(lambda e: e.matmul(ps_gk[:, 0:256], lhsT=C.c("ones", 1), rhs=gb[:], start=False, stop=True), reads=[gb] + cf, writes=[ps_gk])
            P.act(lambda e: e.activation(out=ex[:], in_=ps_gk[:, 0:256], func=AF.Exp, scale=-1.0), reads=[ps_gk], writes=[ex])
            P.act(lambda e: e.activation(out=lsp[:], in_=ex[:], func=AF.Ln, bias=tmp["one"][:, 0:1]), reads=[ex, tmp["one"]], writes=[lsp])
            P.pe(lambda e: e.matmul(ps_gk[:, 256:512], lhsT=C.c("tri16"), rhs=lsp[:], start=True, stop=True), reads=[lsp] + cf, writes=[ps_gk])
            P.pe(lambda e: e.matmul(ps_bl[:, 0:256], lhsT=C.c("bones16"), rhs=lsp[:], start=True, stop=True), reads=[lsp] + cf, writes=[ps_bl])
            for h in range(4):
                P.pe(lambda e, h=h: e.matmul(ps_bT[:, h * 128:(h + 1) * 128], lhsT=lsp[:, h * 64:(h + 1) * 64], rhs=C.c("tri16"), start=True, stop=True),
                     reads=[lsp] + cf, writes=[ps_bT])
            for h in range(4):
                P.pe(lambda e, h=h: e.matmul(ps_blT[:, h * 2:(h + 1) * 2], lhsT=lsp[:, h * 64:(h + 1) * 64], rhs=C.c("chunkind16"), start=True, stop=True),
                     reads=[lsp] + cf, writes=[ps_blT])
            P.dve(lambda e: e.tensor_copy(out=ex[:], in_=ps_gk[:, 256:512]), reads=[ps_gk], writes=[ex])
            P.dve(lambda e: e.tensor_tensor(out=ex[:], in0=ps_bl[:, 0:256], in1=ex[:], op=ALU.subtract), reads=[ps_bl, ex], writes=[ex])
            P.act(lambda e: e.activation(out=ex[:], in_=ex[:], func=AF.Exp), reads=[ex], writes=[ex])
            P.dve(lambda e: e.tensor_tensor(out=kend[:], in0=kN, in1=ex[:], op=ALU.mult), reads=[tokN[i], ex], writes=[kend])
            P.pool(lambda e: e.tensor_copy(out=vb[:], in_=vN), reads=[tokN[i]], writes=[vb])
            P.act(lambda e: e.activation(out=ebT[:], in_=ps_bT[:], func=AF.Exp), reads=[ps_bT], writes=[ebT])
            P.dve(lambda e: e.scalar_tensor_tensor(out=qdT[:].rearrange("d h t -> d (h t)"), in0=qT[i][:].rearrange("d h t -> d (h t)"), scalar=0.125,
                                                   in1=ebT[:], op0=ALU.mult, op1=ALU.mult), reads=[qT[i], ebT], writes=[qdT])
            P.act(lambda e: e.activation(out=ebT[:], in_=ps_bT[:], func=AF.Exp, scale=-1.0), reads=[ps_bT], writes=[ebT])
            P.dve(lambda e: e.tensor_tensor(out=kiT[:].rearrange("d h t -> d (h t)"), in0=kT[i][:].rearrange("d h t -> d (h t)"), in1=ebT[:], op=ALU.mult),
                  reads=[kT[i], ebT], writes=[kiT])
            P.act(lambda e: e.activation(out=dec[:], in_=ps_blT[:], func=AF.Exp), reads=[ps_blT], writes=[dec])
            for h in range(4):
                P.pe(lambda e, h=h: e.matmul(ps_at[:, h * 128:(h + 1) * 128], lhsT=kiT[:, h, :], rhs=qdT[:, h, :], start=True, stop=True),
                     reads=[kiT, qdT], writes=[ps_at])
            P.dve(lambda e: e.tensor_tensor(out=AT[:], in0=ps_at[:].rearrange("p (h t) -> p h t", h=4),
                                            in1=C.c("tri01").unsqueeze(1).to_broadcast([128, 4, 128]), op=ALU.mult), reads=[ps_at] + cf, writes=[AT])
            for c in range(2):
                rows = slice(c * 64, (c + 1) * 64)
                for h in range(4):
                    P.pe(lambda e, h=h, rows=rows, c=c: e.matmul(ps_loc[c][:, h * 128:(h + 1) * 128], lhsT=kend[rows, h * 64:(h + 1) * 64],
                                                                 rhs=vb[rows, h * 128:(h + 1) * 128], start=True, stop=True),
                         reads=[kend, vb], writes=[ps_loc[c]])
            for c in range(2):
                P.dve(lambda e, c=c: e.tensor_tensor(out=S[:], in0=S[:], in1=dec[:].rearrange("d (h c) -> d h c", c=2)[:, :, c:c + 1].to_broadcast([64, 4, 128]),
                                                     op=ALU.mult), reads=[S, dec], writes=[S])
                P.dve(lambda e, c=c: e.tensor_tensor(out=S[:].rearrange("d h e -> d (h e)"), in0=S[:].rearrange("d h e -> d (h e)"), in1=ps_loc[c][:], op=ALU.add),
                      reads=[S, ps_loc[c]], writes=[S])
                if c == 0:
                    P.act(lambda e: e.copy(out=Sb[1][:], in_=S[:]), reads=[S], writes=[Sb[1]])
            for h in range(4):
                cols = slice(h * 128, (h + 1) * 128)
                P.pe(lambda e, h=h, cols=cols: e.matmul(ps_o[:, cols], lhsT=AT[:, h, :], rhs=vb[:, cols], start=True, stop=False),
                     reads=[AT, vb], writes=[ps_o])
                for c in range(2):
                    rows = slice(c * 64, (c + 1) * 64)
                    P.pe(lambda e, h=h, cols=cols, rows=rows, c=c: e.matmul(ps_o[rows, cols], lhsT=qdT[:, h, rows], rhs=Sb[c][:, h, :], start=False, stop=(c == 1)),
                         reads=[qdT, Sb[c]], writes=[ps_o])
            P.act(lambda e: e.copy(out=Sb[0][:], in_=S[:]), reads=[S], writes=[Sb[0]])
            y = yo[t % 2]
            norm_gate(P, ps_o[:], [ps_o], gN, [tokN[i]], nwb[:].unsqueeze(1).to_broadcast([128, 4, 128]), [nwb], 4, 128, y, tmp, False)
            P.dma("sp", ymix.t[tok, 1536:2048], y[:], reads=[y], writes=[ymix])
            bg_tick(P, 1)
        bg_end(P)


def host_param(name, arr):
    a = np.asarray(arr, np.float32)
    if name in ("ssd_conv_w", "gdn_conv_w"):
        L, K, CH = a.shape
        a = a.reshape(L, K, CH // 128, 128).transpose(0, 3, 2, 1)
    elif name in ("norm1_w", "norm2_w"):
        L = a.shape[0]
        a = a.reshape(L, 16, 128).transpose(2, 0, 1)
    elif name == "final_norm_w":
        a = a.reshape(1, -1)
    elif name == "nsa_cmp_pos":
        a = a.transpose(0, 1, 3, 2)
    elif name == "ssd_conv_b":
        L, CH = a.shape
        a = a.reshape(L, CH // 128, 128).transpose(0, 2, 1)
    return np.ascontiguousarray(a)


def bc(ap, shape):
    return ap.to_broadcast(list(shape))


def causal_conv_silu(P, projT, row0, ntiles, cw, cb, dst, dst_off, name, bias=True):
    xpad = [P.sbuf(f"{name}_xpad{i}", [128, SEQ + 3]) for i in range(2)]
    acc = [P.sbuf(f"{name}_acc{i}", [128, SEQ]) for i in range(2)]
    for i in range(2):
        P.pool(lambda e, i=i: e.memset(xpad[i][:, 0:3], 0.0), writes=[xpad[i]])
    for ct in range(ntiles):
        xp = xpad[ct % 2]
        ac = acc[ct % 2]
        P.dma("sp", xp[:, 3:SEQ + 3], projT.t[row0 + ct * 128:row0 + (ct + 1) * 128, :], reads=[projT], writes=[xp])
        eng = P.dve
        eng(lambda e, ct=ct, xp=xp, ac=ac: e.tensor_scalar(out=ac[:], in0=xp[:, 0:SEQ], scalar1=cw[:, ct, 0:1], scalar2=None, op0=ALU.mult),
            reads=[xp, cw], writes=[ac])
        for k in range(1, 4):
            eng(lambda e, ct=ct, xp=xp, ac=ac, k=k: e.scalar_tensor_tensor(out=ac[:], in0=xp[:, k:SEQ + k], scalar=cw[:, ct, k:k + 1], in1=ac[:],
                                                                            op0=ALU.mult, op1=ALU.add), reads=[xp, cw, ac], writes=[ac])
        if bias:
            P.act(lambda e, ct=ct, ac=ac: e.activation(out=dst[:, dst_off + ct, :], in_=ac[:], func=AF.Silu, bias=cb[:, ct:ct + 1]),
                  reads=[ac, cb], writes=[dst])
        else:
            P.act(lambda e, ct=ct, ac=ac: e.activation(out=dst[:, dst_off + ct, :], in_=ac[:], func=AF.Silu), reads=[ac], writes=[dst])


def softplus_small(P, x_ap, xbuf, tmpb, one):
    P.act(lambda e: e.activation(out=x_ap, in_=x_ap, func=AF.Exp), reads=[xbuf], writes=[xbuf])
    P.act(lambda e: e.activation(out=x_ap, in_=x_ap, func=AF.Ln, bias=one[:, 0:1]), reads=[xbuf, one], writes=[xbuf])


def stage_ssd(P, C, projT, projN, ymix, prm, l):
    with P.scope():
        cf = [C.f]
        cw = P.sbuf("ssd_cw", [128, 8, 4])
        cb = P.sbuf("ssd_cb", [128, 8])
        dtb = P.sbuf("ssd_dtb", [128, 8])
        aneg = P.sbuf("ssd_aneg", [128, 8])
        dsk = P.sbuf("ssd_dsk", [128, 8])
        nwb = P.sbuf("ssd_nwb", [128, 512])
        P.dma("sp", cw[:], prm["ssd_conv_w"].t[l], reads=[prm["ssd_conv_w"]], writes=[cw])
        P.dma("sp", cb[:], prm["ssd_conv_b"].t[l], reads=[prm["ssd_conv_b"]], writes=[cb])
        P.dma("sp", dtb[:], prm["ssd_dt_bias"].t[l:l + 1, :].partition_broadcast(128), reads=[prm["ssd_dt_bias"]], writes=[dtb])
        P.dma("sp", aneg[:], prm["ssd_a_log"].t[l:l + 1, :].partition_broadcast(128), reads=[prm["ssd_a_log"]], writes=[aneg])
        P.dma("sp", dsk[:], prm["ssd_d"].t[l:l + 1, :].partition_broadcast(128), reads=[prm["ssd_d"]], writes=[dsk])
        P.dma("sp", nwb[:], prm["ssd_norm_w"].t[l:l + 1, :].partition_broadcast(128), reads=[prm["ssd_norm_w"]], writes=[nwb])
        P.act(lambda e: e.activation(out=aneg[:], in_=aneg[:], func=AF.Exp), reads=[aneg], writes=[aneg])
        P.dve(lambda e: e.tensor_scalar(out=aneg[:], in0=aneg[:], scalar1=-1.0, scalar2=None, op0=ALU.mult), reads=[aneg], writes=[aneg])
        act = P.sbuf("ssd_act", [128, 8, SEQ])
        with P.scope():
            causal_conv_silu(P, projT, PT_SXBC, 8, cw, cb, act, 0, "ssd")
        BCb = P.sbuf("ssd_BCb", [128, 4, SEQ], BF16)
        for k in range(4):
            (P.dve if k % 2 == 0 else P.pool)(lambda e, k=k: e.tensor_copy(out=BCb[:, k, :], in_=act[:, 4 + k, :]), reads=[act], writes=[BCb])
        tmp = ng_tmp(P)
        S = P.sbuf("ssd_S", [128, 8, 64])
        Sb = [P.sbuf(f"ssd_Sb{i}", [128, 8, 64], BF16) for i in range(2)]
        P.dve(lambda e: e.memset(S[:], 0.0), writes=[S])
        P.dve(lambda e: e.memset(Sb[0][:], 0.0), writes=[Sb[0]])
        tokN = [P.sbuf(f"ssd_tokN{i}", [128, 520]) for i in range(2)]
        xN = P.sbuf("ssd_xN", [128, 512])
        BNb = P.sbuf("ssd_BNb", [128, 256], BF16)
        dt8 = P.sbuf("ssd_dt8", [128, 8])
        a8 = P.sbuf("ssd_a8", [128, 8])
        dw8 = P.sbuf("ssd_dw8", [128, 8])
        ac16 = P.sbuf("ssd_ac16", [128, 2, 8])
        e32 = P.sbuf("ssd_e32", [128, 32])
        Aexp = P.sbuf("ssd_Aexp", [128, 8, 128])
        seg = P.sbuf("ssd_seg", [128, 8, 128])
        CBm = P.sbuf("ssd_CBm", [128, 2, 128])
        MT = P.sbuf("ssd_MT", [128, 8, 128], BF16)
        xdt = P.sbuf("ssd_xdt", [128, 8, 64], BF16)
        xw = P.sbuf("ssd_xw", [128, 8, 64], BF16)
        y1 = P.sbuf("ssd_y1", [128, 512])
        y2 = P.sbuf("ssd_y2", [128, 512])
        yo = [P.sbuf(f"ssd_yo{i}", [128, 512]) for i in range(2)]
        psA = P.psum("ssd_psA", [128, 512])
        psB = P.psum("ssd_psB", [128, 512])
        psC = P.psum("ssd_psC", [128, 512])
        psD = P.psum("ssd_psD", [128, 512])
        psE = P.psum("ssd_psE", [128, 512])
        psF = P.psum("ssd_psF", [128, 512])
        psG = [P.psum(f"ssd_psG{i}", [128, 512]) for i in range(2)]

        bg_begin(P)

        def load(t):
            P.dma("sp", tokN[t % 2][:], projN.t[t * 128:(t + 1) * 128, PN_SZ:PN_SZ + 520], reads=[projN], writes=[tokN[t % 2]])

        load(0)
        for t in range(NT):
            if t + 1 < NT:
                load(t + 1)
            tk = tokN[t % 2]
            tok = slice(t * 128, (t + 1) * 128)
            for k in range(4):
                P.pe(lambda e, k=k: e.transpose(out=psA[:, k * 128:(k + 1) * 128], in_=act[:, k, tok], identity=C.c("ident")), reads=[act] + cf, writes=[psA])
            for k in range(2):
                P.pe(lambda e, k=k: e.transpose(out=psB[:, k * 128:(k + 1) * 128], in_=act[:, 4 + k, tok], identity=C.c("ident")), reads=[act] + cf, writes=[psB])
            P.act(lambda e: e.copy(out=xN[:], in_=psA[:]), reads=[psA], writes=[xN])
            P.dve(lambda e: e.tensor_copy(out=BNb[:], in_=psB[:, 0:256]), reads=[psB], writes=[BNb])
            P.dve(lambda e: e.tensor_tensor(out=dt8[:], in0=tk[:, 512:520], in1=dtb[:], op=ALU.add), reads=[tk, dtb], writes=[dt8])
            softplus_small(P, dt8[:], dt8, None, tmp["one"])
            P.dve(lambda e: e.tensor_tensor(out=a8[:], in0=dt8[:], in1=aneg[:], op=ALU.mult), reads=[dt8, aneg], writes=[a8])
            P.dve(lambda e: e.tensor_tensor(out=Aexp[:], in0=bc(a8[:].unsqueeze(2), [128, 8, 128]), in1=bc(C.c("tri01").unsqueeze(1), [128, 8, 128]), op=ALU.mult),
                  reads=[a8] + cf, writes=[Aexp])
            P.dve(lambda e: e.tensor_tensor(out=ac16[:], in0=bc(a8[:].unsqueeze(1), [128, 2, 8]), in1=bc(C.c("chunkind").unsqueeze(2), [128, 2, 8]), op=ALU.mult),
                  reads=[a8] + cf, writes=[ac16])
            P.pe(lambda e: e.matmul(psC[:], lhsT=C.c("su01"), rhs=Aexp[:, 0:4, :].rearrange("p h i -> p (h i)"), start=True, stop=True), reads=[Aexp] + cf, writes=[psC])
            P.pe(lambda e: e.matmul(psD[:], lhsT=C.c("su01"), rhs=Aexp[:, 4:8, :].rearrange("p h i -> p (h i)"), start=True, stop=True), reads=[Aexp] + cf, writes=[psD])
            P.act(lambda e: e.activation(out=seg[:, 0:4, :].rearrange("p h i -> p (h i)"), in_=psC[:], func=AF.Exp), reads=[psC], writes=[seg])
            P.act(lambda e: e.activation(out=seg[:, 4:8, :].rearrange("p h i -> p (h i)"), in_=psD[:], func=AF.Exp), reads=[psD], writes=[seg])
            P.pe(lambda e: e.matmul(psE[:, 0:8], lhsT=C.c("tri01"), rhs=a8[:], start=True, stop=True), reads=[a8] + cf, writes=[psE])
            P.pe(lambda e: e.matmul(psE[:, 8:16], lhsT=C.c("su01"), rhs=a8[:], start=True, stop=True), reads=[a8] + cf, writes=[psE])
            P.pe(lambda e: e.matmul(psE[:, 16:32], lhsT=C.c("ones"), rhs=ac16[:].rearrange("p c h -> p (c h)"), start=True, stop=True), reads=[ac16] + cf, writes=[psE])
            P.act(lambda e: e.activation(out=e32[:], in_=psE[:, 0:32], func=AF.Exp), reads=[psE], writes=[e32])
            ea = e32[:, 0:8]
            w8 = e32[:, 8:16]
            for g in range(2):
                P.pe(lambda e, g=g: e.matmul(psB[:, 256 + g * 128:256 + (g + 1) * 128], lhsT=BCb[:, g, tok], rhs=BCb[:, 2 + g, tok], start=True, stop=True),
                     reads=[BCb], writes=[psB])
            P.dve(lambda e: e.tensor_tensor(out=CBm[:], in0=psB[:, 256:512].rearrange("p (g i) -> p g i", g=2), in1=bc(C.c("tri01").unsqueeze(1), [128, 2, 128]), op=ALU.mult),
                  reads=[psB] + cf, writes=[CBm])
            P.dve(lambda e: e.tensor_tensor(out=MT[:].rearrange("p (g r) i -> p g r i", g=2), in0=seg[:].rearrange("p (g r) i -> p g r i", g=2),
                                            in1=bc(CBm[:].unsqueeze(2), [128, 2, 4, 128]), op=ALU.mult), reads=[seg, CBm], writes=[MT])
            P.dve(lambda e: e.tensor_tensor(out=dw8[:], in0=dt8[:], in1=w8, op=ALU.mult), reads=[dt8, e32], writes=[dw8])
            P.pool(lambda e: e.tensor_tensor(out=xdt[:], in0=xN[:].rearrange("p (h q) -> p h q", h=8), in1=bc(dt8[:].unsqueeze(2), [128, 8, 64]), op=ALU.mult),
                   reads=[xN, dt8], writes=[xdt])
            P.pool(lambda e: e.tensor_tensor(out=xw[:], in0=xN[:].rearrange("p (h q) -> p h q", h=8), in1=bc(dw8[:].unsqueeze(2), [128, 8, 64]), op=ALU.mult),
                   reads=[xN, dw8], writes=[xw])
            for h in range(8):
                P.pe(lambda e, h=h: e.matmul(psF[:, h * 64:(h + 1) * 64], lhsT=MT[:, h, :], rhs=xdt[:, h, :], start=True, stop=True), reads=[MT, xdt], writes=[psF])
            for c in range(2):
                rows = slice(c * 64, (c + 1) * 64)
                for g in range(2):
                    P.pe(lambda e, c=c, g=g, rows=rows: e.matmul(psG[c][:, g * 256:(g + 1) * 256], lhsT=BNb[rows, g * 128:(g + 1) * 128],
                                                                 rhs=xw[rows, 4 * g:4 * g + 4, :].rearrange("p h q -> p (h q)"), start=True, stop=True),
                         reads=[BNb, xw], writes=[psG[c]])
            for c in range(2):
                P.dve(lambda e, c=c: e.tensor_tensor(out=S[:], in0=S[:], in1=bc(e32[:, 16 + 8 * c:24 + 8 * c].unsqueeze(2), [128, 8, 64]), op=ALU.mult),
                      reads=[S, e32], writes=[S])
                P.dve(lambda e, c=c: e.tensor_tensor(out=S[:].rearrange("p h q -> p (h q)"), in0=S[:].rearrange("p h q -> p (h q)"), in1=psG[c][:], op=ALU.add),
                      reads=[S, psG[c]], writes=[S])
                if c == 0:
                    P.act(lambda e: e.copy(out=Sb[1][:], in_=S[:]), reads=[S], writes=[Sb[1]])
            for c in range(2):
                rows = slice(c * 64, (c + 1) * 64)
                for g in range(2):
                    P.pe(lambda e, c=c, g=g, rows=rows: e.matmul(psC[rows, g * 256:(g + 1) * 256], lhsT=BCb[:, 2 + g, t * 128 + c * 64:t * 128 + (c + 1) * 64],
                                                                 rhs=Sb[c][:, 4 * g:4 * g + 4, :].rearrange("p h q -> p (h q)"), start=True, stop=True),
                         reads=[BCb, Sb[c]], writes=[psC])
            P.act(lambda e: e.copy(out=Sb[0][:], in_=S[:]), reads=[S], writes=[Sb[0]])
            P.dve(lambda e: e.tensor_tensor(out=y1[:].rearrange("p (h q) -> p h q", h=8), in0=psC[:].rearrange("p (h q) -> p h q", h=8),
                                            in1=bc(ea.unsqueeze(2), [128, 8, 64]), op=ALU.mult), reads=[psC, e32], writes=[y1])
            P.dve(lambda e: e.tensor_tensor(out=y1[:], in0=y1[:], in1=psF[:], op=ALU.add), reads=[y1, psF], writes=[y1])
            P.pool(lambda e: e.tensor_tensor(out=y2[:].rearrange("p (h q) -> p h q", h=8), in0=xN[:].rearrange("p (h q) -> p h q", h=8),
                                             in1=bc(dsk[:].unsqueeze(2), [128, 8, 64]), op=ALU.mult), reads=[xN, dsk], writes=[y2])
            P.pool(lambda e: e.tensor_tensor(out=y1[:], in0=y1[:], in1=y2[:], op=ALU.add), reads=[y1, y2], writes=[y1])
            y = yo[t % 2]
            norm_gate(P, y1[:], [y1], tk[:, 0:512], [tk], nwb[:].rearrange("p (g e) -> p g e", g=2), [nwb], 2, 256, y, tmp, True)
            P.dma("sp", ymix.t[tok, 512:1024], y[:], reads=[y], writes=[ymix])
            bg_tick(P, 2)
        bg_end(P)


def stage_gdn(P, C, projT, projN, ymix, prm, l):
    H = 4
    with P.scope():
        cf = [C.f]
        cw = P.sbuf("gdn_cw", [128, 12, 4])
        dtb = P.sbuf("gdn_dtb", [128, 4])
        aneg = P.sbuf("gdn_aneg", [128, 4])
        nwb = P.sbuf("gdn_nwb", [128, 128])
        P.dma("sp", cw[:], prm["gdn_conv_w"].t[l], reads=[prm["gdn_conv_w"]], writes=[cw])
        P.dma("sp", dtb[:], prm["gdn_dt_bias"].t[l:l + 1, :].partition_broadcast(128), reads=[prm["gdn_dt_bias"]], writes=[dtb])
        P.dma("sp", aneg[:], prm["gdn_a_log"].t[l:l + 1, :].partition_broadcast(128), reads=[prm["gdn_a_log"]], writes=[aneg])
        P.dma("sp", nwb[:], prm["gdn_norm_w"].t[l:l + 1, :].partition_broadcast(128), reads=[prm["gdn_norm_w"]], writes=[nwb])
        P.act(lambda e: e.activation(out=aneg[:], in_=aneg[:], func=AF.Exp), reads=[aneg], writes=[aneg])
        P.dve(lambda e: e.tensor_scalar(out=aneg[:], in0=aneg[:], scalar1=-1.0, scalar2=None, op0=ALU.mult), reads=[aneg], writes=[aneg])
        tmp = ng_tmp(P)
        qkvb = P.sbuf("gdn_qkvb", [128, 12, SEQ], BF16)
        with P.scope():
            cvt = P.sbuf("gdn_cvt", [128, 1, SEQ])
            sq = P.sbuf("gdn_sq", [128, SEQ])
            rinv = P.sbuf("gdn_rinv", [128, SEQ])
            pss = [P.psum(f"gdn_pss{i}", [128, 512]) for i in range(4)]
            xpad = [P.sbuf(f"gdn_xpad{i}", [128, SEQ + 3]) for i in range(2)]
            acc = P.sbuf("gdn_acc", [128, SEQ])
            for i in range(2):
                P.pool(lambda e, i=i: e.memset(xpad[i][:, 0:3], 0.0), writes=[xpad[i]])
            for ct in range(12):
                xp = xpad[ct % 2]
                P.dma("sp", xp[:, 3:SEQ + 3], projT.t[PT_GQKV + ct * 128:PT_GQKV + (ct + 1) * 128, :], reads=[projT], writes=[xp])
                P.dve(lambda e, ct=ct, xp=xp: e.tensor_scalar(out=acc[:], in0=xp[:, 0:SEQ], scalar1=cw[:, ct, 0:1], scalar2=None, op0=ALU.mult),
                      reads=[xp, cw], writes=[acc])
                for k in range(1, 4):
                    P.dve(lambda e, ct=ct, xp=xp, k=k: e.scalar_tensor_tensor(out=acc[:], in0=xp[:, k:SEQ + k], scalar=cw[:, ct, k:k + 1], in1=acc[:],
                                                                               op0=ALU.mult, op1=ALU.add), reads=[xp, cw, acc], writes=[acc])
                if ct >= 8:
                    P.act(lambda e, ct=ct: e.activation(out=qkvb[:, ct, :], in_=acc[:], func=AF.Silu), reads=[acc], writes=[qkvb])
                    continue
                P.act(lambda e: e.activation(out=cvt[:, 0, :], in_=acc[:], func=AF.Silu), reads=[acc], writes=[cvt])
                P.act(lambda e: e.activation(out=sq[:], in_=cvt[:, 0, :], func=AF.Square), reads=[cvt], writes=[sq])
                for n in range(4):
                    P.pe(lambda e, n=n: e.matmul(pss[n][:], lhsT=C.c("ones"), rhs=sq[:, n * 512:(n + 1) * 512], start=True, stop=True), reads=[sq] + cf, writes=[pss[n]])
                    P.act(lambda e, n=n: e.activation(out=rinv[:, n * 512:(n + 1) * 512], in_=pss[n][:], func=AF.Sqrt, bias=tmp["eps"][:, 0:1]),
                          reads=[pss[n], tmp["eps"]], writes=[rinv])
                P.dve(lambda e: e.reciprocal(out=rinv[:], in_=rinv[:]), reads=[rinv], writes=[rinv])
                scl = 128.0 ** -0.5 if ct < 4 else 1.0
                P.dve(lambda e, ct=ct, scl=scl: e.scalar_tensor_tensor(out=qkvb[:, ct, :], in0=cvt[:, 0, :], scalar=scl, in1=rinv[:], op0=ALU.mult, op1=ALU.mult),
                      reads=[cvt, rinv], writes=[qkvb])
        dbg(1)
        S = P.sbuf("gdn_S", [128, H, 128])
        Sb = P.sbuf("gdn_Sb", [128, H, 128], BF16)
        P.dve(lambda e: e.memset(S[:], 0.0), writes=[S])
        P.dve(lambda e: e.memset(Sb[:], 0.0), writes=[Sb])
        tokN = [P.sbuf(f"gdn_tokN{i}", [128, 520]) for i in range(2)]

        def f4(name, dt=F32):
            return P.sbuf("gdn_" + name, [128, H, 128], dt)

        b4 = P.sbuf("gdn_b4", [128, 4])
        g4 = P.sbuf("gdn_g4", [128, 4])
        gc8 = P.sbuf("gdn_gc8", [128, 2, 4])
        e16 = P.sbuf("gdn_e16", [128, 16])
        bg4 = P.sbuf("gdn_bg4", [128, 4])
        Gt, Gs, DTm, Dm, egb = f4("Gt"), f4("Gs"), f4("DTm"), f4("Dm"), f4("egb")
        A, AT, TT, vb, Kg, u = f4("A"), f4("AT"), f4("TT"), f4("vb"), f4("Kg"), f4("u")
        X = [f4("X0"), f4("X1")]
        XT = [f4("XT0"), f4("XT1")]
        aqkT, wT, qdT, kend, vnew = f4("aqkT", BF16), f4("wT", BF16), f4("qdT", BF16), f4("kend", BF16), f4("vnew", BF16)
        yo = [P.sbuf(f"gdn_yo{i}", [128, 512]) for i in range(2)]
        B0 = P.psum("gdn_B0", [128, 512])
        B1 = P.psum("gdn_B1", [128, 512])
        B2 = P.psum("gdn_B2", [128, 512])
        B3 = P.psum("gdn_B3", [128, 512])
        B4 = P.psum("gdn_B4", [128, 1024], BF16)
        B5 = P.psum("gdn_B5", [128, 512])
        B6 = P.psum("gdn_B6", [128, 512])
        B7 = P.psum("gdn_B7", [128, 512])

        def v4(ap):
            return ap.rearrange("p (h i) -> p h i", h=H)

        def fl(ap):
            return ap.rearrange("p h i -> p (h i)")

        bg_begin(P)

        def load(t):
            P.dma("sp", tokN[t % 2][:], projN.t[t * 128:(t + 1) * 128, PN_GZ:PN_GZ + 520], reads=[projN], writes=[tokN[t % 2]])

        tri = C.c("tri01")
        su = C.c("su01")
        load(0)
        for t in range(NT):
            if t + 1 < NT:
                load(t + 1)
            tk = tokN[t % 2]
            tok = slice(t * 128, (t + 1) * 128)
            P.act(lambda e: e.activation(out=b4[:], in_=tk[:, 512:516], func=AF.Sigmoid), reads=[tk], writes=[b4])
            P.dve(lambda e: e.tensor_tensor(out=g4[:], in0=tk[:, 516:520], in1=dtb[:], op=ALU.add), reads=[tk, dtb], writes=[g4])
            softplus_small(P, g4[:], g4, None, tmp["one"])
            P.dve(lambda e: e.tensor_tensor(out=g4[:], in0=g4[:], in1=aneg[:], op=ALU.mult), reads=[g4, aneg], writes=[g4])
            P.dve(lambda e: e.tensor_tensor(out=Gt[:], in0=bc(g4[:].unsqueeze(2), [128, H, 128]), in1=bc(tri.unsqueeze(1), [128, H, 128]), op=ALU.mult),
                  reads=[g4] + cf, writes=[Gt])
            P.pool(lambda e: e.tensor_tensor(out=Gs[:], in0=bc(g4[:].unsqueeze(2), [128, H, 128]), in1=bc(su.unsqueeze(1), [128, H, 128]), op=ALU.mult),
                   reads=[g4] + cf, writes=[Gs])
            P.dve(lambda e: e.tensor_tensor(out=gc8[:], in0=bc(g4[:].unsqueeze(1), [128, 2, 4]), in1=bc(C.c("chunkind").unsqueeze(2), [128, 2, 4]), op=ALU.mult),
                  reads=[g4] + cf, writes=[gc8])
            P.pe(lambda e: e.matmul(B0[:], lhsT=su, rhs=fl(Gt[:]), start=True, stop=True), reads=[Gt] + cf, writes=[B0])
            P.pe(lambda e: e.matmul(B1[:], lhsT=tri, rhs=fl(Gs[:]), start=True, stop=True), reads=[Gs] + cf, writes=[B1])
            P.pe(lambda e: e.matmul(B2[:], lhsT=C.c("ones"), rhs=fl(Gt[:]), start=True, stop=True), reads=[Gt] + cf, writes=[B2])
            P.pe(lambda e: e.matmul(B3[:, 0:4], lhsT=tri, rhs=g4[:], start=True, stop=True), reads=[g4] + cf, writes=[B3])
            P.pe(lambda e: e.matmul(B3[:, 4:8], lhsT=su, rhs=g4[:], start=True, stop=True), reads=[g4] + cf, writes=[B3])
            P.pe(lambda e: e.matmul(B3[:, 8:16], lhsT=C.c("ones"), rhs=gc8[:].rearrange("p c h -> p (c h)"), start=True, stop=True), reads=[gc8] + cf, writes=[B3])
            P.act(lambda e: e.activation(out=fl(DTm[:]), in_=B0[:], func=AF.Exp), reads=[B0], writes=[DTm])
            P.act(lambda e: e.activation(out=fl(Dm[:]), in_=B1[:], func=AF.Exp), reads=[B1], writes=[Dm])
            P.act(lambda e: e.activation(out=fl(egb[:]), in_=B2[:], func=AF.Exp), reads=[B2], writes=[egb])
            P.act(lambda e: e.activation(out=e16[:], in_=B3[:, 0:16], func=AF.Exp), reads=[B3], writes=[e16])
            P.pool(lambda e: e.tensor_tensor(out=DTm[:], in0=DTm[:], in1=bc(tri.unsqueeze(1), [128, H, 128]), op=ALU.mult), reads=[DTm] + cf, writes=[DTm])
            P.pool(lambda e: e.tensor_tensor(out=Dm[:], in0=Dm[:], in1=bc(su.unsqueeze(1), [128, H, 128]), op=ALU.mult), reads=[Dm] + cf, writes=[Dm])
            P.dve(lambda e: e.tensor_tensor(out=bg4[:], in0=b4[:], in1=e16[:, 0:4], op=ALU.mult), reads=[b4, e16], writes=[bg4])
            dbg(2)
            for h in range(H):
                P.pe(lambda e, h=h: e.transpose(out=B4[:, h * 128:(h + 1) * 128], in_=qkvb[:, 4 + h, tok], identity=C.identb[:]), reads=[qkvb, C.identb], writes=[B4])
            for h in range(H):
                P.pe(lambda e, h=h: e.transpose(out=B4[:, 512 + h * 128:512 + (h + 1) * 128], in_=qkvb[:, 8 + h, tok], identity=C.identb[:]), reads=[qkvb, C.identb], writes=[B4])
            P.dve(lambda e: e.tensor_tensor(out=vb[:], in0=v4(B4[:, 512:1024]), in1=bc(b4[:].unsqueeze(2), [128, H, 128]), op=ALU.mult), reads=[B4, b4], writes=[vb])
            P.dve(lambda e: e.tensor_tensor(out=Kg[:], in0=v4(B4[:, 0:512]), in1=bc(bg4[:].unsqueeze(2), [128, H, 128]), op=ALU.mult), reads=[B4, bg4], writes=[Kg])
            P.dve(lambda e: e.tensor_tensor(out=kend[:], in0=v4(B4[:, 0:512]), in1=bc(e16[:, 4:8].unsqueeze(2), [128, H, 128]), op=ALU.mult), reads=[B4, e16], writes=[kend])
            dbg(3)
            for h in range(H):
                P.pe(lambda e, h=h: e.matmul(B5[:, h * 128:(h + 1) * 128], lhsT=qkvb[:, 4 + h, tok], rhs=qkvb[:, 4 + h, tok], start=True, stop=True), reads=[qkvb], writes=[B5])
            for h in range(H):
                P.pe(lambda e, h=h: e.matmul(B6[:, h * 128:(h + 1) * 128], lhsT=qkvb[:, 4 + h, tok], rhs=qkvb[:, h, tok], start=True, stop=True), reads=[qkvb], writes=[B6])
            P.dve(lambda e: e.tensor_tensor(out=A[:], in0=v4(B5[:]), in1=Dm[:], op=ALU.mult), reads=[B5, Dm], writes=[A])
            P.dve(lambda e: e.tensor_tensor(out=A[:], in0=A[:], in1=bc(b4[:].unsqueeze(2), [128, H, 128]), op=ALU.mult), reads=[A, b4], writes=[A])
            P.dve(lambda e: e.tensor_tensor(out=aqkT[:], in0=v4(B6[:]), in1=DTm[:], op=ALU.mult), reads=[B6, DTm], writes=[aqkT])
            P.pool(lambda e: e.tensor_tensor(out=qdT[:], in0=qkvb[:, 0:4, tok], in1=egb[:], op=ALU.mult), reads=[qkvb, egb], writes=[qdT])
            dbg(4)
            for h in range(H):
                P.pe(lambda e, h=h: e.transpose(out=B0[:, h * 128:(h + 1) * 128], in_=A[:, h, :], identity=C.c("ident")), reads=[A] + cf, writes=[B0])
            dbg(4.3)
            P.act(lambda e: e.copy(out=fl(AT[:]), in_=B0[:]), reads=[B0], writes=[AT])
            dbg(4.6)
            P.dve(lambda e: e.scalar_tensor_tensor(out=TT[:], in0=v4(B0[:]), scalar=-1.0, in1=bc(C.c("ident").unsqueeze(1), [128, H, 128]), op0=ALU.mult, op1=ALU.add),
                  reads=[B0] + cf, writes=[TT])
            dbg(5)
            Xc, XTc = A, AT
            for k in range(1, 6):
                Xn, XTn = X[k % 2], XT[k % 2]
                for h in range(H):
                    P.pe(lambda e, h=h, Xc=Xc, XTc=XTc: e.matmul(B1[:, h * 128:(h + 1) * 128], lhsT=XTc[:, h, :], rhs=Xc[:, h, :], start=True, stop=True),
                         reads=[Xc, XTc], writes=[B1])
                if k < 5:
                    for h in range(H):
                        P.pe(lambda e, h=h, Xc=Xc, XTc=XTc: e.matmul(B2[:, h * 128:(h + 1) * 128], lhsT=Xc[:, h, :], rhs=XTc[:, h, :], start=True, stop=True),
                             reads=[Xc, XTc], writes=[B2])
                P.act(lambda e, Xn=Xn: e.copy(out=fl(Xn[:]), in_=B1[:]), reads=[B1], writes=[Xn])
                if k < 5:
                    P.dve(lambda e, XTn=XTn: e.tensor_copy(out=fl(XTn[:]), in_=B2[:]), reads=[B2], writes=[XTn])
                for h in range(H):
                    P.pe(lambda e, h=h, Xn=Xn: e.matmul(B5[:, h * 128:(h + 1) * 128], lhsT=Xn[:, h, :], rhs=TT[:, h, :], start=True, stop=True),
                         reads=[Xn, TT], writes=[B5])
                P.dve(lambda e: e.tensor_tensor(out=fl(TT[:]), in0=fl(TT[:]), in1=B5[:], op=ALU.add), reads=[TT, B5], writes=[TT])
                Xc, XTc = Xn, XTn
            dbg(6)
            for h in range(H):
                P.pe(lambda e, h=h: e.matmul(B0[:, h * 128:(h + 1) * 128], lhsT=TT[:, h, :], rhs=vb[:, h, :], start=True, stop=True), reads=[TT, vb], writes=[B0])
            for h in range(H):
                P.pe(lambda e, h=h: e.matmul(B5[:, h * 128:(h + 1) * 128], lhsT=Kg[:, h, :], rhs=TT[:, h, :], start=True, stop=True), reads=[TT, Kg], writes=[B5])
            P.act(lambda e: e.copy(out=fl(u[:]), in_=B0[:]), reads=[B0], writes=[u])
            P.dve(lambda e: e.tensor_copy(out=fl(wT[:]), in_=B5[:]), reads=[B5], writes=[wT])
            dbg(7)
            for c in range(2):
                rows = slice(c * 64, (c + 1) * 64)
                for h in range(H):
                    P.pe(lambda e, h=h, rows=rows: e.matmul(B6[rows, h * 128:(h + 1) * 128], lhsT=wT[:, h, rows], rhs=Sb[:, h, :], start=True, stop=True),
                         reads=[wT, Sb], writes=[B6])
                P.dve(lambda e, rows=rows: e.tensor_tensor(out=fl(vnew[rows]), in0=fl(u[rows]), in1=B6[rows, :], op=ALU.subtract), reads=[u, B6], writes=[vnew])
                for h in range(H):
                    P.pe(lambda e, h=h, rows=rows: e.matmul(B7[rows, h * 128:(h + 1) * 128], lhsT=qdT[:, h, rows], rhs=Sb[:, h, :], start=True, stop=False),
                         reads=[qdT, Sb], writes=[B7])
                    P.pe(lambda e, h=h, rows=rows: e.matmul(B7[rows, h * 128:(h + 1) * 128], lhsT=aqkT[rows, h, rows], rhs=vnew[rows, h, :], start=False, stop=True),
                         reads=[aqkT, vnew], writes=[B7])
                for h in range(H):
                    P.pe(lambda e, h=h, rows=rows: e.matmul(B1[:, h * 128:(h + 1) * 128], lhsT=kend[rows, h, :], rhs=vnew[rows, h, :], start=True, stop=True),
                         reads=[kend, vnew], writes=[B1])
                P.dve(lambda e, c=c: e.tensor_tensor(out=S[:], in0=S[:], in1=bc(e16[:, 8 + 4 * c:12 + 4 * c].unsqueeze(2), [128, H, 128]), op=ALU.mult),
                      reads=[S, e16], writes=[S])
                P.dve(lambda e: e.tensor_tensor(out=fl(S[:]), in0=fl(S[:]), in1=B1[:], op=ALU.add), reads=[S, B1], writes=[S])
                P.act(lambda e: e.copy(out=Sb[:], in_=S[:]), reads=[S], writes=[Sb])
            dbg(8)
            y = yo[t % 2]
            norm_gate(P, B7[:], [B7], tk[:, 0:512], [tk], bc(nwb[:].unsqueeze(1), [128, 4, 128]), [nwb], 4, 128, y, tmp, False)
            P.dma("sp", ymix.t[tok, 1024:1536], y[:], reads=[y], writes=[ymix])
            bg_tick(P, 2)
        bg_end(P)


NBIG = 30000.0


def t5_bucket_np(rel):
    n = np.maximum(rel, 0)
    exact = 16
    large = exact + (np.log(np.maximum(n, 1).astype(np.float32) / np.float32(exact)) / np.float32(math.log(128 / 16)) * np.float32(32 - exact)).astype(np.int32)
    return np.where(n < exact, n, np.minimum(large, 31)).astype(np.int64)


def nsa_host_tables(rel_bias):
    rb = np.asarray(rel_bias, np.float32)
    ki = np.arange(128)[:, None]
    qi = np.arange(128)[None, :]
    r0 = qi - ki
    r128 = 128 + qi - ki
    qq = np.arange(128)[:, None]
    mm = np.arange(248)[None, :]
    rc = qq - 16 * (mm - 120) - 31
    tab = np.concatenate([
        rb[t5_bucket_np(r0)].transpose(0, 2, 1).reshape(128, 8 * 128),
        rb[t5_bucket_np(r128)].transpose(0, 2, 1).reshape(128, 8 * 128),
        rb[t5_bucket_np(rc)].transpose(0, 2, 1).reshape(128, 8 * 248)], axis=1)
    t31 = np.broadcast_to(rb[31][None, :], (128, 8)).copy()
    return np.ascontiguousarray(tab, np.float32), np.ascontiguousarray(t31, np.float32)


def nsa_host_consts():
    ki = np.arange(128)[:, None]
    qi = np.arange(128)[None, :]
    qq = np.arange(128)[:, None]
    mm = np.arange(248)[None, :]
    rc = qq - 16 * (mm - 120) - 31
    m0 = np.where(qi - ki >= 0, 0.0, -NBIG)
    msk = np.concatenate([
        np.broadcast_to(m0[:, None, :], (128, 8, 128)).reshape(128, -1),
        np.zeros((128, 8 * 128)),
        np.broadcast_to(np.where(rc >= 0, 0.0, -NBIG)[:, None, :], (128, 8, 248)).reshape(128, -1)], axis=1)
    mtri = np.where(ki > qi, 0.0, -NBIG)
    k = np.arange(128)[:, None]
    j = np.arange(32)[None, :]
    ov = ((16 * k <= 64 * j + 63) & (16 * k + 31 >= 64 * j) & (k < 127)).astype(np.float32)
    keep = np.zeros((128, 16, 32)); addc = np.zeros((128, 16, 32))
    for qb in range(16):
        cur = (2 * qb + (np.arange(128) >= 64))[:, None]
        blk = np.arange(32)[None, :]
        forced = (blk == 0) | (blk == cur) | (blk == cur - 1)
        fut = blk > cur
        keep[:, qb, :] = (~forced & ~fut)
        addc[:, qb, :] = np.where(fut, -1e30, np.where(forced, 1e9, 0.0))
    E = np.zeros((128, 2048))
    E[:32] = (np.arange(2048)[None, :] // 64) == np.arange(32)[:, None]
    parts = dict(msk=msk, mtri=mtri, ov=ov, keep=keep.reshape(128, -1), addc=addc.reshape(128, -1), E=E)
    cols = {}
    o = 0
    arrs = []
    for n, a in parts.items():
        cols[n] = (o, a.shape[1]); o += a.shape[1]; arrs.append(a.astype(np.float32))
    return np.concatenate(arrs, axis=1), cols


NSA_CST_NP, NSA_CST_COLS = nsa_host_consts()


def stage_nsa(P, C, projT, projN, ymix, prm, l):
    G, R = 2, 4
    with P.scope():
        cf = [C.f]
        tmp = ng_tmp(P)
        one = tmp["one"]
        Bn0 = P.sbuf("nsa_Bn0", [128, 8, 128], BF16)
        Bn1 = P.sbuf("nsa_Bn1", [128, 8, 128], BF16)
        Mtri = P.sbuf("nsa_Mtri", [128, 4, 128], BF16)
        FT = P.sbuf("nsa_FT", [128, 8, 248])
        ovb = P.sbuf("nsa_ovb", [128, 32], BF16)
        keep = P.sbuf("nsa_keep", [128, 16, 32])
        addc = P.sbuf("nsa_addc", [128, 16, 32])
        Eb = P.sbuf("nsa_Eb", [32, 2048], BF16)
        ncst = prm["nsa_cst"]
        cc = NSA_CST_COLS
        P.dma("sp", keep[:].rearrange("p a b -> p (a b)"), ncst.t[:, cc["keep"][0]:cc["keep"][0] + 512], reads=[ncst], writes=[keep])
        P.dma("sp", addc[:].rearrange("p a b -> p (a b)"), ncst.t[:, cc["addc"][0]:cc["addc"][0] + 512], reads=[ncst], writes=[addc])
        with P.scope():
            tb = P.sbuf("nsa_tb", [128, 4032])
            mk = P.sbuf("nsa_mk", [128, 4032])
            t31 = P.sbuf("nsa_t31", [128, 8])
            st = P.sbuf("nsa_st", [128, 2048])
            P.dma("sp", tb[:], prm["nsa_tab"].t[:, :], reads=[prm["nsa_tab"]], writes=[tb])
            P.dma("sp", mk[:], ncst.t[:, cc["msk"][0]:cc["msk"][0] + 4032], reads=[ncst], writes=[mk])
            P.dma("sp", t31[:], prm["nsa_t31"].t[:, :], reads=[prm["nsa_t31"]], writes=[t31])
            for (o, w, dst) in ((0, 128, Bn0), (1024, 128, Bn1), (2048, 248, FT)):
                v = tb[:, o:o + 8 * w].rearrange("p (h x) -> p h x", h=8)
                P.dve(lambda e, v=v, w=w: e.tensor_tensor(out=v, in0=v, in1=bc(t31[:].unsqueeze(2), [128, 8, w]), op=ALU.subtract), reads=[tb, t31], writes=[tb])
                P.dve(lambda e, v=v, w=w, o=o, dst=dst: e.tensor_tensor(out=dst[:], in0=v, in1=mk[:, o:o + 8 * w].rearrange("p (h x) -> p h x", h=8), op=ALU.add),
                      reads=[tb, mk], writes=[dst])
            P.dma("sp", st[:, 0:128], ncst.t[:, cc["mtri"][0]:cc["mtri"][0] + 128], reads=[ncst], writes=[st])
            P.dve(lambda e: e.tensor_copy(out=Mtri[:], in_=bc(st[:, 0:128].unsqueeze(1), [128, 4, 128])), reads=[st], writes=[Mtri])
            P.dma("sp", st[:, 128:160], ncst.t[:, cc["ov"][0]:cc["ov"][0] + 32], reads=[ncst], writes=[st])
            P.dve(lambda e: e.tensor_copy(out=ovb[:], in_=st[:, 128:160]), reads=[st], writes=[ovb])
            P.dma("sp", st[0:32, :], ncst.t[0:32, cc["E"][0]:cc["E"][0] + 2048], reads=[ncst], writes=[st])
            P.dve(lambda e: e.tensor_copy(out=Eb[:], in_=st[0:32, :]), reads=[st], writes=[Eb])
        dbg(0.1)
        qTb = P.sbuf("nsa_qTb", [64, 8, SEQ], BF16)
        ksT = P.sbuf("nsa_ksT", [64, 2, SEQ], BF16)
        kwT = P.sbuf("nsa_kwT", [64, 2, SEQ], BF16)
        vsb = P.sbuf("nsa_vsb", [128, 16, 2, 65], BF16)
        vwb = P.sbuf("nsa_vwb", [128, 16, 2, 65], BF16)
        gts = P.sbuf("nsa_gts", [128, 16, 24])
        kcT = P.sbuf("nsa_kcT", [64, 2, 128], BF16)
        vcx = P.sbuf("nsa_vcx", [128, 2, 96], BF16)
        with P.scope():
            stg = [P.sbuf(f"nsa_stg{i}", [64, SEQ]) for i in range(2)]
            n = 0
            for h in range(8):
                s_ = stg[n % 2]; n += 1
                P.dma("sp", s_[:], projT.t[PT_NQ + h * 64:PT_NQ + (h + 1) * 64, :], reads=[projT], writes=[s_])
                P.act(lambda e, h=h, s_=s_: e.activation(out=qTb[:, h, :], in_=s_[:], func=AF.Copy, scale=0.125), reads=[s_], writes=[qTb])
            for (r0, dst) in ((PT_NKS, ksT), (PT_NKW, kwT)):
                for g in range(2):
                    s_ = stg[n % 2]; n += 1
                    P.dma("sp", s_[:], projT.t[r0 + g * 64:r0 + (g + 1) * 64, :], reads=[projT], writes=[s_])
                    P.dve(lambda e, g=g, s_=s_, dst=dst: e.tensor_copy(out=dst[:, g, :], in_=s_[:]), reads=[s_], writes=[dst])
            tn = P.sbuf("nsa_tn", [128, 16, 280])
            P.dma("sp", tn[:], projN.t[:, 0:280].rearrange("(t p) c -> p t c", p=128), reads=[projN], writes=[tn])
            P.pool(lambda e: e.memset(vsb[:], 1.0), writes=[vsb])
            P.pool(lambda e: e.memset(vwb[:], 1.0), writes=[vwb])
            P.dve(lambda e: e.tensor_copy(out=vsb[:, :, :, 0:64], in_=tn[:, :, 0:128].rearrange("p t (g d) -> p t g d", g=2)), reads=[tn], writes=[vsb])
            P.dve(lambda e: e.tensor_copy(out=vwb[:, :, :, 0:64], in_=tn[:, :, 128:256].rearrange("p t (g d) -> p t g d", g=2)), reads=[tn], writes=[vwb])
            P.act(lambda e: e.activation(out=gts[:], in_=tn[:, :, 256:280], func=AF.Sigmoid), reads=[tn], writes=[gts])
            dbg(0.2)
            tT = P.sbuf("nsa_tT", [64, 2, SEQ])
            tG = P.sbuf("nsa_tG", [64, 2, 16, 129], BF16)
            P.pool(lambda e: e.memset(tG[:], 0.0), writes=[tG])
            w1f = P.sbuf("nsa_w1f", [64, 32, 64])
            w1b = P.sbuf("nsa_w1b", [64, 32, 64], BF16)
            w2f = P.sbuf("nsa_w2f", [64, 64])
            w2b = P.sbuf("nsa_w2b", [64, 64], BF16)
            posT = P.sbuf("nsa_posT", [64, 32])
            posb = P.sbuf("nsa_posb", [64, 32], BF16)
            cvec = P.sbuf("nsa_cvec", [64, 1])
            hid = P.sbuf("nsa_hid", [64, 2, 128], BF16)
            ps_h = P.psum("nsa_ps_h", [64, 512])
            ps_c = P.psum("nsa_ps_c", [64, 8])
            ps_o = P.psum("nsa_ps_o", [128, 512])
            P.dve(lambda e: e.memset(hid[:], 0.0), writes=[hid])
            for kv in range(2):
                r0 = PT_NKC if kv == 0 else PT_NVC
                for g in range(2):
                    P.dma("sp", tT[:, g, :], projT.t[r0 + g * 64:r0 + (g + 1) * 64, :], reads=[projT], writes=[tT])
                    P.dve(lambda e, g=g: e.tensor_copy(out=tG[:, g, :, 0:128], in_=tT[:, g, :].rearrange("p (n s) -> p s n", s=16)), reads=[tT], writes=[tG])
                w1src = prm["nsa_cmp_w1"].t[l, kv].rearrange("(j d) o -> d j o", d=64)
                P.dma("sp", w1f[:], w1src, reads=[prm["nsa_cmp_w1"]], writes=[w1f])
                P.pool(lambda e: e.tensor_copy(out=w1b[:], in_=w1f[:]), reads=[w1f], writes=[w1b])
                P.dma("sp", w2f[:], prm["nsa_cmp_w2"].t[l, kv], reads=[prm["nsa_cmp_w2"]], writes=[w2f])
                P.dve(lambda e: e.tensor_copy(out=w2b[:], in_=w2f[:]), reads=[w2f], writes=[w2b])
                P.dma("sp", posT[:], prm["nsa_cmp_pos"].t[l, kv], reads=[prm["nsa_cmp_pos"]], writes=[posT])
                P.dve(lambda e: e.tensor_copy(out=posb[:], in_=posT[:]), reads=[posT], writes=[posb])
                for j in range(32):
                    P.pe(lambda e, j=j: e.matmul(ps_c[:, 0:1], lhsT=w1b[:, j, :], rhs=posb[:, j:j + 1], start=(j == 0), stop=(j == 31)), reads=[w1b, posb], writes=[ps_c])
                P.dve(lambda e: e.tensor_copy(out=cvec[:], in_=ps_c[:, 0:1]), reads=[ps_c], writes=[cvec])
                dbg(0.3 + 0.3 * kv)
                for g in range(2):
                    rows = slice(g * 64, (g + 1) * 64)
                    dbg(0.32 + 0.03 * g + 0.3 * kv)
                    for j in range(32):
                        P.pe(lambda e, j=j, g=g, rows=rows: e.matmul(ps_h[:, g * 128:(g + 1) * 128], lhsT=w1b[:, j, :], rhs=tG[:, g, j % 16, (j // 16):(j // 16) + 128],
                                                                    start=(j == 0), stop=(j == 31)), reads=[w1b, tG], writes=[ps_h])
                for g in range(2):
                    P.act(lambda e, g=g: e.activation(out=hid[:, g, :], in_=ps_h[:, g * 128:(g + 1) * 128], func=AF.Silu, bias=cvec[:, 0:1]),
                          reads=[ps_h, cvec], writes=[hid])
                dbg(0.4 + 0.3 * kv)
                if kv == 0:
                    P.pe(lambda e: e.matmul(ps_h[:, 256:512], lhsT=w2b[:], rhs=hid[:].rearrange("o g n -> o (g n)"), start=True, stop=True), reads=[w2b, hid], writes=[ps_h])
                    P.dve(lambda e: e.tensor_copy(out=kcT[:].rearrange("o g n -> o (g n)"), in_=ps_h[:, 256:512]), reads=[ps_h], writes=[kcT])
                else:
                    for g in range(2):
                        P.pe(lambda e, g=g: e.matmul(ps_o[:, g * 64:(g + 1) * 64], lhsT=hid[:, g, :], rhs=w2b[:], start=True, stop=True), reads=[w2b, hid], writes=[ps_o])
                    P.dve(lambda e: e.tensor_copy(out=vcx[:, :, 0:64], in_=ps_o[:, 0:128].rearrange("p (g d) -> p g d", g=2)), reads=[ps_o], writes=[vcx])
                    P.dve(lambda e: e.tensor_copy(out=vcx[:, :, 64:96], in_=bc(ovb[:].unsqueeze(1), [128, 2, 32])), reads=[ovb], writes=[vcx])
        dbg(1)
        ps_sc = [P.psum(f"nsa_ps_sc{i}", [128, 512]) for i in range(2)]
        ps_os = P.psum("nsa_ps_os", [128, 512])
        ps_ow = P.psum("nsa_ps_ow", [128, 512])
        ps_ocs = [P.psum(f"nsa_ps_oc{i}", [128, 512]) for i in range(2)]
        ps_cs = P.psum("nsa_ps_cs", [128, 512])
        ps_tr = P.psum("nsa_ps_tr", [128, 1024], BF16)
        sc = P.sbuf("nsa_sc", [128, 4, 128])
        ssum = P.sbuf("nsa_ssum", [128, 4])
        pnb = P.sbuf("nsa_pnb", [128, 4, 128], BF16)
        pT = P.sbuf("nsa_pT", [128, 4, 128], BF16)
        imp = P.sbuf("nsa_imp", [128, 32])
        mx8 = P.sbuf("nsa_mx8", [128, 8])
        negm = P.sbuf("nsa_negm", [128, 32], BF16)
        negTs = [P.sbuf(f"nsa_negT{i}", [32, 4, 128], BF16) for i in range(2)]
        eT = [P.sbuf(f"nsa_eT{i}", [128, 4, 128], BF16) for i in range(2)]
        cs = P.sbuf("nsa_cs", [128, 3, 4])
        ya = P.sbuf("nsa_ya", [128, 4, 64])
        yb = P.sbuf("nsa_yb", [128, 4, 64])
        yt = [P.sbuf(f"nsa_yt{i}", [128, 512]) for i in range(2)]
        nsc = 0
        bg_begin(P)

        def v3(ap, r=4):
            return ap.rearrange("p (r q) -> p r q", r=r)

        def phaseA(qb, g, slot):
            qtok = slice(qb * 128, (qb + 1) * 128)
            hs = slice(4 * g, 4 * g + 4)
            ps_oc_s = ps_ocs[slot]
            negT_s = negTs[slot]
            for r in range(R):
                P.pe(lambda e, r=r: e.matmul(ps_cs[:, r * 128:(r + 1) * 128], lhsT=qTb[:, 4 * g + r, qtok], rhs=kcT[:, g, :], start=True, stop=True),
                     reads=[qTb, kcT], writes=[ps_cs])
            m0 = 120 - 8 * qb
            P.dve(lambda e: e.tensor_tensor(out=sc[:], in0=v3(ps_cs[:]), in1=FT[:, hs, m0:m0 + 128], op=ALU.add), reads=[ps_cs, FT], writes=[sc])
            P.act(lambda e: e.activation(out=sc[:], in_=sc[:], func=AF.Exp), reads=[sc], writes=[sc])
            P.dve(lambda e: e.tensor_reduce(out=ssum[:], in_=sc[:], axis=AX.X, op=ALU.add), reads=[sc], writes=[ssum])
            P.dve(lambda e: e.tensor_scalar(out=ssum[:], in0=ssum[:], scalar1=1e-30, scalar2=None, op0=ALU.max), reads=[ssum], writes=[ssum])
            P.dve(lambda e: e.reciprocal(out=ssum[:], in_=ssum[:]), reads=[ssum], writes=[ssum])
            P.dve(lambda e: e.tensor_tensor(out=pnb[:], in0=sc[:], in1=bc(ssum[:].unsqueeze(2), [128, 4, 128]), op=ALU.mult), reads=[sc, ssum], writes=[pnb])
            yield
            for r in range(R):
                P.pe(lambda e, r=r: e.transpose(out=ps_tr[:, r * 128:(r + 1) * 128], in_=pnb[:, r, :], identity=C.identb[:]), reads=[pnb, C.identb], writes=[ps_tr])
            P.act(lambda e: e.copy(out=pT[:].rearrange("p r q -> p (r q)"), in_=ps_tr[:, 0:512]), reads=[ps_tr], writes=[pT])
            yield
            for r in range(R):
                P.pe(lambda e, r=r: e.matmul(ps_oc_s[:, r * 96:(r + 1) * 96], lhsT=pT[:, r, :], rhs=vcx[:, g, :], start=True, stop=True), reads=[pT, vcx], writes=[ps_oc_s])
            oc4 = ps_oc_s[:, 0:384].rearrange("p (r x) -> p r x", r=4)
            P.dve(lambda e: e.tensor_reduce(out=imp[:], in_=oc4[:, :, 64:96].rearrange("p r j -> p j r"), axis=AX.X, op=ALU.add), reads=[ps_oc_s], writes=[imp])
            P.dve(lambda e: e.tensor_tensor(out=imp[:], in0=imp[:], in1=keep[:, qb, :], op=ALU.mult), reads=[imp, keep], writes=[imp])
            P.dve(lambda e: e.tensor_tensor(out=imp[:], in0=imp[:], in1=addc[:, qb, :], op=ALU.add), reads=[imp, addc], writes=[imp])
            P.dve(lambda e: e.max(out=mx8[:], in_=imp[:]), reads=[imp], writes=[mx8])
            P.dve(lambda e: e.tensor_scalar(out=imp[:], in0=imp[:], scalar1=mx8[:, 7:8], scalar2=None, op0=ALU.is_ge), reads=[imp, mx8], writes=[imp])
            P.dve(lambda e: e.tensor_scalar(out=negm[:], in0=imp[:], scalar1=-1.0, scalar2=NBIG, op0=ALU.add, op1=ALU.mult), reads=[imp], writes=[negm])
            yield
            P.pe(lambda e: e.transpose(out=ps_tr[0:32, 512:640], in_=negm[:], identity=C.identb[:]), reads=[negm, C.identb], writes=[ps_tr])
            P.dve(lambda e: e.tensor_copy(out=negT_s[:], in_=bc(ps_tr[0:32, 512:640].unsqueeze(1), [32, 4, 128])), reads=[ps_tr], writes=[negT_s])

        def phaseB(qb, g, slot, gen):
            nonlocal nsc
            it = 0
            qtok = slice(qb * 128, (qb + 1) * 128)
            hs = slice(4 * g, 4 * g + 4)
            y = yt[qb % 2]
            ps_oc_s = ps_ocs[slot]
            negT_s = negTs[slot]
            oc4 = ps_oc_s[:, 0:384].rearrange("p (r x) -> p r x", r=4)
            for kt in range(qb + 1):
                ps = ps_sc[nsc % 2]; et = eT[nsc % 2]; nsc += 1
                ktok = slice(kt * 128, (kt + 1) * 128)
                near = kt >= qb - 1
                P.pe(lambda e, ps=ps, ktok=ktok: e.matmul(v3(ps[:]), lhsT=ksT[:, g, ktok], rhs=qTb[:, hs, qtok], start=True, stop=False), reads=[ksT, qTb], writes=[ps])
                P.pe(lambda e, ps=ps, ktok=ktok, near=near: e.matmul(v3(ps[:]), lhsT=Eb[:, ktok], rhs=negT_s[:], start=False, stop=not near), reads=[Eb, negT_s], writes=[ps])
                if near:
                    Bn = Bn0 if kt == qb else Bn1
                    P.pe(lambda e, ps=ps, Bn=Bn: e.matmul(v3(ps[:]), lhsT=C.identb[:], rhs=Bn[:, hs, :], start=False, stop=True), reads=[Bn, C.identb], writes=[ps])
                P.act(lambda e, ps=ps, et=et: e.activation(out=et[:].rearrange("p r q -> p (r q)"), in_=ps[:], func=AF.Exp), reads=[ps], writes=[et])
                for r in range(R):
                    P.pe(lambda e, r=r, et=et, kt=kt: e.matmul(ps_os[:, r * 65:(r + 1) * 65], lhsT=et[:, r, :], rhs=vsb[:, kt, g, :], start=(kt == 0 and r == 0), stop=(kt == qb), skip_group_check=True),
                         reads=[et, vsb], writes=[ps_os])
                it += 1
                if it % 3 == 2 and gen is not None:
                    next(gen, None)
            kt0 = max(0, qb - 4)
            for kt in range(kt0, qb + 1):
                ps = ps_sc[nsc % 2]; et = eT[nsc % 2]; nsc += 1
                ktok = slice(kt * 128, (kt + 1) * 128)
                dl = qb - kt
                extra = {0: Bn0[:, hs, :], 1: Bn1[:, hs, :], 4: Mtri[:]}.get(dl)
                P.pe(lambda e, ps=ps, ktok=ktok, extra=extra: e.matmul(v3(ps[:]), lhsT=kwT[:, g, ktok], rhs=qTb[:, hs, qtok], start=True, stop=extra is None),
                     reads=[kwT, qTb], writes=[ps])
                if extra is not None:
                    P.pe(lambda e, ps=ps, extra=extra: e.matmul(v3(ps[:]), lhsT=C.identb[:], rhs=extra, start=False, stop=True), reads=[Bn0, Bn1, Mtri, C.identb], writes=[ps])
                P.act(lambda e, ps=ps, et=et: e.activation(out=et[:].rearrange("p r q -> p (r q)"), in_=ps[:], func=AF.Exp), reads=[ps], writes=[et])
                for r in range(R):
                    P.pe(lambda e, r=r, et=et, kt=kt: e.matmul(ps_ow[:, r * 65:(r + 1) * 65], lhsT=et[:, r, :], rhs=vwb[:, kt, g, :], start=(kt == kt0 and r == 0), stop=(kt == qb), skip_group_check=True),
                         reads=[et, vwb], writes=[ps_ow])
                it += 1
                if it % 3 == 2 and gen is not None:
                    next(gen, None)
            if gen is not None:
                for _ in gen:
                    pass
            os4 = ps_os[:, 0:260].rearrange("p (r x) -> p r x", r=4)
            ow4 = ps_ow[:, 0:260].rearrange("p (r x) -> p r x", r=4)
            g3 = gts[:, qb, 12 * g:12 * g + 12].rearrange("p (r b) -> p b r", b=3)
            P.dve(lambda e: e.reciprocal(out=cs[:, 1, :], in_=os4[:, :, 64]), reads=[ps_os], writes=[cs])
            P.dve(lambda e: e.reciprocal(out=cs[:, 2, :], in_=ow4[:, :, 64]), reads=[ps_ow], writes=[cs])
            P.dve(lambda e: e.memset(cs[:, 0, :], 1.0), writes=[cs])
            P.dve(lambda e: e.tensor_tensor(out=cs[:], in0=cs[:], in1=g3, op=ALU.mult), reads=[cs, gts], writes=[cs])
            P.dve(lambda e: e.tensor_tensor(out=ya[:], in0=oc4[:, :, 0:64], in1=bc(cs[:, 0, :].unsqueeze(2), [128, 4, 64]), op=ALU.mult), reads=[ps_oc_s, cs], writes=[ya])
            P.dve(lambda e: e.tensor_tensor(out=yb[:], in0=os4[:, :, 0:64], in1=bc(cs[:, 1, :].unsqueeze(2), [128, 4, 64]), op=ALU.mult), reads=[ps_os, cs], writes=[yb])
            P.pool(lambda e: e.tensor_tensor(out=ya[:], in0=ya[:], in1=yb[:], op=ALU.add), reads=[ya, yb], writes=[ya])
            P.dve(lambda e: e.tensor_tensor(out=yb[:], in0=ow4[:, :, 0:64], in1=bc(cs[:, 2, :].unsqueeze(2), [128, 4, 64]), op=ALU.mult), reads=[ps_ow, cs], writes=[yb])
            P.pool(lambda e: e.tensor_tensor(out=y[:, g * 256:(g + 1) * 256].rearrange("p (r d) -> p r d", r=4), in0=ya[:], in1=yb[:], op=ALU.add), reads=[ya, yb], writes=[y])

        blocks = [(qb, g) for qb in range(NT) for g in range(G)]
        for _ in phaseA(blocks[0][0], blocks[0][1], 0):
            pass
        for bi, (qb, g) in enumerate(blocks):
            gen = phaseA(blocks[bi + 1][0], blocks[bi + 1][1], (bi + 1) % 2) if bi + 1 < len(blocks) else None
            if gen is not None:
                next(gen, None)
            phaseB(qb, g, bi % 2, gen)
            if g == G - 1:
                qtok = slice(qb * 128, (qb + 1) * 128)
                y = yt[qb % 2]
                P.dma("sp", ymix.t[qtok, 0:512], y[:], reads=[y], writes=[ymix])
                bg_tick(P, 4)
                dbg(2 + qb)
        bg_end(P)


WIN_OFF = {}
_o = 0
for (_c0, _n, _r0) in PT_GROUPS:
    WIN_OFF[("T", _c0)] = _o; _o += 16 * _n
for (_c0, _n, _r0) in PN_GROUPS:
    WIN_OFF[("N", _c0)] = _o; _o += 16 * _n
WIN_TOTAL = _o
WOUT_TOTAL = 4 * 16 * 512
W1_TOTAL = 32 * 16 * 256
W2_TOTAL = 4 * 64 * 512


class Background:
    def __init__(self, P, prm, l, wsc):
        self.P = P
        self.jobs = []
        self.loaded = self.cast = self.stored = 0
        self.bufs = None

        def add(src3, srcbuf, nk, nc_, dst, off):
            if nk * nc_ > 4096:
                h = nk // 2
                add(src3[:, 0:h, :], srcbuf, h, nc_, dst, off)
                add(src3[:, h:nk, :], srcbuf, nk - h, nc_, dst, off + h * nc_)
            else:
                self.jobs.append((src3, srcbuf, nk, nc_, dst, off))

        wv = prm["w_in"].t[l].rearrange("(k p) n -> p k n", p=128)
        for (c0, nc_, r0) in PT_GROUPS:
            add(wv[:, :, c0:c0 + nc_], prm["w_in"], 16, nc_, wsc["win"], WIN_OFF[("T", c0)])
        for (c0, nc_, o0) in PN_GROUPS:
            add(wv[:, :, c0:c0 + nc_], prm["w_in"], 16, nc_, wsc["win"], WIN_OFF[("N", c0)])
        wv = prm["w_out"].t[l].rearrange("(k p) n -> p k n", p=128)
        for ct in range(4):
            add(wv[:, :, ct * 512:(ct + 1) * 512], prm["w_out"], 16, 512, wsc["wout"], ct * 8192)
        wv = prm["mlp_w1"].t[l].rearrange("(k p) n -> p k n", p=128)
        for hp in range(32):
            add(wv[:, :, hp * 256:(hp + 1) * 256], prm["mlp_w1"], 16, 256, wsc["w1"], hp * 4096)
        wv = prm["mlp_w2"].t[l].rearrange("(k p) n -> p k n", p=128)
        for ct in range(4):
            for kg in range(8):
                add(wv[:, kg * 8:(kg + 1) * 8, ct * 512:(ct + 1) * 512], prm["mlp_w2"], 8, 512, wsc["w2"], (ct * 8 + kg) * 4096)

    def alloc(self):
        P = self.P
        self.bufs = dict(f=[P.sbuf(f"bg_f{i}", [128, 4096]) for i in range(2)], b=[P.sbuf(f"bg_b{i}", [128, 4096], BF16) for i in range(2)])

    def done(self):
        return self.stored >= len(self.jobs)

    def step(self, load=True):
        if self.bufs is None:
            return
        P = self.P
        f, b_ = self.bufs["f"], self.bufs["b"]
        if self.stored < self.cast:
            j = self.stored
            src3, srcbuf, nk, nc_, dst, off = self.jobs[j]
            tot = nk * nc_
            P.dma("sp", dst.t[:, off:off + tot], b_[j % 2][:, 0:tot], reads=[b_[j % 2]], writes=[dst])
            self.stored += 1
        if self.cast < self.loaded:
            j = self.cast
            tot = self.jobs[j][2] * self.jobs[j][3]
            P.pool(lambda e: e.tensor_copy(out=b_[j % 2][:, 0:tot], in_=f[j % 2][:, 0:tot]), reads=[f[j % 2]], writes=[b_[j % 2]])
            self.cast += 1
        if load and self.loaded < len(self.jobs):
            j = self.loaded
            src3, srcbuf, nk, nc_, dst, off = self.jobs[j]
            P.dma("sp", f[j % 2][:, 0:nk * nc_].rearrange("p (k c) -> p k c", k=nk), src3, reads=[srcbuf], writes=[f[j % 2]])
            self.loaded += 1

    def flush(self):
        while self.stored < self.loaded:
            self.step(load=False)

    def release(self):
        self.flush()
        self.bufs = None


def bg_begin(P):
    if getattr(P, "bg", None) is not None and not P.bg.done():
        P.bg.alloc()


def bg_tick(P, n=1):
    if getattr(P, "bg", None) is not None:
        for _ in range(n):
            P.bg.step()


def bg_end(P):
    if getattr(P, "bg", None) is not None and P.bg.bufs is not None:
        P.bg.release()


def stage_convert_all(P, bg):
    if bg is None or bg.done():
        return
    with P.scope():
        bg.alloc()
        while bg.loaded < len(bg.jobs):
            bg.step()
        bg.release()


def stage_mod(P, C, cT, ada_w, ada_b, modT, gsc, nlayers):
    with P.scope():
        cf = [C.f]
        ca = P.sbuf("mod_ca", [128, 16, BPC])
        P.dma("sp", ca[:], cT.t[:, :, :], reads=[cT], writes=[ca])
        P.act(lambda e: e.activation(out=ca[:], in_=ca[:], func=AF.Silu), reads=[ca], writes=[ca])
        wst = [P.sbuf(f"mod_w{i}", [128, 16, 512]) for i in range(2)]
        brow = [P.sbuf(f"mod_b{i}", [1, 512]) for i in range(2)]
        grow = [P.sbuf(f"mod_g{i}", [BPC, 512]) for i in range(2)]
        ps_f = P.psum("mod_psf", [128, 512])
        ps_g = [P.psum(f"mod_psg{i}", [BPC, 512]) for i in range(2)]
        n = 0
        for l in range(nlayers):
            wv = ada_w.t[l].rearrange("(k p) n -> p k n", p=128)
            for seg in range(6):
                for ct in range(4):
                    w = wst[n % 2]; br = brow[n % 2]
                    c0 = seg * 2048 + ct * 512
                    P.dma("sp", w[:], wv[:, :, c0:c0 + 512], reads=[ada_w], writes=[w])
                    P.dma("sp", br[:], ada_b.t[l:l + 1, c0:c0 + 512], reads=[ada_b], writes=[br])
                    if seg in (2, 5):
                        pg = ps_g[n % 2]; gr = grow[n % 2]
                        for k in range(16):
                            P.pe(lambda e, k=k, w=w, pg=pg: e.matmul(pg[:], lhsT=ca[:, k, :], rhs=w[:, k, :], start=(k == 0), stop=False), reads=[ca, w], writes=[pg])
                        P.pe(lambda e, br=br, pg=pg: e.matmul(pg[:], lhsT=C.c("ones", 1)[:, 0:BPC], rhs=br[:], start=False, stop=True), reads=[br] + cf, writes=[pg])
                        P.act(lambda e, pg=pg, gr=gr: e.copy(out=gr[:], in_=pg[:]), reads=[pg], writes=[gr])
                        P.dma("sp", gsc.t[l, 0 if seg == 2 else 1, :, ct * 512:(ct + 1) * 512], gr[:], reads=[gr], writes=[gsc])
                    else:
                        si = {0: 0, 1: 1, 3: 2, 4: 3}[seg]
                        for cc in range(4):
                            col = ((si * 16) + ct * 4 + cc) * BPC
                            for k in range(16):
                                P.pe(lambda e, k=k, w=w, cc=cc, col=col: e.matmul(ps_f[:, col:col + BPC], lhsT=w[:, k, cc * 128:(cc + 1) * 128], rhs=ca[:, k, :],
                                                                                 start=(k == 0), stop=False), reads=[ca, w], writes=[ps_f])
                            P.pe(lambda e, br=br, cc=cc, col=col: e.matmul(ps_f[:, col:col + BPC], lhsT=br[:, cc * 128:(cc + 1) * 128], rhs=C.c("ones", 1)[:, 0:BPC],
                                                                          start=False, stop=True), reads=[br] + cf, writes=[ps_f])
                    n += 1
            P.dve(lambda e, l=l: e.tensor_copy(out=modT[:, l].rearrange("p s k b -> p (s k b)"), in_=ps_f[:, 0:4 * 16 * BPC]), reads=[ps_f], writes=[modT])


def to_featmajor(P, C, src, src_ap_fn, ntt, hT, norm, scl=None, shf=None, pools=None):
    xt, xb, ss, ps_tr, eps = pools["xt"], pools["xb"], pools["ss"], pools["ps_tr"], pools["eps"]
    for tt in range(ntt):
        x = xt[tt % 2]; xn = xb[tt % 2]; s1 = ss[tt % 2]
        P.dma("sp", x[:], src_ap_fn(tt), reads=[src], writes=[x])
        if norm:
            P.pool(lambda e, s1=s1: e.memset(s1[:], 0.0), writes=[s1])
            P.act(lambda e, x=x, xn=xn, s1=s1: e.activation(out=xn[:], in_=x[:], func=AF.Square, accum_out=s1[:, 0:1]), reads=[x, s1], writes=[xn, s1])
            P.act(lambda e, s1=s1: e.activation(out=s1[:, 0:1], in_=s1[:, 0:1], func=AF.Sqrt, scale=1.0 / D_MODEL, bias=eps[:, 0:1]), reads=[s1, eps], writes=[s1])
            P.dve(lambda e, s1=s1: e.reciprocal(out=s1[:, 0:1], in_=s1[:, 0:1]), reads=[s1], writes=[s1])
            P.dve(lambda e, x=x, xn=xn, s1=s1: e.tensor_scalar(out=xn[:], in0=x[:], scalar1=s1[:, 0:1], scalar2=None, op0=ALU.mult), reads=[x, s1], writes=[xn])
        else:
            P.pool(lambda e, x=x, xn=xn: e.tensor_copy(out=xn[:], in_=x[:]), reads=[x], writes=[xn])
        for half in range(2):
            pt = ps_tr[(2 * tt + half) % len(ps_tr)]
            for kk in range(8):
                k = half * 8 + kk
                P.pe(lambda e, k=k, kk=kk, xn=xn, pt=pt: e.transpose(out=pt[:, kk * 128:(kk + 1) * 128], in_=xn[:, k * 128:(k + 1) * 128], identity=C.identb[:]),
                     reads=[xn, C.identb], writes=[pt])
            dst = hT[:, half * 8:half * 8 + 8, tt * 128:(tt + 1) * 128]
            src3 = pt[:].rearrange("p (k t) -> p k t", k=8)
            if scl is None:
                P.act(lambda e, dst=dst, src3=src3: e.copy(out=dst, in_=src3), reads=[pt], writes=[hT])
            else:
                tm = pools["tm"][(2 * tt + half) % 2]
                P.dve(lambda e, src3=src3, tm=tm, half=half: e.tensor_tensor(out=tm[:], in0=src3, in1=bc(scl[:, half * 8:half * 8 + 8].unsqueeze(2), [128, 8, 128]), op=ALU.mult),
                      reads=[pt] + pools["affb"], writes=[tm])
                P.pool(lambda e, dst=dst, tm=tm, half=half: e.tensor_tensor(out=dst, in0=tm[:], in1=bc(shf[:, half * 8:half * 8 + 8].unsqueeze(2), [128, 8, 128]), op=ALU.add),
                       reads=[tm] + pools["affb"], writes=[hT])


def fm_pools(P, affine):
    d = dict(xt=[P.sbuf(f"fm_xt{i}", [128, D_MODEL]) for i in range(2)],
             xb=[P.sbuf(f"fm_xb{i}", [128, D_MODEL], BF16) for i in range(2)],
             ss=[P.sbuf(f"fm_ss{i}", [128, 1]) for i in range(2)],
             ps_tr=[P.psum(f"fm_pst{i}", [128, 1024], BF16) for i in range(2)],
             eps=P.sbuf("fm_eps", [128, 1]))
    P.pool(lambda e: e.memset(d["eps"][:], EPS), writes=[d["eps"]])
    if affine:
        d["tm"] = [P.sbuf(f"fm_tm{i}", [128, 8, 128]) for i in range(2)]
    return d


def affine_vecs(P, modT, l, b, which, nw, scl, shf):
    P.dve(lambda e: e.tensor_scalar(out=scl[:], in0=modT[:, l, 2 * which + 1, :, b], scalar1=1.0, scalar2=None, op0=ALU.add), reads=[modT], writes=[scl])
    P.dve(lambda e: e.tensor_tensor(out=scl[:], in0=scl[:], in1=nw, op=ALU.mult), reads=[scl], writes=[scl])
    P.dve(lambda e: e.tensor_copy(out=shf[:], in_=modT[:, l, 2 * which, :, b]), reads=[modT], writes=[shf])


def stage_inproj(P, C, xsrc, xsrc_fn, modT, nw1T, win, projT, projN, l, b):
    with P.scope():
        hT = P.sbuf("ip_hT", [128, 16, SEQ], BF16)
        scl = P.sbuf("ip_scl", [128, 16]); shf = P.sbuf("ip_shf", [128, 16])
        affine_vecs(P, modT, l, b, 0, nw1T[:, l, :], scl, shf)
        with P.scope():
            pools = fm_pools(P, True)
            pools["affb"] = [scl, shf]
            to_featmajor(P, C, xsrc, xsrc_fn, NT, hT, True, scl[:], shf[:], pools)
        NWB = 3
        wb = [P.sbuf(f"ip_wb{i}", [128, 16, 512], BF16) for i in range(NWB)]
        ev = [P.sbuf(f"ip_ev{i}", [128, 2048]) for i in range(2)]
        ps = [P.psum(f"ip_ps{i}", [128, 512]) for i in range(4)]
        groups = [("T",) + g for g in PT_GROUPS] + [("N",) + g for g in PN_GROUPS]

        def wload(i):
            kind, c0, nc_, _ = groups[i]
            off = WIN_OFF[(kind, c0)]
            wbb = wb[i % NWB]
            P.dma("sp", wbb[:, :, 0:nc_], win.t[:, off:off + 16 * nc_].rearrange("p (k c) -> p k c", k=16), reads=[win], writes=[wbb])

        npp = 0
        nev = 0
        wload(0)
        wload(1)
        for gi, (kind, c0, nc_, dst0) in enumerate(groups):
            if gi + 2 < len(groups):
                wload(gi + 2)
            wbb = wb[gi % NWB]
            if kind == "T":
                e_ = ev[nev % 2]; nev += 1
                for tq in range(4):
                    p_ = ps[npp % 4]; npp += 1
                    for k in range(16):
                        P.pe(lambda e, k=k, p_=p_, wbb=wbb, nc_=nc_, tq=tq: e.matmul(p_[0:nc_, :], lhsT=wbb[:, k, 0:nc_], rhs=hT[:, k, tq * 512:(tq + 1) * 512], start=(k == 0), stop=(k == 15)),
                             reads=[wbb, hT], writes=[p_])
                    if tq % 2 == 0:
                        P.act(lambda e, p_=p_, e_=e_, nc_=nc_, tq=tq: e.copy(out=e_[0:nc_, tq * 512:(tq + 1) * 512], in_=p_[0:nc_, :]), reads=[p_], writes=[e_])
                    else:
                        P.dve(lambda e, p_=p_, e_=e_, nc_=nc_, tq=tq: e.tensor_copy(out=e_[0:nc_, tq * 512:(tq + 1) * 512], in_=p_[0:nc_, :]), reads=[p_], writes=[e_])
                P.dma("sp", projT.t[dst0:dst0 + nc_, :], e_[0:nc_, :], reads=[e_], writes=[projT])
            else:
                for t4 in range(4):
                    e_ = ev[nev % 2]; nev += 1
                    for ti in range(4):
                        tt = t4 * 4 + ti
                        p_ = ps[npp % 4]; npp += 1
                        for k in range(16):
                            P.pe(lambda e, k=k, p_=p_, wbb=wbb, nc_=nc_, tt=tt: e.matmul(p_[:, 0:nc_], lhsT=hT[:, k, tt * 128:(tt + 1) * 128], rhs=wbb[:, k, 0:nc_], start=(k == 0), stop=(k == 15)),
                                 reads=[wbb, hT], writes=[p_])
                        if ti % 2 == 0:
                            P.act(lambda e, p_=p_, e_=e_, nc_=nc_, ti=ti: e.copy(out=e_[:, ti * 512:ti * 512 + nc_], in_=p_[:, 0:nc_]), reads=[p_], writes=[e_])
                        else:
                            P.dve(lambda e, p_=p_, e_=e_, nc_=nc_, ti=ti: e.tensor_copy(out=e_[:, ti * 512:ti * 512 + nc_], in_=p_[:, 0:nc_]), reads=[p_], writes=[e_])
                    P.dma("sp", projN.t[t4 * 512:(t4 + 1) * 512, dst0:dst0 + nc_].rearrange("(i p) c -> p i c", p=128),
                          e_[:].rearrange("p (i c) -> p i c", i=4)[:, :, 0:nc_], reads=[e_], writes=[projN])


def stage_outproj(P, C, ymix, xsrc, xsrc_fn, xdst, xdst_fn, gsc, wout, l, b):
    with P.scope():
        yT = P.sbuf("op_yT", [128, 16, SEQ], BF16)
        with P.scope():
            pools = fm_pools(P, False)
            to_featmajor(P, C, ymix, lambda tt: ymix.t[tt * 128:(tt + 1) * 128, :], NT, yT, False, None, None, pools)
        wob = P.sbuf("op_wob", [128, 16, D_MODEL], BF16)
        gb = P.sbuf("op_gb", [128, D_MODEL])
        P.dma("sp", gb[:], gsc.t[l, 0, b:b + 1, :].partition_broadcast(128), reads=[gsc], writes=[gb])
        for ct in range(4):
            P.dma("sp", wob[:, :, ct * 512:(ct + 1) * 512], wout.t[:, ct * 8192:(ct + 1) * 8192].rearrange("p (k c) -> p k c", k=16), reads=[wout], writes=[wob])
        xt = [P.sbuf(f"op_xt{i}", [128, D_MODEL]) for i in range(2)]
        xo = [P.sbuf(f"op_xo{i}", [128, D_MODEL]) for i in range(2)]
        ps = [P.psum(f"op_ps{i}", [128, 512]) for i in range(8)]
        for tt in range(NT):
            x = xt[tt % 2]; o = xo[tt % 2]
            P.dma("sp", x[:], xsrc_fn(tt), reads=[xsrc], writes=[x])
            for ct in range(4):
                p_ = ps[(tt * 4 + ct) % 8]
                for k in range(16):
                    P.pe(lambda e, k=k, p_=p_, ct=ct, tt=tt: e.matmul(p_[:], lhsT=yT[:, k, tt * 128:(tt + 1) * 128], rhs=wob[:, k, ct * 512:(ct + 1) * 512], start=(k == 0), stop=(k == 15)),
                         reads=[yT, wob], writes=[p_])
                cs = slice(ct * 512, (ct + 1) * 512)
                P.dve(lambda e, p_=p_, o=o, cs=cs: e.tensor_tensor(out=o[:, cs], in0=p_[:], in1=gb[:, cs], op=ALU.mult), reads=[p_, gb], writes=[o])
                P.pool(lambda e, o=o, x=x, cs=cs: e.tensor_tensor(out=o[:, cs], in0=o[:, cs], in1=x[:, cs], op=ALU.add), reads=[o, x], writes=[o])
            P.dma("sp", xdst_fn(tt), o[:], reads=[o], writes=[xdst])


def stage_mlp(P, C, xsrc, xsrc_fn, xdst, xdst_fn, modT, nw2T, gsc, w1s, w2s, l, b):
    HC = 64
    with P.scope():
        scl = P.sbuf("ml_scl", [128, 16]); shf = P.sbuf("ml_shf", [128, 16])
        affine_vecs(P, modT, l, b, 1, nw2T[:, l, :], scl, shf)
        gb = P.sbuf("ml_gb", [128, D_MODEL])
        P.dma("sp", gb[:], gsc.t[l, 1, b:b + 1, :].partition_broadcast(128), reads=[gsc], writes=[gb])
        hT = P.sbuf("ml_hT", [128, 16, 512], BF16)
        uT = P.sbuf("ml_uT", [128, HC, 512], BF16)
        pools = fm_pools(P, True)
        pools["affb"] = [scl, shf]
        NWB = 4
        wbuf = [P.sbuf(f"ml_wb{i}", [128, 4096], BF16) for i in range(NWB)]
        rl = [P.sbuf(f"ml_rl{i}", [128, 512]) for i in range(2)]
        xo = [P.sbuf(f"ml_xo{i}", [128, 1024]) for i in range(2)]
        ps = [P.psum(f"ml_ps{i}", [128, 512]) for i in range(6)]
        NTILE = 64
        total = (SEQ // 512) * NTILE
        state = {"n": 0}

        def wload(j):
            jj = j % NTILE
            wbb = wbuf[j % NWB]
            if jj < 32:
                P.dma("sp", wbb[:], w1s.t[:, jj * 4096:(jj + 1) * 4096], reads=[w1s], writes=[wbb])
            else:
                P.dma("sp", wbb[:], w2s.t[:, (jj - 32) * 4096:(jj - 31) * 4096], reads=[w2s], writes=[wbb])

        PRE = 3
        for j in range(PRE):
            wload(j)
        nps = 0
        j = 0
        for t5 in range(SEQ // 512):
            to_featmajor(P, C, xsrc, lambda tt, t5=t5: xsrc_fn(t5 * 4 + tt), 4, hT, True, scl[:], shf[:], pools)
            for hp in range(32):
                if j + PRE < total:
                    wload(j + PRE)
                wbb = wbuf[j % NWB]; j += 1
                w3 = wbb[:].rearrange("p (k c) -> p k c", k=16)
                for cc in range(2):
                    hc = hp * 2 + cc
                    p_ = ps[nps % 6]; nps += 1
                    r_ = rl[hc % 2]
                    for k in range(16):
                        P.pe(lambda e, k=k, p_=p_, w3=w3, cc=cc: e.matmul(p_[:], lhsT=w3[:, k, cc * 128:(cc + 1) * 128], rhs=hT[:, k, :], start=(k == 0), stop=(k == 15)),
                             reads=[wbb, hT], writes=[p_])
                    P.act(lambda e, p_=p_, r_=r_: e.activation(out=r_[:], in_=p_[:], func=AF.Relu), reads=[p_], writes=[r_])
                    P.dve(lambda e, r_=r_, hc=hc: e.tensor_tensor(out=uT[:, hc, :], in0=r_[:], in1=r_[:], op=ALU.mult), reads=[r_], writes=[uT])
            for ct in range(4):
                pa = [ps[(nps + i) % 6] for i in range(4)]
                nps += 4
                for kg in range(8):
                    if j + PRE < total:
                        wload(j + PRE)
                    wbb = wbuf[j % NWB]; j += 1
                    w3 = wbb[:].rearrange("p (k c) -> p k c", k=8)
                    for kk in range(8):
                        k = kg * 8 + kk
                        for ti in range(4):
                            P.pe(lambda e, k=k, kk=kk, ti=ti, w3=w3, pa=pa: e.matmul(pa[ti][:], lhsT=uT[:, k, ti * 128:(ti + 1) * 128], rhs=w3[:, kk, :], start=(k == 0), stop=(k == HC - 1)),
                                 reads=[uT, wbb], writes=[pa[ti]])
                for ti in range(4):
                    tt = t5 * 4 + ti
                    o = xo[(ct * 4 + ti) % 2]
                    P.dma("sp", o[:, 512:1024], xsrc_fn(tt)[:, ct * 512:(ct + 1) * 512], reads=[xsrc], writes=[o])
                    P.dve(lambda e, o=o, ti=ti, pa=pa, ct=ct: e.tensor_tensor(out=o[:, 0:512], in0=pa[ti][:], in1=gb[:, ct * 512:(ct + 1) * 512], op=ALU.mult),
                          reads=[pa[ti], gb], writes=[o])
                    P.pool(lambda e, o=o: e.tensor_tensor(out=o[:, 0:512], in0=o[:, 0:512], in1=o[:, 512:1024], op=ALU.add), reads=[o], writes=[o])
                    P.dma("sp", xdst_fn(tt)[:, ct * 512:(ct + 1) * 512], o[:, 0:512], reads=[o], writes=[xdst])


def stage_final(P, C, xsrc, xsrc_fn, fnw, out, out_fn, nseq):
    with P.scope():
        nwb = P.sbuf("fn_nwb", [128, D_MODEL])
        P.dma("sp", nwb[:], fnw.t[0:1, :].partition_broadcast(128), reads=[fnw], writes=[nwb])
        eps = P.sbuf("fn_eps", [128, 1])
        P.pool(lambda e: e.memset(eps[:], EPS), writes=[eps])
        xt = [P.sbuf(f"fn_xt{i}", [128, D_MODEL]) for i in range(2)]
        sq = [P.sbuf(f"fn_sq{i}", [128, D_MODEL]) for i in range(2)]
        ss = [P.sbuf(f"fn_ss{i}", [128, 1]) for i in range(2)]
        for i in range(nseq * NT):
            x = xt[i % 2]; q = sq[i % 2]; s1 = ss[i % 2]
            P.dma("sp", x[:], xsrc_fn(i), reads=[xsrc], writes=[x])
            P.pool(lambda e, s1=s1: e.memset(s1[:], 0.0), writes=[s1])
            P.act(lambda e, x=x, q=q, s1=s1: e.activation(out=q[:], in_=x[:], func=AF.Square, accum_out=s1[:, 0:1]), reads=[x, s1], writes=[q, s1])
            P.act(lambda e, s1=s1: e.activation(out=s1[:, 0:1], in_=s1[:, 0:1], func=AF.Sqrt, scale=1.0 / D_MODEL, bias=eps[:, 0:1]), reads=[s1, eps], writes=[s1])
            P.dve(lambda e, s1=s1: e.reciprocal(out=s1[:, 0:1], in_=s1[:, 0:1]), reads=[s1], writes=[s1])
            P.dve(lambda e, x=x, q=q, s1=s1: e.scalar_tensor_tensor(out=q[:], in0=x[:], scalar=s1[:, 0:1], in1=nwb[:], op0=ALU.mult, op1=ALU.mult), reads=[x, s1, nwb], writes=[q])
            P.dma("sp", out_fn(i), q[:], reads=[q], writes=[out])


SMALL_PARAMS = ["gla_gate_w2", "gla_gate_b", "gla_norm_w", "ssd_conv_w", "ssd_conv_b", "ssd_dt_bias", "ssd_a_log", "ssd_d", "ssd_norm_w",
                "gdn_conv_w", "gdn_dt_bias", "gdn_a_log", "gdn_norm_w", "nsa_cmp_pos", "nsa_cmp_w1", "nsa_cmp_w2"]
BIG_PARAMS = ["ada_w", "ada_b", "w_in", "w_out", "mlp_w1", "mlp_w2"]


def build(nlayers=DEPTH, nseq=BPC, shapes=None):
    nc = bass.Bass("TRN2", target_bir_lowering=False)
    st = ExitStack()
    with st:
        P = Prog(nc, st)

        def ext(name, shape):
            return Buf(nc.dram_tensor(name, list(shape), F32, kind="ExternalInput").ap(), name)

        x = ext("x", [nseq, SEQ, D_MODEL])
        cT = ext("cT", [128, 16, BPC])
        prm = {k: ext(k, shapes[k]) for k in SMALL_PARAMS + BIG_PARAMS + ["nw1T", "nw2T", "fnw", "nsa_tab", "nsa_t31", "nsa_cst", "cst"]}
        out = Buf(nc.dram_tensor("out", [nseq, SEQ, D_MODEL], F32, kind="ExternalOutput").ap(), "out")
        xres = P.dram("xres", [nseq, SEQ, D_MODEL])
        projT = P.dram("projT", [PT_ROWS, SEQ])
        projN = P.dram("projN", [SEQ, PN_COLS])
        ymix = P.dram("ymix", [SEQ, D_MODEL])
        gsc = P.dram("gsc", [nlayers, 2, BPC, D_MODEL])
        C = Consts(P, prm["cst"])
        modT = P.sbuf("modT", [128, nlayers, 4, 16, BPC])
        nw1T = P.sbuf("nw1T", [128, shapes["nw1T"][1], 16])
        nw2T = P.sbuf("nw2T", [128, shapes["nw2T"][1], 16])
        P.dma("sp", nw1T[:], prm["nw1T"].t[:, :, :], reads=[prm["nw1T"]], writes=[nw1T])
        P.dma("sp", nw2T[:], prm["nw2T"].t[:, :, :], reads=[prm["nw2T"]], writes=[nw2T])
        if "mod" not in DBG["skip"]:
            stage_mod(P, C, cT, prm["ada_w"], prm["ada_b"], modT, gsc, nlayers)
        wsc = dict(win=P.dram("wsc_win", [128, WIN_TOTAL], BF16), wout=P.dram("wsc_wout", [128, WOUT_TOTAL], BF16),
                   w1=P.dram("wsc_w1", [128, W1_TOTAL], BF16), w2=P.dram("wsc_w2", [128, W2_TOTAL], BF16))
        wscs = [wsc, dict(win=P.dram("wsc_win2", [128, WIN_TOTAL], BF16), wout=P.dram("wsc_wout2", [128, WOUT_TOTAL], BF16),
                          w1=P.dram("wsc_w12", [128, W1_TOTAL], BF16), w2=P.dram("wsc_w22", [128, W2_TOTAL], BF16))]
        P.bg = None
        stage_convert_all(P, Background(P, prm, 0, wscs[0]))
        for l in range(nlayers):
            wsc = wscs[l % 2]
            for b in range(nseq):
                if l == 0:
                    xs, xs_fn = x, (lambda tt, b=b: x.t[b, tt * 128:(tt + 1) * 128, :])
                else:
                    xs, xs_fn = xres, (lambda tt, b=b: xres.t[b, tt * 128:(tt + 1) * 128, :])
                xr_fn = (lambda tt, b=b: xres.t[b, tt * 128:(tt + 1) * 128, :])
                stage_inproj(P, C, xs, xs_fn, modT, nw1T, wsc["win"], projT, projN, l, b)
                if b == nseq - 1 and l + 1 < nlayers:
                    P.bg = Background(P, prm, l + 1, wscs[(l + 1) % 2])
                stage_nsa(P, C, projT, projN, ymix, prm, l)
                stage_ssd(P, C, projT, projN, ymix, prm, l)
                stage_gdn(P, C, projT, projN, ymix, prm, l)
                stage_gla(P, C, projT, projN, ymix, prm, l)
                if P.bg is not None:
                    stage_convert_all(P, P.bg)
                    P.bg = None
                stage_outproj(P, C, ymix, xs, xs_fn, xres, xr_fn, gsc, wsc["wout"], l, b)
                stage_mlp(P, C, xres, xr_fn, xres, xr_fn, modT, nw2T, gsc, wsc["w1"], wsc["w2"], l, b)
        stage_final(P, C, xres, lambda i: xres.t[i // NT, (i % NT) * 128:(i % NT + 1) * 128, :], prm["fnw"],
                    out, lambda i: out.t[i // NT, (i % NT) * 128:(i % NT + 1) * 128, :], nseq)
        P.finish()
        ninstr = P.ninstr
    return nc, ninstr


def host_inputs(inputs, nlayers=DEPTH):
    d = {}
    for k in SMALL_PARAMS:
        d[k] = host_param(k, inputs[k][:nlayers])
    for k in BIG_PARAMS:
        d[k] = np.ascontiguousarray(np.asarray(inputs[k][:nlayers], np.float32))
    d["nw1T"] = host_param("norm1_w", inputs["norm1_w"][:nlayers])
    d["nw2T"] = host_param("norm2_w", inputs["norm2_w"][:nlayers])
    d["fnw"] = host_param("final_norm_w", inputs["final_norm_w"])
    d["nsa_tab"], d["nsa_t31"] = nsa_host_tables(inputs["rel_bias"])
    d["nsa_cst"] = NSA_CST_NP
    d["cst"] = CST_NP
    return d


def core_inputs(inputs, shared, core, nseq=BPC):
    xs = np.ascontiguousarray(np.asarray(inputs["x"][core * BPC:core * BPC + nseq], np.float32))
    c = np.asarray(inputs["c"][core * BPC:(core + 1) * BPC], np.float32)
    cT = np.ascontiguousarray(c.T.reshape(16, 128, BPC).transpose(1, 0, 2))
    m = dict(shared)
    m["x"] = xs
    m["cT"] = cT
    return m


_CACHE = {}


def kernel(**inputs):
    shared = host_inputs(inputs)
    shapes = {k: v.shape for k, v in shared.items()}
    if "nc" not in _CACHE:
        _CACHE["nc"] = build(DEPTH, BPC, shapes)[0]
    nc = _CACHE["nc"]
    in_maps = [core_inputs(inputs, shared, c) for c in range(NCORES)]
    res = run_bass_kernel_spmd(nc, in_maps, core_ids=list(range(NCORES)))
    out = np.concatenate([r["out"] for r in res.results], axis=0)
    return out.astype(np.float32)
```

```python
import math
from contextlib import ExitStack, contextmanager
import numpy as np
import concourse.bass as bass
import concourse.mybir as mybir
from concourse.bass_utils import run_bass_kernel_spmd

F32 = mybir.dt.float32
BF16 = mybir.dt.bfloat16
AF = mybir.ActivationFunctionType
ALU = mybir.AluOpType
AX = mybir.AxisListType

EPOCH = 12000
SAME_ENGINE_SYNC = True

D_MODEL = 2048
SEQ = 2048
DEPTH = 4
NCORES = 8
BPC = 2
IN_COLS = 6456
EPS = 1e-6
NT = SEQ // 128


class StopStage(Exception):
    pass


DBG = {"stop": 99, "skip": set()}


def dbg(k):
    if DBG["stop"] <= k:
        DBG["P"].dead = True


class Buf:
    __slots__ = ("t", "name", "lw", "rd", "excl")

    def __init__(self, t, name="", excl=False):
        self.t = t
        self.name = name
        self.lw = None
        self.rd = {}
        self.excl = excl

    def __getitem__(self, k):
        return self.t[k]


class Prog:
    ENGS = ("pe", "act", "dve", "pool", "sp")

    def __init__(self, nc, stack):
        self.nc = nc
        self.stack = stack
        self.cnt = {e: 0 for e in ("pe", "act", "dve", "pool")}
        self.sems = {}
        self.seen = {e: {} for e in self.ENGS}
        self.dslots = {}
        self.dnext = {}
        self.E = dict(pe=nc.tensor, act=nc.scalar, dve=nc.vector, pool=nc.gpsimd, sp=nc.sync)
        self.ninstr = 0
        self.base_stack = stack
        self.uid = 0
        self.dead = False
        DBG["P"] = self

    def sem(self, name):
        return self.base_stack.enter_context(self.nc.semaphore(name))

    def sbuf(self, name, shape, dt=F32):
        self.uid += 1
        t = self.stack.enter_context(self.nc.sbuf_tensor(f"{name}_{self.uid}", list(shape), dt))
        return Buf(t, name)

    def psum(self, name, shape, dt=F32):
        self.uid += 1
        t = self.stack.enter_context(self.nc.psum_tensor(f"{name}_{self.uid}", list(shape), dt))
        return Buf(t, name, excl=True)

    def dram(self, name, shape, dt=F32, kind="Internal"):
        t = self.nc.dram_tensor(name, list(shape), dt, kind=kind)
        return Buf(t.ap(), name)

    def _esem(self, eng, idx):
        ep = idx // EPOCH
        k = (eng, ep)
        if k not in self.sems:
            self.sems[k] = self.sem(f"s_{eng}_{ep}")
        return self.sems[k], (idx % EPOCH) + 1

    def _wait(self, eng, ev):
        q, idx = ev
        if isinstance(q, str):
            if q == eng and (eng == "pe" or not SAME_ENGINE_SYNC):
                return
            if self.seen[eng].get(q, -1) >= idx:
                return
            self.seen[eng][q] = idx
            s, v = self._esem(q, idx)
        else:
            if self.seen[eng].get(q, -1) >= idx:
                return
            self.seen[eng][q] = idx
            s = self.dslots[q[0]][q[1]][0]
            v = idx
        self.E[eng].wait_ge(s, v)

    def _deps(self, eng, reads, writes):
        for b in reads:
            if b.lw is not None:
                self._wait(eng, b.lw)
            if b.excl:
                for q, i in list(b.rd.items()):
                    if q != eng:
                        self._wait(eng, (q, i))
        for b in writes:
            if b.lw is not None:
                self._wait(eng, b.lw)
            for q, i in list(b.rd.items()):
                self._wait(eng, (q, i))

    def _mark(self, ev, reads, writes):
        q, idx = ev
        for b in reads:
            if b.rd.get(q, -1) < idx:
                b.rd[q] = idx
        for b in writes:
            b.lw = ev
            b.rd = {}

    def op(self, eng, fn, reads=(), writes=()):
        if self.dead:
            return
        self._deps(eng, reads, writes)
        idx = self.cnt[eng]
        self.cnt[eng] += 1
        s, v = self._esem(eng, idx)
        fn(self.E[eng]).then_inc(s, 1)
        self._mark((eng, idx), reads, writes)
        self.ninstr += 1

    def pe(self, fn, reads=(), writes=()):
        self.op("pe", fn, reads, writes)

    def act(self, fn, reads=(), writes=()):
        self.op("act", fn, reads, writes)

    def dve(self, fn, reads=(), writes=()):
        self.op("dve", fn, reads, writes)

    def pool(self, fn, reads=(), writes=()):
        self.op("pool", fn, reads, writes)

    def dma(self, eng, out_ap, in_ap, reads=(), writes=(), nslots=8, **kw):
        if self.dead:
            return
        if eng not in self.dslots:
            self.dslots[eng] = [[self.sem(f"d_{eng}_{i}"), 0] for i in range(nslots)]
            self.dnext[eng] = 0
        si = self.dnext[eng]
        self.dnext[eng] = (si + 1) % len(self.dslots[eng])
        slot = self.dslots[eng][si]
        q = (eng, si)
        if slot[1] > 0:
            self._wait(eng, (q, slot[1]))
        self._deps(eng, reads, writes)
        slot[1] += 16
        self.E[eng].dma_start(out=out_ap, in_=in_ap, **kw).then_inc(slot[0], 16)
        self._mark((q, slot[1]), reads, writes)
        self.ninstr += 1

    def all_events(self):
        evs = []
        for e in ("pe", "act", "dve", "pool"):
            if self.cnt[e] > 0:
                evs.append((e, self.cnt[e] - 1))
        for eng, slots in self.dslots.items():
            for si, (s, c) in enumerate(slots):
                if c > 0:
                    evs.append(((eng, si), c))
        return evs

    def barrier(self, engs=None):
        evs = self.all_events()
        for e in (engs or self.ENGS):
            for ev in evs:
                if ev[0] == e and e == "pe":
                    continue
                self._wait(e, ev)

    @contextmanager
    def scope(self):
        old = self.stack
        try:
            with ExitStack() as st:
                self.stack = st
                try:
                    yield
                finally:
                    self.barrier()
        finally:
            self.stack = old

    def finish(self):
        self.barrier(["sp"])


def make_consts():
    p = np.arange(128)[:, None]
    f = np.arange(128)[None, :]
    same = (p // 64) == (f // 64)
    cols = {}
    parts = []

    def add(name, arr):
        cols[name] = (sum(a.shape[1] for a in parts), arr.shape[1])
        parts.append(arr.astype(np.float32))

    add("ident", (p == f))
    add("tri01", same & (p <= f))
    add("stri01", same & (p < f))
    add("su01", same & (p > f))
    add("sl01", same & (p >= f))
    add("bones", same)
    add("ones", np.ones((128, 128)))
    add("chunkind", (p // 64) == np.arange(2)[None, :])
    add("tri16", (same & (p <= f)) * (-1.0 / 16.0))
    add("bones16", same * (-1.0 / 16.0))
    add("chunkind16", ((p // 64) == np.arange(2)[None, :]) * (-1.0 / 16.0))
    return np.concatenate(parts, axis=1), cols


CST_NP, CST_COLS = make_consts()


class Consts:
    def __init__(self, P, cst_dram):
        self.P = P
        n = CST_NP.shape[1]
        self.f = P.sbuf("cst_f", [128, n])
        P.dma("sp", self.f[:], cst_dram.t[:, :], reads=[cst_dram], writes=[self.f])
        self.identb = P.sbuf("identb", [128, 128], BF16)
        P.dve(lambda e: e.tensor_copy(out=self.identb[:], in_=self.c("ident")), reads=[self.f], writes=[self.identb])

    def c(self, name, rows=128):
        o, w = CST_COLS[name]
        return self.f[0:rows, o:o + w]


def norm_gate(P, src_ap, src_bufs, z_ap, z_bufs, nw_ap, nw_bufs, G, gsz, out, tmp, gate_first):
    a, b, ss, sg = tmp["a"], tmp["b"], tmp["ss"], tmp["sg"]
    n = G * gsz
    P.act(lambda e: e.activation(out=sg[:, 0:n], in_=z_ap, func=AF.Silu), reads=z_bufs, writes=[sg])
    if gate_first:
        P.dve(lambda e: e.tensor_tensor(out=a[:, 0:n], in0=src_ap, in1=sg[:, 0:n], op=ALU.mult),
              reads=list(src_bufs) + [sg], writes=[a])
    else:
        P.dve(lambda e: e.tensor_copy(out=a[:, 0:n], in_=src_ap), reads=list(src_bufs), writes=[a])
    P.act(lambda e: e.activation(out=b[:, 0:n], in_=a[:, 0:n], func=AF.Square), reads=[a], writes=[b])
    P.dve(lambda e: e.tensor_reduce(out=ss[:, 0:G], in_=b[:, 0:n].rearrange("p (g e) -> p g e", g=G), axis=AX.X, op=ALU.add),
          reads=[b], writes=[ss])
    P.act(lambda e: e.activation(out=ss[:, 0:G], in_=ss[:, 0:G], func=AF.Sqrt, scale=1.0 / gsz, bias=tmp["eps"][:, 0:1]),
          reads=[ss, tmp["eps"]], writes=[ss])
    P.dve(lambda e: e.reciprocal(out=ss[:, 0:G], in_=ss[:, 0:G]), reads=[ss], writes=[ss])
    P.dve(lambda e: e.tensor_tensor(out=b[:, 0:n].rearrange("p (g e) -> p g e", g=G),
                                    in0=a[:, 0:n].rearrange("p (g e) -> p g e", g=G),
                                    in1=ss[:, 0:G].unsqueeze(2).to_broadcast([128, G, gsz]), op=ALU.mult),
          reads=[a, ss], writes=[b])
    if gate_first:
        P.pool(lambda e: e.tensor_tensor(out=out[:, 0:n].rearrange("p (g e) -> p g e", g=G),
                                         in0=b[:, 0:n].rearrange("p (g e) -> p g e", g=G), in1=nw_ap, op=ALU.mult),
               reads=[b] + list(nw_bufs), writes=[out])
    else:
        P.pool(lambda e: e.tensor_tensor(out=a[:, 0:n].rearrange("p (g e) -> p g e", g=G),
                                         in0=b[:, 0:n].rearrange("p (g e) -> p g e", g=G), in1=nw_ap, op=ALU.mult),
               reads=[b] + list(nw_bufs), writes=[a])
        P.pool(lambda e: e.tensor_tensor(out=out[:, 0:n], in0=a[:, 0:n], in1=sg[:, 0:n], op=ALU.mult),
               reads=[a, sg], writes=[out])


def ng_tmp(P):
    t = dict(a=P.sbuf("ng_a", [128, 512]), b=P.sbuf("ng_b", [128, 512]), ss=P.sbuf("ng_ss", [128, 8]),
             sg=P.sbuf("ng_sg", [128, 512]), eps=P.sbuf("ng_eps", [128, 1]), one=P.sbuf("ng_one", [128, 1]))
    P.pool(lambda e: e.memset(t["eps"][:], EPS), writes=[t["eps"]])
    P.pool(lambda e: e.memset(t["one"][:], 1.0), writes=[t["one"]])
    return t


PT_NQ, PT_NKC, PT_NVC, PT_NKS, PT_NKW = 0, 512, 640, 768, 896
PT_SXBC = 1024
PT_GQKV = 2048
PT_LQ, PT_LK, PT_LLR = 3584, 3840, 4096
PT_ROWS = 4112
PN_NVS, PN_NVW, PN_NGATE, PN_SZ, PN_SDT = 0, 128, 256, 280, 792
PN_GZ, PN_GBETA, PN_GA, PN_LK, PN_LV, PN_LG = 800, 1312, 1316, 1320, 1576, 2088
PN_COLS = 2600
PT_GROUPS = ([(0 + 128 * i, 128, PT_NQ + 128 * i) for i in range(4)] +
             [(512, 128, PT_NKC), (640, 128, PT_NVC), (768, 128, PT_NKS), (1024, 128, PT_NKW)] +
             [(1816 + 128 * i, 128, PT_SXBC + 128 * i) for i in range(8)] +
             [(2848 + 128 * i, 128, PT_GQKV + 128 * i) for i in range(12)] +
             [(4904 + 128 * i, 128, PT_LQ + 128 * i) for i in range(2)] +
             [(5160 + 128 * i, 128, PT_LK + 128 * i) for i in range(2)] +
             [(6440, 16, PT_LLR)])
PN_GROUPS = [(896, 128, PN_NVS), (1152, 512, PN_NVW), (1664, 152, PN_NVW + 512), (2840, 8, PN_SDT),
             (4384, 512, PN_GZ), (4896, 8, PN_GBETA), (5160, 256, PN_LK), (5416, 512, PN_LV), (5928, 512, PN_LG)]


def stage_gla(P, C, projT, projN, ymix, prm, l):
    with P.scope():
        w2 = P.sbuf("gla_w2", [16, 256])
        gb = P.sbuf("gla_gb", [1, 256])
        nwb = P.sbuf("gla_nwb", [128, 128])
        P.dma("sp", w2[:], prm["gla_gate_w2"].t[l], reads=[prm["gla_gate_w2"]], writes=[w2])
        P.dma("sp", gb[:], prm["gla_gate_b"].t[l:l + 1, :], reads=[prm["gla_gate_b"]], writes=[gb])
        P.dma("sp", nwb[:], prm["gla_norm_w"].t[l:l + 1, :].partition_broadcast(128), reads=[prm["gla_norm_w"]], writes=[nwb])
        S = P.sbuf("gla_S", [64, 4, 128])
        Sb = [P.sbuf(f"gla_Sb{i}", [64, 4, 128], BF16) for i in range(2)]
        P.dve(lambda e: e.memset(S[:], 0.0), writes=[S])
        P.dve(lambda e: e.memset(Sb[0][:], 0.0), writes=[Sb[0]])
        tmp = ng_tmp(P)
        NB = 2
        qT = [P.sbuf(f"gla_qT{i}", [64, 4, 128]) for i in range(NB)]
        kT = [P.sbuf(f"gla_kT{i}", [64, 4, 128]) for i in range(NB)]
        lrT = [P.sbuf(f"gla_lrT{i}", [16, 128]) for i in range(NB)]
        tokN = [P.sbuf(f"gla_tokN{i}", [128, 1280]) for i in range(NB)]
        lsp = P.sbuf("gla_lsp", [128, 256])
        ex = P.sbuf("gla_ex", [128, 256])
        kend = P.sbuf("gla_kend", [128, 256], BF16)
        vb = P.sbuf("gla_vb", [128, 512], BF16)
        ebT = P.sbuf("gla_ebT", [64, 512])
        qdT = P.sbuf("gla_qdT", [64, 4, 128], BF16)
        kiT = P.sbuf("gla_kiT", [64, 4, 128], BF16)
        dec = P.sbuf("gla_dec", [64, 8])
        AT = P.sbuf("gla_AT", [128, 4, 128], BF16)
        yo = [P.sbuf(f"gla_yo{i}", [128, 512]) for i in range(2)]
        ps_gk = P.psum("gla_ps_gk", [128, 512])
        ps_bl = P.psum("gla_ps_bl", [128, 512])
        ps_bT = P.psum("gla_ps_bT", [64, 512])
        ps_blT = P.psum("gla_ps_blT", [64, 8])
        ps_at = P.psum("gla_ps_at", [128, 512])
        ps_o = P.psum("gla_ps_o", [128, 512])
        ps_loc = [P.psum(f"gla_ps_loc{i}", [64, 512]) for i in range(2)]
        cf = [C.f]
        bg_begin(P)

        def load(t):
            i = t % NB
            tok = slice(t * 128, (t + 1) * 128)
            P.dma("sp", qT[i][:], projT.t[PT_LQ:PT_LQ + 256, tok].rearrange("(h d) t -> d h t", d=64), reads=[projT], writes=[qT[i]])
            P.dma("sp", kT[i][:], projT.t[PT_LK:PT_LK + 256, tok].rearrange("(h d) t -> d h t", d=64), reads=[projT], writes=[kT[i]])
            P.dma("sp", lrT[i][:], projT.t[PT_LLR:PT_LLR + 16, tok], reads=[projT], writes=[lrT[i]])
            P.dma("sp", tokN[i][:], projN.t[tok, PN_LK:PN_LK + 1280], reads=[projN], writes=[tokN[i]])

        load(0)
        for t in range(NT):
            if t + 1 < NT:
                load(t + 1)
            i = t % NB
            tok = slice(t * 128, (t + 1) * 128)
            kN = tokN[i][:, 0:256]
            vN = tokN[i][:, 256:768]
            gN = tokN[i][:, 768:1280]
            P.pe(lambda e: e.matmul(ps_gk[:, 0:256], lhsT=lrT[i][:], rhs=w2[:], start=True, stop=False), reads=[lrT[i], w2], writes=[ps_gk])
            P.pe(lambda e: e.matmul(ps_gk[:, 0:256], lhsT=C.c("ones", 1), rhs=gb[:], start=False, stop=True), reads=[gb] + cf, writes=[ps_gk])
            P.act(lambda e: e.activation(out=ex[:], in_=ps_gk[:, 0:256], func=AF.Exp, scale=-1.0), reads=[ps_gk], writes=[ex])
            P.act(lambda e: e.activation(out=lsp[:], in_=ex[:], func=AF.Ln, bias=tmp["one"][:, 0:1]), reads=[ex, tmp["one"]], writes=[lsp])
            P.pe(lambda e: e.matmul(ps_gk[:, 256:512], lhsT=C.c("tri16"), rhs=lsp[:], start=True, stop=True), reads=[lsp] + cf, writes=[ps_gk])
            P.pe(lambda e: e.matmul(ps_bl[:, 0:256], lhsT=C.c("bones16"), rhs=lsp[:], start=True, stop=True), reads=[lsp] + cf, writes=[ps_bl])
            for h in range(4):
                P.pe(lambda e, h=h: e.matmul(ps_bT[:, h * 128:(h + 1) * 128], lhsT=lsp[:, h * 64:(h + 1) * 64], rhs=C.c("tri16"), start=True, stop=True),
                     reads=[lsp] + cf, writes=[ps_bT])
            for h in range(4):
                P.pe(lambda e, h=h: e.matmul(ps_blT[:, h * 2:(h + 1) * 2], lhsT=lsp[:, h * 64:(h + 1) * 64], rhs=C.c("chunkind16"), start=True, stop=True),
                     reads=[lsp] + cf, writes=[ps_blT])
            P.dve(lambda e: e.tensor_copy(out=ex[:], in_=ps_gk[:, 256:512]), reads=[ps_gk], writes=[ex])
            P.dve(lambda e: e.tensor_tensor(out=ex[:], in0=ps_bl[:, 0:256], in1=ex[:], op=ALU.subtract), reads=[ps_bl, ex], writes=[ex])
            P.act(lambda e: e.activation(out=ex[:], in_=ex[:], func=AF.Exp), reads=[ex], writes=[ex])
            P.dve(lambda e: e.tensor_tensor(out=kend[:], in0=kN, in1=ex[:], op=ALU.mult), reads=[tokN[i], ex], writes=[kend])
            P.pool(lambda e: e.tensor_copy(out=vb[:], in_=vN), reads=[tokN[i]], writes=[vb])
            P.act(lambda e: e.activation(out=ebT[:], in_=ps_bT[:], func=AF.Exp), reads=[ps_bT], writes=[ebT])
            P.dve(lambda e: e.scalar_tensor_tensor(out=qdT[:].rearrange("d h t -> d (h t)"), in0=qT[i][:].rearrange("d h t -> d (h t)"), scalar=0.125,
                                                   in1=ebT[:], op0=ALU.mult, op1=ALU.mult), reads=[qT[i], ebT], writes=[qdT])
            P.act(lambda e: e.activation(out=ebT[:], in_=ps_bT[:], func=AF.Exp, scale=-1.0), reads=[ps_bT], writes=[ebT])
            P.dve(lambda e: e.tensor_tensor(out=kiT[:].rearrange("d h t -> d (h t)"), in0=kT[i][:].rearrange("d h t -> d (h t)"), in1=ebT[:], op=ALU.mult),
                  reads=[kT[i], ebT], writes=[kiT])
            P.act(lambda e: e.activation(out=dec[:], in_=ps_blT[:], func=AF.Exp), reads=[ps_blT], writes=[dec])
            for h in range(4):
                P.pe(lambda e, h=h: e.matmul(ps_at[:, h * 128:(h + 1) * 128], lhsT=kiT[:, h, :], rhs=qdT[:, h, :], start=True, stop=True),
                     reads=[kiT, qdT], writes=[ps_at])
            P.dve(lambda e: e.tensor_tensor(out=AT[:], in0=ps_at[:].rearrange("p (h t) -> p h t", h=4),
                                            in1=C.c("tri01").unsqueeze(1).to_broadcast([128, 4, 128]), op=ALU.mult), reads=[ps_at] + cf, writes=[AT])
            for c in range(2):
                rows = slice(c * 64, (c + 1) * 64)
                for h in range(4):
                    P.pe(lambda e, h=h, rows=rows, c=c: e.matmul(ps_loc[c][:, h * 128:(h + 1) * 128], lhsT=kend[rows, h * 64:(h + 1) * 64],
                                                                 rhs=vb[rows, h * 128:(h + 1) * 128], start=True, stop=True),
                         reads=[kend, vb], writes=[ps_loc[c]])
            for c in range(2):
                P.dve(lambda e, c=c: e.tensor_tensor(out=S[:], in0=S[:], in1=dec[:].rearrange("d (h c) -> d h c", c=2)[:, :, c:c + 1].to_broadcast([64, 4, 128]),
                                                     op=ALU.mult), reads=[S, dec], writes=[S])
                P.dve(lambda e, c=c: e.tensor_tensor(out=S[:].rearrange("d h e -> d (h e)"), in0=S[:].rearrange("d h e -> d (h e)"), in1=ps_loc[c][:], op=ALU.add),
                      reads=[S, ps_loc[c]], writes=[S])
                if c == 0:
                    P.act(lambda e: e.copy(out=Sb[1][:], in_=S[:]), reads=[S], writes=[Sb[1]])
            for h in range(4):
                cols = slice(h * 128, (h + 1) * 128)
                P.pe(lambda e, h=h, cols=cols: e.matmul(ps_o[:, cols], lhsT=AT[:, h, :], rhs=vb[:, cols], start=True, stop=False),
                     reads=[AT, vb], writes=[ps_o])
                for c in range(2):
                    rows = slice(c * 64, (c + 1) * 64)
                    P.pe(lambda e, h=h, cols=cols, rows=rows, c=c: e.matmul(ps_o[rows, cols], lhsT=qdT[:, h, rows], rhs=Sb[c][:, h, :], start=False, stop=(c == 1)),
                         reads=[qdT, Sb[c]], writes=[ps_o])
            P.act(lambda e: e.copy(out=Sb[0][:], in_=S[:]), reads=[S], writes=[Sb[0]])
            y = yo[t % 2]
            norm_gate(P, ps_o[:], [ps_o], gN, [tokN[i]], nwb[:].unsqueeze(1).to_broadcast([128, 4, 128]), [nwb], 4, 128, y, tmp, False)
            P.dma("sp", ymix.t[tok, 1536:2048], y[:], reads=[y], writes=[ymix])
            bg_tick(P, 1)
        bg_end(P)


def host_param(name, arr):
    a = np.asarray(arr, np.float32)
    if name in ("ssd_conv_w", "gdn_conv_w"):
        L, K, CH = a.shape
        a = a.reshape(L, K, CH // 128, 128).transpose(0, 3, 2, 1)
    elif name in ("norm1_w", "norm2_w"):
        L = a.shape[0]
        a = a.reshape(L, 16, 128).transpose(2, 0, 1)
    elif name == "final_norm_w":
        a = a.reshape(1, -1)
    elif name == "nsa_cmp_pos":
        a = a.transpose(0, 1, 3, 2)
    elif name == "ssd_conv_b":
        L, CH = a.shape
        a = a.reshape(L, CH // 128, 128).transpose(0, 2, 1)
    return np.ascontiguousarray(a)


def bc(ap, shape):
    return ap.to_broadcast(list(shape))


def causal_conv_silu(P, projT, row0, ntiles, cw, cb, dst, dst_off, name, bias=True):
    xpad = [P.sbuf(f"{name}_xpad{i}", [128, SEQ + 3]) for i in range(2)]
    acc = [P.sbuf(f"{name}_acc{i}", [128, SEQ]) for i in range(2)]
    for i in range(2):
        P.pool(lambda e, i=i: e.memset(xpad[i][:, 0:3], 0.0), writes=[xpad[i]])
    for ct in range(ntiles):
        xp = xpad[ct % 2]
        ac = acc[ct % 2]
        P.dma("sp", xp[:, 3:SEQ + 3], projT.t[row0 + ct * 128:row0 + (ct + 1) * 128, :], reads=[projT], writes=[xp])
        eng = P.dve
        eng(lambda e, ct=ct, xp=xp, ac=ac: e.tensor_scalar(out=ac[:], in0=xp[:, 0:SEQ], scalar1=cw[:, ct, 0:1], scalar2=None, op0=ALU.mult),
            reads=[xp, cw], writes=[ac])
        for k in range(1, 4):
            eng(lambda e, ct=ct, xp=xp, ac=ac, k=k: e.scalar_tensor_tensor(out=ac[:], in0=xp[:, k:SEQ + k], scalar=cw[:, ct, k:k + 1], in1=ac[:],
                                                                            op0=ALU.mult, op1=ALU.add), reads=[xp, cw, ac], writes=[ac])
        if bias:
            P.act(lambda e, ct=ct, ac=ac: e.activation(out=dst[:, dst_off + ct, :], in_=ac[:], func=AF.Silu, bias=cb[:, ct:ct + 1]),
                  reads=[ac, cb], writes=[dst])
        else:
            P.act(lambda e, ct=ct, ac=ac: e.activation(out=dst[:, dst_off + ct, :], in_=ac[:], func=AF.Silu), reads=[ac], writes=[dst])


def softplus_small(P, x_ap, xbuf, tmpb, one):
    P.act(lambda e: e.activation(out=x_ap, in_=x_ap, func=AF.Exp), reads=[xbuf], writes=[xbuf])
    P.act(lambda e: e.activation(out=x_ap, in_=x_ap, func=AF.Ln, bias=one[:, 0:1]), reads=[xbuf, one], writes=[xbuf])


def stage_ssd(P, C, projT, projN, ymix, prm, l):
    with P.scope():
        cf = [C.f]
        cw = P.sbuf("ssd_cw", [128, 8, 4])
        cb = P.sbuf("ssd_cb", [128, 8])
        dtb = P.sbuf("ssd_dtb", [128, 8])
        aneg = P.sbuf("ssd_aneg", [128, 8])
        dsk = P.sbuf("ssd_dsk", [128, 8])
        nwb = P.sbuf("ssd_nwb", [128, 512])
        P.dma("sp", cw[:], prm["ssd_conv_w"].t[l], reads=[prm["ssd_conv_w"]], writes=[cw])
        P.dma("sp", cb[:], prm["ssd_conv_b"].t[l], reads=[prm["ssd_conv_b"]], writes=[cb])
        P.dma("sp", dtb[:], prm["ssd_dt_bias"].t[l:l + 1, :].partition_broadcast(128), reads=[prm["ssd_dt_bias"]], writes=[dtb])
        P.dma("sp", aneg[:], prm["ssd_a_log"].t[l:l + 1, :].partition_broadcast(128), reads=[prm["ssd_a_log"]], writes=[aneg])
        P.dma("sp", dsk[:], prm["ssd_d"].t[l:l + 1, :].partition_broadcast(128), reads=[prm["ssd_d"]], writes=[dsk])
        P.dma("sp", nwb[:], prm["ssd_norm_w"].t[l:l + 1, :].partition_broadcast(128), reads=[prm["ssd_norm_w"]], writes=[nwb])
        P.act(lambda e: e.activation(out=aneg[:], in_=aneg[:], func=AF.Exp), reads=[aneg], writes=[aneg])
        P.dve(lambda e: e.tensor_scalar(out=aneg[:], in0=aneg[:], scalar1=-1.0, scalar2=None, op0=ALU.mult), reads=[aneg], writes=[aneg])
        act = P.sbuf("ssd_act", [128, 8, SEQ])
        with P.scope():
            causal_conv_silu(P, projT, PT_SXBC, 8, cw, cb, act, 0, "ssd")
        BCb = P.sbuf("ssd_BCb", [128, 4, SEQ], BF16)
        for k in range(4):
            (P.dve if k % 2 == 0 else P.pool)(lambda e, k=k: e.tensor_copy(out=BCb[:, k, :], in_=act[:, 4 + k, :]), reads=[act], writes=[BCb])
        tmp = ng_tmp(P)
        S = P.sbuf("ssd_S", [128, 8, 64])
        Sb = [P.sbuf(f"ssd_Sb{i}", [128, 8, 64], BF16) for i in range(2)]
        P.dve(lambda e: e.memset(S[:], 0.0), writes=[S])
        P.dve(lambda e: e.memset(Sb[0][:], 0.0), writes=[Sb[0]])
        tokN = [P.sbuf(f"ssd_tokN{i}", [128, 520]) for i in range(2)]
        xN = P.sbuf("ssd_xN", [128, 512])
        BNb = P.sbuf("ssd_BNb", [128, 256], BF16)
        dt8 = P.sbuf("ssd_dt8", [128, 8])
        a8 = P.sbuf("ssd_a8", [128, 8])
        dw8 = P.sbuf("ssd_dw8", [128, 8])
        ac16 = P.sbuf("ssd_ac16", [128, 2, 8])
        e32 = P.sbuf("ssd_e32", [128, 32])
        Aexp = P.sbuf("ssd_Aexp", [128, 8, 128])
        seg = P.sbuf("ssd_seg", [128, 8, 128])
        CBm = P.sbuf("ssd_CBm", [128, 2, 128])
        MT = P.sbuf("ssd_MT", [128, 8, 128], BF16)
        xdt = P.sbuf("ssd_xdt", [128, 8, 64], BF16)
        xw = P.sbuf("ssd_xw", [128, 8, 64], BF16)
        y1 = P.sbuf("ssd_y1", [128, 512])
        y2 = P.sbuf("ssd_y2", [128, 512])
        yo = [P.sbuf(f"ssd_yo{i}", [128, 512]) for i in range(2)]
        psA = P.psum("ssd_psA", [128, 512])
        psB = P.psum("ssd_psB", [128, 512])
        psC = P.psum("ssd_psC", [128, 512])
        psD = P.psum("ssd_psD", [128, 512])
        psE = P.psum("ssd_psE", [128, 512])
        psF = P.psum("ssd_psF", [128, 512])
        psG = [P.psum(f"ssd_psG{i}", [128, 512]) for i in range(2)]

        bg_begin(P)

        def load(t):
            P.dma("sp", tokN[t % 2][:], projN.t[t * 128:(t + 1) * 128, PN_SZ:PN_SZ + 520], reads=[projN], writes=[tokN[t % 2]])

        load(0)
        for t in range(NT):
            if t + 1 < NT:
                load(t + 1)
            tk = tokN[t % 2]
            tok = slice(t * 128, (t + 1) * 128)
            for k in range(4):
                P.pe(lambda e, k=k: e.transpose(out=psA[:, k * 128:(k + 1) * 128], in_=act[:, k, tok], identity=C.c("ident")), reads=[act] + cf, writes=[psA])
            for k in range(2):
                P.pe(lambda e, k=k: e.transpose(out=psB[:, k * 128:(k + 1) * 128], in_=act[:, 4 + k, tok], identity=C.c("ident")), reads=[act] + cf, writes=[psB])
            P.act(lambda e: e.copy(out=xN[:], in_=psA[:]), reads=[psA], writes=[xN])
            P.dve(lambda e: e.tensor_copy(out=BNb[:], in_=psB[:, 0:256]), reads=[psB], writes=[BNb])
            P.dve(lambda e: e.tensor_tensor(out=dt8[:], in0=tk[:, 512:520], in1=dtb[:], op=ALU.add), reads=[tk, dtb], writes=[dt8])
            softplus_small(P, dt8[:], dt8, None, tmp["one"])
            P.dve(lambda e: e.tensor_tensor(out=a8[:], in0=dt8[:], in1=aneg[:], op=ALU.mult), reads=[dt8, aneg], writes=[a8])
            P.dve(lambda e: e.tensor_tensor(out=Aexp[:], in0=bc(a8[:].unsqueeze(2), [128, 8, 128]), in1=bc(C.c("tri01").unsqueeze(1), [128, 8, 128]), op=ALU.mult),
                  reads=[a8] + cf, writes=[Aexp])
            P.dve(lambda e: e.tensor_tensor(out=ac16[:], in0=bc(a8[:].unsqueeze(1), [128, 2, 8]), in1=bc(C.c("chunkind").unsqueeze(2), [128, 2, 8]), op=ALU.mult),
                  reads=[a8] + cf, writes=[ac16])
            P.pe(lambda e: e.matmul(psC[:], lhsT=C.c("su01"), rhs=Aexp[:, 0:4, :].rearrange("p h i -> p (h i)"), start=True, stop=True), reads=[Aexp] + cf, writes=[psC])
            P.pe(lambda e: e.matmul(psD[:], lhsT=C.c("su01"), rhs=Aexp[:, 4:8, :].rearrange("p h i -> p (h i)"), start=True, stop=True), reads=[Aexp] + cf, writes=[psD])
            P.act(lambda e: e.activation(out=seg[:, 0:4, :].rearrange("p h i -> p (h i)"), in_=psC[:], func=AF.Exp), reads=[psC], writes=[seg])
            P.act(lambda e: e.activation(out=seg[:, 4:8, :].rearrange("p h i -> p (h i)"), in_=psD[:], func=AF.Exp), reads=[psD], writes=[seg])
            P.pe(lambda e: e.matmul(psE[:, 0:8], lhsT=C.c("tri01"), rhs=a8[:], start=True, stop=True), reads=[a8] + cf, writes=[psE])
            P.pe(lambda e: e.matmul(psE[:, 8:16], lhsT=C.c("su01"), rhs=a8[:], start=True, stop=True), reads=[a8] + cf, writes=[psE])
            P.pe(lambda e: e.matmul(psE[:, 16:32], lhsT=C.c("ones"), rhs=ac16[:].rearrange("p c h -> p (c h)"), start=True, stop=True), reads=[ac16] + cf, writes=[psE])
            P.act(lambda e: e.activation(out=e32[:], in_=psE[:, 0:32], func=AF.Exp), reads=[psE], writes=[e32])
            ea = e32[:, 0:8]
            w8 = e32[:, 8:16]
            for g in range(2):
                P.pe(lambda e, g=g: e.matmul(psB[:, 256 + g * 128:256 + (g + 1) * 128], lhsT=BCb[:, g, tok], rhs=BCb[:, 2 + g, tok], start=True, stop=True),
                     reads=[BCb], writes=[psB])
            P.dve(lambda e: e.tensor_tensor(out=CBm[:], in0=psB[:, 256:512].rearrange("p (g i) -> p g i", g=2), in1=bc(C.c("tri01").unsqueeze(1), [128, 2, 128]), op=ALU.mult),
                  reads=[psB] + cf, writes=[CBm])
            P.dve(lambda e: e.tensor_tensor(out=MT[:].rearrange("p (g r) i -> p g r i", g=2), in0=seg[:].rearrange("p (g r) i -> p g r i", g=2),
                                            in1=bc(CBm[:].unsqueeze(2), [128, 2, 4, 128]), op=ALU.mult), reads=[seg, CBm], writes=[MT])
            P.dve(lambda e: e.tensor_tensor(out=dw8[:], in0=dt8[:], in1=w8, op=ALU.mult), reads=[dt8, e32], writes=[dw8])
            P.pool(lambda e: e.tensor_tensor(out=xdt[:], in0=xN[:].rearrange("p (h q) -> p h q", h=8), in1=bc(dt8[:].unsqueeze(2), [128, 8, 64]), op=ALU.mult),
                   reads=[xN, dt8], writes=[xdt])
            P.pool(lambda e: e.tensor_tensor(out=xw[:], in0=xN[:].rearrange("p (h q) -> p h q", h=8), in1=bc(dw8[:].unsqueeze(2), [128, 8, 64]), op=ALU.mult),
                   reads=[xN, dw8], writes=[xw])
            for h in range(8):
                P.pe(lambda e, h=h: e.matmul(psF[:, h * 64:(h + 1) * 64], lhsT=MT[:, h, :], rhs=xdt[:, h, :], start=True, stop=True), reads=[MT, xdt], writes=[psF])
            for c in range(2):
                rows = slice(c * 64, (c + 1) * 64)
                for g in range(2):
                    P.pe(lambda e, c=c, g=g, rows=rows: e.matmul(psG[c][:, g * 256:(g + 1) * 256], lhsT=BNb[rows, g * 128:(g + 1) * 128],
                                                                 rhs=xw[rows, 4 * g:4 * g + 4, :].rearrange("p h q -> p (h q)"), start=True, stop=True),
                         reads=[BNb, xw], writes=[psG[c]])
            for c in range(2):
                P.dve(lambda e, c=c: e.tensor_tensor(out=S[:], in0=S[:], in1=bc(e32[:, 16 + 8 * c:24 + 8 * c].unsqueeze(2), [128, 8, 64]), op=ALU.mult),
                      reads=[S, e32], writes=[S])
                P.dve(lambda e, c=c: e.tensor_tensor(out=S[:].rearrange("p h q -> p (h q)"), in0=S[:].rearrange("p h q -> p (h q)"), in1=psG[c][:], op=ALU.add),
                      reads=[S, psG[c]], writes=[S])
                if c == 0:
                    P.act(lambda e: e.copy(out=Sb[1][:], in_=S[:]), reads=[S], writes=[Sb[1]])
            for c in range(2):
                rows = slice(c * 64, (c + 1) * 64)
                for g in range(2):
                    P.pe(lambda e, c=c, g=g, rows=rows: e.matmul(psC[rows, g * 256:(g + 1) * 256], lhsT=BCb[:, 2 + g, t * 128 + c * 64:t * 128 + (c + 1) * 64],
                                                                 rhs=Sb[c][:, 4 * g:4 * g + 4, :].rearrange("p h q -> p (h q)"), start=True, stop=True),
                         reads=[BCb, Sb[c]], writes=[psC])
            P.act(lambda e: e.copy(out=Sb[0][:], in_=S[:]), reads=[S], writes=[Sb[0]])
            P.dve(lambda e: e.tensor_tensor(out=y1[:].rearrange("p (h q) -> p h q", h=8), in0=psC[:].rearrange("p (h q) -> p h q", h=8),
                                            in1=bc(ea.unsqueeze(2), [128, 8, 64]), op=ALU.mult), reads=[psC, e32], writes=[y1])
            P.dve(lambda e: e.tensor_tensor(out=y1[:], in0=y1[:], in1=psF[:], op=ALU.add), reads=[y1, psF], writes=[y1])
            P.pool(lambda e: e.tensor_tensor(out=y2[:].rearrange("p (h q) -> p h q", h=8), in0=xN[:].rearrange("p (h q) -> p h q", h=8),
                                             in1=bc(dsk[:].unsqueeze(2), [128, 8, 64]), op=ALU.mult), reads=[xN, dsk], writes=[y2])
            P.pool(lambda e: e.tensor_tensor(out=y1[:], in0=y1[:], in1=y2[:], op=ALU.add), reads=[y1, y2], writes=[y1])
            y = yo[t % 2]
            norm_gate(P, y1[:], [y1], tk[:, 0:512], [tk], nwb[:].rearrange("p (g e) -> p g e", g=2), [nwb], 2, 256, y, tmp, True)
            P.dma("sp", ymix.t[tok, 512:1024], y[:], reads=[y], writes=[ymix])
            bg_tick(P, 2)
        bg_end(P)


def stage_gdn(P, C, projT, projN, ymix, prm, l):
    H = 4
    with P.scope():
        cf = [C.f]
        cw = P.sbuf("gdn_cw", [128, 12, 4])
        dtb = P.sbuf("gdn_dtb", [128, 4])
        aneg = P.sbuf("gdn_aneg", [128, 4])
        nwb = P.sbuf("gdn_nwb", [128, 128])
        P.dma("sp", cw[:], prm["gdn_conv_w"].t[l], reads=[prm["gdn_conv_w"]], writes=[cw])
        P.dma("sp", dtb[:], prm["gdn_dt_bias"].t[l:l + 1, :].partition_broadcast(128), reads=[prm["gdn_dt_bias"]], writes=[dtb])
        P.dma("sp", aneg[:], prm["gdn_a_log"].t[l:l + 1, :].partition_broadcast(128), reads=[prm["gdn_a_log"]], writes=[aneg])
        P.dma("sp", nwb[:], prm["gdn_norm_w"].t[l:l + 1, :].partition_broadcast(128), reads=[prm["gdn_norm_w"]], writes=[nwb])
        P.act(lambda e: e.activation(out=aneg[:], in_=aneg[:], func=AF.Exp), reads=[aneg], writes=[aneg])
        P.dve(lambda e: e.tensor_scalar(out=aneg[:], in0=aneg[:], scalar1=-1.0, scalar2=None, op0=ALU.mult), reads=[aneg], writes=[aneg])
        tmp = ng_tmp(P)
        qkvb = P.sbuf("gdn_qkvb", [128, 12, SEQ], BF16)
        with P.scope():
            cvt = P.sbuf("gdn_cvt", [128, 1, SEQ])
            sq = P.sbuf("gdn_sq", [128, SEQ])
            rinv = P.sbuf("gdn_rinv", [128, SEQ])
            pss = [P.psum(f"gdn_pss{i}", [128, 512]) for i in range(4)]
            xpad = [P.sbuf(f"gdn_xpad{i}", [128, SEQ + 3]) for i in range(2)]
            acc = P.sbuf("gdn_acc", [128, SEQ])
            for i in range(2):
                P.pool(lambda e, i=i: e.memset(xpad[i][:, 0:3], 0.0), writes=[xpad[i]])
            for ct in range(12):
                xp = xpad[ct % 2]
                P.dma("sp", xp[:, 3:SEQ + 3], projT.t[PT_GQKV + ct * 128:PT_GQKV + (ct + 1) * 128, :], reads=[projT], writes=[xp])
                P.dve(lambda e, ct=ct, xp=xp: e.tensor_scalar(out=acc[:], in0=xp[:, 0:SEQ], scalar1=cw[:, ct, 0:1], scalar2=None, op0=ALU.mult),
                      reads=[xp, cw], writes=[acc])
                for k in range(1, 4):
                    P.dve(lambda e, ct=ct, xp=xp, k=k: e.scalar_tensor_tensor(out=acc[:], in0=xp[:, k:SEQ + k], scalar=cw[:, ct, k:k + 1], in1=acc[:],
                                                                               op0=ALU.mult, op1=ALU.add), reads=[xp, cw, acc], writes=[acc])
                if ct >= 8:
                    P.act(lambda e, ct=ct: e.activation(out=qkvb[:, ct, :], in_=acc[:], func=AF.Silu), reads=[acc], writes=[qkvb])
                    continue
                P.act(lambda e: e.activation(out=cvt[:, 0, :], in_=acc[:], func=AF.Silu), reads=[acc], writes=[cvt])
                P.act(lambda e: e.activation(out=sq[:], in_=cvt[:, 0, :], func=AF.Square), reads=[cvt], writes=[sq])
                for n in range(4):
                    P.pe(lambda e, n=n: e.matmul(pss[n][:], lhsT=C.c("ones"), rhs=sq[:, n * 512:(n + 1) * 512], start=True, stop=True), reads=[sq] + cf, writes=[pss[n]])
                    P.act(lambda e, n=n: e.activation(out=rinv[:, n * 512:(n + 1) * 512], in_=pss[n][:], func=AF.Sqrt, bias=tmp["eps"][:, 0:1]),
                          reads=[pss[n], tmp["eps"]], writes=[rinv])
                P.dve(lambda e: e.reciprocal(out=rinv[:], in_=rinv[:]), reads=[rinv], writes=[rinv])
                scl = 128.0 ** -0.5 if ct < 4 else 1.0
                P.dve(lambda e, ct=ct, scl=scl: e.scalar_tensor_tensor(out=qkvb[:, ct, :], in0=cvt[:, 0, :], scalar=scl, in1=rinv[:], op0=ALU.mult, op1=ALU.mult),
                      reads=[cvt, rinv], writes=[qkvb])
        dbg(1)
        S = P.sbuf("gdn_S", [128, H, 128])
        Sb = P.sbuf("gdn_Sb", [128, H, 128], BF16)
        P.dve(lambda e: e.memset(S[:], 0.0), writes=[S])
        P.dve(lambda e: e.memset(Sb[:], 0.0), writes=[Sb])
        tokN = [P.sbuf(f"gdn_tokN{i}", [128, 520]) for i in range(3)]

        def f4(name, dt=F32):
            return P.sbuf("gdn_" + name, [128, H, 128], dt)

        b4 = P.sbuf("gdn_b4", [128, 4])
        g4 = P.sbuf("gdn_g4", [128, 4])
        gc8 = P.sbuf("gdn_gc8", [128, 2, 4])
        e16s = [P.sbuf(f"gdn_e16_{i}", [128, 16]) for i in range(2)]
        bg4 = P.sbuf("gdn_bg4", [128, 4])
        Gt, Gs, DTm, Dm, egb = f4("Gt"), f4("Gs"), f4("DTm"), f4("Dm"), f4("egb")
        A, AT, TT, vb, Kg = f4("A"), f4("AT"), f4("TT"), f4("vb"), f4("Kg")
        us = [f4("u0"), f4("u1")]
        X = [f4("X0"), f4("X1")]
        XT = [f4("XT0"), f4("XT1")]
        aqkTs, wTs, qdTs, kends = [[f4(f"{n}{i}", BF16) for i in range(2)] for n in ("aqkT", "wT", "qdT", "kend")]
        vnew = f4("vnew", BF16)
        yo = [P.sbuf(f"gdn_yo{i}", [128, 512]) for i in range(2)]
        B0 = P.psum("gdn_B0", [128, 512])
        B1 = P.psum("gdn_B1", [128, 512])
        B2 = P.psum("gdn_B2", [128, 512])
        B3 = P.psum("gdn_B3", [128, 512])
        B4 = P.psum("gdn_B4", [128, 1024], BF16)
        B5 = P.psum("gdn_B5", [128, 512])
        B6 = P.psum("gdn_B6", [128, 512])
        B7 = P.psum("gdn_B7", [128, 512])

        def v4(ap):
            return ap.rearrange("p (h i) -> p h i", h=H)

        def fl(ap):
            return ap.rearrange("p h i -> p (h i)")

        bg_begin(P)

        def load(t):
            P.dma("sp", tokN[t % 3][:], projN.t[t * 128:(t + 1) * 128, PN_GZ:PN_GZ + 520], reads=[projN], writes=[tokN[t % 3]])

        tri = C.c("tri01")
        su = C.c("su01")
        def pre(t, adv):
            tk = tokN[t % 3]
            e16 = e16s[t % 2]; u = us[t % 2]; aqkT = aqkTs[t % 2]; wT = wTs[t % 2]; qdT = qdTs[t % 2]; kend = kends[t % 2]
            tok = slice(t * 128, (t + 1) * 128)
            P.act(lambda e: e.activation(out=b4[:], in_=tk[:, 512:516], func=AF.Sigmoid), reads=[tk], writes=[b4])
            P.dve(lambda e: e.tensor_tensor(out=g4[:], in0=tk[:, 516:520], in1=dtb[:], op=ALU.add), reads=[tk, dtb], writes=[g4])
            softplus_small(P, g4[:], g4, None, tmp["one"])
            P.dve(lambda e: e.tensor_tensor(out=g4[:], in0=g4[:], in1=aneg[:], op=ALU.mult), reads=[g4, aneg], writes=[g4])
            P.dve(lambda e: e.tensor_tensor(out=Gt[:], in0=bc(g4[:].unsqueeze(2), [128, H, 128]), in1=bc(tri.unsqueeze(1), [128, H, 128]), op=ALU.mult),
                  reads=[g4] + cf, writes=[Gt])
            P.pool(lambda e: e.tensor_tensor(out=Gs[:], in0=bc(g4[:].unsqueeze(2), [128, H, 128]), in1=bc(su.unsqueeze(1), [128, H, 128]), op=ALU.mult),
                   reads=[g4] + cf, writes=[Gs])
            P.dve(lambda e: e.tensor_tensor(out=gc8[:], in0=bc(g4[:].unsqueeze(1), [128, 2, 4]), in1=bc(C.c("chunkind").unsqueeze(2), [128, 2, 4]), op=ALU.mult),
                  reads=[g4] + cf, writes=[gc8])
            P.pe(lambda e: e.matmul(B0[:], lhsT=su, rhs=fl(Gt[:]), start=True, stop=True), reads=[Gt] + cf, writes=[B0])
            P.pe(lambda e: e.matmul(B1[:], lhsT=tri, rhs=fl(Gs[:]), start=True, stop=True), reads=[Gs] + cf, writes=[B1])
            P.pe(lambda e: e.matmul(B2[:], lhsT=C.c("ones"), rhs=fl(Gt[:]), start=True, stop=True), reads=[Gt] + cf, writes=[B2])
            P.pe(lambda e: e.matmul(B3[:, 0:4], lhsT=tri, rhs=g4[:], start=True, stop=True), reads=[g4] + cf, writes=[B3])
            P.pe(lambda e: e.matmul(B3[:, 4:8], lhsT=su, rhs=g4[:], start=True, stop=True), reads=[g4] + cf, writes=[B3])
            P.pe(lambda e: e.matmul(B3[:, 8:16], lhsT=C.c("ones"), rhs=gc8[:].rearrange("p c h -> p (c h)"), start=True, stop=True), reads=[gc8] + cf, writes=[B3])
            P.act(lambda e: e.activation(out=fl(DTm[:]), in_=B0[:], func=AF.Exp), reads=[B0], writes=[DTm])
            P.act(lambda e: e.activation(out=fl(Dm[:]), in_=B1[:], func=AF.Exp), reads=[B1], writes=[Dm])
            P.act(lambda e: e.activation(out=fl(egb[:]), in_=B2[:], func=AF.Exp), reads=[B2], writes=[egb])
            P.act(lambda e: e.activation(out=e16[:], in_=B3[:, 0:16], func=AF.Exp), reads=[B3], writes=[e16])
            P.pool(lambda e: e.tensor_tensor(out=DTm[:], in0=DTm[:], in1=bc(tri.unsqueeze(1), [128, H, 128]), op=ALU.mult), reads=[DTm] + cf, writes=[DTm])
            P.pool(lambda e: e.tensor_tensor(out=Dm[:], in0=Dm[:], in1=bc(su.unsqueeze(1), [128, H, 128]), op=ALU.mult), reads=[Dm] + cf, writes=[Dm])
            P.dve(lambda e: e.tensor_tensor(out=bg4[:], in0=b4[:], in1=e16[:, 0:4], op=ALU.mult), reads=[b4, e16], writes=[bg4])
            dbg(2)
            adv()
            for h in range(H):
                P.pe(lambda e, h=h: e.transpose(out=B4[:, h * 128:(h + 1) * 128], in_=qkvb[:, 4 + h, tok], identity=C.identb[:]), reads=[qkvb, C.identb], writes=[B4])
            for h in range(H):
                P.pe(lambda e, h=h: e.transpose(out=B4[:, 512 + h * 128:512 + (h + 1) * 128], in_=qkvb[:, 8 + h, tok], identity=C.identb[:]), reads=[qkvb, C.identb], writes=[B4])
            P.dve(lambda e: e.tensor_tensor(out=vb[:], in0=v4(B4[:, 512:1024]), in1=bc(b4[:].unsqueeze(2), [128, H, 128]), op=ALU.mult), reads=[B4, b4], writes=[vb])
            P.dve(lambda e: e.tensor_tensor(out=Kg[:], in0=v4(B4[:, 0:512]), in1=bc(bg4[:].unsqueeze(2), [128, H, 128]), op=ALU.mult), reads=[B4, bg4], writes=[Kg])
            P.dve(lambda e: e.tensor_tensor(out=kend[:], in0=v4(B4[:, 0:512]), in1=bc(e16[:, 4:8].unsqueeze(2), [128, H, 128]), op=ALU.mult), reads=[B4, e16], writes=[kend])
            dbg(3)
            adv()
            for h in range(H):
                P.pe(lambda e, h=h: e.matmul(B0[:, h * 128:(h + 1) * 128], lhsT=qkvb[:, 4 + h, tok], rhs=qkvb[:, 4 + h, tok], start=True, stop=True), reads=[qkvb], writes=[B0])
            for h in range(H):
                P.pe(lambda e, h=h: e.matmul(B1[:, h * 128:(h + 1) * 128], lhsT=qkvb[:, 4 + h, tok], rhs=qkvb[:, h, tok], start=True, stop=True), reads=[qkvb], writes=[B1])
            P.dve(lambda e: e.tensor_tensor(out=A[:], in0=v4(B0[:]), in1=Dm[:], op=ALU.mult), reads=[B0, Dm], writes=[A])
            P.dve(lambda e: e.tensor_tensor(out=A[:], in0=A[:], in1=bc(b4[:].unsqueeze(2), [128, H, 128]), op=ALU.mult), reads=[A, b4], writes=[A])
            P.dve(lambda e: e.tensor_tensor(out=aqkT[:], in0=v4(B1[:]), in1=DTm[:], op=ALU.mult), reads=[B1, DTm], writes=[aqkT])
            P.pool(lambda e: e.tensor_tensor(out=qdT[:], in0=qkvb[:, 0:4, tok], in1=egb[:], op=ALU.mult), reads=[qkvb, egb], writes=[qdT])
            dbg(4)
            adv()
            for h in range(H):
                P.pe(lambda e, h=h: e.transpose(out=B2[:, h * 128:(h + 1) * 128], in_=A[:, h, :], identity=C.c("ident")), reads=[A] + cf, writes=[B2])
            dbg(4.3)
            adv()
            P.act(lambda e: e.copy(out=fl(AT[:]), in_=B2[:]), reads=[B2], writes=[AT])
            dbg(4.6)
            adv()
            P.dve(lambda e: e.scalar_tensor_tensor(out=TT[:], in0=v4(B2[:]), scalar=-1.0, in1=bc(C.c("ident").unsqueeze(1), [128, H, 128]), op0=ALU.mult, op1=ALU.add),
                  reads=[B2] + cf, writes=[TT])
            dbg(5)
            adv()
            Xc, XTc = A, AT
            for k in range(1, 6):
                Xn, XTn = X[k % 2], XT[k % 2]
                for h in range(H):
                    P.pe(lambda e, h=h, Xc=Xc, XTc=XTc: e.matmul(B0[:, h * 128:(h + 1) * 128], lhsT=XTc[:, h, :], rhs=Xc[:, h, :], start=True, stop=True),
                         reads=[Xc, XTc], writes=[B0])
                if k < 5:
                    for h in range(H):
                        P.pe(lambda e, h=h, Xc=Xc, XTc=XTc: e.matmul(B1[:, h * 128:(h + 1) * 128], lhsT=Xc[:, h, :], rhs=XTc[:, h, :], start=True, stop=True),
                             reads=[Xc, XTc], writes=[B1])
                P.act(lambda e, Xn=Xn: e.copy(out=fl(Xn[:]), in_=B0[:]), reads=[B0], writes=[Xn])
                if k < 5:
                    P.dve(lambda e, XTn=XTn: e.tensor_copy(out=fl(XTn[:]), in_=B1[:]), reads=[B1], writes=[XTn])
                for h in range(H):
                    P.pe(lambda e, h=h, Xn=Xn: e.matmul(B2[:, h * 128:(h + 1) * 128], lhsT=Xn[:, h, :], rhs=TT[:, h, :], start=True, stop=True),
                         reads=[Xn, TT], writes=[B2])
                P.dve(lambda e: e.tensor_tensor(out=fl(TT[:]), in0=fl(TT[:]), in1=B2[:], op=ALU.add), reads=[TT, B2], writes=[TT])
                adv()
                Xc, XTc = Xn, XTn
            dbg(6)
            adv()
            for h in range(H):
                P.pe(lambda e, h=h: e.matmul(B0[:, h * 128:(h + 1) * 128], lhsT=TT[:, h, :], rhs=vb[:, h, :], start=True, stop=True), reads=[TT, vb], writes=[B0])
            for h in range(H):
                P.pe(lambda e, h=h: e.matmul(B1[:, h * 128:(h + 1) * 128], lhsT=Kg[:, h, :], rhs=TT[:, h, :], start=True, stop=True), reads=[TT, Kg], writes=[B1])
            P.act(lambda e: e.copy(out=fl(u[:]), in_=B0[:]), reads=[B0], writes=[u])
            P.dve(lambda e: e.tensor_copy(out=fl(wT[:]), in_=B1[:]), reads=[B1], writes=[wT])

        def scan(t):
            tk = tokN[t % 3]
            tok = slice(t * 128, (t + 1) * 128)
            e16 = e16s[t % 2]; u = us[t % 2]; aqkT = aqkTs[t % 2]; wT = wTs[t % 2]; qdT = qdTs[t % 2]; kend = kends[t % 2]
            for c in range(2):
                rows = slice(c * 64, (c + 1) * 64)
                for h in range(H):
                    P.pe(lambda e, h=h, rows=rows: e.matmul(B5[rows, h * 128:(h + 1) * 128], lhsT=wT[:, h, rows], rhs=Sb[:, h, :], start=True, stop=True),
                         reads=[wT, Sb], writes=[B5])
                P.dve(lambda e, rows=rows: e.tensor_tensor(out=fl(vnew[rows]), in0=fl(u[rows]), in1=B5[rows, :], op=ALU.subtract), reads=[u, B5], writes=[vnew])
                yield
                for h in range(H):
                    P.pe(lambda e, h=h, rows=rows: e.matmul(B7[rows, h * 128:(h + 1) * 128], lhsT=qdT[:, h, rows], rhs=Sb[:, h, :], start=True, stop=False),
                         reads=[qdT, Sb], writes=[B7])
                    P.pe(lambda e, h=h, rows=rows: e.matmul(B7[rows, h * 128:(h + 1) * 128], lhsT=aqkT[rows, h, rows], rhs=vnew[rows, h, :], start=False, stop=True),
                         reads=[aqkT, vnew], writes=[B7])
                for h in range(H):
                    P.pe(lambda e, h=h, rows=rows: e.matmul(B6[:, h * 128:(h + 1) * 128], lhsT=kend[rows, h, :], rhs=vnew[rows, h, :], start=True, stop=True),
                         reads=[kend, vnew], writes=[B6])
                yield
                P.dve(lambda e, c=c: e.tensor_tensor(out=S[:], in0=S[:], in1=bc(e16[:, 8 + 4 * c:12 + 4 * c].unsqueeze(2), [128, H, 128]), op=ALU.mult),
                      reads=[S, e16], writes=[S])
                P.dve(lambda e: e.tensor_tensor(out=fl(S[:]), in0=fl(S[:]), in1=B6[:], op=ALU.add), reads=[S, B6], writes=[S])
                P.act(lambda e: e.copy(out=Sb[:], in_=S[:]), reads=[S], writes=[Sb])
            yield
            y = yo[t % 2]
            norm_gate(P, B7[:], [B7], tk[:, 0:512], [tk], bc(nwb[:].unsqueeze(1), [128, 4, 128]), [nwb], 4, 128, y, tmp, False)
            P.dma("sp", ymix.t[tok, 1024:1536], y[:], reads=[y], writes=[ymix])
            yield

        load(0)
        load(1)
        pre(0, lambda: None)
        for t in range(NT):
            if t + 2 < NT:
                load(t + 2)
            gen = scan(t)
            adv = (lambda gen=gen: next(gen, None))
            if t + 1 < NT:
                pre(t + 1, adv)
            for _ in gen:
                pass
            bg_tick(P, 2)
        bg_end(P)


NBIG = 30000.0


def t5_bucket_np(rel):
    n = np.maximum(rel, 0)
    exact = 16
    large = exact + (np.log(np.maximum(n, 1).astype(np.float32) / np.float32(exact)) / np.float32(math.log(128 / 16)) * np.float32(32 - exact)).astype(np.int32)
    return np.where(n < exact, n, np.minimum(large, 31)).astype(np.int64)


def nsa_host_tables(rel_bias):
    rb = np.asarray(rel_bias, np.float32)
    ki = np.arange(128)[:, None]
    qi = np.arange(128)[None, :]
    r0 = qi - ki
    r128 = 128 + qi - ki
    qq = np.arange(128)[:, None]
    mm = np.arange(248)[None, :]
    rc = qq - 16 * (mm - 120) - 31
    tab = np.concatenate([
        rb[t5_bucket_np(r0)].transpose(0, 2, 1).reshape(128, 8 * 128),
        rb[t5_bucket_np(r128)].transpose(0, 2, 1).reshape(128, 8 * 128),
        rb[t5_bucket_np(rc)].transpose(0, 2, 1).reshape(128, 8 * 248)], axis=1)
    t31 = np.broadcast_to(rb[31][None, :], (128, 8)).copy()
    return np.ascontiguousarray(tab, np.float32), np.ascontiguousarray(t31, np.float32)


def nsa_host_consts():
    ki = np.arange(128)[:, None]
    qi = np.arange(128)[None, :]
    qq = np.arange(128)[:, None]
    mm = np.arange(248)[None, :]
    rc = qq - 16 * (mm - 120) - 31
    m0 = np.where(qi - ki >= 0, 0.0, -NBIG)
    msk = np.concatenate([
        np.broadcast_to(m0[:, None, :], (128, 8, 128)).reshape(128, -1),
        np.zeros((128, 8 * 128)),
        np.broadcast_to(np.where(rc >= 0, 0.0, -NBIG)[:, None, :], (128, 8, 248)).reshape(128, -1)], axis=1)
    mtri = np.where(ki > qi, 0.0, -NBIG)
    k = np.arange(128)[:, None]
    j = np.arange(32)[None, :]
    ov = ((16 * k <= 64 * j + 63) & (16 * k + 31 >= 64 * j) & (k < 127)).astype(np.float32)
    keep = np.zeros((128, 16, 32)); addc = np.zeros((128, 16, 32))
    for qb in range(16):
        cur = (2 * qb + (np.arange(128) >= 64))[:, None]
        blk = np.arange(32)[None, :]
        forced = (blk == 0) | (blk == cur) | (blk == cur - 1)
        fut = blk > cur
        keep[:, qb, :] = (~forced & ~fut)
        addc[:, qb, :] = np.where(fut, -1e30, np.where(forced, 1e9, 0.0))
    E = np.zeros((128, 2048))
    E[:32] = (np.arange(2048)[None, :] // 64) == np.arange(32)[:, None]
    parts = dict(msk=msk, mtri=mtri, ov=ov, keep=keep.reshape(128, -1), addc=addc.reshape(128, -1), E=E)
    cols = {}
    o = 0
    arrs = []
    for n, a in parts.items():
        cols[n] = (o, a.shape[1]); o += a.shape[1]; arrs.append(a.astype(np.float32))
    return np.concatenate(arrs, axis=1), cols


NSA_CST_NP, NSA_CST_COLS = nsa_host_consts()


def stage_nsa(P, C, projT, projN, ymix, prm, l):
    G, R = 2, 4
    with P.scope():
        cf = [C.f]
        tmp = ng_tmp(P)
        one = tmp["one"]
        Bn0 = P.sbuf("nsa_Bn0", [128, 8, 128], BF16)
        Bn1 = P.sbuf("nsa_Bn1", [128, 8, 128], BF16)
        Mtri = P.sbuf("nsa_Mtri", [128, 4, 128], BF16)
        FT = P.sbuf("nsa_FT", [128, 8, 248])
        ovb = P.sbuf("nsa_ovb", [128, 32], BF16)
        keep = P.sbuf("nsa_keep", [128, 16, 32])
        addc = P.sbuf("nsa_addc", [128, 16, 32])
        Eb = P.sbuf("nsa_Eb", [32, 2048], BF16)
        ncst = prm["nsa_cst"]
        cc = NSA_CST_COLS
        P.dma("sp", keep[:].rearrange("p a b -> p (a b)"), ncst.t[:, cc["keep"][0]:cc["keep"][0] + 512], reads=[ncst], writes=[keep])
        P.dma("sp", addc[:].rearrange("p a b -> p (a b)"), ncst.t[:, cc["addc"][0]:cc["addc"][0] + 512], reads=[ncst], writes=[addc])
        with P.scope():
            tb = P.sbuf("nsa_tb", [128, 4032])
            mk = P.sbuf("nsa_mk", [128, 4032])
            t31 = P.sbuf("nsa_t31", [128, 8])
            st = P.sbuf("nsa_st", [128, 2048])
            P.dma("sp", tb[:], prm["nsa_tab"].t[:, :], reads=[prm["nsa_tab"]], writes=[tb])
            P.dma("sp", mk[:], ncst.t[:, cc["msk"][0]:cc["msk"][0] + 4032], reads=[ncst], writes=[mk])
            P.dma("sp", t31[:], prm["nsa_t31"].t[:, :], reads=[prm["nsa_t31"]], writes=[t31])
            for (o, w, dst) in ((0, 128, Bn0), (1024, 128, Bn1), (2048, 248, FT)):
                v = tb[:, o:o + 8 * w].rearrange("p (h x) -> p h x", h=8)
                P.dve(lambda e, v=v, w=w: e.tensor_tensor(out=v, in0=v, in1=bc(t31[:].unsqueeze(2), [128, 8, w]), op=ALU.subtract), reads=[tb, t31], writes=[tb])
                P.dve(lambda e, v=v, w=w, o=o, dst=dst: e.tensor_tensor(out=dst[:], in0=v, in1=mk[:, o:o + 8 * w].rearrange("p (h x) -> p h x", h=8), op=ALU.add),
                      reads=[tb, mk], writes=[dst])
            P.dma("sp", st[:, 0:128], ncst.t[:, cc["mtri"][0]:cc["mtri"][0] + 128], reads=[ncst], writes=[st])
            P.dve(lambda e: e.tensor_copy(out=Mtri[:], in_=bc(st[:, 0:128].unsqueeze(1), [128, 4, 128])), reads=[st], writes=[Mtri])
            P.dma("sp", st[:, 128:160], ncst.t[:, cc["ov"][0]:cc["ov"][0] + 32], reads=[ncst], writes=[st])
            P.dve(lambda e: e.tensor_copy(out=ovb[:], in_=st[:, 128:160]), reads=[st], writes=[ovb])
            P.dma("sp", st[0:32, :], ncst.t[0:32, cc["E"][0]:cc["E"][0] + 2048], reads=[ncst], writes=[st])
            P.dve(lambda e: e.tensor_copy(out=Eb[:], in_=st[0:32, :]), reads=[st], writes=[Eb])
        dbg(0.1)
        qTb = P.sbuf("nsa_qTb", [64, 8, SEQ], BF16)
        ksT = P.sbuf("nsa_ksT", [64, 2, SEQ], BF16)
        kwT = P.sbuf("nsa_kwT", [64, 2, SEQ], BF16)
        vsb = P.sbuf("nsa_vsb", [128, 16, 2, 65], BF16)
        vwb = P.sbuf("nsa_vwb", [128, 16, 2, 65], BF16)
        gts = P.sbuf("nsa_gts", [128, 16, 24])
        kcT = P.sbuf("nsa_kcT", [64, 2, 128], BF16)
        vcx = P.sbuf("nsa_vcx", [128, 2, 96], BF16)
        with P.scope():
            stg = [P.sbuf(f"nsa_stg{i}", [64, SEQ]) for i in range(2)]
            n = 0
            for h in range(8):
                s_ = stg[n % 2]; n += 1
                P.dma("sp", s_[:], projT.t[PT_NQ + h * 64:PT_NQ + (h + 1) * 64, :], reads=[projT], writes=[s_])
                P.act(lambda e, h=h, s_=s_: e.activation(out=qTb[:, h, :], in_=s_[:], func=AF.Copy, scale=0.125), reads=[s_], writes=[qTb])
            for (r0, dst) in ((PT_NKS, ksT), (PT_NKW, kwT)):
                for g in range(2):
                    s_ = stg[n % 2]; n += 1
                    P.dma("sp", s_[:], projT.t[r0 + g * 64:r0 + (g + 1) * 64, :], reads=[projT], writes=[s_])
                    P.dve(lambda e, g=g, s_=s_, dst=dst: e.tensor_copy(out=dst[:, g, :], in_=s_[:]), reads=[s_], writes=[dst])
            tn = P.sbuf("nsa_tn", [128, 16, 280])
            P.dma("sp", tn[:], projN.t[:, 0:280].rearrange("(t p) c -> p t c", p=128), reads=[projN], writes=[tn])
            P.pool(lambda e: e.memset(vsb[:], 1.0), writes=[vsb])
            P.pool(lambda e: e.memset(vwb[:], 1.0), writes=[vwb])
            P.dve(lambda e: e.tensor_copy(out=vsb[:, :, :, 0:64], in_=tn[:, :, 0:128].rearrange("p t (g d) -> p t g d", g=2)), reads=[tn], writes=[vsb])
            P.dve(lambda e: e.tensor_copy(out=vwb[:, :, :, 0:64], in_=tn[:, :, 128:256].rearrange("p t (g d) -> p t g d", g=2)), reads=[tn], writes=[vwb])
            P.act(lambda e: e.activation(out=gts[:], in_=tn[:, :, 256:280], func=AF.Sigmoid), reads=[tn], writes=[gts])
            dbg(0.2)
            tT = P.sbuf("nsa_tT", [64, 2, SEQ])
            tG = P.sbuf("nsa_tG", [64, 2, 16, 129], BF16)
            P.pool(lambda e: e.memset(tG[:], 0.0), writes=[tG])
            w1f = P.sbuf("nsa_w1f", [64, 32, 64])
            w1b = P.sbuf("nsa_w1b", [64, 32, 64], BF16)
            w2f = P.sbuf("nsa_w2f", [64, 64])
            w2b = P.sbuf("nsa_w2b", [64, 64], BF16)
            posT = P.sbuf("nsa_posT", [64, 32])
            posb = P.sbuf("nsa_posb", [64, 32], BF16)
            cvec = P.sbuf("nsa_cvec", [64, 1])
            hid = P.sbuf("nsa_hid", [64, 2, 128], BF16)
            ps_h = P.psum("nsa_ps_h", [64, 512])
            ps_c = P.psum("nsa_ps_c", [64, 8])
            ps_o = P.psum("nsa_ps_o", [128, 512])
            P.dve(lambda e: e.memset(hid[:], 0.0), writes=[hid])
            for kv in range(2):
                r0 = PT_NKC if kv == 0 else PT_NVC
                for g in range(2):
                    P.dma("sp", tT[:, g, :], projT.t[r0 + g * 64:r0 + (g + 1) * 64, :], reads=[projT], writes=[tT])
                    P.dve(lambda e, g=g: e.tensor_copy(out=tG[:, g, :, 0:128], in_=tT[:, g, :].rearrange("p (n s) -> p s n", s=16)), reads=[tT], writes=[tG])
                w1src = prm["nsa_cmp_w1"].t[l, kv].rearrange("(j d) o -> d j o", d=64)
                P.dma("sp", w1f[:], w1src, reads=[prm["nsa_cmp_w1"]], writes=[w1f])
                P.pool(lambda e: e.tensor_copy(out=w1b[:], in_=w1f[:]), reads=[w1f], writes=[w1b])
                P.dma("sp", w2f[:], prm["nsa_cmp_w2"].t[l, kv], reads=[prm["nsa_cmp_w2"]], writes=[w2f])
                P.dve(lambda e: e.tensor_copy(out=w2b[:], in_=w2f[:]), reads=[w2f], writes=[w2b])
                P.dma("sp", posT[:], prm["nsa_cmp_pos"].t[l, kv], reads=[prm["nsa_cmp_pos"]], writes=[posT])
                P.dve(lambda e: e.tensor_copy(out=posb[:], in_=posT[:]), reads=[posT], writes=[posb])
                for j in range(32):
                    P.pe(lambda e, j=j: e.matmul(ps_c[:, 0:1], lhsT=w1b[:, j, :], rhs=posb[:, j:j + 1], start=(j == 0), stop=(j == 31)), reads=[w1b, posb], writes=[ps_c])
                P.dve(lambda e: e.tensor_copy(out=cvec[:], in_=ps_c[:, 0:1]), reads=[ps_c], writes=[cvec])
                dbg(0.3 + 0.3 * kv)
                for g in range(2):
                    rows = slice(g * 64, (g + 1) * 64)
                    dbg(0.32 + 0.03 * g + 0.3 * kv)
                    for j in range(32):
                        P.pe(lambda e, j=j, g=g, rows=rows: e.matmul(ps_h[:, g * 128:(g + 1) * 128], lhsT=w1b[:, j, :], rhs=tG[:, g, j % 16, (j // 16):(j // 16) + 128],
                                                                    start=(j == 0), stop=(j == 31)), reads=[w1b, tG], writes=[ps_h])
                for g in range(2):
                    P.act(lambda e, g=g: e.activation(out=hid[:, g, :], in_=ps_h[:, g * 128:(g + 1) * 128], func=AF.Silu, bias=cvec[:, 0:1]),
                          reads=[ps_h, cvec], writes=[hid])
                dbg(0.4 + 0.3 * kv)
                if kv == 0:
                    P.pe(lambda e: e.matmul(ps_h[:, 256:512], lhsT=w2b[:], rhs=hid[:].rearrange("o g n -> o (g n)"), start=True, stop=True), reads=[w2b, hid], writes=[ps_h])
                    P.dve(lambda e: e.tensor_copy(out=kcT[:].rearrange("o g n -> o (g n)"), in_=ps_h[:, 256:512]), reads=[ps_h], writes=[kcT])
                else:
                    for g in range(2):
                        P.pe(lambda e, g=g: e.matmul(ps_o[:, g * 64:(g + 1) * 64], lhsT=hid[:, g, :], rhs=w2b[:], start=True, stop=True), reads=[w2b, hid], writes=[ps_o])
                    P.dve(lambda e: e.tensor_copy(out=vcx[:, :, 0:64], in_=ps_o[:, 0:128].rearrange("p (g d) -> p g d", g=2)), reads=[ps_o], writes=[vcx])
                    P.dve(lambda e: e.tensor_copy(out=vcx[:, :, 64:96], in_=bc(ovb[:].unsqueeze(1), [128, 2, 32])), reads=[ovb], writes=[vcx])
        dbg(1)
        ps_sc = [P.psum(f"nsa_ps_sc{i}", [128, 512]) for i in range(2)]
        ps_os = P.psum("nsa_ps_os", [128, 512])
        ps_ow = P.psum("nsa_ps_ow", [128, 512])
        ps_ocs = [P.psum(f"nsa_ps_oc{i}", [128, 512]) for i in range(2)]
        ps_cs = P.psum("nsa_ps_cs", [128, 512])
        ps_tr = P.psum("nsa_ps_tr", [128, 1024], BF16)
        sc = P.sbuf("nsa_sc", [128, 4, 128])
        ssum = P.sbuf("nsa_ssum", [128, 4])
        pnb = P.sbuf("nsa_pnb", [128, 4, 128], BF16)
        pT = P.sbuf("nsa_pT", [128, 4, 128], BF16)
        imp = P.sbuf("nsa_imp", [128, 32])
        mx8 = P.sbuf("nsa_mx8", [128, 8])
        negm = P.sbuf("nsa_negm", [128, 32], BF16)
        negTs = [P.sbuf(f"nsa_negT{i}", [32, 4, 128], BF16) for i in range(2)]
        eT = [P.sbuf(f"nsa_eT{i}", [128, 4, 128], BF16) for i in range(2)]
        cs = P.sbuf("nsa_cs", [128, 3, 4])
        ya = P.sbuf("nsa_ya", [128, 4, 64])
        yb = P.sbuf("nsa_yb", [128, 4, 64])
        yt = [P.sbuf(f"nsa_yt{i}", [128, 512]) for i in range(2)]
        nsc = 0
        bg_begin(P)

        def v3(ap, r=4):
            return ap.rearrange("p (r q) -> p r q", r=r)

        def phaseA(qb, g, slot):
            qtok = slice(qb * 128, (qb + 1) * 128)
            hs = slice(4 * g, 4 * g + 4)
            ps_oc_s = ps_ocs[slot]
            negT_s = negTs[slot]
            for r in range(R):
                P.pe(lambda e, r=r: e.matmul(ps_cs[:, r * 128:(r + 1) * 128], lhsT=qTb[:, 4 * g + r, qtok], rhs=kcT[:, g, :], start=True, stop=True),
                     reads=[qTb, kcT], writes=[ps_cs])
            m0 = 120 - 8 * qb
            P.dve(lambda e: e.tensor_tensor(out=sc[:], in0=v3(ps_cs[:]), in1=FT[:, hs, m0:m0 + 128], op=ALU.add), reads=[ps_cs, FT], writes=[sc])
            P.act(lambda e: e.activation(out=sc[:], in_=sc[:], func=AF.Exp), reads=[sc], writes=[sc])
            P.dve(lambda e: e.tensor_reduce(out=ssum[:], in_=sc[:], axis=AX.X, op=ALU.add), reads=[sc], writes=[ssum])
            P.dve(lambda e: e.tensor_scalar(out=ssum[:], in0=ssum[:], scalar1=1e-30, scalar2=None, op0=ALU.max), reads=[ssum], writes=[ssum])
            P.dve(lambda e: e.reciprocal(out=ssum[:], in_=ssum[:]), reads=[ssum], writes=[ssum])
            P.dve(lambda e: e.tensor_tensor(out=pnb[:], in0=sc[:], in1=bc(ssum[:].unsqueeze(2), [128, 4, 128]), op=ALU.mult), reads=[sc, ssum], writes=[pnb])
            yield
            for r in range(R):
                P.pe(lambda e, r=r: e.transpose(out=ps_tr[:, r * 128:(r + 1) * 128], in_=pnb[:, r, :], identity=C.identb[:]), reads=[pnb, C.identb], writes=[ps_tr])
            P.act(lambda e: e.copy(out=pT[:].rearrange("p r q -> p (r q)"), in_=ps_tr[:, 0:512]), reads=[ps_tr], writes=[pT])
            yield
            for r in range(R):
                P.pe(lambda e, r=r: e.matmul(ps_oc_s[:, r * 96:(r + 1) * 96], lhsT=pT[:, r, :], rhs=vcx[:, g, :], start=True, stop=True), reads=[pT, vcx], writes=[ps_oc_s])
            oc4 = ps_oc_s[:, 0:384].rearrange("p (r x) -> p r x", r=4)
            P.dve(lambda e: e.tensor_reduce(out=imp[:], in_=oc4[:, :, 64:96].rearrange("p r j -> p j r"), axis=AX.X, op=ALU.add), reads=[ps_oc_s], writes=[imp])
            P.dve(lambda e: e.tensor_tensor(out=imp[:], in0=imp[:], in1=keep[:, qb, :], op=ALU.mult), reads=[imp, keep], writes=[imp])
            P.dve(lambda e: e.tensor_tensor(out=imp[:], in0=imp[:], in1=addc[:, qb, :], op=ALU.add), reads=[imp, addc], writes=[imp])
            P.dve(lambda e: e.max(out=mx8[:], in_=imp[:]), reads=[imp], writes=[mx8])
            P.dve(lambda e: e.tensor_scalar(out=imp[:], in0=imp[:], scalar1=mx8[:, 7:8], scalar2=None, op0=ALU.is_ge), reads=[imp, mx8], writes=[imp])
            P.dve(lambda e: e.tensor_scalar(out=negm[:], in0=imp[:], scalar1=-1.0, scalar2=NBIG, op0=ALU.add, op1=ALU.mult), reads=[imp], writes=[negm])
            yield
            P.pe(lambda e: e.transpose(out=ps_tr[0:32, 512:640], in_=negm[:], identity=C.identb[:]), reads=[negm, C.identb], writes=[ps_tr])
            P.dve(lambda e: e.tensor_copy(out=negT_s[:], in_=bc(ps_tr[0:32, 512:640].unsqueeze(1), [32, 4, 128])), reads=[ps_tr], writes=[negT_s])

        def phaseB(qb, g, slot, gen):
            nonlocal nsc
            it = 0
            qtok = slice(qb * 128, (qb + 1) * 128)
            hs = slice(4 * g, 4 * g + 4)
            y = yt[qb % 2]
            ps_oc_s = ps_ocs[slot]
            negT_s = negTs[slot]
            oc4 = ps_oc_s[:, 0:384].rearrange("p (r x) -> p r x", r=4)
            for kt in range(qb + 1):
                ps = ps_sc[nsc % 2]; et = eT[nsc % 2]; nsc += 1
                ktok = slice(kt * 128, (kt + 1) * 128)
                near = kt >= qb - 1
                P.pe(lambda e, ps=ps, ktok=ktok: e.matmul(v3(ps[:]), lhsT=ksT[:, g, ktok], rhs=qTb[:, hs, qtok], start=True, stop=False), reads=[ksT, qTb], writes=[ps])
                P.pe(lambda e, ps=ps, ktok=ktok, near=near: e.matmul(v3(ps[:]), lhsT=Eb[:, ktok], rhs=negT_s[:], start=False, stop=not near), reads=[Eb, negT_s], writes=[ps])
                if near:
                    Bn = Bn0 if kt == qb else Bn1
                    P.pe(lambda e, ps=ps, Bn=Bn: e.matmul(v3(ps[:]), lhsT=C.identb[:], rhs=Bn[:, hs, :], start=False, stop=True), reads=[Bn, C.identb], writes=[ps])
                P.act(lambda e, ps=ps, et=et: e.activation(out=et[:].rearrange("p r q -> p (r q)"), in_=ps[:], func=AF.Exp), reads=[ps], writes=[et])
                for r in range(R):
                    P.pe(lambda e, r=r, et=et, kt=kt: e.matmul(ps_os[:, r * 65:(r + 1) * 65], lhsT=et[:, r, :], rhs=vsb[:, kt, g, :], start=(kt == 0 and r == 0), stop=(kt == qb), skip_group_check=True),
                         reads=[et, vsb], writes=[ps_os])
                it += 1
                if it % 3 == 2 and gen is not None:
                    next(gen, None)
            kt0 = max(0, qb - 4)
            for kt in range(kt0, qb + 1):
                ps = ps_sc[nsc % 2]; et = eT[nsc % 2]; nsc += 1
                ktok = slice(kt * 128, (kt + 1) * 128)
                dl = qb - kt
                extra = {0: Bn0[:, hs, :], 1: Bn1[:, hs, :], 4: Mtri[:]}.get(dl)
                P.pe(lambda e, ps=ps, ktok=ktok, extra=extra: e.matmul(v3(ps[:]), lhsT=kwT[:, g, ktok], rhs=qTb[:, hs, qtok], start=True, stop=extra is None),
                     reads=[kwT, qTb], writes=[ps])
                if extra is not None:
                    P.pe(lambda e, ps=ps, extra=extra: e.matmul(v3(ps[:]), lhsT=C.identb[:], rhs=extra, start=False, stop=True), reads=[Bn0, Bn1, Mtri, C.identb], writes=[ps])
                P.act(lambda e, ps=ps, et=et: e.activation(out=et[:].rearrange("p r q -> p (r q)"), in_=ps[:], func=AF.Exp), reads=[ps], writes=[et])
                for r in range(R):
                    P.pe(lambda e, r=r, et=et, kt=kt: e.matmul(ps_ow[:, r * 65:(r + 1) * 65], lhsT=et[:, r, :], rhs=vwb[:, kt, g, :], start=(kt == kt0 and r == 0), stop=(kt == qb), skip_group_check=True),
                         reads=[et, vwb], writes=[ps_ow])
                it += 1
                if it % 3 == 2 and gen is not None:
                    next(gen, None)
            if gen is not None:
                for _ in gen:
                    pass
            os4 = ps_os[:, 0:260].rearrange("p (r x) -> p r x", r=4)
            ow4 = ps_ow[:, 0:260].rearrange("p (r x) -> p r x", r=4)
            g3 = gts[:, qb, 12 * g:12 * g + 12].rearrange("p (r b) -> p b r", b=3)
            P.dve(lambda e: e.reciprocal(out=cs[:, 1, :], in_=os4[:, :, 64]), reads=[ps_os], writes=[cs])
            P.dve(lambda e: e.reciprocal(out=cs[:, 2, :], in_=ow4[:, :, 64]), reads=[ps_ow], writes=[cs])
            P.dve(lambda e: e.memset(cs[:, 0, :], 1.0), writes=[cs])
            P.dve(lambda e: e.tensor_tensor(out=cs[:], in0=cs[:], in1=g3, op=ALU.mult), reads=[cs, gts], writes=[cs])
            P.dve(lambda e: e.tensor_tensor(out=ya[:], in0=oc4[:, :, 0:64], in1=bc(cs[:, 0, :].unsqueeze(2), [128, 4, 64]), op=ALU.mult), reads=[ps_oc_s, cs], writes=[ya])
            P.dve(lambda e: e.tensor_tensor(out=yb[:], in0=os4[:, :, 0:64], in1=bc(cs[:, 1, :].unsqueeze(2), [128, 4, 64]), op=ALU.mult), reads=[ps_os, cs], writes=[yb])
            P.pool(lambda e: e.tensor_tensor(out=ya[:], in0=ya[:], in1=yb[:], op=ALU.add), reads=[ya, yb], writes=[ya])
            P.dve(lambda e: e.tensor_tensor(out=yb[:], in0=ow4[:, :, 0:64], in1=bc(cs[:, 2, :].unsqueeze(2), [128, 4, 64]), op=ALU.mult), reads=[ps_ow, cs], writes=[yb])
            P.pool(lambda e: e.tensor_tensor(out=y[:, g * 256:(g + 1) * 256].rearrange("p (r d) -> p r d", r=4), in0=ya[:], in1=yb[:], op=ALU.add), reads=[ya, yb], writes=[y])

        blocks = [(qb, g) for qb in range(NT) for g in range(G)]
        for _ in phaseA(blocks[0][0], blocks[0][1], 0):
            pass
        for bi, (qb, g) in enumerate(blocks):
            gen = phaseA(blocks[bi + 1][0], blocks[bi + 1][1], (bi + 1) % 2) if bi + 1 < len(blocks) else None
            if gen is not None:
                next(gen, None)
            phaseB(qb, g, bi % 2, gen)
            if g == G - 1:
                qtok = slice(qb * 128, (qb + 1) * 128)
                y = yt[qb % 2]
                P.dma("sp", ymix.t[qtok, 0:512], y[:], reads=[y], writes=[ymix])
                bg_tick(P, 4)
                dbg(2 + qb)
        bg_end(P)


WIN_OFF = {}
_o = 0
for (_c0, _n, _r0) in PT_GROUPS:
    WIN_OFF[("T", _c0)] = _o; _o += 16 * _n
for (_c0, _n, _r0) in PN_GROUPS:
    WIN_OFF[("N", _c0)] = _o; _o += 16 * _n
WIN_TOTAL = _o
WOUT_TOTAL = 4 * 16 * 512
W1_TOTAL = 32 * 16 * 256
W2_TOTAL = 4 * 64 * 512


class Background:
    def __init__(self, P, prm, l, wsc):
        self.P = P
        self.jobs = []
        self.loaded = self.cast = self.stored = 0
        self.bufs = None

        def add(src3, srcbuf, nk, nc_, dst, off):
            if nk * nc_ > 4096:
                h = nk // 2
                add(src3[:, 0:h, :], srcbuf, h, nc_, dst, off)
                add(src3[:, h:nk, :], srcbuf, nk - h, nc_, dst, off + h * nc_)
            else:
                self.jobs.append((src3, srcbuf, nk, nc_, dst, off))

        wv = prm["w_in"].t[l].rearrange("(k p) n -> p k n", p=128)
        for (c0, nc_, r0) in PT_GROUPS:
            add(wv[:, :, c0:c0 + nc_], prm["w_in"], 16, nc_, wsc["win"], WIN_OFF[("T", c0)])
        for (c0, nc_, o0) in PN_GROUPS:
            add(wv[:, :, c0:c0 + nc_], prm["w_in"], 16, nc_, wsc["win"], WIN_OFF[("N", c0)])
        wv = prm["w_out"].t[l].rearrange("(k p) n -> p k n", p=128)
        for ct in range(4):
            add(wv[:, :, ct * 512:(ct + 1) * 512], prm["w_out"], 16, 512, wsc["wout"], ct * 8192)
        wv = prm["mlp_w1"].t[l].rearrange("(k p) n -> p k n", p=128)
        for hp in range(32):
            add(wv[:, :, hp * 256:(hp + 1) * 256], prm["mlp_w1"], 16, 256, wsc["w1"], hp * 4096)
        wv = prm["mlp_w2"].t[l].rearrange("(k p) n -> p k n", p=128)
        for ct in range(4):
            for kg in range(8):
                add(wv[:, kg * 8:(kg + 1) * 8, ct * 512:(ct + 1) * 512], prm["mlp_w2"], 8, 512, wsc["w2"], (ct * 8 + kg) * 4096)

    def alloc(self):
        P = self.P
        self.bufs = dict(f=[P.sbuf(f"bg_f{i}", [128, 4096]) for i in range(2)], b=[P.sbuf(f"bg_b{i}", [128, 4096], BF16) for i in range(2)])

    def done(self):
        return self.stored >= len(self.jobs)

    def step(self, load=True):
        if self.bufs is None:
            return
        P = self.P
        f, b_ = self.bufs["f"], self.bufs["b"]
        if self.stored < self.cast:
            j = self.stored
            src3, srcbuf, nk, nc_, dst, off = self.jobs[j]
            tot = nk * nc_
            P.dma("sp", dst.t[:, off:off + tot], b_[j % 2][:, 0:tot], reads=[b_[j % 2]], writes=[dst])
            self.stored += 1
        if self.cast < self.loaded:
            j = self.cast
            tot = self.jobs[j][2] * self.jobs[j][3]
            P.pool(lambda e: e.tensor_copy(out=b_[j % 2][:, 0:tot], in_=f[j % 2][:, 0:tot]), reads=[f[j % 2]], writes=[b_[j % 2]])
            self.cast += 1
        if load and self.loaded < len(self.jobs):
            j = self.loaded
            src3, srcbuf, nk, nc_, dst, off = self.jobs[j]
            P.dma("sp", f[j % 2][:, 0:nk * nc_].rearrange("p (k c) -> p k c", k=nk), src3, reads=[srcbuf], writes=[f[j % 2]])
            self.loaded += 1

    def flush(self):
        while self.stored < self.loaded:
            self.step(load=False)

    def release(self):
        self.flush()
        self.bufs = None


def bg_begin(P):
    if getattr(P, "bg", None) is not None and not P.bg.done():
        P.bg.alloc()


def bg_tick(P, n=1):
    if getattr(P, "bg", None) is not None:
        for _ in range(n):
            P.bg.step()


def bg_end(P):
    if getattr(P, "bg", None) is not None and P.bg.bufs is not None:
        P.bg.release()


def stage_convert_all(P, bg):
    if bg is None or bg.done():
        return
    with P.scope():
        bg.alloc()
        while bg.loaded < len(bg.jobs):
            bg.step()
        bg.release()


def stage_mod(P, C, cT, ada_w, ada_b, modT, gsc, nlayers):
    with P.scope():
        cf = [C.f]
        ca = P.sbuf("mod_ca", [128, 16, BPC])
        P.dma("sp", ca[:], cT.t[:, :, :], reads=[cT], writes=[ca])
        P.act(lambda e: e.activation(out=ca[:], in_=ca[:], func=AF.Silu), reads=[ca], writes=[ca])
        wst = [P.sbuf(f"mod_w{i}", [128, 16, 512]) for i in range(2)]
        brow = [P.sbuf(f"mod_b{i}", [1, 512]) for i in range(2)]
        grow = [P.sbuf(f"mod_g{i}", [BPC, 512]) for i in range(2)]
        ps_f = P.psum("mod_psf", [128, 512])
        ps_g = [P.psum(f"mod_psg{i}", [BPC, 512]) for i in range(2)]
        bg_begin(P)
        n = 0
        for l in range(nlayers):
            wv = ada_w.t[l].rearrange("(k p) n -> p k n", p=128)
            for seg in range(6):
                for ct in range(4):
                    w = wst[n % 2]; br = brow[n % 2]
                    c0 = seg * 2048 + ct * 512
                    P.dma("sp", w[:], wv[:, :, c0:c0 + 512], reads=[ada_w], writes=[w])
                    P.dma("sp", br[:], ada_b.t[l:l + 1, c0:c0 + 512], reads=[ada_b], writes=[br])
                    if seg in (2, 5):
                        pg = ps_g[n % 2]; gr = grow[n % 2]
                        for k in range(16):
                            P.pe(lambda e, k=k, w=w, pg=pg: e.matmul(pg[:], lhsT=ca[:, k, :], rhs=w[:, k, :], start=(k == 0), stop=False), reads=[ca, w], writes=[pg])
                        P.pe(lambda e, br=br, pg=pg: e.matmul(pg[:], lhsT=C.c("ones", 1)[:, 0:BPC], rhs=br[:], start=False, stop=True), reads=[br] + cf, writes=[pg])
                        P.act(lambda e, pg=pg, gr=gr: e.copy(out=gr[:], in_=pg[:]), reads=[pg], writes=[gr])
                        P.dma("sp", gsc.t[l, 0 if seg == 2 else 1, :, ct * 512:(ct + 1) * 512], gr[:], reads=[gr], writes=[gsc])
                    else:
                        si = {0: 0, 1: 1, 3: 2, 4: 3}[seg]
                        for cc in range(4):
                            col = ((si * 16) + ct * 4 + cc) * BPC
                            for k in range(16):
                                P.pe(lambda e, k=k, w=w, cc=cc, col=col: e.matmul(ps_f[:, col:col + BPC], lhsT=w[:, k, cc * 128:(cc + 1) * 128], rhs=ca[:, k, :],
                                                                                 start=(k == 0), stop=False), reads=[ca, w], writes=[ps_f])
                            P.pe(lambda e, br=br, cc=cc, col=col: e.matmul(ps_f[:, col:col + BPC], lhsT=br[:, cc * 128:(cc + 1) * 128], rhs=C.c("ones", 1)[:, 0:BPC],
                                                                          start=False, stop=True), reads=[br] + cf, writes=[ps_f])
                    n += 1
                    bg_tick(P, 2)
            P.dve(lambda e, l=l: e.tensor_copy(out=modT[:, l].rearrange("p s k b -> p (s k b)"), in_=ps_f[:, 0:4 * 16 * BPC]), reads=[ps_f], writes=[modT])
        bg_end(P)


def to_featmajor(P, C, src, src_ap_fn, ntt, hT, norm, scl=None, shf=None, pools=None):
    xt, xb, ss, ps_tr, eps = pools["xt"], pools["xb"], pools["ss"], pools["ps_tr"], pools["eps"]

    def prep(tt):
        x = xt[tt % 2]; xn = xb[tt % 2]; s1 = ss[tt % 2]
        P.dma("sp", x[:], src_ap_fn(tt), reads=[src], writes=[x])
        if norm:
            P.pool(lambda e: e.memset(s1[:], 0.0), writes=[s1])
            P.act(lambda e: e.activation(out=xn[:], in_=x[:], func=AF.Square, accum_out=s1[:, 0:1]), reads=[x, s1], writes=[xn, s1])
            P.act(lambda e: e.activation(out=s1[:, 0:1], in_=s1[:, 0:1], func=AF.Sqrt, scale=1.0 / D_MODEL, bias=eps[:, 0:1]), reads=[s1, eps], writes=[s1])
            P.dve(lambda e: e.reciprocal(out=s1[:, 0:1], in_=s1[:, 0:1]), reads=[s1], writes=[s1])
            P.dve(lambda e: e.tensor_scalar(out=xn[:], in0=x[:], scalar1=s1[:, 0:1], scalar2=None, op0=ALU.mult), reads=[x, s1], writes=[xn])
        else:
            P.pool(lambda e: e.tensor_copy(out=xn[:], in_=x[:]), reads=[x], writes=[xn])

    def trans(tt):
        xn = xb[tt % 2]
        for half in range(2):
            pt = ps_tr[(2 * tt + half) % len(ps_tr)]
            for kk in range(8):
                k = half * 8 + kk
                P.pe(lambda e, k=k, kk=kk: e.transpose(out=pt[:, kk * 128:(kk + 1) * 128], in_=xn[:, k * 128:(k + 1) * 128], identity=C.identb[:]),
                     reads=[xn, C.identb], writes=[pt])
            dst = hT[:, half * 8:half * 8 + 8, tt * 128:(tt + 1) * 128]
            src3 = pt[:].rearrange("p (k t) -> p k t", k=8)
            if scl is None:
                P.act(lambda e: e.copy(out=dst, in_=src3), reads=[pt], writes=[hT])
            else:
                tm = pools["tm"][(2 * tt + half) % 2]
                P.dve(lambda e: e.tensor_tensor(out=tm[:], in0=src3, in1=bc(scl[:, half * 8:half * 8 + 8].unsqueeze(2), [128, 8, 128]), op=ALU.mult),
                      reads=[pt] + pools["affb"], writes=[tm])
                P.pool(lambda e: e.tensor_tensor(out=dst, in0=tm[:], in1=bc(shf[:, half * 8:half * 8 + 8].unsqueeze(2), [128, 8, 128]), op=ALU.add),
                       reads=[tm] + pools["affb"], writes=[hT])

    prep(0)
    for tt in range(ntt):
        if tt + 1 < ntt:
            prep(tt + 1)
        trans(tt)


def fm_pools(P, affine):
    d = dict(xt=[P.sbuf(f"fm_xt{i}", [128, D_MODEL]) for i in range(2)],
             xb=[P.sbuf(f"fm_xb{i}", [128, D_MODEL], BF16) for i in range(2)],
             ss=[P.sbuf(f"fm_ss{i}", [128, 1]) for i in range(2)],
             ps_tr=[P.psum(f"fm_pst{i}", [128, 1024], BF16) for i in range(2)],
             eps=P.sbuf("fm_eps", [128, 1]))
    P.pool(lambda e: e.memset(d["eps"][:], EPS), writes=[d["eps"]])
    if affine:
        d["tm"] = [P.sbuf(f"fm_tm{i}", [128, 8, 128]) for i in range(2)]
    return d


def affine_vecs(P, modT, l, b, which, nw, scl, shf):
    P.dve(lambda e: e.tensor_scalar(out=scl[:], in0=modT[:, l, 2 * which + 1, :, b], scalar1=1.0, scalar2=None, op0=ALU.add), reads=[modT], writes=[scl])
    P.dve(lambda e: e.tensor_tensor(out=scl[:], in0=scl[:], in1=nw, op=ALU.mult), reads=[scl], writes=[scl])
    P.dve(lambda e: e.tensor_copy(out=shf[:], in_=modT[:, l, 2 * which, :, b]), reads=[modT], writes=[shf])


def stage_inproj(P, C, xsrc, xsrc_fn, modT, nw1T, win, projT, projN, l, b):
    with P.scope():
        hT = P.sbuf("ip_hT", [128, 16, SEQ], BF16)
        scl = P.sbuf("ip_scl", [128, 16]); shf = P.sbuf("ip_shf", [128, 16])
        affine_vecs(P, modT, l, b, 0, nw1T[:, l, :], scl, shf)
        with P.scope():
            pools = fm_pools(P, True)
            pools["affb"] = [scl, shf]
            to_featmajor(P, C, xsrc, xsrc_fn, NT, hT, True, scl[:], shf[:], pools)
        NWB = 3
        wb = [P.sbuf(f"ip_wb{i}", [128, 16, 512], BF16) for i in range(NWB)]
        ev = [P.sbuf(f"ip_ev{i}", [128, 2048]) for i in range(2)]
        ps = [P.psum(f"ip_ps{i}", [128, 512]) for i in range(4)]
        groups = [("T",) + g for g in PT_GROUPS] + [("N",) + g for g in PN_GROUPS]

        def wload(i):
            kind, c0, nc_, _ = groups[i]
            off = WIN_OFF[(kind, c0)]
            wbb = wb[i % NWB]
            P.dma("sp", wbb[:, :, 0:nc_], win.t[:, off:off + 16 * nc_].rearrange("p (k c) -> p k c", k=16), reads=[win], writes=[wbb])

        npp = 0
        nev = 0
        wload(0)
        wload(1)
        for gi, (kind, c0, nc_, dst0) in enumerate(groups):
            if gi + 2 < len(groups):
                wload(gi + 2)
            wbb = wb[gi % NWB]
            if kind == "T":
                e_ = ev[nev % 2]; nev += 1
                for tq in range(4):
                    p_ = ps[npp % 4]; npp += 1
                    for k in range(16):
                        P.pe(lambda e, k=k, p_=p_, wbb=wbb, nc_=nc_, tq=tq: e.matmul(p_[0:nc_, :], lhsT=wbb[:, k, 0:nc_], rhs=hT[:, k, tq * 512:(tq + 1) * 512], start=(k == 0), stop=(k == 15)),
                             reads=[wbb, hT], writes=[p_])
                    if tq % 2 == 0:
                        P.act(lambda e, p_=p_, e_=e_, nc_=nc_, tq=tq: e.copy(out=e_[0:nc_, tq * 512:(tq + 1) * 512], in_=p_[0:nc_, :]), reads=[p_], writes=[e_])
                    else:
                        P.dve(lambda e, p_=p_, e_=e_, nc_=nc_, tq=tq: e.tensor_copy(out=e_[0:nc_, tq * 512:(tq + 1) * 512], in_=p_[0:nc_, :]), reads=[p_], writes=[e_])
                P.dma("sp", projT.t[dst0:dst0 + nc_, :], e_[0:nc_, :], reads=[e_], writes=[projT])
            else:
                for t4 in range(4):
                    e_ = ev[nev % 2]; nev += 1
                    for ti in range(4):
                        tt = t4 * 4 + ti
                        p_ = ps[npp % 4]; npp += 1
                        for k in range(16):
                            P.pe(lambda e, k=k, p_=p_, wbb=wbb, nc_=nc_, tt=tt: e.matmul(p_[:, 0:nc_], lhsT=hT[:, k, tt * 128:(tt + 1) * 128], rhs=wbb[:, k, 0:nc_], start=(k == 0), stop=(k == 15)),
                                 reads=[wbb, hT], writes=[p_])
                        if ti % 2 == 0:
                            P.act(lambda e, p_=p_, e_=e_, nc_=nc_, ti=ti: e.copy(out=e_[:, ti * 512:ti * 512 + nc_], in_=p_[:, 0:nc_]), reads=[p_], writes=[e_])
                        else:
                            P.dve(lambda e, p_=p_, e_=e_, nc_=nc_, ti=ti: e.tensor_copy(out=e_[:, ti * 512:ti * 512 + nc_], in_=p_[:, 0:nc_]), reads=[p_], writes=[e_])
                    P.dma("sp", projN.t[t4 * 512:(t4 + 1) * 512, dst0:dst0 + nc_].rearrange("(i p) c -> p i c", p=128),
                          e_[:].rearrange("p (i c) -> p i c", i=4)[:, :, 0:nc_], reads=[e_], writes=[projN])


def stage_outproj(P, C, ymix, xsrc, xsrc_fn, xdst, xdst_fn, gsc, wout, l, b):
    with P.scope():
        yT = P.sbuf("op_yT", [128, 16, SEQ], BF16)
        with P.scope():
            pools = fm_pools(P, False)
            to_featmajor(P, C, ymix, lambda tt: ymix.t[tt * 128:(tt + 1) * 128, :], NT, yT, False, None, None, pools)
        wob = P.sbuf("op_wob", [128, 16, D_MODEL], BF16)
        gb = P.sbuf("op_gb", [128, D_MODEL])
        P.dma("sp", gb[:], gsc.t[l, 0, b:b + 1, :].partition_broadcast(128), reads=[gsc], writes=[gb])
        for ct in range(4):
            P.dma("sp", wob[:, :, ct * 512:(ct + 1) * 512], wout.t[:, ct * 8192:(ct + 1) * 8192].rearrange("p (k c) -> p k c", k=16), reads=[wout], writes=[wob])
        xt = [P.sbuf(f"op_xt{i}", [128, D_MODEL]) for i in range(2)]
        xo = [P.sbuf(f"op_xo{i}", [128, D_MODEL]) for i in range(2)]
        ps = [P.psum(f"op_ps{i}", [128, 512]) for i in range(8)]
        for tt in range(NT):
            x = xt[tt % 2]; o = xo[tt % 2]
            P.dma("sp", x[:], xsrc_fn(tt), reads=[xsrc], writes=[x])
            for ct in range(4):
                p_ = ps[(tt * 4 + ct) % 8]
                for k in range(16):
                    P.pe(lambda e, k=k, p_=p_, ct=ct, tt=tt: e.matmul(p_[:], lhsT=yT[:, k, tt * 128:(tt + 1) * 128], rhs=wob[:, k, ct * 512:(ct + 1) * 512], start=(k == 0), stop=(k == 15)),
                         reads=[yT, wob], writes=[p_])
                cs = slice(ct * 512, (ct + 1) * 512)
                P.dve(lambda e, p_=p_, o=o, cs=cs: e.tensor_tensor(out=o[:, cs], in0=p_[:], in1=gb[:, cs], op=ALU.mult), reads=[p_, gb], writes=[o])
                P.pool(lambda e, o=o, x=x, cs=cs: e.tensor_tensor(out=o[:, cs], in0=o[:, cs], in1=x[:, cs], op=ALU.add), reads=[o, x], writes=[o])
            P.dma("sp", xdst_fn(tt), o[:], reads=[o], writes=[xdst])


def stage_mlp(P, C, xsrc, xsrc_fn, xdst, xdst_fn, modT, nw2T, gsc, w1s, w2s, l, b):
    HC = 64
    with P.scope():
        scl = P.sbuf("ml_scl", [128, 16]); shf = P.sbuf("ml_shf", [128, 16])
        affine_vecs(P, modT, l, b, 1, nw2T[:, l, :], scl, shf)
        gb = P.sbuf("ml_gb", [128, D_MODEL])
        P.dma("sp", gb[:], gsc.t[l, 1, b:b + 1, :].partition_broadcast(128), reads=[gsc], writes=[gb])
        hT = P.sbuf("ml_hT", [128, 16, 512], BF16)
        uT = P.sbuf("ml_uT", [128, HC, 512], BF16)
        pools = fm_pools(P, True)
        pools["affb"] = [scl, shf]
        NWB = 4
        wbuf = [P.sbuf(f"ml_wb{i}", [128, 4096], BF16) for i in range(NWB)]
        rl = [P.sbuf(f"ml_rl{i}", [128, 512]) for i in range(2)]
        xo = [P.sbuf(f"ml_xo{i}", [128, 1024]) for i in range(2)]
        ps = [P.psum(f"ml_ps{i}", [128, 512]) for i in range(6)]
        NTILE = 64
        total = (SEQ // 512) * NTILE
        state = {"n": 0}

        def wload(j):
            jj = j % NTILE
            wbb = wbuf[j % NWB]
            if jj < 32:
                P.dma("sp", wbb[:], w1s.t[:, jj * 4096:(jj + 1) * 4096], reads=[w1s], writes=[wbb])
            else:
                P.dma("sp", wbb[:], w2s.t[:, (jj - 32) * 4096:(jj - 31) * 4096], reads=[w2s], writes=[wbb])

        PRE = 3
        for j in range(PRE):
            wload(j)
        nps = 0
        j = 0
        for t5 in range(SEQ // 512):
            to_featmajor(P, C, xsrc, lambda tt, t5=t5: xsrc_fn(t5 * 4 + tt), 4, hT, True, scl[:], shf[:], pools)
            for hp in range(32):
                if j + PRE < total:
                    wload(j + PRE)
                wbb = wbuf[j % NWB]; j += 1
                w3 = wbb[:].rearrange("p (k c) -> p k c", k=16)
                for cc in range(2):
                    hc = hp * 2 + cc
                    p_ = ps[nps % 6]; nps += 1
                    r_ = rl[hc % 2]
                    for k in range(16):
                        P.pe(lambda e, k=k, p_=p_, w3=w3, cc=cc: e.matmul(p_[:], lhsT=w3[:, k, cc * 128:(cc + 1) * 128], rhs=hT[:, k, :], start=(k == 0), stop=(k == 15)),
                             reads=[wbb, hT], writes=[p_])
                    P.act(lambda e, p_=p_, r_=r_: e.activation(out=r_[:], in_=p_[:], func=AF.Relu), reads=[p_], writes=[r_])
                    P.dve(lambda e, r_=r_, hc=hc: e.tensor_tensor(out=uT[:, hc, :], in0=r_[:], in1=r_[:], op=ALU.mult), reads=[r_], writes=[uT])
            for ct in range(4):
                pa = [ps[(nps + i) % 6] for i in range(4)]
                nps += 4
                for kg in range(8):
                    if j + PRE < total:
                        wload(j + PRE)
                    wbb = wbuf[j % NWB]; j += 1
                    w3 = wbb[:].rearrange("p (k c) -> p k c", k=8)
                    for kk in range(8):
                        k = kg * 8 + kk
                        for ti in range(4):
                            P.pe(lambda e, k=k, kk=kk, ti=ti, w3=w3, pa=pa: e.matmul(pa[ti][:], lhsT=uT[:, k, ti * 128:(ti + 1) * 128], rhs=w3[:, kk, :], start=(k == 0), stop=(k == HC - 1)),
                                 reads=[uT, wbb], writes=[pa[ti]])
                for ti in range(4):
                    tt = t5 * 4 + ti
                    o = xo[(ct * 4 + ti) % 2]
                    P.dma("sp", o[:, 512:1024], xsrc_fn(tt)[:, ct * 512:(ct + 1) * 512], reads=[xsrc], writes=[o])
                    P.dve(lambda e, o=o, ti=ti, pa=pa, ct=ct: e.tensor_tensor(out=o[:, 0:512], in0=pa[ti][:], in1=gb[:, ct * 512:(ct + 1) * 512], op=ALU.mult),
                          reads=[pa[ti], gb], writes=[o])
                    P.pool(lambda e, o=o: e.tensor_tensor(out=o[:, 0:512], in0=o[:, 0:512], in1=o[:, 512:1024], op=ALU.add), reads=[o], writes=[o])
                    P.dma("sp", xdst_fn(tt)[:, ct * 512:(ct + 1) * 512], o[:, 0:512], reads=[o], writes=[xdst])


def stage_final(P, C, xsrc, xsrc_fn, fnw, out, out_fn, nseq):
    with P.scope():
        nwb = P.sbuf("fn_nwb", [128, D_MODEL])
        P.dma("sp", nwb[:], fnw.t[0:1, :].partition_broadcast(128), reads=[fnw], writes=[nwb])
        eps = P.sbuf("fn_eps", [128, 1])
        P.pool(lambda e: e.memset(eps[:], EPS), writes=[eps])
        xt = [P.sbuf(f"fn_xt{i}", [128, D_MODEL]) for i in range(2)]
        sq = [P.sbuf(f"fn_sq{i}", [128, D_MODEL]) for i in range(2)]
        ss = [P.sbuf(f"fn_ss{i}", [128, 1]) for i in range(2)]
        for i in range(nseq * NT):
            x = xt[i % 2]; q = sq[i % 2]; s1 = ss[i % 2]
            P.dma("sp", x[:], xsrc_fn(i), reads=[xsrc], writes=[x])
            P.pool(lambda e, s1=s1: e.memset(s1[:], 0.0), writes=[s1])
            P.act(lambda e, x=x, q=q, s1=s1: e.activation(out=q[:], in_=x[:], func=AF.Square, accum_out=s1[:, 0:1]), reads=[x, s1], writes=[q, s1])
            P.act(lambda e, s1=s1: e.activation(out=s1[:, 0:1], in_=s1[:, 0:1], func=AF.Sqrt, scale=1.0 / D_MODEL, bias=eps[:, 0:1]), reads=[s1, eps], writes=[s1])
            P.dve(lambda e, s1=s1: e.reciprocal(out=s1[:, 0:1], in_=s1[:, 0:1]), reads=[s1], writes=[s1])
            P.dve(lambda e, x=x, q=q, s1=s1: e.scalar_tensor_tensor(out=q[:], in0=x[:], scalar=s1[:, 0:1], in1=nwb[:], op0=ALU.mult, op1=ALU.mult), reads=[x, s1, nwb], writes=[q])
            P.dma("sp", out_fn(i), q[:], reads=[q], writes=[out])


SMALL_PARAMS = ["gla_gate_w2", "gla_gate_b", "gla_norm_w", "ssd_conv_w", "ssd_conv_b", "ssd_dt_bias", "ssd_a_log", "ssd_d", "ssd_norm_w",
                "gdn_conv_w", "gdn_dt_bias", "gdn_a_log", "gdn_norm_w", "nsa_cmp_pos", "nsa_cmp_w1", "nsa_cmp_w2"]
BIG_PARAMS = ["ada_w", "ada_b", "w_in", "w_out", "mlp_w1", "mlp_w2"]


def build(nlayers=DEPTH, nseq=BPC, shapes=None):
    nc = bass.Bass("TRN2", target_bir_lowering=False)
    st = ExitStack()
    with st:
        P = Prog(nc, st)

        def ext(name, shape):
            return Buf(nc.dram_tensor(name, list(shape), F32, kind="ExternalInput").ap(), name)

        x = ext("x", [nseq, SEQ, D_MODEL])
        cT = ext("cT", [128, 16, BPC])
        prm = {k: ext(k, shapes[k]) for k in SMALL_PARAMS + BIG_PARAMS + ["nw1T", "nw2T", "fnw", "nsa_tab", "nsa_t31", "nsa_cst", "cst"]}
        out = Buf(nc.dram_tensor("out", [nseq, SEQ, D_MODEL], F32, kind="ExternalOutput").ap(), "out")
        xres = P.dram("xres", [nseq, SEQ, D_MODEL])
        projT = P.dram("projT", [PT_ROWS, SEQ])
        projN = P.dram("projN", [SEQ, PN_COLS])
        ymix = P.dram("ymix", [SEQ, D_MODEL])
        gsc = P.dram("gsc", [nlayers, 2, BPC, D_MODEL])
        C = Consts(P, prm["cst"])
        modT = P.sbuf("modT", [128, nlayers, 4, 16, BPC])
        nw1T = P.sbuf("nw1T", [128, shapes["nw1T"][1], 16])
        nw2T = P.sbuf("nw2T", [128, shapes["nw2T"][1], 16])
        P.dma("sp", nw1T[:], prm["nw1T"].t[:, :, :], reads=[prm["nw1T"]], writes=[nw1T])
        P.dma("sp", nw2T[:], prm["nw2T"].t[:, :, :], reads=[prm["nw2T"]], writes=[nw2T])
        P.bg = None
        wsc = dict(win=P.dram("wsc_win", [128, WIN_TOTAL], BF16), wout=P.dram("wsc_wout", [128, WOUT_TOTAL], BF16),
                   w1=P.dram("wsc_w1", [128, W1_TOTAL], BF16), w2=P.dram("wsc_w2", [128, W2_TOTAL], BF16))
        wscs = [wsc, dict(win=P.dram("wsc_win2", [128, WIN_TOTAL], BF16), wout=P.dram("wsc_wout2", [128, WOUT_TOTAL], BF16),
                          w1=P.dram("wsc_w12", [128, W1_TOTAL], BF16), w2=P.dram("wsc_w22", [128, W2_TOTAL], BF16))]
        P.bg = Background(P, prm, 0, wscs[0])
        stage_mod(P, C, cT, prm["ada_w"], prm["ada_b"], modT, gsc, nlayers)
        stage_convert_all(P, P.bg)
        P.bg = None
        for l in range(nlayers):
            wsc = wscs[l % 2]
            for b in range(nseq):
                if l == 0:
                    xs, xs_fn = x, (lambda tt, b=b: x.t[b, tt * 128:(tt + 1) * 128, :])
                else:
                    xs, xs_fn = xres, (lambda tt, b=b: xres.t[b, tt * 128:(tt + 1) * 128, :])
                xr_fn = (lambda tt, b=b: xres.t[b, tt * 128:(tt + 1) * 128, :])
                stage_inproj(P, C, xs, xs_fn, modT, nw1T, wsc["win"], projT, projN, l, b)
                if b == nseq - 1 and l + 1 < nlayers:
                    P.bg = Background(P, prm, l + 1, wscs[(l + 1) % 2])
                stage_nsa(P, C, projT, projN, ymix, prm, l)
                stage_ssd(P, C, projT, projN, ymix, prm, l)
                stage_gdn(P, C, projT, projN, ymix, prm, l)
                stage_gla(P, C, projT, projN, ymix, prm, l)
                if P.bg is not None:
                    stage_convert_all(P, P.bg)
                    P.bg = None
                stage_outproj(P, C, ymix, xs, xs_fn, xres, xr_fn, gsc, wsc["wout"], l, b)
                stage_mlp(P, C, xres, xr_fn, xres, xr_fn, modT, nw2T, gsc, wsc["w1"], wsc["w2"], l, b)
        stage_final(P, C, xres, lambda i: xres.t[i // NT, (i % NT) * 128:(i % NT + 1) * 128, :], prm["fnw"],
                    out, lambda i: out.t[i // NT, (i % NT) * 128:(i % NT + 1) * 128, :], nseq)
        P.finish()
        ninstr = P.ninstr
    return nc, ninstr


def host_inputs(inputs, nlayers=DEPTH):
    d = {}
    for k in SMALL_PARAMS:
        d[k] = host_param(k, inputs[k][:nlayers])
    for k in BIG_PARAMS:
        d[k] = np.ascontiguousarray(np.asarray(inputs[k][:nlayers], np.float32))
    d["nw1T"] = host_param("norm1_w", inputs["norm1_w"][:nlayers])
    d["nw2T"] = host_param("norm2_w", inputs["norm2_w"][:nlayers])
    d["fnw"] = host_param("final_norm_w", inputs["final_norm_w"])
    d["nsa_tab"], d["nsa_t31"] = nsa_host_tables(inputs["rel_bias"])
    d["nsa_cst"] = NSA_CST_NP
    d["cst"] = CST_NP
    return d


def core_inputs(inputs, shared, core, nseq=BPC):
    xs = np.ascontiguousarray(np.asarray(inputs["x"][core * BPC:core * BPC + nseq], np.float32))
    c = np.asarray(inputs["c"][core * BPC:(core + 1) * BPC], np.float32)
    cT = np.ascontiguousarray(c.T.reshape(16, 128, BPC).transpose(1, 0, 2))
    m = dict(shared)
    m["x"] = xs
    m["cT"] = cT
    return m


_CACHE = {}


def kernel(**inputs):
    shared = host_inputs(inputs)
    shapes = {k: v.shape for k, v in shared.items()}
    if "nc" not in _CACHE:
        _CACHE["nc"] = build(DEPTH, BPC, shapes)[0]
    nc = _CACHE["nc"]
    in_maps = [core_inputs(inputs, shared, c) for c in range(NCORES)]
    res = run_bass_kernel_spmd(nc, in_maps, core_ids=list(range(NCORES)))
    out = np.concatenate([r["out"] for r in res.results], axis=0)
    return out.astype(np.float32)
```

```python
import math
from contextlib import ExitStack, contextmanager
import numpy as np
import concourse.bass as bass
import concourse.mybir as mybir
from concourse.bass_utils import run_bass_kernel_spmd

F32 = mybir.dt.float32
BF16 = mybir.dt.bfloat16
AF = mybir.ActivationFunctionType
ALU = mybir.AluOpType
AX = mybir.AxisListType

EPOCH = 12000
SAME_ENGINE_SYNC = True

D_MODEL = 2048
SEQ = 2048
DEPTH = 4
NCORES = 8
BPC = 2
IN_COLS = 6456
EPS = 1e-6
NT = SEQ // 128


class StopStage(Exception):
    pass


DBG = {"stop": 99, "skip": set()}


def dbg(k):
    if DBG["stop"] <= k:
        DBG["P"].dead = True


class Buf:
    __slots__ = ("t", "name", "lw", "rd", "excl")

    def __init__(self, t, name="", excl=False):
        self.t = t
        self.name = name
        self.lw = None
        self.rd = {}
        self.excl = excl

    def __getitem__(self, k):
        return self.t[k]


class Prog:
    ENGS = ("pe", "act", "dve", "pool", "sp")

    def __init__(self, nc, stack):
        self.nc = nc
        self.stack = stack
        self.cnt = {e: 0 for e in ("pe", "act", "dve", "pool")}
        self.sems = {}
        self.seen = {e: {} for e in self.ENGS}
        self.dslots = {}
        self.dnext = {}
        self.E = dict(pe=nc.tensor, act=nc.scalar, dve=nc.vector, pool=nc.gpsimd, sp=nc.sync)
        self.ninstr = 0
        self.base_stack = stack
        self.uid = 0
        self.dead = False
        DBG["P"] = self

    def sem(self, name):
        return self.base_stack.enter_context(self.nc.semaphore(name))

    def sbuf(self, name, shape, dt=F32):
        self.uid += 1
        t = self.stack.enter_context(self.nc.sbuf_tensor(f"{name}_{self.uid}", list(shape), dt))
        return Buf(t, name)

    def psum(self, name, shape, dt=F32):
        self.uid += 1
        t = self.stack.enter_context(self.nc.psum_tensor(f"{name}_{self.uid}", list(shape), dt))
        return Buf(t, name, excl=True)

    def dram(self, name, shape, dt=F32, kind="Internal"):
        t = self.nc.dram_tensor(name, list(shape), dt, kind=kind)
        return Buf(t.ap(), name)

    def _esem(self, eng, idx):
        ep = idx // EPOCH
        k = (eng, ep)
        if k not in self.sems:
            self.sems[k] = self.sem(f"s_{eng}_{ep}")
        return self.sems[k], (idx % EPOCH) + 1

    def _wait(self, eng, ev):
        q, idx = ev
        if isinstance(q, str):
            if q == eng and (eng == "pe" or not SAME_ENGINE_SYNC):
                return
            if self.seen[eng].get(q, -1) >= idx:
                return
            self.seen[eng][q] = idx
            s, v = self._esem(q, idx)
        else:
            if self.seen[eng].get(q, -1) >= idx:
                return
            self.seen[eng][q] = idx
            s = self.dslots[q[0]][q[1]][0]
            v = idx
        self.E[eng].wait_ge(s, v)

    def _deps(self, eng, reads, writes):
        for b in reads:
            if b.lw is not None:
                self._wait(eng, b.lw)
            if b.excl:
                for q, i in list(b.rd.items()):
                    if q != eng:
                        self._wait(eng, (q, i))
        for b in writes:
            if b.lw is not None:
                self._wait(eng, b.lw)
            for q, i in list(b.rd.items()):
                self._wait(eng, (q, i))

    def _mark(self, ev, reads, writes):
        q, idx = ev
        for b in reads:
            if b.rd.get(q, -1) < idx:
                b.rd[q] = idx
        for b in writes:
            b.lw = ev
            b.rd = {}

    def op(self, eng, fn, reads=(), writes=()):
        if self.dead:
            return
        self._deps(eng, reads, writes)
        idx = self.cnt[eng]
        self.cnt[eng] += 1
        s, v = self._esem(eng, idx)
        fn(self.E[eng]).then_inc(s, 1)
        self._mark((eng, idx), reads, writes)
        self.ninstr += 1

    def pe(self, fn, reads=(), writes=()):
        self.op("pe", fn, reads, writes)

    def act(self, fn, reads=(), writes=()):
        self.op("act", fn, reads, writes)

    def dve(self, fn, reads=(), writes=()):
        self.op("dve", fn, reads, writes)

    def pool(self, fn, reads=(), writes=()):
        self.op("pool", fn, reads, writes)

    def dma(self, eng, out_ap, in_ap, reads=(), writes=(), nslots=8, **kw):
        if self.dead:
            return
        if eng not in self.dslots:
            self.dslots[eng] = [[self.sem(f"d_{eng}_{i}"), 0] for i in range(nslots)]
            self.dnext[eng] = 0
        si = self.dnext[eng]
        self.dnext[eng] = (si + 1) % len(self.dslots[eng])
        slot = self.dslots[eng][si]
        q = (eng, si)
        if slot[1] > 0:
            self._wait(eng, (q, slot[1]))
        self._deps(eng, reads, writes)
        slot[1] += 16
        self.E[eng].dma_start(out=out_ap, in_=in_ap, **kw).then_inc(slot[0], 16)
        self._mark((q, slot[1]), reads, writes)
        self.ninstr += 1

    def all_events(self):
        evs = []
        for e in ("pe", "act", "dve", "pool"):
            if self.cnt[e] > 0:
                evs.append((e, self.cnt[e] - 1))
        for eng, slots in self.dslots.items():
            for si, (s, c) in enumerate(slots):
                if c > 0:
                    evs.append(((eng, si), c))
        return evs

    def barrier(self, engs=None):
        evs = self.all_events()
        for e in (engs or self.ENGS):
            for ev in evs:
                if ev[0] == e and e == "pe":
                    continue
                self._wait(e, ev)

    @contextmanager
    def scope(self):
        old = self.stack
        try:
            with ExitStack() as st:
                self.stack = st
                try:
                    yield
                finally:
                    self.barrier()
        finally:
            self.stack = old

    def finish(self):
        self.barrier(["sp"])


def make_consts():
    p = np.arange(128)[:, None]
    f = np.arange(128)[None, :]
    same = (p // 64) == (f // 64)
    cols = {}
    parts = []

    def add(name, arr):
        cols[name] = (sum(a.shape[1] for a in parts), arr.shape[1])
        parts.append(arr.astype(np.float32))

    add("ident", (p == f))
    add("tri01", same & (p <= f))
    add("stri01", same & (p < f))
    add("su01", same & (p > f))
    add("sl01", same & (p >= f))
    add("bones", same)
    add("ones", np.ones((128, 128)))
    add("chunkind", (p // 64) == np.arange(2)[None, :])
    add("tri16", (same & (p <= f)) * (-1.0 / 16.0))
    add("bones16", same * (-1.0 / 16.0))
    add("chunkind16", ((p // 64) == np.arange(2)[None, :]) * (-1.0 / 16.0))
    return np.concatenate(parts, axis=1), cols


CST_NP, CST_COLS = make_consts()


class Consts:
    def __init__(self, P, cst_dram):
        self.P = P
        n = CST_NP.shape[1]
        self.f = P.sbuf("cst_f", [128, n])
        P.dma("sp", self.f[:], cst_dram.t[:, :], reads=[cst_dram], writes=[self.f])
        self.identb = P.sbuf("identb", [128, 128], BF16)
        P.dve(lambda e: e.tensor_copy(out=self.identb[:], in_=self.c("ident")), reads=[self.f], writes=[self.identb])

    def c(self, name, rows=128):
        o, w = CST_COLS[name]
        return self.f[0:rows, o:o + w]


def norm_gate(P, src_ap, src_bufs, z_ap, z_bufs, nw_ap, nw_bufs, G, gsz, out, tmp, gate_first):
    a, b, ss, sg = tmp["a"], tmp["b"], tmp["ss"], tmp["sg"]
    n = G * gsz
    P.act(lambda e: e.activation(out=sg[:, 0:n], in_=z_ap, func=AF.Silu), reads=z_bufs, writes=[sg])
    if gate_first:
        P.dve(lambda e: e.tensor_tensor(out=a[:, 0:n], in0=src_ap, in1=sg[:, 0:n], op=ALU.mult),
              reads=list(src_bufs) + [sg], writes=[a])
    else:
        P.dve(lambda e: e.tensor_copy(out=a[:, 0:n], in_=src_ap), reads=list(src_bufs), writes=[a])
    P.act(lambda e: e.activation(out=b[:, 0:n], in_=a[:, 0:n], func=AF.Square), reads=[a], writes=[b])
    P.dve(lambda e: e.tensor_reduce(out=ss[:, 0:G], in_=b[:, 0:n].rearrange("p (g e) -> p g e", g=G), axis=AX.X, op=ALU.add),
          reads=[b], writes=[ss])
    P.act(lambda e: e.activation(out=ss[:, 0:G], in_=ss[:, 0:G], func=AF.Sqrt, scale=1.0 / gsz, bias=tmp["eps"][:, 0:1]),
          reads=[ss, tmp["eps"]], writes=[ss])
    P.dve(lambda e: e.reciprocal(out=ss[:, 0:G], in_=ss[:, 0:G]), reads=[ss], writes=[ss])
    P.dve(lambda e: e.tensor_tensor(out=b[:, 0:n].rearrange("p (g e) -> p g e", g=G),
                                    in0=a[:, 0:n].rearrange("p (g e) -> p g e", g=G),
                                    in1=ss[:, 0:G].unsqueeze(2).to_broadcast([128, G, gsz]), op=ALU.mult),
          reads=[a, ss], writes=[b])
    if gate_first:
        P.pool(lambda e: e.tensor_tensor(out=out[:, 0:n].rearrange("p (g e) -> p g e", g=G),
                                         in0=b[:, 0:n].rearrange("p (g e) -> p g e", g=G), in1=nw_ap, op=ALU.mult),
               reads=[b] + list(nw_bufs), writes=[out])
    else:
        P.pool(lambda e: e.tensor_tensor(out=a[:, 0:n].rearrange("p (g e) -> p g e", g=G),
                                         in0=b[:, 0:n].rearrange("p (g e) -> p g e", g=G), in1=nw_ap, op=ALU.mult),
               reads=[b] + list(nw_bufs), writes=[a])
        P.pool(lambda e: e.tensor_tensor(out=out[:, 0:n], in0=a[:, 0:n], in1=sg[:, 0:n], op=ALU.mult),
               reads=[a, sg], writes=[out])


def ng_tmp(P):
    t = dict(a=P.sbuf("ng_a", [128, 512]), b=P.sbuf("ng_b", [128, 512]), ss=P.sbuf("ng_ss", [128, 8]),
             sg=P.sbuf("ng_sg", [128, 512]), eps=P.sbuf("ng_eps", [128, 1]), one=P.sbuf("ng_one", [128, 1]))
    P.pool(lambda e: e.memset(t["eps"][:], EPS), writes=[t["eps"]])
    P.pool(lambda e: e.memset(t["one"][:], 1.0), writes=[t["one"]])
    return t


PT_NQ, PT_NKC, PT_NVC, PT_NKS, PT_NKW = 0, 512, 640, 768, 896
PT_SXBC = 1024
PT_GQKV = 2048
PT_LQ, PT_LK, PT_LLR = 3584, 3840, 4096
PT_ROWS = 4112
PN_NVS, PN_NVW, PN_NGATE, PN_SZ, PN_SDT = 0, 128, 256, 280, 792
PN_GZ, PN_GBETA, PN_GA, PN_LK, PN_LV, PN_LG = 800, 1312, 1316, 1320, 1576, 2088
PN_COLS = 2600
PT_GROUPS = ([(0 + 128 * i, 128, PT_NQ + 128 * i) for i in range(4)] +
             [(512, 128, PT_NKC), (640, 128, PT_NVC), (768, 128, PT_NKS), (1024, 128, PT_NKW)] +
             [(1816 + 128 * i, 128, PT_SXBC + 128 * i) for i in range(8)] +
             [(2848 + 128 * i, 128, PT_GQKV + 128 * i) for i in range(12)] +
             [(4904 + 128 * i, 128, PT_LQ + 128 * i) for i in range(2)] +
             [(5160 + 128 * i, 128, PT_LK + 128 * i) for i in range(2)] +
             [(6440, 16, PT_LLR)])
PN_GROUPS = [(896, 128, PN_NVS), (1152, 512, PN_NVW), (1664, 152, PN_NVW + 512), (2840, 8, PN_SDT),
             (4384, 512, PN_GZ), (4896, 8, PN_GBETA), (5160, 256, PN_LK), (5416, 512, PN_LV), (5928, 512, PN_LG)]


def stage_gla(P, C, projT, projN, ymix, prm, l):
    with P.scope():
        w2 = P.sbuf("gla_w2", [16, 256])
        gb = P.sbuf("gla_gb", [1, 256])
        nwb = P.sbuf("gla_nwb", [128, 128])
        P.dma("sp", w2[:], prm["gla_gate_w2"].t[l], reads=[prm["gla_gate_w2"]], writes=[w2])
        P.dma("sp", gb[:], prm["gla_gate_b"].t[l:l + 1, :], reads=[prm["gla_gate_b"]], writes=[gb])
        P.dma("sp", nwb[:], prm["gla_norm_w"].t[l:l + 1, :].partition_broadcast(128), reads=[prm["gla_norm_w"]], writes=[nwb])
        S = P.sbuf("gla_S", [64, 4, 128])
        Sb = [P.sbuf(f"gla_Sb{i}", [64, 4, 128], BF16) for i in range(2)]
        P.dve(lambda e: e.memset(S[:], 0.0), writes=[S])
        P.dve(lambda e: e.memset(Sb[0][:], 0.0), writes=[Sb[0]])
        tmp = ng_tmp(P)
        NB = 2
        qT = [P.sbuf(f"gla_qT{i}", [64, 4, 128]) for i in range(NB)]
        kT = [P.sbuf(f"gla_kT{i}", [64, 4, 128]) for i in range(NB)]
        lrT = [P.sbuf(f"gla_lrT{i}", [16, 128]) for i in range(NB)]
        tokN = [P.sbuf(f"gla_tokN{i}", [128, 1280]) for i in range(NB)]
        lsp = P.sbuf("gla_lsp", [128, 256])
        ex = P.sbuf("gla_ex", [128, 256])
        kend = P.sbuf("gla_kend", [128, 256], BF16)
        vb = P.sbuf("gla_vb", [128, 512], BF16)
        ebT = P.sbuf("gla_ebT", [64, 512])
        qdT = P.sbuf("gla_qdT", [64, 4, 128], BF16)
        kiT = P.sbuf("gla_kiT", [64, 4, 128], BF16)
        dec = P.sbuf("gla_dec", [64, 8])
        AT = P.sbuf("gla_AT", [128, 4, 128], BF16)
        yo = [P.sbuf(f"gla_yo{i}", [128, 512]) for i in range(2)]
        ps_gk = P.psum("gla_ps_gk", [128, 512])
        ps_bl = P.psum("gla_ps_bl", [128, 512])
        ps_bT = P.psum("gla_ps_bT", [64, 512])
        ps_blT = P.psum("gla_ps_blT", [64, 8])
        ps_at = P.psum("gla_ps_at", [128, 512])
        ps_o = P.psum("gla_ps_o", [128, 512])
        ps_loc = [P.psum(f"gla_ps_loc{i}", [64, 512]) for i in range(2)]
        cf = [C.f]
        bg_begin(P)

        def load(t):
            i = t % NB
            tok = slice(t * 128, (t + 1) * 128)
            P.dma("sp", qT[i][:], projT.t[PT_LQ:PT_LQ + 256, tok].rearrange("(h d) t -> d h t", d=64), reads=[projT], writes=[qT[i]])
            P.dma("sp", kT[i][:], projT.t[PT_LK:PT_LK + 256, tok].rearrange("(h d) t -> d h t", d=64), reads=[projT], writes=[kT[i]])
            P.dma("sp", lrT[i][:], projT.t[PT_LLR:PT_LLR + 16, tok], reads=[projT], writes=[lrT[i]])
            P.dma("sp", tokN[i][:], projN.t[tok, PN_LK:PN_LK + 1280], reads=[projN], writes=[tokN[i]])

        load(0)
        for t in range(NT):
            if t + 1 < NT:
                load(t + 1)
            i = t % NB
            tok = slice(t * 128, (t + 1) * 128)
            kN = tokN[i][:, 0:256]
            vN = tokN[i][:, 256:768]
            gN = tokN[i][:, 768:1280]
            P.pe(lambda e: e.matmul(ps_gk[:, 0:256], lhsT=lrT[i][:], rhs=w2[:], start=True, stop=False), reads=[lrT[i], w2], writes=[ps_gk])
            P.pe(lambda e: e.matmul(ps_gk[:, 0:256], lhsT=C.c("ones", 1), rhs=gb[:], start=False, stop=True), reads=[gb] + cf, writes=[ps_gk])
            P.act(lambda e: e.activation(out=ex[:], in_=ps_gk[:, 0:256], func=AF.Exp, scale=-1.0), reads=[ps_gk], writes=[ex])
            P.act(lambda e: e.activation(out=lsp[:], in_=ex[:], func=AF.Ln, bias=tmp["one"][:, 0:1]), reads=[ex, tmp["one"]], writes=[lsp])
            P.pe(lambda e: e.matmul(ps_gk[:, 256:512], lhsT=C.c("tri16"), rhs=lsp[:], start=True, stop=True), reads=[lsp] + cf, writes=[ps_gk])
            P.pe(lambda e: e.matmul(ps_bl[:, 0:256], lhsT=C.c("bones16"), rhs=lsp[:], start=True, stop=True), reads=[lsp] + cf, writes=[ps_bl])
            for h in range(4):
                P.pe(lambda e, h=h: e.matmul(ps_bT[:, h * 128:(h + 1) * 128], lhsT=lsp[:, h * 64:(h + 1) * 64], rhs=C.c("tri16"), start=True, stop=True),
                     reads=[lsp] + cf, writes=[ps_bT])
            for h in range(4):
                P.pe(lambda e, h=h: e.matmul(ps_blT[:, h * 2:(h + 1) * 2], lhsT=lsp[:, h * 64:(h + 1) * 64], rhs=C.c("chunkind16"), start=True, stop=True),
                     reads=[lsp] + cf, writes=[ps_blT])
            P.dve(lambda e: e.tensor_copy(out=ex[:], in_=ps_gk[:, 256:512]), reads=[ps_gk], writes=[ex])
            P.dve(lambda e: e.tensor_tensor(out=ex[:], in0=ps_bl[:, 0:256], in1=ex[:], op=ALU.subtract), reads=[ps_bl, ex], writes=[ex])
            P.act(lambda e: e.activation(out=ex[:], in_=ex[:], func=AF.Exp), reads=[ex], writes=[ex])
            P.dve(lambda e: e.tensor_tensor(out=kend[:], in0=kN, in1=ex[:], op=ALU.mult), reads=[tokN[i], ex], writes=[kend])
            P.pool(lambda e: e.tensor_copy(out=vb[:], in_=vN), reads=[tokN[i]], writes=[vb])
            P.act(lambda e: e.activation(out=ebT[:], in_=ps_bT[:], func=AF.Exp), reads=[ps_bT], writes=[ebT])
            P.dve(lambda e: e.scalar_tensor_tensor(out=qdT[:].rearrange("d h t -> d (h t)"), in0=qT[i][:].rearrange("d h t -> d (h t)"), scalar=0.125,
                                                   in1=ebT[:], op0=ALU.mult, op1=ALU.mult), reads=[qT[i], ebT], writes=[qdT])
            P.act(lambda e: e.activation(out=ebT[:], in_=ps_bT[:], func=AF.Exp, scale=-1.0), reads=[ps_bT], writes=[ebT])
            P.dve(lambda e: e.tensor_tensor(out=kiT[:].rearrange("d h t -> d (h t)"), in0=kT[i][:].rearrange("d h t -> d (h t)"), in1=ebT[:], op=ALU.mult),
                  reads=[kT[i], ebT], writes=[kiT])
            P.act(lambda e: e.activation(out=dec[:], in_=ps_blT[:], func=AF.Exp), reads=[ps_blT], writes=[dec])
            for h in range(4):
                P.pe(lambda e, h=h: e.matmul(ps_at[:, h * 128:(h + 1) * 128], lhsT=kiT[:, h, :], rhs=qdT[:, h, :], start=True, stop=True),
                     reads=[kiT, qdT], writes=[ps_at])
            P.dve(lambda e: e.tensor_tensor(out=AT[:], in0=ps_at[:].rearrange("p (h t) -> p h t", h=4),
                                            in1=C.c("tri01").unsqueeze(1).to_broadcast([128, 4, 128]), op=ALU.mult), reads=[ps_at] + cf, writes=[AT])
            for c in range(2):
                rows = slice(c * 64, (c + 1) * 64)
                for h in range(4):
                    P.pe(lambda e, h=h, rows=rows, c=c: e.matmul(ps_loc[c][:, h * 128:(h + 1) * 128], lhsT=kend[rows, h * 64:(h + 1) * 64],
                                                                 rhs=vb[rows, h * 128:(h + 1) * 128], start=True, stop=True),
                         reads=[kend, vb], writes=[ps_loc[c]])
            for c in range(2):
                P.dve(lambda e, c=c: e.tensor_tensor(out=S[:], in0=S[:], in1=dec[:].rearrange("d (h c) -> d h c", c=2)[:, :, c:c + 1].to_broadcast([64, 4, 128]),
                                                     op=ALU.mult), reads=[S, dec], writes=[S])
                P.dve(lambda e, c=c: e.tensor_tensor(out=S[:].rearrange("d h e -> d (h e)"), in0=S[:].rearrange("d h e -> d (h e)"), in1=ps_loc[c][:], op=ALU.add),
                      reads=[S, ps_loc[c]], writes=[S])
                if c == 0:
                    P.act(lambda e: e.copy(out=Sb[1][:], in_=S[:]), reads=[S], writes=[Sb[1]])
            for h in range(4):
                cols = slice(h * 128, (h + 1) * 128)
                P.pe(lambda e, h=h, cols=cols: e.matmul(ps_o[:, cols], lhsT=AT[:, h, :], rhs=vb[:, cols], start=True, stop=False),
                     reads=[AT, vb], writes=[ps_o])
                for c in range(2):
                    rows = slice(c * 64, (c + 1) * 64)
                    P.pe(lambda e, h=h, cols=cols, rows=rows, c=c: e.matmul(ps_o[rows, cols], lhsT=qdT[:, h, rows], rhs=Sb[c][:, h, :], start=False, stop=(c == 1)),
                         reads=[qdT, Sb[c]], writes=[ps_o])
            P.act(lambda e: e.copy(out=Sb[0][:], in_=S[:]), reads=[S], writes=[Sb[0]])
            y = yo[t % 2]
            norm_gate(P, ps_o[:], [ps_o], gN, [tokN[i]], nwb[:].unsqueeze(1).to_broadcast([128, 4, 128]), [nwb], 4, 128, y, tmp, False)
            P.dma("sp", ymix.t[tok, 1536:2048], y[:], reads=[y], writes=[ymix])
            bg_tick(P, 1)
        bg_end(P)


def host_param(name, arr):
    a = np.asarray(arr, np.float32)
    if name in ("ssd_conv_w", "gdn_conv_w"):
        L, K, CH = a.shape
        a = a.reshape(L, K, CH // 128, 128).transpose(0, 3, 2, 1)
    elif name in ("norm1_w", "norm2_w"):
        L = a.shape[0]
        a = a.reshape(L, 16, 128).transpose(2, 0, 1)
    elif name == "final_norm_w":
        a = a.reshape(1, -1)
    elif name == "nsa_cmp_pos":
        a = a.transpose(0, 1, 3, 2)
    elif name == "ssd_conv_b":
        L, CH = a.shape
        a = a.reshape(L, CH // 128, 128).transpose(0, 2, 1)
    return np.ascontiguousarray(a)


def bc(ap, shape):
    return ap.to_broadcast(list(shape))


def causal_conv_silu(P, projT, row0, ntiles, cw, cb, dst, dst_off, name, bias=True):
    xpad = [P.sbuf(f"{name}_xpad{i}", [128, SEQ + 3]) for i in range(2)]
    acc = [P.sbuf(f"{name}_acc{i}", [128, SEQ]) for i in range(2)]
    for i in range(2):
        P.pool(lambda e, i=i: e.memset(xpad[i][:, 0:3], 0.0), writes=[xpad[i]])
    for ct in range(ntiles):
        xp = xpad[ct % 2]
        ac = acc[ct % 2]
        P.dma("sp", xp[:, 3:SEQ + 3], projT.t[row0 + ct * 128:row0 + (ct + 1) * 128, :], reads=[projT], writes=[xp])
        eng = P.dve
        eng(lambda e, ct=ct, xp=xp, ac=ac: e.tensor_scalar(out=ac[:], in0=xp[:, 0:SEQ], scalar1=cw[:, ct, 0:1], scalar2=None, op0=ALU.mult),
            reads=[xp, cw], writes=[ac])
        for k in range(1, 4):
            eng(lambda e, ct=ct, xp=xp, ac=ac, k=k: e.scalar_tensor_tensor(out=ac[:], in0=xp[:, k:SEQ + k], scalar=cw[:, ct, k:k + 1], in1=ac[:],
                                                                            op0=ALU.mult, op1=ALU.add), reads=[xp, cw, ac], writes=[ac])
        if bias:
            P.act(lambda e, ct=ct, ac=ac: e.activation(out=dst[:, dst_off + ct, :], in_=ac[:], func=AF.Silu, bias=cb[:, ct:ct + 1]),
                  reads=[ac, cb], writes=[dst])
        else:
            P.act(lambda e, ct=ct, ac=ac: e.activation(out=dst[:, dst_off + ct, :], in_=ac[:], func=AF.Silu), reads=[ac], writes=[dst])


def softplus_small(P, x_ap, xbuf, tmpb, one):
    P.act(lambda e: e.activation(out=x_ap, in_=x_ap, func=AF.Exp), reads=[xbuf], writes=[xbuf])
    P.act(lambda e: e.activation(out=x_ap, in_=x_ap, func=AF.Ln, bias=one[:, 0:1]), reads=[xbuf, one], writes=[xbuf])


def stage_ssd(P, C, projT, projN, ymix, prm, l):
    with P.scope():
        cf = [C.f]
        cw = P.sbuf("ssd_cw", [128, 8, 4])
        cb = P.sbuf("ssd_cb", [128, 8])
        dtb = P.sbuf("ssd_dtb", [128, 8])
        aneg = P.sbuf("ssd_aneg", [128, 8])
        dsk = P.sbuf("ssd_dsk", [128, 8])
        nwb = P.sbuf("ssd_nwb", [128, 512])
        P.dma("sp", cw[:], prm["ssd_conv_w"].t[l], reads=[prm["ssd_conv_w"]], writes=[cw])
        P.dma("sp", cb[:], prm["ssd_conv_b"].t[l], reads=[prm["ssd_conv_b"]], writes=[cb])
        P.dma("sp", dtb[:], prm["ssd_dt_bias"].t[l:l + 1, :].partition_broadcast(128), reads=[prm["ssd_dt_bias"]], writes=[dtb])
        P.dma("sp", aneg[:], prm["ssd_a_log"].t[l:l + 1, :].partition_broadcast(128), reads=[prm["ssd_a_log"]], writes=[aneg])
        P.dma("sp", dsk[:], prm["ssd_d"].t[l:l + 1, :].partition_broadcast(128), reads=[prm["ssd_d"]], writes=[dsk])
        P.dma("sp", nwb[:], prm["ssd_norm_w"].t[l:l + 1, :].partition_broadcast(128), reads=[prm["ssd_norm_w"]], writes=[nwb])
        P.act(lambda e: e.activation(out=aneg[:], in_=aneg[:], func=AF.Exp), reads=[aneg], writes=[aneg])
        P.dve(lambda e: e.tensor_scalar(out=aneg[:], in0=aneg[:], scalar1=-1.0, scalar2=None, op0=ALU.mult), reads=[aneg], writes=[aneg])
        act = P.sbuf("ssd_act", [128, 8, SEQ])
        with P.scope():
            causal_conv_silu(P, projT, PT_SXBC, 8, cw, cb, act, 0, "ssd")
        BCb = P.sbuf("ssd_BCb", [128, 4, SEQ], BF16)
        for k in range(4):
            (P.dve if k % 2 == 0 else P.pool)(lambda e, k=k: e.tensor_copy(out=BCb[:, k, :], in_=act[:, 4 + k, :]), reads=[act], writes=[BCb])
        tmp = ng_tmp(P)
        S = P.sbuf("ssd_S", [128, 8, 64])
        Sb = [P.sbuf(f"ssd_Sb{i}", [128, 8, 64], BF16) for i in range(2)]
        P.dve(lambda e: e.memset(S[:], 0.0), writes=[S])
        P.dve(lambda e: e.memset(Sb[0][:], 0.0), writes=[Sb[0]])
        tokN = [P.sbuf(f"ssd_tokN{i}", [128, 520]) for i in range(2)]
        xN = P.sbuf("ssd_xN", [128, 512])
        BNb = P.sbuf("ssd_BNb", [128, 256], BF16)
        dt8 = P.sbuf("ssd_dt8", [128, 8])
        a8 = P.sbuf("ssd_a8", [128, 8])
        dw8 = P.sbuf("ssd_dw8", [128, 8])
        ac16 = P.sbuf("ssd_ac16", [128, 2, 8])
        e32 = P.sbuf("ssd_e32", [128, 32])
        Aexp = P.sbuf("ssd_Aexp", [128, 8, 128])
        seg = P.sbuf("ssd_seg", [128, 8, 128])
        CBm = P.sbuf("ssd_CBm", [128, 2, 128])
        MT = P.sbuf("ssd_MT", [128, 8, 128], BF16)
        xdt = P.sbuf("ssd_xdt", [128, 8, 64], BF16)
        xw = P.sbuf("ssd_xw", [128, 8, 64], BF16)
        y1 = P.sbuf("ssd_y1", [128, 512])
        y2 = P.sbuf("ssd_y2", [128, 512])
        yo = [P.sbuf(f"ssd_yo{i}", [128, 512]) for i in range(2)]
        psA = P.psum("ssd_psA", [128, 512])
        psB = P.psum("ssd_psB", [128, 512])
        psC = P.psum("ssd_psC", [128, 512])
        psD = P.psum("ssd_psD", [128, 512])
        psE = P.psum("ssd_psE", [128, 512])
        psF = P.psum("ssd_psF", [128, 512])
        psG = [P.psum(f"ssd_psG{i}", [128, 512]) for i in range(2)]

        bg_begin(P)

        def load(t):
            P.dma("sp", tokN[t % 2][:], projN.t[t * 128:(t + 1) * 128, PN_SZ:PN_SZ + 520], reads=[projN], writes=[tokN[t % 2]])

        load(0)
        for t in range(NT):
            if t + 1 < NT:
                load(t + 1)
            tk = tokN[t % 2]
            tok = slice(t * 128, (t + 1) * 128)
            for k in range(4):
                P.pe(lambda e, k=k: e.transpose(out=psA[:, k * 128:(k + 1) * 128], in_=act[:, k, tok], identity=C.c("ident")), reads=[act] + cf, writes=[psA])
            for k in range(2):
                P.pe(lambda e, k=k: e.transpose(out=psB[:, k * 128:(k + 1) * 128], in_=act[:, 4 + k, tok], identity=C.c("ident")), reads=[act] + cf, writes=[psB])
            P.act(lambda e: e.copy(out=xN[:], in_=psA[:]), reads=[psA], writes=[xN])
            P.dve(lambda e: e.tensor_copy(out=BNb[:], in_=psB[:, 0:256]), reads=[psB], writes=[BNb])
            P.dve(lambda e: e.tensor_tensor(out=dt8[:], in0=tk[:, 512:520], in1=dtb[:], op=ALU.add), reads=[tk, dtb], writes=[dt8])
            softplus_small(P, dt8[:], dt8, None, tmp["one"])
            P.dve(lambda e: e.tensor_tensor(out=a8[:], in0=dt8[:], in1=aneg[:], op=ALU.mult), reads=[dt8, aneg], writes=[a8])
            P.dve(lambda e: e.tensor_tensor(out=Aexp[:], in0=bc(a8[:].unsqueeze(2), [128, 8, 128]), in1=bc(C.c("tri01").unsqueeze(1), [128, 8, 128]), op=ALU.mult),
                  reads=[a8] + cf, writes=[Aexp])
            P.dve(lambda e: e.tensor_tensor(out=ac16[:], in0=bc(a8[:].unsqueeze(1), [128, 2, 8]), in1=bc(C.c("chunkind").unsqueeze(2), [128, 2, 8]), op=ALU.mult),
                  reads=[a8] + cf, writes=[ac16])
            P.pe(lambda e: e.matmul(psC[:], lhsT=C.c("su01"), rhs=Aexp[:, 0:4, :].rearrange("p h i -> p (h i)"), start=True, stop=True), reads=[Aexp] + cf, writes=[psC])
            P.pe(lambda e: e.matmul(psD[:], lhsT=C.c("su01"), rhs=Aexp[:, 4:8, :].rearrange("p h i -> p (h i)"), start=True, stop=True), reads=[Aexp] + cf, writes=[psD])
            P.act(lambda e: e.activation(out=seg[:, 0:4, :].rearrange("p h i -> p (h i)"), in_=psC[:], func=AF.Exp), reads=[psC], writes=[seg])
            P.act(lambda e: e.activation(out=seg[:, 4:8, :].rearrange("p h i -> p (h i)"), in_=psD[:], func=AF.Exp), reads=[psD], writes=[seg])
            P.pe(lambda e: e.matmul(psE[:, 0:8], lhsT=C.c("tri01"), rhs=a8[:], start=True, stop=True), reads=[a8] + cf, writes=[psE])
            P.pe(lambda e: e.matmul(psE[:, 8:16], lhsT=C.c("su01"), rhs=a8[:], start=True, stop=True), reads=[a8] + cf, writes=[psE])
            P.pe(lambda e: e.matmul(psE[:, 16:32], lhsT=C.c("ones"), rhs=ac16[:].rearrange("p c h -> p (c h)"), start=True, stop=True), reads=[ac16] + cf, writes=[psE])
            P.act(lambda e: e.activation(out=e32[:], in_=psE[:, 0:32], func=AF.Exp), reads=[psE], writes=[e32])
            ea = e32[:, 0:8]
            w8 = e32[:, 8:16]
            for g in range(2):
                P.pe(lambda e, g=g: e.matmul(psB[:, 256 + g * 128:256 + (g + 1) * 128], lhsT=BCb[:, g, tok], rhs=BCb[:, 2 + g, tok], start=True, stop=True),
                     reads=[BCb], writes=[psB])
            P.dve(lambda e: e.tensor_tensor(out=CBm[:], in0=psB[:, 256:512].rearrange("p (g i) -> p g i", g=2), in1=bc(C.c("tri01").unsqueeze(1), [128, 2, 128]), op=ALU.mult),
                  reads=[psB] + cf, writes=[CBm])
            P.dve(lambda e: e.tensor_tensor(out=MT[:].rearrange("p (g r) i -> p g r i", g=2), in0=seg[:].rearrange("p (g r) i -> p g r i", g=2),
                                            in1=bc(CBm[:].unsqueeze(2), [128, 2, 4, 128]), op=ALU.mult), reads=[seg, CBm], writes=[MT])
            P.dve(lambda e: e.tensor_tensor(out=dw8[:], in0=dt8[:], in1=w8, op=ALU.mult), reads=[dt8, e32], writes=[dw8])
            P.pool(lambda e: e.tensor_tensor(out=xdt[:], in0=xN[:].rearrange("p (h q) -> p h q", h=8), in1=bc(dt8[:].unsqueeze(2), [128, 8, 64]), op=ALU.mult),
                   reads=[xN, dt8], writes=[xdt])
            P.pool(lambda e: e.tensor_tensor(out=xw[:], in0=xN[:].rearrange("p (h q) -> p h q", h=8), in1=bc(dw8[:].unsqueeze(2), [128, 8, 64]), op=ALU.mult),
                   reads=[xN, dw8], writes=[xw])
            for h in range(8):
                P.pe(lambda e, h=h: e.matmul(psF[:, h * 64:(h + 1) * 64], lhsT=MT[:, h, :], rhs=xdt[:, h, :], start=True, stop=True), reads=[MT, xdt], writes=[psF])
            for c in range(2):
                rows = slice(c * 64, (c + 1) * 64)
                for g in range(2):
                    P.pe(lambda e, c=c, g=g, rows=rows: e.matmul(psG[c][:, g * 256:(g + 1) * 256], lhsT=BNb[rows, g * 128:(g + 1) * 128],
                                                                 rhs=xw[rows, 4 * g:4 * g + 4, :].rearrange("p h q -> p (h q)"), start=True, stop=True),
                         reads=[BNb, xw], writes=[psG[c]])
            for c in range(2):
                P.dve(lambda e, c=c: e.tensor_tensor(out=S[:], in0=S[:], in1=bc(e32[:, 16 + 8 * c:24 + 8 * c].unsqueeze(2), [128, 8, 64]), op=ALU.mult),
                      reads=[S, e32], writes=[S])
                P.dve(lambda e, c=c: e.tensor_tensor(out=S[:].rearrange("p h q -> p (h q)"), in0=S[:].rearrange("p h q -> p (h q)"), in1=psG[c][:], op=ALU.add),
                      reads=[S, psG[c]], writes=[S])
                if c == 0:
                    P.act(lambda e: e.copy(out=Sb[1][:], in_=S[:]), reads=[S], writes=[Sb[1]])
            for c in range(2):
                rows = slice(c * 64, (c + 1) * 64)
                for g in range(2):
                    P.pe(lambda e, c=c, g=g, rows=rows: e.matmul(psC[rows, g * 256:(g + 1) * 256], lhsT=BCb[:, 2 + g, t * 128 + c * 64:t * 128 + (c + 1) * 64],
                                                                 rhs=Sb[c][:, 4 * g:4 * g + 4, :].rearrange("p h q -> p (h q)"), start=True, stop=True),
                         reads=[BCb, Sb[c]], writes=[psC])
            P.act(lambda e: e.copy(out=Sb[0][:], in_=S[:]), reads=[S], writes=[Sb[0]])
            P.dve(lambda e: e.tensor_tensor(out=y1[:].rearrange("p (h q) -> p h q", h=8), in0=psC[:].rearrange("p (h q) -> p h q", h=8),
                                            in1=bc(ea.unsqueeze(2), [128, 8, 64]), op=ALU.mult), reads=[psC, e32], writes=[y1])
            P.dve(lambda e: e.tensor_tensor(out=y1[:], in0=y1[:], in1=psF[:], op=ALU.add), reads=[y1, psF], writes=[y1])
            P.pool(lambda e: e.tensor_tensor(out=y2[:].rearrange("p (h q) -> p h q", h=8), in0=xN[:].rearrange("p (h q) -> p h q", h=8),
                                             in1=bc(dsk[:].unsqueeze(2), [128, 8, 64]), op=ALU.mult), reads=[xN, dsk], writes=[y2])
            P.pool(lambda e: e.tensor_tensor(out=y1[:], in0=y1[:], in1=y2[:], op=ALU.add), reads=[y1, y2], writes=[y1])
            y = yo[t % 2]
            norm_gate(P, y1[:], [y1], tk[:, 0:512], [tk], nwb[:].rearrange("p (g e) -> p g e", g=2), [nwb], 2, 256, y, tmp, True)
            P.dma("sp", ymix.t[tok, 512:1024], y[:], reads=[y], writes=[ymix])
            bg_tick(P, 2)
        bg_end(P)


def stage_gdn(P, C, projT, projN, ymix, prm, l):
    H = 4
    with P.scope():
        cf = [C.f]
        cw = P.sbuf("gdn_cw", [128, 12, 4])
        dtb = P.sbuf("gdn_dtb", [128, 4])
        aneg = P.sbuf("gdn_aneg", [128, 4])
        nwb = P.sbuf("gdn_nwb", [128, 128])
        P.dma("sp", cw[:], prm["gdn_conv_w"].t[l], reads=[prm["gdn_conv_w"]], writes=[cw])
        P.dma("sp", dtb[:], prm["gdn_dt_bias"].t[l:l + 1, :].partition_broadcast(128), reads=[prm["gdn_dt_bias"]], writes=[dtb])
        P.dma("sp", aneg[:], prm["gdn_a_log"].t[l:l + 1, :].partition_broadcast(128), reads=[prm["gdn_a_log"]], writes=[aneg])
        P.dma("sp", nwb[:], prm["gdn_norm_w"].t[l:l + 1, :].partition_broadcast(128), reads=[prm["gdn_norm_w"]], writes=[nwb])
        P.act(lambda e: e.activation(out=aneg[:], in_=aneg[:], func=AF.Exp), reads=[aneg], writes=[aneg])
        P.dve(lambda e: e.tensor_scalar(out=aneg[:], in0=aneg[:], scalar1=-1.0, scalar2=None, op0=ALU.mult), reads=[aneg], writes=[aneg])
        tmp = ng_tmp(P)
        qkvb = P.sbuf("gdn_qkvb", [128, 12, SEQ], BF16)
        with P.scope():
            cvt = P.sbuf("gdn_cvt", [128, 1, SEQ])
            sq = P.sbuf("gdn_sq", [128, SEQ])
            rinv = P.sbuf("gdn_rinv", [128, SEQ])
            pss = [P.psum(f"gdn_pss{i}", [128, 512]) for i in range(4)]
            xpad = [P.sbuf(f"gdn_xpad{i}", [128, SEQ + 3]) for i in range(2)]
            acc = P.sbuf("gdn_acc", [128, SEQ])
            for i in range(2):
                P.pool(lambda e, i=i: e.memset(xpad[i][:, 0:3], 0.0), writes=[xpad[i]])
            for ct in range(12):
                xp = xpad[ct % 2]
                P.dma("sp", xp[:, 3:SEQ + 3], projT.t[PT_GQKV + ct * 128:PT_GQKV + (ct + 1) * 128, :], reads=[projT], writes=[xp])
                P.dve(lambda e, ct=ct, xp=xp: e.tensor_scalar(out=acc[:], in0=xp[:, 0:SEQ], scalar1=cw[:, ct, 0:1], scalar2=None, op0=ALU.mult),
                      reads=[xp, cw], writes=[acc])
                for k in range(1, 4):
                    P.dve(lambda e, ct=ct, xp=xp, k=k: e.scalar_tensor_tensor(out=acc[:], in0=xp[:, k:SEQ + k], scalar=cw[:, ct, k:k + 1], in1=acc[:],
                                                                               op0=ALU.mult, op1=ALU.add), reads=[xp, cw, acc], writes=[acc])
                if ct >= 8:
                    P.act(lambda e, ct=ct: e.activation(out=qkvb[:, ct, :], in_=acc[:], func=AF.Silu), reads=[acc], writes=[qkvb])
                    continue
                P.act(lambda e: e.activation(out=cvt[:, 0, :], in_=acc[:], func=AF.Silu), reads=[acc], writes=[cvt])
                P.act(lambda e: e.activation(out=sq[:], in_=cvt[:, 0, :], func=AF.Square), reads=[cvt], writes=[sq])
                for n in range(4):
                    P.pe(lambda e, n=n: e.matmul(pss[n][:], lhsT=C.c("ones"), rhs=sq[:, n * 512:(n + 1) * 512], start=True, stop=True), reads=[sq] + cf, writes=[pss[n]])
                    P.act(lambda e, n=n: e.activation(out=rinv[:, n * 512:(n + 1) * 512], in_=pss[n][:], func=AF.Sqrt, bias=tmp["eps"][:, 0:1]),
                          reads=[pss[n], tmp["eps"]], writes=[rinv])
                P.dve(lambda e: e.reciprocal(out=rinv[:], in_=rinv[:]), reads=[rinv], writes=[rinv])
                scl = 128.0 ** -0.5 if ct < 4 else 1.0
                P.dve(lambda e, ct=ct, scl=scl: e.scalar_tensor_tensor(out=qkvb[:, ct, :], in0=cvt[:, 0, :], scalar=scl, in1=rinv[:], op0=ALU.mult, op1=ALU.mult),
                      reads=[cvt, rinv], writes=[qkvb])
        dbg(1)
        S = P.sbuf("gdn_S", [128, H, 128])
        Sb = P.sbuf("gdn_Sb", [128, H, 128], BF16)
        P.dve(lambda e: e.memset(S[:], 0.0), writes=[S])
        P.dve(lambda e: e.memset(Sb[:], 0.0), writes=[Sb])
        tokN = [P.sbuf(f"gdn_tokN{i}", [128, 520]) for i in range(3)]

        def f4(name, dt=F32):
            return P.sbuf("gdn_" + name, [128, H, 128], dt)

        b4 = P.sbuf("gdn_b4", [128, 4])
        g4 = P.sbuf("gdn_g4", [128, 4])
        gc8 = P.sbuf("gdn_gc8", [128, 2, 4])
        e16s = [P.sbuf(f"gdn_e16_{i}", [128, 16]) for i in range(2)]
        bg4 = P.sbuf("gdn_bg4", [128, 4])
        Gt, Gs, DTm, Dm, egb = f4("Gt"), f4("Gs"), f4("DTm"), f4("Dm"), f4("egb")
        A, AT, TT, vb, Kg = f4("A"), f4("AT"), f4("TT"), f4("vb"), f4("Kg")
        us = [f4("u0"), f4("u1")]
        X = [f4("X0"), f4("X1")]
        XT = [f4("XT0"), f4("XT1")]
        aqkTs, wTs, qdTs, kends = [[f4(f"{n}{i}", BF16) for i in range(2)] for n in ("aqkT", "wT", "qdT", "kend")]
        vnew = f4("vnew", BF16)
        yo = [P.sbuf(f"gdn_yo{i}", [128, 512]) for i in range(2)]
        B0 = P.psum("gdn_B0", [128, 512])
        B1 = P.psum("gdn_B1", [128, 512])
        B2 = P.psum("gdn_B2", [128, 512])
        B3 = P.psum("gdn_B3", [128, 512])
        B4 = P.psum("gdn_B4", [128, 1024], BF16)
        B5 = P.psum("gdn_B5", [128, 512])
        B6 = P.psum("gdn_B6", [128, 512])
        B7 = P.psum("gdn_B7", [128, 512])

        def v4(ap):
            return ap.rearrange("p (h i) -> p h i", h=H)

        def fl(ap):
            return ap.rearrange("p h i -> p (h i)")

        bg_begin(P)

        def load(t):
            P.dma("sp", tokN[t % 3][:], projN.t[t * 128:(t + 1) * 128, PN_GZ:PN_GZ + 520], reads=[projN], writes=[tokN[t % 3]])

        tri = C.c("tri01")
        su = C.c("su01")
        def pre(t, adv):
            tk = tokN[t % 3]
            e16 = e16s[t % 2]; u = us[t % 2]; aqkT = aqkTs[t % 2]; wT = wTs[t % 2]; qdT = qdTs[t % 2]; kend = kends[t % 2]
            tok = slice(t * 128, (t + 1) * 128)
            P.act(lambda e: e.activation(out=b4[:], in_=tk[:, 512:516], func=AF.Sigmoid), reads=[tk], writes=[b4])
            P.dve(lambda e: e.tensor_tensor(out=g4[:], in0=tk[:, 516:520], in1=dtb[:], op=ALU.add), reads=[tk, dtb], writes=[g4])
            softplus_small(P, g4[:], g4, None, tmp["one"])
            P.dve(lambda e: e.tensor_tensor(out=g4[:], in0=g4[:], in1=aneg[:], op=ALU.mult), reads=[g4, aneg], writes=[g4])
            P.dve(lambda e: e.tensor_tensor(out=Gt[:], in0=bc(g4[:].unsqueeze(2), [128, H, 128]), in1=bc(tri.unsqueeze(1), [128, H, 128]), op=ALU.mult),
                  reads=[g4] + cf, writes=[Gt])
            P.pool(lambda e: e.tensor_tensor(out=Gs[:], in0=bc(g4[:].unsqueeze(2), [128, H, 128]), in1=bc(su.unsqueeze(1), [128, H, 128]), op=ALU.mult),
                   reads=[g4] + cf, writes=[Gs])
            P.dve(lambda e: e.tensor_tensor(out=gc8[:], in0=bc(g4[:].unsqueeze(1), [128, 2, 4]), in1=bc(C.c("chunkind").unsqueeze(2), [128, 2, 4]), op=ALU.mult),
                  reads=[g4] + cf, writes=[gc8])
            P.pe(lambda e: e.matmul(B0[:], lhsT=su, rhs=fl(Gt[:]), start=True, stop=True), reads=[Gt] + cf, writes=[B0])
            P.pe(lambda e: e.matmul(B1[:], lhsT=tri, rhs=fl(Gs[:]), start=True, stop=True), reads=[Gs] + cf, writes=[B1])
            P.pe(lambda e: e.matmul(B2[:], lhsT=C.c("ones"), rhs=fl(Gt[:]), start=True, stop=True), reads=[Gt] + cf, writes=[B2])
            P.pe(lambda e: e.matmul(B3[:, 0:4], lhsT=tri, rhs=g4[:], start=True, stop=True), reads=[g4] + cf, writes=[B3])
            P.pe(lambda e: e.matmul(B3[:, 4:8], lhsT=su, rhs=g4[:], start=True, stop=True), reads=[g4] + cf, writes=[B3])
            P.pe(lambda e: e.matmul(B3[:, 8:16], lhsT=C.c("ones"), rhs=gc8[:].rearrange("p c h -> p (c h)"), start=True, stop=True), reads=[gc8] + cf, writes=[B3])
            P.act(lambda e: e.activation(out=fl(DTm[:]), in_=B0[:], func=AF.Exp), reads=[B0], writes=[DTm])
            P.act(lambda e: e.activation(out=fl(Dm[:]), in_=B1[:], func=AF.Exp), reads=[B1], writes=[Dm])
            P.act(lambda e: e.activation(out=fl(egb[:]), in_=B2[:], func=AF.Exp), reads=[B2], writes=[egb])
            P.act(lambda e: e.activation(out=e16[:], in_=B3[:, 0:16], func=AF.Exp), reads=[B3], writes=[e16])
            P.pool(lambda e: e.tensor_tensor(out=DTm[:], in0=DTm[:], in1=bc(tri.unsqueeze(1), [128, H, 128]), op=ALU.mult), reads=[DTm] + cf, writes=[DTm])
            P.pool(lambda e: e.tensor_tensor(out=Dm[:], in0=Dm[:], in1=bc(su.unsqueeze(1), [128, H, 128]), op=ALU.mult), reads=[Dm] + cf, writes=[Dm])
            P.dve(lambda e: e.tensor_tensor(out=bg4[:], in0=b4[:], in1=e16[:, 0:4], op=ALU.mult), reads=[b4, e16], writes=[bg4])
            dbg(2)
            adv()
            for h in range(H):
                P.pe(lambda e, h=h: e.transpose(out=B4[:, h * 128:(h + 1) * 128], in_=qkvb[:, 4 + h, tok], identity=C.identb[:]), reads=[qkvb, C.identb], writes=[B4])
            for h in range(H):
                P.pe(lambda e, h=h: e.transpose(out=B4[:, 512 + h * 128:512 + (h + 1) * 128], in_=qkvb[:, 8 + h, tok], identity=C.identb[:]), reads=[qkvb, C.identb], writes=[B4])
            P.dve(lambda e: e.tensor_tensor(out=vb[:], in0=v4(B4[:, 512:1024]), in1=bc(b4[:].unsqueeze(2), [128, H, 128]), op=ALU.mult), reads=[B4, b4], writes=[vb])
            P.dve(lambda e: e.tensor_tensor(out=Kg[:], in0=v4(B4[:, 0:512]), in1=bc(bg4[:].unsqueeze(2), [128, H, 128]), op=ALU.mult), reads=[B4, bg4], writes=[Kg])
            P.dve(lambda e: e.tensor_tensor(out=kend[:], in0=v4(B4[:, 0:512]), in1=bc(e16[:, 4:8].unsqueeze(2), [128, H, 128]), op=ALU.mult), reads=[B4, e16], writes=[kend])
            dbg(3)
            adv()
            for h in range(H):
                P.pe(lambda e, h=h: e.matmul(B0[:, h * 128:(h + 1) * 128], lhsT=qkvb[:, 4 + h, tok], rhs=qkvb[:, 4 + h, tok], start=True, stop=True), reads=[qkvb], writes=[B0])
            for h in range(H):
                P.pe(lambda e, h=h: e.matmul(B1[:, h * 128:(h + 1) * 128], lhsT=qkvb[:, 4 + h, tok], rhs=qkvb[:, h, tok], start=True, stop=True), reads=[qkvb], writes=[B1])
            P.dve(lambda e: e.tensor_tensor(out=A[:], in0=v4(B0[:]), in1=Dm[:], op=ALU.mult), reads=[B0, Dm], writes=[A])
            P.dve(lambda e: e.tensor_tensor(out=A[:], in0=A[:], in1=bc(b4[:].unsqueeze(2), [128, H, 128]), op=ALU.mult), reads=[A, b4], writes=[A])
            P.dve(lambda e: e.tensor_tensor(out=aqkT[:], in0=v4(B1[:]), in1=DTm[:], op=ALU.mult), reads=[B1, DTm], writes=[aqkT])
            P.pool(lambda e: e.tensor_tensor(out=qdT[:], in0=qkvb[:, 0:4, tok], in1=egb[:], op=ALU.mult), reads=[qkvb, egb], writes=[qdT])
            dbg(4)
            adv()
            for h in range(H):
                P.pe(lambda e, h=h: e.transpose(out=B2[:, h * 128:(h + 1) * 128], in_=A[:, h, :], identity=C.c("ident")), reads=[A] + cf, writes=[B2])
            dbg(4.3)
            adv()
            P.act(lambda e: e.copy(out=fl(AT[:]), in_=B2[:]), reads=[B2], writes=[AT])
            dbg(4.6)
            adv()
            P.dve(lambda e: e.scalar_tensor_tensor(out=TT[:], in0=v4(B2[:]), scalar=-1.0, in1=bc(C.c("ident").unsqueeze(1), [128, H, 128]), op0=ALU.mult, op1=ALU.add),
                  reads=[B2] + cf, writes=[TT])
            dbg(5)
            adv()
            Xc, XTc = A, AT
            for k in range(1, 6):
                Xn, XTn = X[k % 2], XT[k % 2]
                for h in range(H):
                    P.pe(lambda e, h=h, Xc=Xc, XTc=XTc: e.matmul(B0[:, h * 128:(h + 1) * 128], lhsT=XTc[:, h, :], rhs=Xc[:, h, :], start=True, stop=True),
                         reads=[Xc, XTc], writes=[B0])
                if k < 5:
                    for h in range(H):
                        P.pe(lambda e, h=h, Xc=Xc, XTc=XTc: e.matmul(B1[:, h * 128:(h + 1) * 128], lhsT=Xc[:, h, :], rhs=XTc[:, h, :], start=True, stop=True),
                             reads=[Xc, XTc], writes=[B1])
                P.act(lambda e, Xn=Xn: e.copy(out=fl(Xn[:]), in_=B0[:]), reads=[B0], writes=[Xn])
                if k < 5:
                    P.dve(lambda e, XTn=XTn: e.tensor_copy(out=fl(XTn[:]), in_=B1[:]), reads=[B1], writes=[XTn])
                for h in range(H):
                    P.pe(lambda e, h=h, Xn=Xn: e.matmul(B2[:, h * 128:(h + 1) * 128], lhsT=Xn[:, h, :], rhs=TT[:, h, :], start=True, stop=True),
                         reads=[Xn, TT], writes=[B2])
                P.dve(lambda e: e.tensor_tensor(out=fl(TT[:]), in0=fl(TT[:]), in1=B2[:], op=ALU.add), reads=[TT, B2], writes=[TT])
                adv()
                Xc, XTc = Xn, XTn
            dbg(6)
            adv()
            for h in range(H):
                P.pe(lambda e, h=h: e.matmul(B0[:, h * 128:(h + 1) * 128], lhsT=TT[:, h, :], rhs=vb[:, h, :], start=True, stop=True), reads=[TT, vb], writes=[B0])
            for h in range(H):
                P.pe(lambda e, h=h: e.matmul(B1[:, h * 128:(h + 1) * 128], lhsT=Kg[:, h, :], rhs=TT[:, h, :], start=True, stop=True), reads=[TT, Kg], writes=[B1])
            P.act(lambda e: e.copy(out=fl(u[:]), in_=B0[:]), reads=[B0], writes=[u])
            P.dve(lambda e: e.tensor_copy(out=fl(wT[:]), in_=B1[:]), reads=[B1], writes=[wT])

        def scan(t):
            tk = tokN[t % 3]
            tok = slice(t * 128, (t + 1) * 128)
            e16 = e16s[t % 2]; u = us[t % 2]; aqkT = aqkTs[t % 2]; wT = wTs[t % 2]; qdT = qdTs[t % 2]; kend = kends[t % 2]
            for c in range(2):
                rows = slice(c * 64, (c + 1) * 64)
                for h in range(H):
                    P.pe(lambda e, h=h, rows=rows: e.matmul(B5[rows, h * 128:(h + 1) * 128], lhsT=wT[:, h, rows], rhs=Sb[:, h, :], start=True, stop=True),
                         reads=[wT, Sb], writes=[B5])
                P.dve(lambda e, rows=rows: e.tensor_tensor(out=fl(vnew[rows]), in0=fl(u[rows]), in1=B5[rows, :], op=ALU.subtract), reads=[u, B5], writes=[vnew])
                yield
                for h in range(H):
                    P.pe(lambda e, h=h, rows=rows: e.matmul(B7[rows, h * 128:(h + 1) * 128], lhsT=qdT[:, h, rows], rhs=Sb[:, h, :], start=True, stop=False),
                         reads=[qdT, Sb], writes=[B7])
                    P.pe(lambda e, h=h, rows=rows: e.matmul(B7[rows, h * 128:(h + 1) * 128], lhsT=aqkT[rows, h, rows], rhs=vnew[rows, h, :], start=False, stop=True),
                         reads=[aqkT, vnew], writes=[B7])
                for h in range(H):
                    P.pe(lambda e, h=h, rows=rows: e.matmul(B6[:, h * 128:(h + 1) * 128], lhsT=kend[rows, h, :], rhs=vnew[rows, h, :], start=True, stop=True),
                         reads=[kend, vnew], writes=[B6])
                yield
                P.dve(lambda e, c=c: e.tensor_tensor(out=S[:], in0=S[:], in1=bc(e16[:, 8 + 4 * c:12 + 4 * c].unsqueeze(2), [128, H, 128]), op=ALU.mult),
                      reads=[S, e16], writes=[S])
                P.dve(lambda e: e.tensor_tensor(out=fl(S[:]), in0=fl(S[:]), in1=B6[:], op=ALU.add), reads=[S, B6], writes=[S])
                P.act(lambda e: e.copy(out=Sb[:], in_=S[:]), reads=[S], writes=[Sb])
            yield
            y = yo[t % 2]
            norm_gate(P, B7[:], [B7], tk[:, 0:512], [tk], bc(nwb[:].unsqueeze(1), [128, 4, 128]), [nwb], 4, 128, y, tmp, False)
            P.dma("sp", ymix.t[tok, 1024:1536], y[:], reads=[y], writes=[ymix])
            yield

        load(0)
        load(1)
        pre(0, lambda: None)
        for t in range(NT):
            if t + 2 < NT:
                load(t + 2)
            gen = scan(t)
            adv = (lambda gen=gen: next(gen, None))
            if t + 1 < NT:
                pre(t + 1, adv)
            for _ in gen:
                pass
            bg_tick(P, 2)
        bg_end(P)


NBIG = 30000.0


def t5_bucket_np(rel):
    n = np.maximum(rel, 0)
    exact = 16
    large = exact + (np.log(np.maximum(n, 1).astype(np.float32) / np.float32(exact)) / np.float32(math.log(128 / 16)) * np.float32(32 - exact)).astype(np.int32)
    return np.where(n < exact, n, np.minimum(large, 31)).astype(np.int64)


def nsa_host_tables(rel_bias):
    rb = np.asarray(rel_bias, np.float32)
    ki = np.arange(128)[:, None]
    qi = np.arange(128)[None, :]
    r0 = qi - ki
    r128 = 128 + qi - ki
    qq = np.arange(128)[:, None]
    mm = np.arange(248)[None, :]
    rc = qq - 16 * (mm - 120) - 31
    tab = np.concatenate([
        rb[t5_bucket_np(r0)].transpose(0, 2, 1).reshape(128, 8 * 128),
        rb[t5_bucket_np(r128)].transpose(0, 2, 1).reshape(128, 8 * 128),
        rb[t5_bucket_np(rc)].transpose(0, 2, 1).reshape(128, 8 * 248)], axis=1)
    t31 = np.broadcast_to(rb[31][None, :], (128, 8)).copy()
    return np.ascontiguousarray(tab, np.float32), np.ascontiguousarray(t31, np.float32)


def nsa_host_consts():
    ki = np.arange(128)[:, None]
    qi = np.arange(128)[None, :]
    qq = np.arange(128)[:, None]
    mm = np.arange(248)[None, :]
    rc = qq - 16 * (mm - 120) - 31
    m0 = np.where(qi - ki >= 0, 0.0, -NBIG)
    msk = np.concatenate([
        np.broadcast_to(m0[:, None, :], (128, 8, 128)).reshape(128, -1),
        np.zeros((128, 8 * 128)),
        np.broadcast_to(np.where(rc >= 0, 0.0, -NBIG)[:, None, :], (128, 8, 248)).reshape(128, -1)], axis=1)
    mtri = np.where(ki > qi, 0.0, -NBIG)
    k = np.arange(128)[:, None]
    j = np.arange(32)[None, :]
    ov = ((16 * k <= 64 * j + 63) & (16 * k + 31 >= 64 * j) & (k < 127)).astype(np.float32)
    keep = np.zeros((128, 16, 32)); addc = np.zeros((128, 16, 32))
    for qb in range(16):
        cur = (2 * qb + (np.arange(128) >= 64))[:, None]
        blk = np.arange(32)[None, :]
        forced = (blk == 0) | (blk == cur) | (blk == cur - 1)
        fut = blk > cur
        keep[:, qb, :] = (~forced & ~fut)
        addc[:, qb, :] = np.where(fut, -1e30, np.where(forced, 1e9, 0.0))
    E = np.zeros((128, 2048))
    E[:32] = (np.arange(2048)[None, :] // 64) == np.arange(32)[:, None]
    parts = dict(msk=msk, mtri=mtri, ov=ov, keep=keep.reshape(128, -1), addc=addc.reshape(128, -1), E=E)
    cols = {}
    o = 0
    arrs = []
    for n, a in parts.items():
        cols[n] = (o, a.shape[1]); o += a.shape[1]; arrs.append(a.astype(np.float32))
    return np.concatenate(arrs, axis=1), cols


NSA_CST_NP, NSA_CST_COLS = nsa_host_consts()


def stage_nsa(P, C, projT, projN, ymix, prm, l):
    G, R = 2, 4
    with P.scope():
        cf = [C.f]
        tmp = ng_tmp(P)
        one = tmp["one"]
        Bn0 = P.sbuf("nsa_Bn0", [128, 8, 128], BF16)
        Bn1 = P.sbuf("nsa_Bn1", [128, 8, 128], BF16)
        Mtri = P.sbuf("nsa_Mtri", [128, 4, 128], BF16)
        FT = P.sbuf("nsa_FT", [128, 8, 248])
        ovb = P.sbuf("nsa_ovb", [128, 32], BF16)
        keep = P.sbuf("nsa_keep", [128, 16, 32])
        addc = P.sbuf("nsa_addc", [128, 16, 32])
        Eb = P.sbuf("nsa_Eb", [32, 2048], BF16)
        ncst = prm["nsa_cst"]
        cc = NSA_CST_COLS
        P.dma("sp", keep[:].rearrange("p a b -> p (a b)"), ncst.t[:, cc["keep"][0]:cc["keep"][0] + 512], reads=[ncst], writes=[keep])
        P.dma("sp", addc[:].rearrange("p a b -> p (a b)"), ncst.t[:, cc["addc"][0]:cc["addc"][0] + 512], reads=[ncst], writes=[addc])
        with P.scope():
            tb = P.sbuf("nsa_tb", [128, 4032])
            mk = P.sbuf("nsa_mk", [128, 4032])
            t31 = P.sbuf("nsa_t31", [128, 8])
            st = P.sbuf("nsa_st", [128, 2048])
            P.dma("sp", tb[:], prm["nsa_tab"].t[:, :], reads=[prm["nsa_tab"]], writes=[tb])
            P.dma("sp", mk[:], ncst.t[:, cc["msk"][0]:cc["msk"][0] + 4032], reads=[ncst], writes=[mk])
            P.dma("sp", t31[:], prm["nsa_t31"].t[:, :], reads=[prm["nsa_t31"]], writes=[t31])
            for (o, w, dst) in ((0, 128, Bn0), (1024, 128, Bn1), (2048, 248, FT)):
                v = tb[:, o:o + 8 * w].rearrange("p (h x) -> p h x", h=8)
                P.dve(lambda e, v=v, w=w: e.tensor_tensor(out=v, in0=v, in1=bc(t31[:].unsqueeze(2), [128, 8, w]), op=ALU.subtract), reads=[tb, t31], writes=[tb])
                P.dve(lambda e, v=v, w=w, o=o, dst=dst: e.tensor_tensor(out=dst[:], in0=v, in1=mk[:, o:o + 8 * w].rearrange("p (h x) -> p h x", h=8), op=ALU.add),
                      reads=[tb, mk], writes=[dst])
            P.dma("sp", st[:, 0:128], ncst.t[:, cc["mtri"][0]:cc["mtri"][0] + 128], reads=[ncst], writes=[st])
            P.dve(lambda e: e.tensor_copy(out=Mtri[:], in_=bc(st[:, 0:128].unsqueeze(1), [128, 4, 128])), reads=[st], writes=[Mtri])
            P.dma("sp", st[:, 128:160], ncst.t[:, cc["ov"][0]:cc["ov"][0] + 32], reads=[ncst], writes=[st])
            P.dve(lambda e: e.tensor_copy(out=ovb[:], in_=st[:, 128:160]), reads=[st], writes=[ovb])
            P.dma("sp", st[0:32, :], ncst.t[0:32, cc["E"][0]:cc["E"][0] + 2048], reads=[ncst], writes=[st])
            P.dve(lambda e: e.tensor_copy(out=Eb[:], in_=st[0:32, :]), reads=[st], writes=[Eb])
        dbg(0.1)
        qTb = P.sbuf("nsa_qTb", [64, 8, SEQ], BF16)
        ksT = P.sbuf("nsa_ksT", [64, 2, SEQ], BF16)
        kwT = P.sbuf("nsa_kwT", [64, 2, SEQ], BF16)
        vsb = P.sbuf("nsa_vsb", [128, 16, 2, 65], BF16)
        vwb = P.sbuf("nsa_vwb", [128, 16, 2, 65], BF16)
        gts = P.sbuf("nsa_gts", [128, 16, 24])
        kcT = P.sbuf("nsa_kcT", [64, 2, 128], BF16)
        vcx = P.sbuf("nsa_vcx", [128, 2, 96], BF16)
        with P.scope():
            stg = [P.sbuf(f"nsa_stg{i}", [64, SEQ]) for i in range(2)]
            n = 0
            for h in range(8):
                s_ = stg[n % 2]; n += 1
                P.dma("sp", s_[:], projT.t[PT_NQ + h * 64:PT_NQ + (h + 1) * 64, :], reads=[projT], writes=[s_])
                P.act(lambda e, h=h, s_=s_: e.activation(out=qTb[:, h, :], in_=s_[:], func=AF.Copy, scale=0.125), reads=[s_], writes=[qTb])
            for (r0, dst) in ((PT_NKS, ksT), (PT_NKW, kwT)):
                for g in range(2):
                    s_ = stg[n % 2]; n += 1
                    P.dma("sp", s_[:], projT.t[r0 + g * 64:r0 + (g + 1) * 64, :], reads=[projT], writes=[s_])
                    P.dve(lambda e, g=g, s_=s_, dst=dst: e.tensor_copy(out=dst[:, g, :], in_=s_[:]), reads=[s_], writes=[dst])
            tn = P.sbuf("nsa_tn", [128, 16, 280])
            P.dma("sp", tn[:], projN.t[:, 0:280].rearrange("(t p) c -> p t c", p=128), reads=[projN], writes=[tn])
            P.pool(lambda e: e.memset(vsb[:], 1.0), writes=[vsb])
            P.pool(lambda e: e.memset(vwb[:], 1.0), writes=[vwb])
            P.dve(lambda e: e.tensor_copy(out=vsb[:, :, :, 0:64], in_=tn[:, :, 0:128].rearrange("p t (g d) -> p t g d", g=2)), reads=[tn], writes=[vsb])
            P.dve(lambda e: e.tensor_copy(out=vwb[:, :, :, 0:64], in_=tn[:, :, 128:256].rearrange("p t (g d) -> p t g d", g=2)), reads=[tn], writes=[vwb])
            P.act(lambda e: e.activation(out=gts[:], in_=tn[:, :, 256:280], func=AF.Sigmoid), reads=[tn], writes=[gts])
            dbg(0.2)
            tT = P.sbuf("nsa_tT", [64, 2, SEQ])
            tG = P.sbuf("nsa_tG", [64, 2, 16, 129], BF16)
            P.pool(lambda e: e.memset(tG[:], 0.0), writes=[tG])
            w1f = P.sbuf("nsa_w1f", [64, 32, 64])
            w1b = P.sbuf("nsa_w1b", [64, 32, 64], BF16)
            w2f = P.sbuf("nsa_w2f", [64, 64])
            w2b = P.sbuf("nsa_w2b", [64, 64], BF16)
            posT = P.sbuf("nsa_posT", [64, 32])
            posb = P.sbuf("nsa_posb", [64, 32], BF16)
            cvec = P.sbuf("nsa_cvec", [64, 1])
            hid = P.sbuf("nsa_hid", [64, 2, 128], BF16)
            ps_h = P.psum("nsa_ps_h", [64, 512])
            ps_c = P.psum("nsa_ps_c", [64, 8])
            ps_o = P.psum("nsa_ps_o", [128, 512])
            P.dve(lambda e: e.memset(hid[:], 0.0), writes=[hid])
            for kv in range(2):
                r0 = PT_NKC if kv == 0 else PT_NVC
                for g in range(2):
                    P.dma("sp", tT[:, g, :], projT.t[r0 + g * 64:r0 + (g + 1) * 64, :], reads=[projT], writes=[tT])
                    P.dve(lambda e, g=g: e.tensor_copy(out=tG[:, g, :, 0:128], in_=tT[:, g, :].rearrange("p (n s) -> p s n", s=16)), reads=[tT], writes=[tG])
                w1src = prm["nsa_cmp_w1"].t[l, kv].rearrange("(j d) o -> d j o", d=64)
                P.dma("sp", w1f[:], w1src, reads=[prm["nsa_cmp_w1"]], writes=[w1f])
                P.pool(lambda e: e.tensor_copy(out=w1b[:], in_=w1f[:]), reads=[w1f], writes=[w1b])
                P.dma("sp", w2f[:], prm["nsa_cmp_w2"].t[l, kv], reads=[prm["nsa_cmp_w2"]], writes=[w2f])
                P.dve(lambda e: e.tensor_copy(out=w2b[:], in_=w2f[:]), reads=[w2f], writes=[w2b])
                P.dma("sp", posT[:], prm["nsa_cmp_pos"].t[l, kv], reads=[prm["nsa_cmp_pos"]], writes=[posT])
                P.dve(lambda e: e.tensor_copy(out=posb[:], in_=posT[:]), reads=[posT], writes=[posb])
                for j in range(32):
                    P.pe(lambda e, j=j: e.matmul(ps_c[:, 0:1], lhsT=w1b[:, j, :], rhs=posb[:, j:j + 1], start=(j == 0), stop=(j == 31)), reads=[w1b, posb], writes=[ps_c])
                P.dve(lambda e: e.tensor_copy(out=cvec[:], in_=ps_c[:, 0:1]), reads=[ps_c], writes=[cvec])
                dbg(0.3 + 0.3 * kv)
                for g in range(2):
                    rows = slice(g * 64, (g + 1) * 64)
                    dbg(0.32 + 0.03 * g + 0.3 * kv)
                    for j in range(32):
                        P.pe(lambda e, j=j, g=g, rows=rows: e.matmul(ps_h[:, g * 128:(g + 1) * 128], lhsT=w1b[:, j, :], rhs=tG[:, g, j % 16, (j // 16):(j // 16) + 128],
                                                                    start=(j == 0), stop=(j == 31)), reads=[w1b, tG], writes=[ps_h])
                for g in range(2):
                    P.act(lambda e, g=g: e.activation(out=hid[:, g, :], in_=ps_h[:, g * 128:(g + 1) * 128], func=AF.Silu, bias=cvec[:, 0:1]),
                          reads=[ps_h, cvec], writes=[hid])
                dbg(0.4 + 0.3 * kv)
                if kv == 0:
                    P.pe(lambda e: e.matmul(ps_h[:, 256:512], lhsT=w2b[:], rhs=hid[:].rearrange("o g n -> o (g n)"), start=True, stop=True), reads=[w2b, hid], writes=[ps_h])
                    P.dve(lambda e: e.tensor_copy(out=kcT[:].rearrange("o g n -> o (g n)"), in_=ps_h[:, 256:512]), reads=[ps_h], writes=[kcT])
                else:
                    for g in range(2):
                        P.pe(lambda e, g=g: e.matmul(ps_o[:, g * 64:(g + 1) * 64], lhsT=hid[:, g, :], rhs=w2b[:], start=True, stop=True), reads=[w2b, hid], writes=[ps_o])
                    P.dve(lambda e: e.tensor_copy(out=vcx[:, :, 0:64], in_=ps_o[:, 0:128].rearrange("p (g d) -> p g d", g=2)), reads=[ps_o], writes=[vcx])
                    P.dve(lambda e: e.tensor_copy(out=vcx[:, :, 64:96], in_=bc(ovb[:].unsqueeze(1), [128, 2, 32])), reads=[ovb], writes=[vcx])
        dbg(1)
        ps_sc = [P.psum(f"nsa_ps_sc{i}", [128, 512]) for i in range(2)]
        ps_os = P.psum("nsa_ps_os", [128, 512])
        ps_ow = P.psum("nsa_ps_ow", [128, 512])
        ps_ocs = [P.psum(f"nsa_ps_oc{i}", [128, 512]) for i in range(2)]
        ps_cs = P.psum("nsa_ps_cs", [128, 512])
        ps_tr = P.psum("nsa_ps_tr", [128, 1024], BF16)
        sc = P.sbuf("nsa_sc", [128, 4, 128])
        ssum = P.sbuf("nsa_ssum", [128, 4])
        pnb = P.sbuf("nsa_pnb", [128, 4, 128], BF16)
        pT = P.sbuf("nsa_pT", [128, 4, 128], BF16)
        imp = P.sbuf("nsa_imp", [128, 32])
        mx8 = P.sbuf("nsa_mx8", [128, 8])
        negm = P.sbuf("nsa_negm", [128, 32], BF16)
        negTs = [P.sbuf(f"nsa_negT{i}", [32, 4, 128], BF16) for i in range(2)]
        eT = [P.sbuf(f"nsa_eT{i}", [128, 4, 128], BF16) for i in range(2)]
        cs = P.sbuf("nsa_cs", [128, 3, 4])
        ya = P.sbuf("nsa_ya", [128, 4, 64])
        yb = P.sbuf("nsa_yb", [128, 4, 64])
        yt = [P.sbuf(f"nsa_yt{i}", [128, 512]) for i in range(2)]
        nsc = 0
        bg_begin(P)

        def v3(ap, r=4):
            return ap.rearrange("p (r q) -> p r q", r=r)

        def phaseA(qb, g, slot):
            qtok = slice(qb * 128, (qb + 1) * 128)
            hs = slice(4 * g, 4 * g + 4)
            ps_oc_s = ps_ocs[slot]
            negT_s = negTs[slot]
            for r in range(R):
                P.pe(lambda e, r=r: e.matmul(ps_cs[:, r * 128:(r + 1) * 128], lhsT=qTb[:, 4 * g + r, qtok], rhs=kcT[:, g, :], start=True, stop=True),
                     reads=[qTb, kcT], writes=[ps_cs])
            m0 = 120 - 8 * qb
            P.dve(lambda e: e.tensor_tensor(out=sc[:], in0=v3(ps_cs[:]), in1=FT[:, hs, m0:m0 + 128], op=ALU.add), reads=[ps_cs, FT], writes=[sc])
            P.act(lambda e: e.activation(out=sc[:], in_=sc[:], func=AF.Exp), reads=[sc], writes=[sc])
            P.dve(lambda e: e.tensor_reduce(out=ssum[:], in_=sc[:], axis=AX.X, op=ALU.add), reads=[sc], writes=[ssum])
            P.dve(lambda e: e.tensor_scalar(out=ssum[:], in0=ssum[:], scalar1=1e-30, scalar2=None, op0=ALU.max), reads=[ssum], writes=[ssum])
            P.dve(lambda e: e.reciprocal(out=ssum[:], in_=ssum[:]), reads=[ssum], writes=[ssum])
            P.dve(lambda e: e.tensor_tensor(out=pnb[:], in0=sc[:], in1=bc(ssum[:].unsqueeze(2), [128, 4, 128]), op=ALU.mult), reads=[sc, ssum], writes=[pnb])
            yield
            for r in range(R):
                P.pe(lambda e, r=r: e.transpose(out=ps_tr[:, r * 128:(r + 1) * 128], in_=pnb[:, r, :], identity=C.identb[:]), reads=[pnb, C.identb], writes=[ps_tr])
            P.act(lambda e: e.copy(out=pT[:].rearrange("p r q -> p (r q)"), in_=ps_tr[:, 0:512]), reads=[ps_tr], writes=[pT])
            yield
            for r in range(R):
                P.pe(lambda e, r=r: e.matmul(ps_oc_s[:, r * 96:(r + 1) * 96], lhsT=pT[:, r, :], rhs=vcx[:, g, :], start=True, stop=True), reads=[pT, vcx], writes=[ps_oc_s])
            oc4 = ps_oc_s[:, 0:384].rearrange("p (r x) -> p r x", r=4)
            P.dve(lambda e: e.tensor_reduce(out=imp[:], in_=oc4[:, :, 64:96].rearrange("p r j -> p j r"), axis=AX.X, op=ALU.add), reads=[ps_oc_s], writes=[imp])
            P.dve(lambda e: e.tensor_tensor(out=imp[:], in0=imp[:], in1=keep[:, qb, :], op=ALU.mult), reads=[imp, keep], writes=[imp])
            P.dve(lambda e: e.tensor_tensor(out=imp[:], in0=imp[:], in1=addc[:, qb, :], op=ALU.add), reads=[imp, addc], writes=[imp])
            P.dve(lambda e: e.max(out=mx8[:], in_=imp[:]), reads=[imp], writes=[mx8])
            P.dve(lambda e: e.tensor_scalar(out=imp[:], in0=imp[:], scalar1=mx8[:, 7:8], scalar2=None, op0=ALU.is_ge), reads=[imp, mx8], writes=[imp])
            P.dve(lambda e: e.tensor_scalar(out=negm[:], in0=imp[:], scalar1=-1.0, scalar2=NBIG, op0=ALU.add, op1=ALU.mult), reads=[imp], writes=[negm])
            yield
            P.pe(lambda e: e.transpose(out=ps_tr[0:32, 512:640], in_=negm[:], identity=C.identb[:]), reads=[negm, C.identb], writes=[ps_tr])
            P.dve(lambda e: e.tensor_copy(out=negT_s[:], in_=bc(ps_tr[0:32, 512:640].unsqueeze(1), [32, 4, 128])), reads=[ps_tr], writes=[negT_s])

        def phaseB(qb, g, slot, gen):
            nonlocal nsc
            it = 0
            qtok = slice(qb * 128, (qb + 1) * 128)
            hs = slice(4 * g, 4 * g + 4)
            y = yt[qb % 2]
            ps_oc_s = ps_ocs[slot]
            negT_s = negTs[slot]
            oc4 = ps_oc_s[:, 0:384].rearrange("p (r x) -> p r x", r=4)
            kt0 = max(0, qb - 4)
            iters = [("s", kt) for kt in range(qb + 1)] + [("w", kt) for kt in range(kt0, qb + 1)]
            slots = []

            def scores(ii):
                nonlocal nsc
                br, kt = iters[ii]
                ps = ps_sc[nsc % 2]; et = eT[nsc % 2]; nsc += 1
                slots.append((ps, et))
                ktok = slice(kt * 128, (kt + 1) * 128)
                if br == "s":
                    near = kt >= qb - 1
                    P.pe(lambda e: e.matmul(v3(ps[:]), lhsT=ksT[:, g, ktok], rhs=qTb[:, hs, qtok], start=True, stop=False), reads=[ksT, qTb], writes=[ps])
                    P.pe(lambda e: e.matmul(v3(ps[:]), lhsT=Eb[:, ktok], rhs=negT_s[:], start=False, stop=not near), reads=[Eb, negT_s], writes=[ps])
                    if near:
                        Bn = Bn0 if kt == qb else Bn1
                        P.pe(lambda e: e.matmul(v3(ps[:]), lhsT=C.identb[:], rhs=Bn[:, hs, :], start=False, stop=True), reads=[Bn, C.identb], writes=[ps])
                else:
                    dl = qb - kt
                    extra = {0: Bn0[:, hs, :], 1: Bn1[:, hs, :], 4: Mtri[:]}.get(dl)
                    P.pe(lambda e: e.matmul(v3(ps[:]), lhsT=kwT[:, g, ktok], rhs=qTb[:, hs, qtok], start=True, stop=extra is None), reads=[kwT, qTb], writes=[ps])
                    if extra is not None:
                        P.pe(lambda e: e.matmul(v3(ps[:]), lhsT=C.identb[:], rhs=extra, start=False, stop=True), reads=[Bn0, Bn1, Mtri, C.identb], writes=[ps])

            scores(0)
            for ii, (br, kt) in enumerate(iters):
                if ii + 1 < len(iters):
                    scores(ii + 1)
                ps, et = slots[ii]
                P.act(lambda e: e.activation(out=et[:].rearrange("p r q -> p (r q)"), in_=ps[:], func=AF.Exp), reads=[ps], writes=[et])
                if br == "s":
                    for r in range(R):
                        P.pe(lambda e, r=r: e.matmul(ps_os[:, r * 65:(r + 1) * 65], lhsT=et[:, r, :], rhs=vsb[:, kt, g, :], start=(kt == 0 and r == 0), stop=(kt == qb), skip_group_check=True),
                             reads=[et, vsb], writes=[ps_os])
                else:
                    for r in range(R):
                        P.pe(lambda e, r=r: e.matmul(ps_ow[:, r * 65:(r + 1) * 65], lhsT=et[:, r, :], rhs=vwb[:, kt, g, :], start=(kt == kt0 and r == 0), stop=(kt == qb), skip_group_check=True),
                             reads=[et, vwb], writes=[ps_ow])
                it += 1
                if it % 3 == 2 and gen is not None:
                    next(gen, None)
            if gen is not None:
                for _ in gen:
                    pass
            os4 = ps_os[:, 0:260].rearrange("p (r x) -> p r x", r=4)
            ow4 = ps_ow[:, 0:260].rearrange("p (r x) -> p r x", r=4)
            g3 = gts[:, qb, 12 * g:12 * g + 12].rearrange("p (r b) -> p b r", b=3)
            P.dve(lambda e: e.reciprocal(out=cs[:, 1, :], in_=os4[:, :, 64]), reads=[ps_os], writes=[cs])
            P.dve(lambda e: e.reciprocal(out=cs[:, 2, :], in_=ow4[:, :, 64]), reads=[ps_ow], writes=[cs])
            P.dve(lambda e: e.memset(cs[:, 0, :], 1.0), writes=[cs])
            P.dve(lambda e: e.tensor_tensor(out=cs[:], in0=cs[:], in1=g3, op=ALU.mult), reads=[cs, gts], writes=[cs])
            P.dve(lambda e: e.tensor_tensor(out=ya[:], in0=oc4[:, :, 0:64], in1=bc(cs[:, 0, :].unsqueeze(2), [128, 4, 64]), op=ALU.mult), reads=[ps_oc_s, cs], writes=[ya])
            P.dve(lambda e: e.tensor_tensor(out=yb[:], in0=os4[:, :, 0:64], in1=bc(cs[:, 1, :].unsqueeze(2), [128, 4, 64]), op=ALU.mult), reads=[ps_os, cs], writes=[yb])
            P.pool(lambda e: e.tensor_tensor(out=ya[:], in0=ya[:], in1=yb[:], op=ALU.add), reads=[ya, yb], writes=[ya])
            P.dve(lambda e: e.tensor_tensor(out=yb[:], in0=ow4[:, :, 0:64], in1=bc(cs[:, 2, :].unsqueeze(2), [128, 4, 64]), op=ALU.mult), reads=[ps_ow, cs], writes=[yb])
            P.pool(lambda e: e.tensor_tensor(out=y[:, g * 256:(g + 1) * 256].rearrange("p (r d) -> p r d", r=4), in0=ya[:], in1=yb[:], op=ALU.add), reads=[ya, yb], writes=[y])

        blocks = [(qb, g) for qb in range(NT) for g in range(G)]
        for _ in phaseA(blocks[0][0], blocks[0][1], 0):
            pass
        for bi, (qb, g) in enumerate(blocks):
            gen = phaseA(blocks[bi + 1][0], blocks[bi + 1][1], (bi + 1) % 2) if bi + 1 < len(blocks) else None
            if gen is not None:
                next(gen, None)
            phaseB(qb, g, bi % 2, gen)
            if g == G - 1:
                qtok = slice(qb * 128, (qb + 1) * 128)
                y = yt[qb % 2]
                P.dma("sp", ymix.t[qtok, 0:512], y[:], reads=[y], writes=[ymix])
                bg_tick(P, 4)
                dbg(2 + qb)
        bg_end(P)


WIN_OFF = {}
_o = 0
for (_c0, _n, _r0) in PT_GROUPS:
    WIN_OFF[("T", _c0)] = _o; _o += 16 * _n
for (_c0, _n, _r0) in PN_GROUPS:
    WIN_OFF[("N", _c0)] = _o; _o += 16 * _n
WIN_TOTAL = _o
WOUT_TOTAL = 4 * 16 * 512
W1_TOTAL = 32 * 16 * 256
W2_TOTAL = 4 * 64 * 512


class Background:
    def __init__(self, P, prm, l, wsc):
        self.P = P
        self.jobs = []
        self.loaded = self.cast = self.stored = 0
        self.bufs = None

        def add(src3, srcbuf, nk, nc_, dst, off):
            if nk * nc_ > 4096:
                h = nk // 2
                add(src3[:, 0:h, :], srcbuf, h, nc_, dst, off)
                add(src3[:, h:nk, :], srcbuf, nk - h, nc_, dst, off + h * nc_)
            else:
                self.jobs.append((src3, srcbuf, nk, nc_, dst, off))

        wv = prm["w_in"].t[l].rearrange("(k p) n -> p k n", p=128)
        for (c0, nc_, r0) in PT_GROUPS:
            add(wv[:, :, c0:c0 + nc_], prm["w_in"], 16, nc_, wsc["win"], WIN_OFF[("T", c0)])
        for (c0, nc_, o0) in PN_GROUPS:
            add(wv[:, :, c0:c0 + nc_], prm["w_in"], 16, nc_, wsc["win"], WIN_OFF[("N", c0)])
        wv = prm["w_out"].t[l].rearrange("(k p) n -> p k n", p=128)
        for ct in range(4):
            add(wv[:, :, ct * 512:(ct + 1) * 512], prm["w_out"], 16, 512, wsc["wout"], ct * 8192)
        wv = prm["mlp_w1"].t[l].rearrange("(k p) n -> p k n", p=128)
        for hp in range(32):
            add(wv[:, :, hp * 256:(hp + 1) * 256], prm["mlp_w1"], 16, 256, wsc["w1"], hp * 4096)
        wv = prm["mlp_w2"].t[l].rearrange("(k p) n -> p k n", p=128)
        for ct in range(4):
            for kg in range(8):
                add(wv[:, kg * 8:(kg + 1) * 8, ct * 512:(ct + 1) * 512], prm["mlp_w2"], 8, 512, wsc["w2"], (ct * 8 + kg) * 4096)

    def alloc(self):
        P = self.P
        self.bufs = dict(f=[P.sbuf(f"bg_f{i}", [128, 4096]) for i in range(2)], b=[P.sbuf(f"bg_b{i}", [128, 4096], BF16) for i in range(2)])

    def done(self):
        return self.stored >= len(self.jobs)

    def step(self, load=True):
        if self.bufs is None:
            return
        P = self.P
        f, b_ = self.bufs["f"], self.bufs["b"]
        if self.stored < self.cast:
            j = self.stored
            src3, srcbuf, nk, nc_, dst, off = self.jobs[j]
            tot = nk * nc_
            P.dma("sp", dst.t[:, off:off + tot], b_[j % 2][:, 0:tot], reads=[b_[j % 2]], writes=[dst])
            self.stored += 1
        if self.cast < self.loaded:
            j = self.cast
            tot = self.jobs[j][2] * self.jobs[j][3]
            P.pool(lambda e: e.tensor_copy(out=b_[j % 2][:, 0:tot], in_=f[j % 2][:, 0:tot]), reads=[f[j % 2]], writes=[b_[j % 2]])
            self.cast += 1
        if load and self.loaded < len(self.jobs):
            j = self.loaded
            src3, srcbuf, nk, nc_, dst, off = self.jobs[j]
            P.dma("sp", f[j % 2][:, 0:nk * nc_].rearrange("p (k c) -> p k c", k=nk), src3, reads=[srcbuf], writes=[f[j % 2]])
            self.loaded += 1

    def flush(self):
        while self.stored < self.loaded:
            self.step(load=False)

    def release(self):
        self.flush()
        self.bufs = None


def bg_begin(P):
    if getattr(P, "bg", None) is not None and not P.bg.done():
        P.bg.alloc()


def bg_tick(P, n=1):
    if getattr(P, "bg", None) is not None:
        for _ in range(n):
            P.bg.step()


def bg_end(P):
    if getattr(P, "bg", None) is not None and P.bg.bufs is not None:
        P.bg.release()


def stage_convert_all(P, bg):
    if bg is None or bg.done():
        return
    with P.scope():
        bg.alloc()
        while bg.loaded < len(bg.jobs):
            bg.step()
        bg.release()


def stage_mod(P, C, cT, ada_w, ada_b, modT, gsc, nlayers):
    with P.scope():
        cf = [C.f]
        ca = P.sbuf("mod_ca", [128, 16, BPC])
        P.dma("sp", ca[:], cT.t[:, :, :], reads=[cT], writes=[ca])
        P.act(lambda e: e.activation(out=ca[:], in_=ca[:], func=AF.Silu), reads=[ca], writes=[ca])
        wst = [P.sbuf(f"mod_w{i}", [128, 16, 512]) for i in range(2)]
        brow = [P.sbuf(f"mod_b{i}", [1, 512]) for i in range(2)]
        grow = [P.sbuf(f"mod_g{i}", [BPC, 512]) for i in range(2)]
        ps_f = P.psum("mod_psf", [128, 512])
        ps_g = [P.psum(f"mod_psg{i}", [BPC, 512]) for i in range(2)]
        bg_begin(P)
        n = 0
        for l in range(nlayers):
            wv = ada_w.t[l].rearrange("(k p) n -> p k n", p=128)
            for seg in range(6):
                for ct in range(4):
                    w = wst[n % 2]; br = brow[n % 2]
                    c0 = seg * 2048 + ct * 512
                    P.dma("sp", w[:], wv[:, :, c0:c0 + 512], reads=[ada_w], writes=[w])
                    P.dma("sp", br[:], ada_b.t[l:l + 1, c0:c0 + 512], reads=[ada_b], writes=[br])
                    if seg in (2, 5):
                        pg = ps_g[n % 2]; gr = grow[n % 2]
                        for k in range(16):
                            P.pe(lambda e, k=k, w=w, pg=pg: e.matmul(pg[:], lhsT=ca[:, k, :], rhs=w[:, k, :], start=(k == 0), stop=False), reads=[ca, w], writes=[pg])
                        P.pe(lambda e, br=br, pg=pg: e.matmul(pg[:], lhsT=C.c("ones", 1)[:, 0:BPC], rhs=br[:], start=False, stop=True), reads=[br] + cf, writes=[pg])
                        P.act(lambda e, pg=pg, gr=gr: e.copy(out=gr[:], in_=pg[:]), reads=[pg], writes=[gr])
                        P.dma("sp", gsc.t[l, 0 if seg == 2 else 1, :, ct * 512:(ct + 1) * 512], gr[:], reads=[gr], writes=[gsc])
                    else:
                        si = {0: 0, 1: 1, 3: 2, 4: 3}[seg]
                        pg = ps_g[n % 2]; gr = grow[n % 2]
                        for k in range(16):
                            P.pe(lambda e, k=k, w=w, pg=pg: e.matmul(pg[:], lhsT=ca[:, k, :], rhs=w[:, k, :], start=(k == 0), stop=False), reads=[ca, w], writes=[pg])
                        P.pe(lambda e, br=br, pg=pg: e.matmul(pg[:], lhsT=C.c("ones", 1)[:, 0:BPC], rhs=br[:], start=False, stop=True), reads=[br] + cf, writes=[pg])
                        P.act(lambda e, pg=pg, gr=gr: e.copy(out=gr[:], in_=pg[:]), reads=[pg], writes=[gr])
                        for cc in range(4):
                            col = ((si * 16) + ct * 4 + cc) * BPC
                            P.pe(lambda e, gr=gr, cc=cc, col=col: e.transpose(out=ps_f[:, col:col + BPC], in_=gr[:, cc * 128:(cc + 1) * 128], identity=C.c("ident", BPC)[:, 0:BPC]),
                                 reads=[gr] + cf, writes=[ps_f])
                    n += 1
                    bg_tick(P, 2)
            P.dve(lambda e, l=l: e.tensor_copy(out=modT[:, l].rearrange("p s k b -> p (s k b)"), in_=ps_f[:, 0:4 * 16 * BPC]), reads=[ps_f], writes=[modT])
        bg_end(P)


def to_featmajor(P, C, src, src_ap_fn, ntt, hT, norm, scl=None, shf=None, pools=None):
    xt, xb, ss, ps_tr, eps = pools["xt"], pools["xb"], pools["ss"], pools["ps_tr"], pools["eps"]

    def prep(tt):
        x = xt[tt % 2]; xn = xb[tt % 2]; s1 = ss[tt % 2]
        P.dma("sp", x[:], src_ap_fn(tt), reads=[src], writes=[x])
        if norm:
            P.pool(lambda e: e.memset(s1[:], 0.0), writes=[s1])
            P.act(lambda e: e.activation(out=xn[:], in_=x[:], func=AF.Square, accum_out=s1[:, 0:1]), reads=[x, s1], writes=[xn, s1])
            P.act(lambda e: e.activation(out=s1[:, 0:1], in_=s1[:, 0:1], func=AF.Sqrt, scale=1.0 / D_MODEL, bias=eps[:, 0:1]), reads=[s1, eps], writes=[s1])
            P.dve(lambda e: e.reciprocal(out=s1[:, 0:1], in_=s1[:, 0:1]), reads=[s1], writes=[s1])
            P.dve(lambda e: e.tensor_scalar(out=xn[:], in0=x[:], scalar1=s1[:, 0:1], scalar2=None, op0=ALU.mult), reads=[x, s1], writes=[xn])
        else:
            P.pool(lambda e: e.tensor_copy(out=xn[:], in_=x[:]), reads=[x], writes=[xn])

    def trans(tt):
        xn = xb[tt % 2]
        for half in range(2):
            pt = ps_tr[(2 * tt + half) % len(ps_tr)]
            for kk in range(8):
                k = half * 8 + kk
                P.pe(lambda e, k=k, kk=kk: e.transpose(out=pt[:, kk * 128:(kk + 1) * 128], in_=xn[:, k * 128:(k + 1) * 128], identity=C.identb[:]),
                     reads=[xn, C.identb], writes=[pt])
            dst = hT[:, half * 8:half * 8 + 8, tt * 128:(tt + 1) * 128]
            src3 = pt[:].rearrange("p (k t) -> p k t", k=8)
            if scl is None:
                P.act(lambda e: e.copy(out=dst, in_=src3), reads=[pt], writes=[hT])
            else:
                tm = pools["tm"][(2 * tt + half) % 2]
                P.dve(lambda e: e.tensor_tensor(out=tm[:], in0=src3, in1=bc(scl[:, half * 8:half * 8 + 8].unsqueeze(2), [128, 8, 128]), op=ALU.mult),
                      reads=[pt] + pools["affb"], writes=[tm])
                P.pool(lambda e: e.tensor_tensor(out=dst, in0=tm[:], in1=bc(shf[:, half * 8:half * 8 + 8].unsqueeze(2), [128, 8, 128]), op=ALU.add),
                       reads=[tm] + pools["affb"], writes=[hT])

    prep(0)
    for tt in range(ntt):
        if tt + 1 < ntt:
            prep(tt + 1)
        trans(tt)


def fm_pools(P, affine):
    d = dict(xt=[P.sbuf(f"fm_xt{i}", [128, D_MODEL]) for i in range(2)],
             xb=[P.sbuf(f"fm_xb{i}", [128, D_MODEL], BF16) for i in range(2)],
             ss=[P.sbuf(f"fm_ss{i}", [128, 1]) for i in range(2)],
             ps_tr=[P.psum(f"fm_pst{i}", [128, 1024], BF16) for i in range(2)],
             eps=P.sbuf("fm_eps", [128, 1]))
    P.pool(lambda e: e.memset(d["eps"][:], EPS), writes=[d["eps"]])
    if affine:
        d["tm"] = [P.sbuf(f"fm_tm{i}", [128, 8, 128]) for i in range(2)]
    return d


def affine_vecs(P, modT, l, b, which, nw, scl, shf):
    P.dve(lambda e: e.tensor_scalar(out=scl[:], in0=modT[:, l, 2 * which + 1, :, b], scalar1=1.0, scalar2=None, op0=ALU.add), reads=[modT], writes=[scl])
    P.dve(lambda e: e.tensor_tensor(out=scl[:], in0=scl[:], in1=nw, op=ALU.mult), reads=[scl], writes=[scl])
    P.dve(lambda e: e.tensor_copy(out=shf[:], in_=modT[:, l, 2 * which, :, b]), reads=[modT], writes=[shf])


def stage_inproj(P, C, xsrc, xsrc_fn, modT, nw1T, win, projT, projN, l, b):
    with P.scope():
        hT = P.sbuf("ip_hT", [128, 16, SEQ], BF16)
        scl = P.sbuf("ip_scl", [128, 16]); shf = P.sbuf("ip_shf", [128, 16])
        affine_vecs(P, modT, l, b, 0, nw1T[:, l, :], scl, shf)
        with P.scope():
            pools = fm_pools(P, True)
            pools["affb"] = [scl, shf]
            to_featmajor(P, C, xsrc, xsrc_fn, NT, hT, True, scl[:], shf[:], pools)
        NWB = 3
        wb = [P.sbuf(f"ip_wb{i}", [128, 16, 512], BF16) for i in range(NWB)]
        ev = [P.sbuf(f"ip_ev{i}", [128, 2048]) for i in range(2)]
        ps = [P.psum(f"ip_ps{i}", [128, 512]) for i in range(4)]
        groups = [("T",) + g for g in PT_GROUPS] + [("N",) + g for g in PN_GROUPS]

        def wload(i):
            kind, c0, nc_, _ = groups[i]
            off = WIN_OFF[(kind, c0)]
            wbb = wb[i % NWB]
            P.dma("sp", wbb[:, :, 0:nc_], win.t[:, off:off + 16 * nc_].rearrange("p (k c) -> p k c", k=16), reads=[win], writes=[wbb])

        npp = 0
        nev = 0
        wload(0)
        wload(1)
        for gi, (kind, c0, nc_, dst0) in enumerate(groups):
            if gi + 2 < len(groups):
                wload(gi + 2)
            wbb = wb[gi % NWB]
            if kind == "T":
                e_ = ev[nev % 2]; nev += 1
                for tq in range(4):
                    p_ = ps[npp % 4]; npp += 1
                    for k in range(16):
                        P.pe(lambda e, k=k, p_=p_, wbb=wbb, nc_=nc_, tq=tq: e.matmul(p_[0:nc_, :], lhsT=wbb[:, k, 0:nc_], rhs=hT[:, k, tq * 512:(tq + 1) * 512], start=(k == 0), stop=(k == 15)),
                             reads=[wbb, hT], writes=[p_])
                    if tq % 2 == 0:
                        P.act(lambda e, p_=p_, e_=e_, nc_=nc_, tq=tq: e.copy(out=e_[0:nc_, tq * 512:(tq + 1) * 512], in_=p_[0:nc_, :]), reads=[p_], writes=[e_])
                    else:
                        P.dve(lambda e, p_=p_, e_=e_, nc_=nc_, tq=tq: e.tensor_copy(out=e_[0:nc_, tq * 512:(tq + 1) * 512], in_=p_[0:nc_, :]), reads=[p_], writes=[e_])
                P.dma("sp", projT.t[dst0:dst0 + nc_, :], e_[0:nc_, :], reads=[e_], writes=[projT])
            else:
                for t4 in range(4):
                    e_ = ev[nev % 2]; nev += 1
                    for ti in range(4):
                        tt = t4 * 4 + ti
                        p_ = ps[npp % 4]; npp += 1
                        for k in range(16):
                            P.pe(lambda e, k=k, p_=p_, wbb=wbb, nc_=nc_, tt=tt: e.matmul(p_[:, 0:nc_], lhsT=hT[:, k, tt * 128:(tt + 1) * 128], rhs=wbb[:, k, 0:nc_], start=(k == 0), stop=(k == 15)),
                                 reads=[wbb, hT], writes=[p_])
                        if ti % 2 == 0:
                            P.act(lambda e, p_=p_, e_=e_, nc_=nc_, ti=ti: e.copy(out=e_[:, ti * 512:ti * 512 + nc_], in_=p_[:, 0:nc_]), reads=[p_], writes=[e_])
                        else:
                            P.dve(lambda e, p_=p_, e_=e_, nc_=nc_, ti=ti: e.tensor_copy(out=e_[:, ti * 512:ti * 512 + nc_], in_=p_[:, 0:nc_]), reads=[p_], writes=[e_])
                    P.dma("sp", projN.t[t4 * 512:(t4 + 1) * 512, dst0:dst0 + nc_].rearrange("(i p) c -> p i c", p=128),
                          e_[:].rearrange("p (i c) -> p i c", i=4)[:, :, 0:nc_], reads=[e_], writes=[projN])


def stage_outproj(P, C, ymix, xsrc, xsrc_fn, xdst, xdst_fn, gsc, wout, l, b):
    with P.scope():
        yT = P.sbuf("op_yT", [128, 16, SEQ], BF16)
        with P.scope():
            pools = fm_pools(P, False)
            to_featmajor(P, C, ymix, lambda tt: ymix.t[tt * 128:(tt + 1) * 128, :], NT, yT, False, None, None, pools)
        wob = P.sbuf("op_wob", [128, 16, D_MODEL], BF16)
        gb = P.sbuf("op_gb", [128, D_MODEL])
        P.dma("sp", gb[:], gsc.t[l, 0, b:b + 1, :].partition_broadcast(128), reads=[gsc], writes=[gb])
        for ct in range(4):
            P.dma("sp", wob[:, :, ct * 512:(ct + 1) * 512], wout.t[:, ct * 8192:(ct + 1) * 8192].rearrange("p (k c) -> p k c", k=16), reads=[wout], writes=[wob])
        xt = [P.sbuf(f"op_xt{i}", [128, D_MODEL]) for i in range(2)]
        xo = [P.sbuf(f"op_xo{i}", [128, D_MODEL]) for i in range(2)]
        ps = [P.psum(f"op_ps{i}", [128, 512]) for i in range(8)]
        for tt in range(NT):
            x = xt[tt % 2]; o = xo[tt % 2]
            P.dma("sp", x[:], xsrc_fn(tt), reads=[xsrc], writes=[x])
            for ct in range(4):
                p_ = ps[(tt * 4 + ct) % 8]
                for k in range(16):
                    P.pe(lambda e, k=k, p_=p_, ct=ct, tt=tt: e.matmul(p_[:], lhsT=yT[:, k, tt * 128:(tt + 1) * 128], rhs=wob[:, k, ct * 512:(ct + 1) * 512], start=(k == 0), stop=(k == 15)),
                         reads=[yT, wob], writes=[p_])
                cs = slice(ct * 512, (ct + 1) * 512)
                P.dve(lambda e, p_=p_, o=o, cs=cs: e.tensor_tensor(out=o[:, cs], in0=p_[:], in1=gb[:, cs], op=ALU.mult), reads=[p_, gb], writes=[o])
                P.pool(lambda e, o=o, x=x, cs=cs: e.tensor_tensor(out=o[:, cs], in0=o[:, cs], in1=x[:, cs], op=ALU.add), reads=[o, x], writes=[o])
            P.dma("sp", xdst_fn(tt), o[:], reads=[o], writes=[xdst])


def stage_mlp(P, C, xsrc, xsrc_fn, xdst, xdst_fn, modT, nw2T, gsc, w1s, w2s, l, b):
    HC = 64
    with P.scope():
        scl = P.sbuf("ml_scl", [128, 16]); shf = P.sbuf("ml_shf", [128, 16])
        affine_vecs(P, modT, l, b, 1, nw2T[:, l, :], scl, shf)
        gb = P.sbuf("ml_gb", [128, D_MODEL])
        P.dma("sp", gb[:], gsc.t[l, 1, b:b + 1, :].partition_broadcast(128), reads=[gsc], writes=[gb])
        hT = P.sbuf("ml_hT", [128, 16, 512], BF16)
        uT = P.sbuf("ml_uT", [128, HC, 512], BF16)
        pools = fm_pools(P, True)
        pools["affb"] = [scl, shf]
        NWB = 4
        wbuf = [P.sbuf(f"ml_wb{i}", [128, 4096], BF16) for i in range(NWB)]
        rl = [P.sbuf(f"ml_rl{i}", [128, 512]) for i in range(2)]
        xo = [P.sbuf(f"ml_xo{i}", [128, 1024]) for i in range(2)]
        ps = [P.psum(f"ml_ps{i}", [128, 512]) for i in range(6)]
        NTILE = 64
        total = (SEQ // 512) * NTILE
        state = {"n": 0}

        def wload(j):
            jj = j % NTILE
            wbb = wbuf[j % NWB]
            if jj < 32:
                P.dma("sp", wbb[:], w1s.t[:, jj * 4096:(jj + 1) * 4096], reads=[w1s], writes=[wbb])
            else:
                P.dma("sp", wbb[:], w2s.t[:, (jj - 32) * 4096:(jj - 31) * 4096], reads=[w2s], writes=[wbb])

        PRE = 3
        for j in range(PRE):
            wload(j)
        nps = 0
        j = 0
        for t5 in range(SEQ // 512):
            to_featmajor(P, C, xsrc, lambda tt, t5=t5: xsrc_fn(t5 * 4 + tt), 4, hT, True, scl[:], shf[:], pools)
            for hp in range(32):
                if j + PRE < total:
                    wload(j + PRE)
                wbb = wbuf[j % NWB]; j += 1
                w3 = wbb[:].rearrange("p (k c) -> p k c", k=16)
                for cc in range(2):
                    hc = hp * 2 + cc
                    p_ = ps[nps % 6]; nps += 1
                    r_ = rl[hc % 2]
                    for k in range(16):
                        P.pe(lambda e, k=k, p_=p_, w3=w3, cc=cc: e.matmul(p_[:], lhsT=w3[:, k, cc * 128:(cc + 1) * 128], rhs=hT[:, k, :], start=(k == 0), stop=(k == 15)),
                             reads=[wbb, hT], writes=[p_])
                    P.act(lambda e, p_=p_, r_=r_: e.activation(out=r_[:], in_=p_[:], func=AF.Relu), reads=[p_], writes=[r_])
                    P.dve(lambda e, r_=r_, hc=hc: e.tensor_tensor(out=uT[:, hc, :], in0=r_[:], in1=r_[:], op=ALU.mult), reads=[r_], writes=[uT])
            for ct in range(4):
                pa = [ps[(nps + i) % 6] for i in range(4)]
                nps += 4
                for kg in range(8):
                    if j + PRE < total:
                        wload(j + PRE)
                    wbb = wbuf[j % NWB]; j += 1
                    w3 = wbb[:].rearrange("p (k c) -> p k c", k=8)
                    for kk in range(8):
                        k = kg * 8 + kk
                        for ti in range(4):
                            P.pe(lambda e, k=k, kk=kk, ti=ti, w3=w3, pa=pa: e.matmul(pa[ti][:], lhsT=uT[:, k, ti * 128:(ti + 1) * 128], rhs=w3[:, kk, :], start=(k == 0), stop=(k == HC - 1)),
                                 reads=[uT, wbb], writes=[pa[ti]])
                for ti in range(4):
                    tt = t5 * 4 + ti
                    o = xo[(ct * 4 + ti) % 2]
                    P.dma("sp", o[:, 512:1024], xsrc_fn(tt)[:, ct * 512:(ct + 1) * 512], reads=[xsrc], writes=[o])
                    P.dve(lambda e, o=o, ti=ti, pa=pa, ct=ct: e.tensor_tensor(out=o[:, 0:512], in0=pa[ti][:], in1=gb[:, ct * 512:(ct + 1) * 512], op=ALU.mult),
                          reads=[pa[ti], gb], writes=[o])
                    P.pool(lambda e, o=o: e.tensor_tensor(out=o[:, 0:512], in0=o[:, 0:512], in1=o[:, 512:1024], op=ALU.add), reads=[o], writes=[o])
                    P.dma("sp", xdst_fn(tt)[:, ct * 512:(ct + 1) * 512], o[:, 0:512], reads=[o], writes=[xdst])


def stage_final(P, C, xsrc, xsrc_fn, fnw, out, out_fn, nseq):
    with P.scope():
        nwb = P.sbuf("fn_nwb", [128, D_MODEL])
        P.dma("sp", nwb[:], fnw.t[0:1, :].partition_broadcast(128), reads=[fnw], writes=[nwb])
        eps = P.sbuf("fn_eps", [128, 1])
        P.pool(lambda e: e.memset(eps[:], EPS), writes=[eps])
        xt = [P.sbuf(f"fn_xt{i}", [128, D_MODEL]) for i in range(2)]
        sq = [P.sbuf(f"fn_sq{i}", [128, D_MODEL]) for i in range(2)]
        ss = [P.sbuf(f"fn_ss{i}", [128, 1]) for i in range(2)]
        for i in range(nseq * NT):
            x = xt[i % 2]; q = sq[i % 2]; s1 = ss[i % 2]
            P.dma("sp", x[:], xsrc_fn(i), reads=[xsrc], writes=[x])
            P.pool(lambda e, s1=s1: e.memset(s1[:], 0.0), writes=[s1])
            P.act(lambda e, x=x, q=q, s1=s1: e.activation(out=q[:], in_=x[:], func=AF.Square, accum_out=s1[:, 0:1]), reads=[x, s1], writes=[q, s1])
            P.act(lambda e, s1=s1: e.activation(out=s1[:, 0:1], in_=s1[:, 0:1], func=AF.Sqrt, scale=1.0 / D_MODEL, bias=eps[:, 0:1]), reads=[s1, eps], writes=[s1])
            P.dve(lambda e, s1=s1: e.reciprocal(out=s1[:, 0:1], in_=s1[:, 0:1]), reads=[s1], writes=[s1])
            P.dve(lambda e, x=x, q=q, s1=s1: e.scalar_tensor_tensor(out=q[:], in0=x[:], scalar=s1[:, 0:1], in1=nwb[:], op0=ALU.mult, op1=ALU.mult), reads=[x, s1, nwb], writes=[q])
            P.dma("sp", out_fn(i), q[:], reads=[q], writes=[out])


SMALL_PARAMS = ["gla_gate_w2", "gla_gate_b", "gla_norm_w", "ssd_conv_w", "ssd_conv_b", "ssd_dt_bias", "ssd_a_log", "ssd_d", "ssd_norm_w",
                "gdn_conv_w", "gdn_dt_bias", "gdn_a_log", "gdn_norm_w", "nsa_cmp_pos", "nsa_cmp_w1", "nsa_cmp_w2"]
BIG_PARAMS = ["ada_w", "ada_b", "w_in", "w_out", "mlp_w1", "mlp_w2"]


def build(nlayers=DEPTH, nseq=BPC, shapes=None):
    nc = bass.Bass("TRN2", target_bir_lowering=False)
    st = ExitStack()
    with st:
        P = Prog(nc, st)

        def ext(name, shape):
            return Buf(nc.dram_tensor(name, list(shape), F32, kind="ExternalInput").ap(), name)

        x = ext("x", [nseq, SEQ, D_MODEL])
        cT = ext("cT", [128, 16, BPC])
        prm = {k: ext(k, shapes[k]) for k in SMALL_PARAMS + BIG_PARAMS + ["nw1T", "nw2T", "fnw", "nsa_tab", "nsa_t31", "nsa_cst", "cst"]}
        out = Buf(nc.dram_tensor("out", [nseq, SEQ, D_MODEL], F32, kind="ExternalOutput").ap(), "out")
        xres = P.dram("xres", [nseq, SEQ, D_MODEL])
        projT = P.dram("projT", [PT_ROWS, SEQ])
        projN = P.dram("projN", [SEQ, PN_COLS])
        ymix = P.dram("ymix", [SEQ, D_MODEL])
        gsc = P.dram("gsc", [nlayers, 2, BPC, D_MODEL])
        C = Consts(P, prm["cst"])
        modT = P.sbuf("modT", [128, nlayers, 4, 16, BPC])
        nw1T = P.sbuf("nw1T", [128, shapes["nw1T"][1], 16])
        nw2T = P.sbuf("nw2T", [128, shapes["nw2T"][1], 16])
        P.dma("sp", nw1T[:], prm["nw1T"].t[:, :, :], reads=[prm["nw1T"]], writes=[nw1T])
        P.dma("sp", nw2T[:], prm["nw2T"].t[:, :, :], reads=[prm["nw2T"]], writes=[nw2T])
        P.bg = None
        wsc = dict(win=P.dram("wsc_win", [128, WIN_TOTAL], BF16), wout=P.dram("wsc_wout", [128, WOUT_TOTAL], BF16),
                   w1=P.dram("wsc_w1", [128, W1_TOTAL], BF16), w2=P.dram("wsc_w2", [128, W2_TOTAL], BF16))
        wscs = [wsc, dict(win=P.dram("wsc_win2", [128, WIN_TOTAL], BF16), wout=P.dram("wsc_wout2", [128, WOUT_TOTAL], BF16),
                          w1=P.dram("wsc_w12", [128, W1_TOTAL], BF16), w2=P.dram("wsc_w22", [128, W2_TOTAL], BF16))]
        P.bg = Background(P, prm, 0, wscs[0])
        stage_mod(P, C, cT, prm["ada_w"], prm["ada_b"], modT, gsc, nlayers)
        stage_convert_all(P, P.bg)
        P.bg = None
        for l in range(nlayers):
            wsc = wscs[l % 2]
            for b in range(nseq):
                if l == 0:
                    xs, xs_fn = x, (lambda tt, b=b: x.t[b, tt * 128:(tt + 1) * 128, :])
                else:
                    xs, xs_fn = xres, (lambda tt, b=b: xres.t[b, tt * 128:(tt + 1) * 128, :])
                xr_fn = (lambda tt, b=b: xres.t[b, tt * 128:(tt + 1) * 128, :])
                stage_inproj(P, C, xs, xs_fn, modT, nw1T, wsc["win"], projT, projN, l, b)
                if b == nseq - 1 and l + 1 < nlayers:
                    P.bg = Background(P, prm, l + 1, wscs[(l + 1) % 2])
                stage_nsa(P, C, projT, projN, ymix, prm, l)
                stage_ssd(P, C, projT, projN, ymix, prm, l)
                stage_gdn(P, C, projT, projN, ymix, prm, l)
                stage_gla(P, C, projT, projN, ymix, prm, l)
                if P.bg is not None:
                    stage_convert_all(P, P.bg)
                    P.bg = None
                stage_outproj(P, C, ymix, xs, xs_fn, xres, xr_fn, gsc, wsc["wout"], l, b)
                stage_mlp(P, C, xres, xr_fn, xres, xr_fn, modT, nw2T, gsc, wsc["w1"], wsc["w2"], l, b)
        stage_final(P, C, xres, lambda i: xres.t[i // NT, (i % NT) * 128:(i % NT + 1) * 128, :], prm["fnw"],
                    out, lambda i: out.t[i // NT, (i % NT) * 128:(i % NT + 1) * 128, :], nseq)
        P.finish()
        ninstr = P.ninstr
    return nc, ninstr


def host_inputs(inputs, nlayers=DEPTH):
    d = {}
    for k in SMALL_PARAMS:
        d[k] = host_param(k, inputs[k][:nlayers])
    for k in BIG_PARAMS:
        d[k] = np.ascontiguousarray(np.asarray(inputs[k][:nlayers], np.float32))
    d["nw1T"] = host_param("norm1_w", inputs["norm1_w"][:nlayers])
    d["nw2T"] = host_param("norm2_w", inputs["norm2_w"][:nlayers])
    d["fnw"] = host_param("final_norm_w", inputs["final_norm_w"])
    d["nsa_tab"], d["nsa_t31"] = nsa_host_tables(inputs["rel_bias"])
    d["nsa_cst"] = NSA_CST_NP
    d["cst"] = CST_NP
    return d


def core_inputs(inputs, shared, core, nseq=BPC):
    xs = np.ascontiguousarray(np.asarray(inputs["x"][core * BPC:core * BPC + nseq], np.float32))
    c = np.asarray(inputs["c"][core * BPC:(core + 1) * BPC], np.float32)
    cT = np.ascontiguousarray(c.T.reshape(16, 128, BPC).transpose(1, 0, 2))
    m = dict(shared)
    m["x"] = xs
    m["cT"] = cT
    return m


_CACHE = {}


def kernel(**inputs):
    shared = host_inputs(inputs)
    shapes = {k: v.shape for k, v in shared.items()}
    if "nc" not in _CACHE:
        _CACHE["nc"] = build(DEPTH, BPC, shapes)[0]
    nc = _CACHE["nc"]
    in_maps = [core_inputs(inputs, shared, c) for c in range(NCORES)]
    res = run_bass_kernel_spmd(nc, in_maps, core_ids=list(range(NCORES)))
    out = np.concatenate([r["out"] for r in res.results], axis=0)
    return out.astype(np.float32)
```

```python
import math
from contextlib import ExitStack, contextmanager
import numpy as np
import concourse.bass as bass
import concourse.mybir as mybir
from concourse.bass_utils import run_bass_kernel_spmd

F32 = mybir.dt.float32
BF16 = mybir.dt.bfloat16
AF = mybir.ActivationFunctionType
ALU = mybir.AluOpType
AX = mybir.AxisListType

EPOCH = 12000
SAME_ENGINE_SYNC = True

D_MODEL = 2048
SEQ = 2048
DEPTH = 4
NCORES = 8
BPC = 2
IN_COLS = 6456
EPS = 1e-6
NT = SEQ // 128


class StopStage(Exception):
    pass


DBG = {"stop": 99, "skip": set()}


def dbg(k):
    if DBG["stop"] <= k:
        DBG["P"].dead = True


class Buf:
    __slots__ = ("t", "name", "lw", "rd", "excl")

    def __init__(self, t, name="", excl=False):
        self.t = t
        self.name = name
        self.lw = None
        self.rd = {}
        self.excl = excl

    def __getitem__(self, k):
        return self.t[k]


class Prog:
    ENGS = ("pe", "act", "dve", "pool", "sp")

    def __init__(self, nc, stack):
        self.nc = nc
        self.stack = stack
        self.cnt = {e: 0 for e in ("pe", "act", "dve", "pool")}
        self.sems = {}
        self.seen = {e: {} for e in self.ENGS}
        self.dslots = {}
        self.dnext = {}
        self.E = dict(pe=nc.tensor, act=nc.scalar, dve=nc.vector, pool=nc.gpsimd, sp=nc.sync)
        self.ninstr = 0
        self.base_stack = stack
        self.uid = 0
        self.dead = False
        DBG["P"] = self

    def sem(self, name):
        return self.base_stack.enter_context(self.nc.semaphore(name))

    def sbuf(self, name, shape, dt=F32):
        self.uid += 1
        t = self.stack.enter_context(self.nc.sbuf_tensor(f"{name}_{self.uid}", list(shape), dt))
        return Buf(t, name)

    def psum(self, name, shape, dt=F32):
        self.uid += 1
        t = self.stack.enter_context(self.nc.psum_tensor(f"{name}_{self.uid}", list(shape), dt))
        return Buf(t, name, excl=True)

    def dram(self, name, shape, dt=F32, kind="Internal"):
        t = self.nc.dram_tensor(name, list(shape), dt, kind=kind)
        return Buf(t.ap(), name)

    def _esem(self, eng, idx):
        ep = idx // EPOCH
        k = (eng, ep)
        if k not in self.sems:
            self.sems[k] = self.sem(f"s_{eng}_{ep}")
        return self.sems[k], (idx % EPOCH) + 1

    def _wait(self, eng, ev):
        q, idx = ev
        if isinstance(q, str):
            if q == eng and (eng == "pe" or not SAME_ENGINE_SYNC):
                return
            if self.seen[eng].get(q, -1) >= idx:
                return
            self.seen[eng][q] = idx
            s, v = self._esem(q, idx)
        else:
            if self.seen[eng].get(q, -1) >= idx:
                return
            self.seen[eng][q] = idx
            s = self.dslots[q[0]][q[1]][0]
            v = idx
        self.E[eng].wait_ge(s, v)

    def _deps(self, eng, reads, writes):
        for b in reads:
            if b.lw is not None:
                self._wait(eng, b.lw)
            if b.excl:
                for q, i in list(b.rd.items()):
                    if q != eng:
                        self._wait(eng, (q, i))
        for b in writes:
            if b.lw is not None:
                self._wait(eng, b.lw)
            for q, i in list(b.rd.items()):
                self._wait(eng, (q, i))

    def _mark(self, ev, reads, writes):
        q, idx = ev
        for b in reads:
            if b.rd.get(q, -1) < idx:
                b.rd[q] = idx
        for b in writes:
            b.lw = ev
            b.rd = {}

    def op(self, eng, fn, reads=(), writes=()):
        if self.dead:
            return
        self._deps(eng, reads, writes)
        idx = self.cnt[eng]
        self.cnt[eng] += 1
        s, v = self._esem(eng, idx)
        fn(self.E[eng]).then_inc(s, 1)
        self._mark((eng, idx), reads, writes)
        self.ninstr += 1

    def pe(self, fn, reads=(), writes=()):
        self.op("pe", fn, reads, writes)

    def act(self, fn, reads=(), writes=()):
        self.op("act", fn, reads, writes)

    def dve(self, fn, reads=(), writes=()):
        self.op("dve", fn, reads, writes)

    def pool(self, fn, reads=(), writes=()):
        self.op("pool", fn, reads, writes)

    def dma(self, eng, out_ap, in_ap, reads=(), writes=(), nslots=8, **kw):
        if self.dead:
            return
        if eng not in self.dslots:
            self.dslots[eng] = [[self.sem(f"d_{eng}_{i}"), 0] for i in range(nslots)]
            self.dnext[eng] = 0
        si = self.dnext[eng]
        self.dnext[eng] = (si + 1) % len(self.dslots[eng])
        slot = self.dslots[eng][si]
        q = (eng, si)
        if slot[1] > 0:
            self._wait(eng, (q, slot[1]))
        self._deps(eng, reads, writes)
        slot[1] += 16
        self.E[eng].dma_start(out=out_ap, in_=in_ap, **kw).then_inc(slot[0], 16)
        self._mark((q, slot[1]), reads, writes)
        self.ninstr += 1

    def all_events(self):
        evs = []
        for e in ("pe", "act", "dve", "pool"):
            if self.cnt[e] > 0:
                evs.append((e, self.cnt[e] - 1))
        for eng, slots in self.dslots.items():
            for si, (s, c) in enumerate(slots):
                if c > 0:
                    evs.append(((eng, si), c))
        return evs

    def barrier(self, engs=None):
        evs = self.all_events()
        for e in (engs or self.ENGS):
            for ev in evs:
                if ev[0] == e and e == "pe":
                    continue
                self._wait(e, ev)

    @contextmanager
    def scope(self):
        old = self.stack
        try:
            with ExitStack() as st:
                self.stack = st
                try:
                    yield
                finally:
                    self.barrier()
        finally:
            self.stack = old

    def finish(self):
        self.barrier(["sp"])


def make_consts():
    p = np.arange(128)[:, None]
    f = np.arange(128)[None, :]
    same = (p // 64) == (f // 64)
    cols = {}
    parts = []

    def add(name, arr):
        cols[name] = (sum(a.shape[1] for a in parts), arr.shape[1])
        parts.append(arr.astype(np.float32))

    add("ident", (p == f))
    add("tri01", same & (p <= f))
    add("stri01", same & (p < f))
    add("su01", same & (p > f))
    add("sl01", same & (p >= f))
    add("bones", same)
    add("ones", np.ones((128, 128)))
    add("chunkind", (p // 64) == np.arange(2)[None, :])
    add("tri16", (same & (p <= f)) * (-1.0 / 16.0))
    add("bones16", same * (-1.0 / 16.0))
    add("chunkind16", ((p // 64) == np.arange(2)[None, :]) * (-1.0 / 16.0))
    return np.concatenate(parts, axis=1), cols


CST_NP, CST_COLS = make_consts()


class Consts:
    def __init__(self, P, cst_dram):
        self.P = P
        n = CST_NP.shape[1]
        self.f = P.sbuf("cst_f", [128, n])
        P.dma("sp", self.f[:], cst_dram.t[:, :], reads=[cst_dram], writes=[self.f])
        self.identb = P.sbuf("identb", [128, 128], BF16)
        P.dve(lambda e: e.tensor_copy(out=self.identb[:], in_=self.c("ident")), reads=[self.f], writes=[self.identb])

    def c(self, name, rows=128):
        o, w = CST_COLS[name]
        return self.f[0:rows, o:o + w]


def norm_gate(P, src_ap, src_bufs, z_ap, z_bufs, nw_ap, nw_bufs, G, gsz, out, tmp, gate_first):
    a, b, ss, sg = tmp["a"], tmp["b"], tmp["ss"], tmp["sg"]
    n = G * gsz
    P.act(lambda e: e.activation(out=sg[:, 0:n], in_=z_ap, func=AF.Silu), reads=z_bufs, writes=[sg])
    if gate_first:
        P.dve(lambda e: e.tensor_tensor(out=a[:, 0:n], in0=src_ap, in1=sg[:, 0:n], op=ALU.mult),
              reads=list(src_bufs) + [sg], writes=[a])
    else:
        P.dve(lambda e: e.tensor_copy(out=a[:, 0:n], in_=src_ap), reads=list(src_bufs), writes=[a])
    P.act(lambda e: e.activation(out=b[:, 0:n], in_=a[:, 0:n], func=AF.Square), reads=[a], writes=[b])
    P.dve(lambda e: e.tensor_reduce(out=ss[:, 0:G], in_=b[:, 0:n].rearrange("p (g e) -> p g e", g=G), axis=AX.X, op=ALU.add),
          reads=[b], writes=[ss])
    P.act(lambda e: e.activation(out=ss[:, 0:G], in_=ss[:, 0:G], func=AF.Sqrt, scale=1.0 / gsz, bias=tmp["eps"][:, 0:1]),
          reads=[ss, tmp["eps"]], writes=[ss])
    P.dve(lambda e: e.reciprocal(out=ss[:, 0:G], in_=ss[:, 0:G]), reads=[ss], writes=[ss])
    P.dve(lambda e: e.tensor_tensor(out=b[:, 0:n].rearrange("p (g e) -> p g e", g=G),
                                    in0=a[:, 0:n].rearrange("p (g e) -> p g e", g=G),
                                    in1=ss[:, 0:G].unsqueeze(2).to_broadcast([128, G, gsz]), op=ALU.mult),
          reads=[a, ss], writes=[b])
    if gate_first:
        P.pool(lambda e: e.tensor_tensor(out=out[:, 0:n].rearrange("p (g e) -> p g e", g=G),
                                         in0=b[:, 0:n].rearrange("p (g e) -> p g e", g=G), in1=nw_ap, op=ALU.mult),
               reads=[b] + list(nw_bufs), writes=[out])
    else:
        P.pool(lambda e: e.tensor_tensor(out=a[:, 0:n].rearrange("p (g e) -> p g e", g=G),
                                         in0=b[:, 0:n].rearrange("p (g e) -> p g e", g=G), in1=nw_ap, op=ALU.mult),
               reads=[b] + list(nw_bufs), writes=[a])
        P.pool(lambda e: e.tensor_tensor(out=out[:, 0:n], in0=a[:, 0:n], in1=sg[:, 0:n], op=ALU.mult),
               reads=[a, sg], writes=[out])


def ng_tmp(P):
    t = dict(a=P.sbuf("ng_a", [128, 512]), b=P.sbuf("ng_b", [128, 512]), ss=P.sbuf("ng_ss", [128, 8]),
             sg=P.sbuf("ng_sg", [128, 512]), eps=P.sbuf("ng_eps", [128, 1]), one=P.sbuf("ng_one", [128, 1]))
    P.pool(lambda e: e.memset(t["eps"][:], EPS), writes=[t["eps"]])
    P.pool(lambda e: e.memset(t["one"][:], 1.0), writes=[t["one"]])
    return t


PT_NQ, PT_NKC, PT_NVC, PT_NKS, PT_NKW = 0, 512, 640, 768, 896
PT_SXBC = 1024
PT_GQKV = 2048
PT_LQ, PT_LK, PT_LLR = 3584, 3840, 4096
PT_ROWS = 4112
PN_NVS, PN_NVW, PN_NGATE, PN_SZ, PN_SDT = 0, 128, 256, 280, 792
PN_GZ, PN_GBETA, PN_GA, PN_LK, PN_LV, PN_LG = 800, 1312, 1316, 1320, 1576, 2088
PN_COLS = 2600
PT_GROUPS = ([(0 + 128 * i, 128, PT_NQ + 128 * i) for i in range(4)] +
             [(512, 128, PT_NKC), (640, 128, PT_NVC), (768, 128, PT_NKS), (1024, 128, PT_NKW)] +
             [(1816 + 128 * i, 128, PT_SXBC + 128 * i) for i in range(8)] +
             [(2848 + 128 * i, 128, PT_GQKV + 128 * i) for i in range(12)] +
             [(4904 + 128 * i, 128, PT_LQ + 128 * i) for i in range(2)] +
             [(5160 + 128 * i, 128, PT_LK + 128 * i) for i in range(2)] +
             [(6440, 16, PT_LLR)])
PN_GROUPS = [(896, 128, PN_NVS), (1152, 512, PN_NVW), (1664, 152, PN_NVW + 512), (2840, 8, PN_SDT),
             (4384, 512, PN_GZ), (4896, 8, PN_GBETA), (5160, 256, PN_LK), (5416, 512, PN_LV), (5928, 512, PN_LG)]


def stage_gla(P, C, projT, projN, ymix, prm, l):
    with P.scope():
        w2 = P.sbuf("gla_w2", [16, 256])
        gb = P.sbuf("gla_gb", [1, 256])
        nwb = P.sbuf("gla_nwb", [128, 128])
        P.dma("sp", w2[:], prm["gla_gate_w2"].t[l], reads=[prm["gla_gate_w2"]], writes=[w2])
        P.dma("sp", gb[:], prm["gla_gate_b"].t[l:l + 1, :], reads=[prm["gla_gate_b"]], writes=[gb])
        P.dma("sp", nwb[:], prm["gla_norm_w"].t[l:l + 1, :].partition_broadcast(128), reads=[prm["gla_norm_w"]], writes=[nwb])
        S = P.sbuf("gla_S", [64, 4, 128])
        Sb = [P.sbuf(f"gla_Sb{i}", [64, 4, 128], BF16) for i in range(2)]
        P.dve(lambda e: e.memset(S[:], 0.0), writes=[S])
        P.dve(lambda e: e.memset(Sb[0][:], 0.0), writes=[Sb[0]])
        tmp = ng_tmp(P)
        NB = 2
        qT = [P.sbuf(f"gla_qT{i}", [64, 4, 128]) for i in range(NB)]
        kT = [P.sbuf(f"gla_kT{i}", [64, 4, 128]) for i in range(NB)]
        lrT = [P.sbuf(f"gla_lrT{i}", [16, 128]) for i in range(NB)]
        tokN = [P.sbuf(f"gla_tokN{i}", [128, 1280]) for i in range(NB)]
        lsp = P.sbuf("gla_lsp", [128, 256])
        ex = P.sbuf("gla_ex", [128, 256])
        kend = P.sbuf("gla_kend", [128, 256], BF16)
        vb = P.sbuf("gla_vb", [128, 512], BF16)
        ebT = P.sbuf("gla_ebT", [64, 512])
        qdT = P.sbuf("gla_qdT", [64, 4, 128], BF16)
        kiT = P.sbuf("gla_kiT", [64, 4, 128], BF16)
        dec = P.sbuf("gla_dec", [64, 8])
        AT = P.sbuf("gla_AT", [128, 4, 128], BF16)
        yo = [P.sbuf(f"gla_yo{i}", [128, 512]) for i in range(2)]
        ps_gk = P.psum("gla_ps_gk", [128, 512])
        ps_bl = P.psum("gla_ps_bl", [128, 512])
        ps_bT = P.psum("gla_ps_bT", [64, 512])
        ps_blT = P.psum("gla_ps_blT", [64, 8])
        ps_at = P.psum("gla_ps_at", [128, 512])
        ps_o = P.psum("gla_ps_o", [128, 512])
        ps_loc = [P.psum(f"gla_ps_loc{i}", [64, 512]) for i in range(2)]
        cf = [C.f]
        bg_begin(P)

        def load(t):
            i = t % NB
            tok = slice(t * 128, (t + 1) * 128)
            P.dma("sp", qT[i][:], projT.t[PT_LQ:PT_LQ + 256, tok].rearrange("(h d) t -> d h t", d=64), reads=[projT], writes=[qT[i]])
            P.dma("sp", kT[i][:], projT.t[PT_LK:PT_LK + 256, tok].rearrange("(h d) t -> d h t", d=64), reads=[projT], writes=[kT[i]])
            P.dma("sp", lrT[i][:], projT.t[PT_LLR:PT_LLR + 16, tok], reads=[projT], writes=[lrT[i]])
            P.dma("sp", tokN[i][:], projN.t[tok, PN_LK:PN_LK + 1280], reads=[projN], writes=[tokN[i]])

        load(0)
        for t in range(NT):
            if t + 1 < NT:
                load(t + 1)
            i = t % NB
            tok = slice(t * 128, (t + 1) * 128)
            kN = tokN[i][:, 0:256]
            vN = tokN[i][:, 256:768]
            gN = tokN[i][:, 768:1280]
            P.pe(lambda e: e.matmul(ps_gk[:, 0:256], lhsT=lrT[i][:], rhs=w2[:], start=True, stop=False), reads=[lrT[i], w2], writes=[ps_gk])
            P.pe(lambda e: e.matmul(ps_gk[:, 0:256], lhsT=C.c("ones", 1), rhs=gb[:], start=False, stop=True), reads=[gb] + cf, writes=[ps_gk])
            P.act(lambda e: e.activation(out=ex[:], in_=ps_gk[:, 0:256], func=AF.Exp, scale=-1.0), reads=[ps_gk], writes=[ex])
            P.act(lambda e: e.activation(out=lsp[:], in_=ex[:], func=AF.Ln, bias=tmp["one"][:, 0:1]), reads=[ex, tmp["one"]], writes=[lsp])
            P.pe(lambda e: e.matmul(ps_gk[:, 256:512], lhsT=C.c("tri16"), rhs=lsp[:], start=True, stop=True), reads=[lsp] + cf, writes=[ps_gk])
            P.pe(lambda e: e.matmul(ps_bl[:, 0:256], lhsT=C.c("bones16"), rhs=lsp[:], start=True, stop=True), reads=[lsp] + cf, writes=[ps_bl])
            for h in range(4):
                P.pe(lambda e, h=h: e.matmul(ps_bT[:, h * 128:(h + 1) * 128], lhsT=lsp[:, h * 64:(h + 1) * 64], rhs=C.c("tri16"), start=True, stop=True),
                     reads=[lsp] + cf, writes=[ps_bT])
            for h in range(4):
                P.pe(lambda e, h=h: e.matmul(ps_blT[:, h * 2:(h + 1) * 2], lhsT=lsp[:, h * 64:(h + 1) * 64], rhs=C.c("chunkind16"), start=True, stop=True),
                     reads=[lsp] + cf, writes=[ps_blT])
            P.dve(lambda e: e.tensor_copy(out=ex[:], in_=ps_gk[:, 256:512]), reads=[ps_gk], writes=[ex])
            P.dve(lambda e: e.tensor_tensor(out=ex[:], in0=ps_bl[:, 0:256], in1=ex[:], op=ALU.subtract), reads=[ps_bl, ex], writes=[ex])
            P.act(lambda e: e.activation(out=ex[:], in_=ex[:], func=AF.Exp), reads=[ex], writes=[ex])
            P.dve(lambda e: e.tensor_tensor(out=kend[:], in0=kN, in1=ex[:], op=ALU.mult), reads=[tokN[i], ex], writes=[kend])
            P.pool(lambda e: e.tensor_copy(out=vb[:], in_=vN), reads=[tokN[i]], writes=[vb])
            P.act(lambda e: e.activation(out=ebT[:], in_=ps_bT[:], func=AF.Exp), reads=[ps_bT], writes=[ebT])
            P.dve(lambda e: e.scalar_tensor_tensor(out=qdT[:].rearrange("d h t -> d (h t)"), in0=qT[i][:].rearrange("d h t -> d (h t)"), scalar=0.125,
                                                   in1=ebT[:], op0=ALU.mult, op1=ALU.mult), reads=[qT[i], ebT], writes=[qdT])
            P.act(lambda e: e.activation(out=ebT[:], in_=ps_bT[:], func=AF.Exp, scale=-1.0), reads=[ps_bT], writes=[ebT])
            P.dve(lambda e: e.tensor_tensor(out=kiT[:].rearrange("d h t -> d (h t)"), in0=kT[i][:].rearrange("d h t -> d (h t)"), in1=ebT[:], op=ALU.mult),
                  reads=[kT[i], ebT], writes=[kiT])
            P.act(lambda e: e.activation(out=dec[:], in_=ps_blT[:], func=AF.Exp), reads=[ps_blT], writes=[dec])
            for h in range(4):
                P.pe(lambda e, h=h: e.matmul(ps_at[:, h * 128:(h + 1) * 128], lhsT=kiT[:, h, :], rhs=qdT[:, h, :], start=True, stop=True),
                     reads=[kiT, qdT], writes=[ps_at])
            P.dve(lambda e: e.tensor_tensor(out=AT[:], in0=ps_at[:].rearrange("p (h t) -> p h t", h=4),
                                            in1=C.c("tri01").unsqueeze(1).to_broadcast([128, 4, 128]), op=ALU.mult), reads=[ps_at] + cf, writes=[AT])
            for c in range(2):
                rows = slice(c * 64, (c + 1) * 64)
                for h in range(4):
                    P.pe(lambda e, h=h, rows=rows, c=c: e.matmul(ps_loc[c][:, h * 128:(h + 1) * 128], lhsT=kend[rows, h * 64:(h + 1) * 64],
                                                                 rhs=vb[rows, h * 128:(h + 1) * 128], start=True, stop=True),
                         reads=[kend, vb], writes=[ps_loc[c]])
            for c in range(2):
                P.dve(lambda e, c=c: e.tensor_tensor(out=S[:], in0=S[:], in1=dec[:].rearrange("d (h c) -> d h c", c=2)[:, :, c:c + 1].to_broadcast([64, 4, 128]),
                                                     op=ALU.mult), reads=[S, dec], writes=[S])
                P.dve(lambda e, c=c: e.tensor_tensor(out=S[:].rearrange("d h e -> d (h e)"), in0=S[:].rearrange("d h e -> d (h e)"), in1=ps_loc[c][:], op=ALU.add),
                      reads=[S, ps_loc[c]], writes=[S])
                if c == 0:
                    P.act(lambda e: e.copy(out=Sb[1][:], in_=S[:]), reads=[S], writes=[Sb[1]])
            for h in range(4):
                cols = slice(h * 128, (h + 1) * 128)
                P.pe(lambda e, h=h, cols=cols: e.matmul(ps_o[:, cols], lhsT=AT[:, h, :], rhs=vb[:, cols], start=True, stop=False),
                     reads=[AT, vb], writes=[ps_o])
                for c in range(2):
                    rows = slice(c * 64, (c + 1) * 64)
                    P.pe(lambda e, h=h, cols=cols, rows=rows, c=c: e.matmul(ps_o[rows, cols], lhsT=qdT[:, h, rows], rhs=Sb[c][:, h, :], start=False, stop=(c == 1)),
                         reads=[qdT, Sb[c]], writes=[ps_o])
            P.act(lambda e: e.copy(out=Sb[0][:], in_=S[:]), reads=[S], writes=[Sb[0]])
            y = yo[t % 2]
            norm_gate(P, ps_o[:], [ps_o], gN, [tokN[i]], nwb[:].unsqueeze(1).to_broadcast([128, 4, 128]), [nwb], 4, 128, y, tmp, False)
            P.dma("sp", ymix.t[tok, 1536:2048], y[:], reads=[y], writes=[ymix])
            bg_tick(P, 1)
        bg_end(P)


def host_param(name, arr):
    a = np.asarray(arr, np.float32)
    if name in ("ssd_conv_w", "gdn_conv_w"):
        L, K, CH = a.shape
        a = a.reshape(L, K, CH // 128, 128).transpose(0, 3, 2, 1)
    elif name in ("norm1_w", "norm2_w"):
        L = a.shape[0]
        a = a.reshape(L, 16, 128).transpose(2, 0, 1)
    elif name == "final_norm_w":
        a = a.reshape(1, -1)
    elif name == "nsa_cmp_pos":
        a = a.transpose(0, 1, 3, 2)
    elif name == "ssd_conv_b":
        L, CH = a.shape
        a = a.reshape(L, CH // 128, 128).transpose(0, 2, 1)
    return np.ascontiguousarray(a)


def bc(ap, shape):
    return ap.to_broadcast(list(shape))


def causal_conv_silu(P, projT, row0, ntiles, cw, cb, dst, dst_off, name, bias=True):
    xpad = [P.sbuf(f"{name}_xpad{i}", [128, SEQ + 3]) for i in range(2)]
    acc = [P.sbuf(f"{name}_acc{i}", [128, SEQ]) for i in range(2)]
    for i in range(2):
        P.pool(lambda e, i=i: e.memset(xpad[i][:, 0:3], 0.0), writes=[xpad[i]])
    for ct in range(ntiles):
        xp = xpad[ct % 2]
        ac = acc[ct % 2]
        P.dma("sp", xp[:, 3:SEQ + 3], projT.t[row0 + ct * 128:row0 + (ct + 1) * 128, :], reads=[projT], writes=[xp])
        eng = P.dve
        eng(lambda e, ct=ct, xp=xp, ac=ac: e.tensor_scalar(out=ac[:], in0=xp[:, 0:SEQ], scalar1=cw[:, ct, 0:1], scalar2=None, op0=ALU.mult),
            reads=[xp, cw], writes=[ac])
        for k in range(1, 4):
            eng(lambda e, ct=ct, xp=xp, ac=ac, k=k: e.scalar_tensor_tensor(out=ac[:], in0=xp[:, k:SEQ + k], scalar=cw[:, ct, k:k + 1], in1=ac[:],
                                                                            op0=ALU.mult, op1=ALU.add), reads=[xp, cw, ac], writes=[ac])
        if bias:
            P.act(lambda e, ct=ct, ac=ac: e.activation(out=dst[:, dst_off + ct, :], in_=ac[:], func=AF.Silu, bias=cb[:, ct:ct + 1]),
                  reads=[ac, cb], writes=[dst])
        else:
            P.act(lambda e, ct=ct, ac=ac: e.activation(out=dst[:, dst_off + ct, :], in_=ac[:], func=AF.Silu), reads=[ac], writes=[dst])


def softplus_small(P, x_ap, xbuf, tmpb, one):
    P.act(lambda e: e.activation(out=x_ap, in_=x_ap, func=AF.Exp), reads=[xbuf], writes=[xbuf])
    P.act(lambda e: e.activation(out=x_ap, in_=x_ap, func=AF.Ln, bias=one[:, 0:1]), reads=[xbuf, one], writes=[xbuf])


def stage_ssd(P, C, projT, projN, ymix, prm, l):
    with P.scope():
        cf = [C.f]
        cw = P.sbuf("ssd_cw", [128, 8, 4])
        cb = P.sbuf("ssd_cb", [128, 8])
        dtb = P.sbuf("ssd_dtb", [128, 8])
        aneg = P.sbuf("ssd_aneg", [128, 8])
        dsk = P.sbuf("ssd_dsk", [128, 8])
        nwb = P.sbuf("ssd_nwb", [128, 512])
        P.dma("sp", cw[:], prm["ssd_conv_w"].t[l], reads=[prm["ssd_conv_w"]], writes=[cw])
        P.dma("sp", cb[:], prm["ssd_conv_b"].t[l], reads=[prm["ssd_conv_b"]], writes=[cb])
        P.dma("sp", dtb[:], prm["ssd_dt_bias"].t[l:l + 1, :].partition_broadcast(128), reads=[prm["ssd_dt_bias"]], writes=[dtb])
        P.dma("sp", aneg[:], prm["ssd_a_log"].t[l:l + 1, :].partition_broadcast(128), reads=[prm["ssd_a_log"]], writes=[aneg])
        P.dma("sp", dsk[:], prm["ssd_d"].t[l:l + 1, :].partition_broadcast(128), reads=[prm["ssd_d"]], writes=[dsk])
        P.dma("sp", nwb[:], prm["ssd_norm_w"].t[l:l + 1, :].partition_broadcast(128), reads=[prm["ssd_norm_w"]], writes=[nwb])
        P.act(lambda e: e.activation(out=aneg[:], in_=aneg[:], func=AF.Exp), reads=[aneg], writes=[aneg])
        P.dve(lambda e: e.tensor_scalar(out=aneg[:], in0=aneg[:], scalar1=-1.0, scalar2=None, op0=ALU.mult), reads=[aneg], writes=[aneg])
        act = P.sbuf("ssd_act", [128, 8, SEQ])
        with P.scope():
            causal_conv_silu(P, projT, PT_SXBC, 8, cw, cb, act, 0, "ssd")
        BCb = P.sbuf("ssd_BCb", [128, 4, SEQ], BF16)
        for k in range(4):
            (P.dve if k % 2 == 0 else P.pool)(lambda e, k=k: e.tensor_copy(out=BCb[:, k, :], in_=act[:, 4 + k, :]), reads=[act], writes=[BCb])
        tmp = ng_tmp(P)
        S = P.sbuf("ssd_S", [128, 8, 64])
        Sb = [P.sbuf(f"ssd_Sb{i}", [128, 8, 64], BF16) for i in range(2)]
        P.dve(lambda e: e.memset(S[:], 0.0), writes=[S])
        P.dve(lambda e: e.memset(Sb[0][:], 0.0), writes=[Sb[0]])
        tokN = [P.sbuf(f"ssd_tokN{i}", [128, 520]) for i in range(2)]
        xN = P.sbuf("ssd_xN", [128, 512])
        BNb = P.sbuf("ssd_BNb", [128, 256], BF16)
        dt8 = P.sbuf("ssd_dt8", [128, 8])
        a8 = P.sbuf("ssd_a8", [128, 8])
        dw8 = P.sbuf("ssd_dw8", [128, 8])
        ac16 = P.sbuf("ssd_ac16", [128, 2, 8])
        e32 = P.sbuf("ssd_e32", [128, 32])
        Aexp = P.sbuf("ssd_Aexp", [128, 8, 128])
        seg = P.sbuf("ssd_seg", [128, 8, 128])
        CBm = P.sbuf("ssd_CBm", [128, 2, 128])
        MT = P.sbuf("ssd_MT", [128, 8, 128], BF16)
        xdt = P.sbuf("ssd_xdt", [128, 8, 64], BF16)
        xw = P.sbuf("ssd_xw", [128, 8, 64], BF16)
        y1 = P.sbuf("ssd_y1", [128, 512])
        y2 = P.sbuf("ssd_y2", [128, 512])
        yo = [P.sbuf(f"ssd_yo{i}", [128, 512]) for i in range(2)]
        psA = P.psum("ssd_psA", [128, 512])
        psB = P.psum("ssd_psB", [128, 512])
        psC = P.psum("ssd_psC", [128, 512])
        psD = P.psum("ssd_psD", [128, 512])
        psE = P.psum("ssd_psE", [128, 512])
        psF = P.psum("ssd_psF", [128, 512])
        psG = [P.psum(f"ssd_psG{i}", [128, 512]) for i in range(2)]

        bg_begin(P)

        def load(t):
            P.dma("sp", tokN[t % 2][:], projN.t[t * 128:(t + 1) * 128, PN_SZ:PN_SZ + 520], reads=[projN], writes=[tokN[t % 2]])

        load(0)
        for t in range(NT):
            if t + 1 < NT:
                load(t + 1)
            tk = tokN[t % 2]
            tok = slice(t * 128, (t + 1) * 128)
            for k in range(4):
                P.pe(lambda e, k=k: e.transpose(out=psA[:, k * 128:(k + 1) * 128], in_=act[:, k, tok], identity=C.c("ident")), reads=[act] + cf, writes=[psA])
            for k in range(2):
                P.pe(lambda e, k=k: e.transpose(out=psB[:, k * 128:(k + 1) * 128], in_=act[:, 4 + k, tok], identity=C.c("ident")), reads=[act] + cf, writes=[psB])
            P.act(lambda e: e.copy(out=xN[:], in_=psA[:]), reads=[psA], writes=[xN])
            P.dve(lambda e: e.tensor_copy(out=BNb[:], in_=psB[:, 0:256]), reads=[psB], writes=[BNb])
            P.dve(lambda e: e.tensor_tensor(out=dt8[:], in0=tk[:, 512:520], in1=dtb[:], op=ALU.add), reads=[tk, dtb], writes=[dt8])
            softplus_small(P, dt8[:], dt8, None, tmp["one"])
            P.dve(lambda e: e.tensor_tensor(out=a8[:], in0=dt8[:], in1=aneg[:], op=ALU.mult), reads=[dt8, aneg], writes=[a8])
            P.dve(lambda e: e.tensor_tensor(out=Aexp[:], in0=bc(a8[:].unsqueeze(2), [128, 8, 128]), in1=bc(C.c("tri01").unsqueeze(1), [128, 8, 128]), op=ALU.mult),
                  reads=[a8] + cf, writes=[Aexp])
            P.dve(lambda e: e.tensor_tensor(out=ac16[:], in0=bc(a8[:].unsqueeze(1), [128, 2, 8]), in1=bc(C.c("chunkind").unsqueeze(2), [128, 2, 8]), op=ALU.mult),
                  reads=[a8] + cf, writes=[ac16])
            P.pe(lambda e: e.matmul(psC[:], lhsT=C.c("su01"), rhs=Aexp[:, 0:4, :].rearrange("p h i -> p (h i)"), start=True, stop=True), reads=[Aexp] + cf, writes=[psC])
            P.pe(lambda e: e.matmul(psD[:], lhsT=C.c("su01"), rhs=Aexp[:, 4:8, :].rearrange("p h i -> p (h i)"), start=True, stop=True), reads=[Aexp] + cf, writes=[psD])
            P.act(lambda e: e.activation(out=seg[:, 0:4, :].rearrange("p h i -> p (h i)"), in_=psC[:], func=AF.Exp), reads=[psC], writes=[seg])
            P.act(lambda e: e.activation(out=seg[:, 4:8, :].rearrange("p h i -> p (h i)"), in_=psD[:], func=AF.Exp), reads=[psD], writes=[seg])
            P.pe(lambda e: e.matmul(psE[:, 0:8], lhsT=C.c("tri01"), rhs=a8[:], start=True, stop=True), reads=[a8] + cf, writes=[psE])
            P.pe(lambda e: e.matmul(psE[:, 8:16], lhsT=C.c("su01"), rhs=a8[:], start=True, stop=True), reads=[a8] + cf, writes=[psE])
            P.pe(lambda e: e.matmul(psE[:, 16:32], lhsT=C.c("ones"), rhs=ac16[:].rearrange("p c h -> p (c h)"), start=True, stop=True), reads=[ac16] + cf, writes=[psE])
            P.act(lambda e: e.activation(out=e32[:], in_=psE[:, 0:32], func=AF.Exp), reads=[psE], writes=[e32])
            ea = e32[:, 0:8]
            w8 = e32[:, 8:16]
            for g in range(2):
                P.pe(lambda e, g=g: e.matmul(psB[:, 256 + g * 128:256 + (g + 1) * 128], lhsT=BCb[:, g, tok], rhs=BCb[:, 2 + g, tok], start=True, stop=True),
                     reads=[BCb], writes=[psB])
            P.dve(lambda e: e.tensor_tensor(out=CBm[:], in0=psB[:, 256:512].rearrange("p (g i) -> p g i", g=2), in1=bc(C.c("tri01").unsqueeze(1), [128, 2, 128]), op=ALU.mult),
                  reads=[psB] + cf, writes=[CBm])
            P.dve(lambda e: e.tensor_tensor(out=MT[:].rearrange("p (g r) i -> p g r i", g=2), in0=seg[:].rearrange("p (g r) i -> p g r i", g=2),
                                            in1=bc(CBm[:].unsqueeze(2), [128, 2, 4, 128]), op=ALU.mult), reads=[seg, CBm], writes=[MT])
            P.dve(lambda e: e.tensor_tensor(out=dw8[:], in0=dt8[:], in1=w8, op=ALU.mult), reads=[dt8, e32], writes=[dw8])
            P.pool(lambda e: e.tensor_tensor(out=xdt[:], in0=xN[:].rearrange("p (h q) -> p h q", h=8), in1=bc(dt8[:].unsqueeze(2), [128, 8, 64]), op=ALU.mult),
                   reads=[xN, dt8], writes=[xdt])
            P.pool(lambda e: e.tensor_tensor(out=xw[:], in0=xN[:].rearrange("p (h q) -> p h q", h=8), in1=bc(dw8[:].unsqueeze(2), [128, 8, 64]), op=ALU.mult),
                   reads=[xN, dw8], writes=[xw])
            for h in range(8):
                P.pe(lambda e, h=h: e.matmul(psF[:, h * 64:(h + 1) * 64], lhsT=MT[:, h, :], rhs=xdt[:, h, :], start=True, stop=True), reads=[MT, xdt], writes=[psF])
            for c in range(2):
                rows = slice(c * 64, (c + 1) * 64)
                for g in range(2):
                    P.pe(lambda e, c=c, g=g, rows=rows: e.matmul(psG[c][:, g * 256:(g + 1) * 256], lhsT=BNb[rows, g * 128:(g + 1) * 128],
                                                                 rhs=xw[rows, 4 * g:4 * g + 4, :].rearrange("p h q -> p (h q)"), start=True, stop=True),
                         reads=[BNb, xw], writes=[psG[c]])
            for c in range(2):
                P.dve(lambda e, c=c: e.tensor_tensor(out=S[:], in0=S[:], in1=bc(e32[:, 16 + 8 * c:24 + 8 * c].unsqueeze(2), [128, 8, 64]), op=ALU.mult),
                      reads=[S, e32], writes=[S])
                P.dve(lambda e, c=c: e.tensor_tensor(out=S[:].rearrange("p h q -> p (h q)"), in0=S[:].rearrange("p h q -> p (h q)"), in1=psG[c][:], op=ALU.add),
                      reads=[S, psG[c]], writes=[S])
                if c == 0:
                    P.act(lambda e: e.copy(out=Sb[1][:], in_=S[:]), reads=[S], writes=[Sb[1]])
            for c in range(2):
                rows = slice(c * 64, (c + 1) * 64)
                for g in range(2):
                    P.pe(lambda e, c=c, g=g, rows=rows: e.matmul(psC[rows, g * 256:(g + 1) * 256], lhsT=BCb[:, 2 + g, t * 128 + c * 64:t * 128 + (c + 1) * 64],
                                                                 rhs=Sb[c][:, 4 * g:4 * g + 4, :].rearrange("p h q -> p (h q)"), start=True, stop=True),
                         reads=[BCb, Sb[c]], writes=[psC])
            P.act(lambda e: e.copy(out=Sb[0][:], in_=S[:]), reads=[S], writes=[Sb[0]])
            P.dve(lambda e: e.tensor_tensor(out=y1[:].rearrange("p (h q) -> p h q", h=8), in0=psC[:].rearrange("p (h q) -> p h q", h=8),
                                            in1=bc(ea.unsqueeze(2), [128, 8, 64]), op=ALU.mult), reads=[psC, e32], writes=[y1])
            P.dve(lambda e: e.tensor_tensor(out=y1[:], in0=y1[:], in1=psF[:], op=ALU.add), reads=[y1, psF], writes=[y1])
            P.pool(lambda e: e.tensor_tensor(out=y2[:].rearrange("p (h q) -> p h q", h=8), in0=xN[:].rearrange("p (h q) -> p h q", h=8),
                                             in1=bc(dsk[:].unsqueeze(2), [128, 8, 64]), op=ALU.mult), reads=[xN, dsk], writes=[y2])
            P.pool(lambda e: e.tensor_tensor(out=y1[:], in0=y1[:], in1=y2[:], op=ALU.add), reads=[y1, y2], writes=[y1])
            y = yo[t % 2]
            norm_gate(P, y1[:], [y1], tk[:, 0:512], [tk], nwb[:].rearrange("p (g e) -> p g e", g=2), [nwb], 2, 256, y, tmp, True)
            P.dma("sp", ymix.t[tok, 512:1024], y[:], reads=[y], writes=[ymix])
            bg_tick(P, 2)
        bg_end(P)


def stage_gdn(P, C, projT, projN, ymix, prm, l):
    H = 4
    with P.scope():
        cf = [C.f]
        cw = P.sbuf("gdn_cw", [128, 12, 4])
        dtb = P.sbuf("gdn_dtb", [128, 4])
        aneg = P.sbuf("gdn_aneg", [128, 4])
        nwb = P.sbuf("gdn_nwb", [128, 128])
        P.dma("sp", cw[:], prm["gdn_conv_w"].t[l], reads=[prm["gdn_conv_w"]], writes=[cw])
        P.dma("sp", dtb[:], prm["gdn_dt_bias"].t[l:l + 1, :].partition_broadcast(128), reads=[prm["gdn_dt_bias"]], writes=[dtb])
        P.dma("sp", aneg[:], prm["gdn_a_log"].t[l:l + 1, :].partition_broadcast(128), reads=[prm["gdn_a_log"]], writes=[aneg])
        P.dma("sp", nwb[:], prm["gdn_norm_w"].t[l:l + 1, :].partition_broadcast(128), reads=[prm["gdn_norm_w"]], writes=[nwb])
        P.act(lambda e: e.activation(out=aneg[:], in_=aneg[:], func=AF.Exp), reads=[aneg], writes=[aneg])
        P.dve(lambda e: e.tensor_scalar(out=aneg[:], in0=aneg[:], scalar1=-1.0, scalar2=None, op0=ALU.mult), reads=[aneg], writes=[aneg])
        tmp = ng_tmp(P)
        qkvb = P.sbuf("gdn_qkvb", [128, 12, SEQ], BF16)
        with P.scope():
            cvt = P.sbuf("gdn_cvt", [128, 1, SEQ])
            sq = P.sbuf("gdn_sq", [128, SEQ])
            rinv = P.sbuf("gdn_rinv", [128, SEQ])
            pss = [P.psum(f"gdn_pss{i}", [128, 512]) for i in range(4)]
            xpad = [P.sbuf(f"gdn_xpad{i}", [128, SEQ + 3]) for i in range(2)]
            acc = P.sbuf("gdn_acc", [128, SEQ])
            for i in range(2):
                P.pool(lambda e, i=i: e.memset(xpad[i][:, 0:3], 0.0), writes=[xpad[i]])
            for ct in range(12):
                xp = xpad[ct % 2]
                P.dma("sp", xp[:, 3:SEQ + 3], projT.t[PT_GQKV + ct * 128:PT_GQKV + (ct + 1) * 128, :], reads=[projT], writes=[xp])
                P.dve(lambda e, ct=ct, xp=xp: e.tensor_scalar(out=acc[:], in0=xp[:, 0:SEQ], scalar1=cw[:, ct, 0:1], scalar2=None, op0=ALU.mult),
                      reads=[xp, cw], writes=[acc])
                for k in range(1, 4):
                    P.dve(lambda e, ct=ct, xp=xp, k=k: e.scalar_tensor_tensor(out=acc[:], in0=xp[:, k:SEQ + k], scalar=cw[:, ct, k:k + 1], in1=acc[:],
                                                                               op0=ALU.mult, op1=ALU.add), reads=[xp, cw, acc], writes=[acc])
                if ct >= 8:
                    P.act(lambda e, ct=ct: e.activation(out=qkvb[:, ct, :], in_=acc[:], func=AF.Silu), reads=[acc], writes=[qkvb])
                    continue
                P.act(lambda e: e.activation(out=cvt[:, 0, :], in_=acc[:], func=AF.Silu), reads=[acc], writes=[cvt])
                P.act(lambda e: e.activation(out=sq[:], in_=cvt[:, 0, :], func=AF.Square), reads=[cvt], writes=[sq])
                for n in range(4):
                    P.pe(lambda e, n=n: e.matmul(pss[n][:], lhsT=C.c("ones"), rhs=sq[:, n * 512:(n + 1) * 512], start=True, stop=True), reads=[sq] + cf, writes=[pss[n]])
                    P.act(lambda e, n=n: e.activation(out=rinv[:, n * 512:(n + 1) * 512], in_=pss[n][:], func=AF.Sqrt, bias=tmp["eps"][:, 0:1]),
                          reads=[pss[n], tmp["eps"]], writes=[rinv])
                P.dve(lambda e: e.reciprocal(out=rinv[:], in_=rinv[:]), reads=[rinv], writes=[rinv])
                scl = 128.0 ** -0.5 if ct < 4 else 1.0
                P.dve(lambda e, ct=ct, scl=scl: e.scalar_tensor_tensor(out=qkvb[:, ct, :], in0=cvt[:, 0, :], scalar=scl, in1=rinv[:], op0=ALU.mult, op1=ALU.mult),
                      reads=[cvt, rinv], writes=[qkvb])
        dbg(1)
        S = P.sbuf("gdn_S", [128, H, 128])
        Sb = P.sbuf("gdn_Sb", [128, H, 128], BF16)
        P.dve(lambda e: e.memset(S[:], 0.0), writes=[S])
        P.dve(lambda e: e.memset(Sb[:], 0.0), writes=[Sb])
        tokN = [P.sbuf(f"gdn_tokN{i}", [128, 520]) for i in range(3)]

        def f4(name, dt=F32):
            return P.sbuf("gdn_" + name, [128, H, 128], dt)

        b4 = P.sbuf("gdn_b4", [128, 4])
        g4 = P.sbuf("gdn_g4", [128, 4])
        gc8 = P.sbuf("gdn_gc8", [128, 2, 4])
        e16s = [P.sbuf(f"gdn_e16_{i}", [128, 16]) for i in range(2)]
        bg4 = P.sbuf("gdn_bg4", [128, 4])
        Gt, Gs, DTm, Dm, egb = f4("Gt"), f4("Gs"), f4("DTm"), f4("Dm"), f4("egb")
        A, AT, TT, vb, Kg = f4("A"), f4("AT"), f4("TT"), f4("vb"), f4("Kg")
        us = [f4("u0"), f4("u1")]
        X = [f4("X0"), f4("X1")]
        XT = [f4("XT0"), f4("XT1")]
        aqkTs, wTs, qdTs, kends = [[f4(f"{n}{i}", BF16) for i in range(2)] for n in ("aqkT", "wT", "qdT", "kend")]
        vnew = f4("vnew", BF16)
        yo = [P.sbuf(f"gdn_yo{i}", [128, 512]) for i in range(2)]
        B0 = P.psum("gdn_B0", [128, 512])
        B1 = P.psum("gdn_B1", [128, 512])
        B2 = P.psum("gdn_B2", [128, 512])
        B3 = P.psum("gdn_B3", [128, 512])
        B4 = P.psum("gdn_B4", [128, 1024], BF16)
        B5 = P.psum("gdn_B5", [128, 512])
        B6 = P.psum("gdn_B6", [128, 512])
        B7 = P.psum("gdn_B7", [128, 512])

        def v4(ap):
            return ap.rearrange("p (h i) -> p h i", h=H)

        def fl(ap):
            return ap.rearrange("p h i -> p (h i)")

        bg_begin(P)

        def load(t):
            P.dma("sp", tokN[t % 3][:], projN.t[t * 128:(t + 1) * 128, PN_GZ:PN_GZ + 520], reads=[projN], writes=[tokN[t % 3]])

        tri = C.c("tri01")
        su = C.c("su01")
        def pre(t, adv):
            tk = tokN[t % 3]
            e16 = e16s[t % 2]; u = us[t % 2]; aqkT = aqkTs[t % 2]; wT = wTs[t % 2]; qdT = qdTs[t % 2]; kend = kends[t % 2]
            tok = slice(t * 128, (t + 1) * 128)
            P.act(lambda e: e.activation(out=b4[:], in_=tk[:, 512:516], func=AF.Sigmoid), reads=[tk], writes=[b4])
            P.dve(lambda e: e.tensor_tensor(out=g4[:], in0=tk[:, 516:520], in1=dtb[:], op=ALU.add), reads=[tk, dtb], writes=[g4])
            softplus_small(P, g4[:], g4, None, tmp["one"])
            P.dve(lambda e: e.tensor_tensor(out=g4[:], in0=g4[:], in1=aneg[:], op=ALU.mult), reads=[g4, aneg], writes=[g4])
            P.dve(lambda e: e.tensor_tensor(out=Gt[:], in0=bc(g4[:].unsqueeze(2), [128, H, 128]), in1=bc(tri.unsqueeze(1), [128, H, 128]), op=ALU.mult),
                  reads=[g4] + cf, writes=[Gt])
            P.pool(lambda e: e.tensor_tensor(out=Gs[:], in0=bc(g4[:].unsqueeze(2), [128, H, 128]), in1=bc(su.unsqueeze(1), [128, H, 128]), op=ALU.mult),
                   reads=[g4] + cf, writes=[Gs])
            P.dve(lambda e: e.tensor_tensor(out=gc8[:], in0=bc(g4[:].unsqueeze(1), [128, 2, 4]), in1=bc(C.c("chunkind").unsqueeze(2), [128, 2, 4]), op=ALU.mult),
                  reads=[g4] + cf, writes=[gc8])
            P.pe(lambda e: e.matmul(B0[:], lhsT=su, rhs=fl(Gt[:]), start=True, stop=True), reads=[Gt] + cf, writes=[B0])
            P.pe(lambda e: e.matmul(B1[:], lhsT=tri, rhs=fl(Gs[:]), start=True, stop=True), reads=[Gs] + cf, writes=[B1])
            P.pe(lambda e: e.matmul(B2[:], lhsT=C.c("ones"), rhs=fl(Gt[:]), start=True, stop=True), reads=[Gt] + cf, writes=[B2])
            P.pe(lambda e: e.matmul(B3[:, 0:4], lhsT=tri, rhs=g4[:], start=True, stop=True), reads=[g4] + cf, writes=[B3])
            P.pe(lambda e: e.matmul(B3[:, 4:8], lhsT=su, rhs=g4[:], start=True, stop=True), reads=[g4] + cf, writes=[B3])
            P.pe(lambda e: e.matmul(B3[:, 8:16], lhsT=C.c("ones"), rhs=gc8[:].rearrange("p c h -> p (c h)"), start=True, stop=True), reads=[gc8] + cf, writes=[B3])
            P.act(lambda e: e.activation(out=fl(DTm[:]), in_=B0[:], func=AF.Exp), reads=[B0], writes=[DTm])
            P.act(lambda e: e.activation(out=fl(Dm[:]), in_=B1[:], func=AF.Exp), reads=[B1], writes=[Dm])
            P.act(lambda e: e.activation(out=fl(egb[:]), in_=B2[:], func=AF.Exp), reads=[B2], writes=[egb])
            P.act(lambda e: e.activation(out=e16[:], in_=B3[:, 0:16], func=AF.Exp), reads=[B3], writes=[e16])
            P.pool(lambda e: e.tensor_tensor(out=DTm[:], in0=DTm[:], in1=bc(tri.unsqueeze(1), [128, H, 128]), op=ALU.mult), reads=[DTm] + cf, writes=[DTm])
            P.pool(lambda e: e.tensor_tensor(out=Dm[:], in0=Dm[:], in1=bc(su.unsqueeze(1), [128, H, 128]), op=ALU.mult), reads=[Dm] + cf, writes=[Dm])
            P.dve(lambda e: e.tensor_tensor(out=bg4[:], in0=b4[:], in1=e16[:, 0:4], op=ALU.mult), reads=[b4, e16], writes=[bg4])
            dbg(2)
            adv()
            for h in range(H):
                P.pe(lambda e, h=h: e.transpose(out=B4[:, h * 128:(h + 1) * 128], in_=qkvb[:, 4 + h, tok], identity=C.identb[:]), reads=[qkvb, C.identb], writes=[B4])
            for h in range(H):
                P.pe(lambda e, h=h: e.transpose(out=B4[:, 512 + h * 128:512 + (h + 1) * 128], in_=qkvb[:, 8 + h, tok], identity=C.identb[:]), reads=[qkvb, C.identb], writes=[B4])
            P.dve(lambda e: e.tensor_tensor(out=vb[:], in0=v4(B4[:, 512:1024]), in1=bc(b4[:].unsqueeze(2), [128, H, 128]), op=ALU.mult), reads=[B4, b4], writes=[vb])
            P.dve(lambda e: e.tensor_tensor(out=Kg[:], in0=v4(B4[:, 0:512]), in1=bc(bg4[:].unsqueeze(2), [128, H, 128]), op=ALU.mult), reads=[B4, bg4], writes=[Kg])
            P.dve(lambda e: e.tensor_tensor(out=kend[:], in0=v4(B4[:, 0:512]), in1=bc(e16[:, 4:8].unsqueeze(2), [128, H, 128]), op=ALU.mult), reads=[B4, e16], writes=[kend])
            dbg(3)
            adv()
            for h in range(H):
                P.pe(lambda e, h=h: e.matmul(B0[:, h * 128:(h + 1) * 128], lhsT=qkvb[:, 4 + h, tok], rhs=qkvb[:, 4 + h, tok], start=True, stop=True), reads=[qkvb], writes=[B0])
            for h in range(H):
                P.pe(lambda e, h=h: e.matmul(B1[:, h * 128:(h + 1) * 128], lhsT=qkvb[:, 4 + h, tok], rhs=qkvb[:, h, tok], start=True, stop=True), reads=[qkvb], writes=[B1])
            P.dve(lambda e: e.tensor_tensor(out=A[:], in0=v4(B0[:]), in1=Dm[:], op=ALU.mult), reads=[B0, Dm], writes=[A])
            P.dve(lambda e: e.tensor_tensor(out=A[:], in0=A[:], in1=bc(b4[:].unsqueeze(2), [128, H, 128]), op=ALU.mult), reads=[A, b4], writes=[A])
            P.dve(lambda e: e.tensor_tensor(out=aqkT[:], in0=v4(B1[:]), in1=DTm[:], op=ALU.mult), reads=[B1, DTm], writes=[aqkT])
            P.pool(lambda e: e.tensor_tensor(out=qdT[:], in0=qkvb[:, 0:4, tok], in1=egb[:], op=ALU.mult), reads=[qkvb, egb], writes=[qdT])
            dbg(4)
            adv()
            for h in range(H):
                P.pe(lambda e, h=h: e.transpose(out=B2[:, h * 128:(h + 1) * 128], in_=A[:, h, :], identity=C.c("ident")), reads=[A] + cf, writes=[B2])
            dbg(4.3)
            adv()
            P.act(lambda e: e.copy(out=fl(AT[:]), in_=B2[:]), reads=[B2], writes=[AT])
            dbg(4.6)
            adv()
            P.dve(lambda e: e.scalar_tensor_tensor(out=TT[:], in0=v4(B2[:]), scalar=-1.0, in1=bc(C.c("ident").unsqueeze(1), [128, H, 128]), op0=ALU.mult, op1=ALU.add),
                  reads=[B2] + cf, writes=[TT])
            dbg(5)
            adv()
            Xc, XTc = A, AT
            for k in range(1, 6):
                Xn, XTn = X[k % 2], XT[k % 2]
                for h in range(H):
                    P.pe(lambda e, h=h, Xc=Xc, XTc=XTc: e.matmul(B0[:, h * 128:(h + 1) * 128], lhsT=XTc[:, h, :], rhs=Xc[:, h, :], start=True, stop=True),
                         reads=[Xc, XTc], writes=[B0])
                if k < 5:
                    for h in range(H):
                        P.pe(lambda e, h=h, Xc=Xc, XTc=XTc: e.matmul(B1[:, h * 128:(h + 1) * 128], lhsT=Xc[:, h, :], rhs=XTc[:, h, :], start=True, stop=True),
                             reads=[Xc, XTc], writes=[B1])
                P.act(lambda e, Xn=Xn: e.copy(out=fl(Xn[:]), in_=B0[:]), reads=[B0], writes=[Xn])
                if k < 5:
                    P.dve(lambda e, XTn=XTn: e.tensor_copy(out=fl(XTn[:]), in_=B1[:]), reads=[B1], writes=[XTn])
                for h in range(H):
                    P.pe(lambda e, h=h, Xn=Xn: e.matmul(B2[:, h * 128:(h + 1) * 128], lhsT=Xn[:, h, :], rhs=TT[:, h, :], start=True, stop=True),
                         reads=[Xn, TT], writes=[B2])
                P.dve(lambda e: e.tensor_tensor(out=fl(TT[:]), in0=fl(TT[:]), in1=B2[:], op=ALU.add), reads=[TT, B2], writes=[TT])
                adv()
                Xc, XTc = Xn, XTn
            dbg(6)
            adv()
            for h in range(H):
                P.pe(lambda e, h=h: e.matmul(B0[:, h * 128:(h + 1) * 128], lhsT=TT[:, h, :], rhs=vb[:, h, :], start=True, stop=True), reads=[TT, vb], writes=[B0])
            for h in range(H):
                P.pe(lambda e, h=h: e.matmul(B1[:, h * 128:(h + 1) * 128], lhsT=Kg[:, h, :], rhs=TT[:, h, :], start=True, stop=True), reads=[TT, Kg], writes=[B1])
            P.act(lambda e: e.copy(out=fl(u[:]), in_=B0[:]), reads=[B0], writes=[u])
            P.dve(lambda e: e.tensor_copy(out=fl(wT[:]), in_=B1[:]), reads=[B1], writes=[wT])

        def scan(t):
            tk = tokN[t % 3]
            tok = slice(t * 128, (t + 1) * 128)
            e16 = e16s[t % 2]; u = us[t % 2]; aqkT = aqkTs[t % 2]; wT = wTs[t % 2]; qdT = qdTs[t % 2]; kend = kends[t % 2]
            for c in range(2):
                rows = slice(c * 64, (c + 1) * 64)
                for h in range(H):
                    P.pe(lambda e, h=h, rows=rows: e.matmul(B5[rows, h * 128:(h + 1) * 128], lhsT=wT[:, h, rows], rhs=Sb[:, h, :], start=True, stop=True),
                         reads=[wT, Sb], writes=[B5])
                P.dve(lambda e, rows=rows: e.tensor_tensor(out=fl(vnew[rows]), in0=fl(u[rows]), in1=B5[rows, :], op=ALU.subtract), reads=[u, B5], writes=[vnew])
                yield
                for h in range(H):
                    P.pe(lambda e, h=h, rows=rows: e.matmul(B7[rows, h * 128:(h + 1) * 128], lhsT=qdT[:, h, rows], rhs=Sb[:, h, :], start=True, stop=False),
                         reads=[qdT, Sb], writes=[B7])
                    P.pe(lambda e, h=h, rows=rows: e.matmul(B7[rows, h * 128:(h + 1) * 128], lhsT=aqkT[rows, h, rows], rhs=vnew[rows, h, :], start=False, stop=True),
                         reads=[aqkT, vnew], writes=[B7])
                for h in range(H):
                    P.pe(lambda e, h=h, rows=rows: e.matmul(B6[:, h * 128:(h + 1) * 128], lhsT=kend[rows, h, :], rhs=vnew[rows, h, :], start=True, stop=True),
                         reads=[kend, vnew], writes=[B6])
                yield
                P.dve(lambda e, c=c: e.tensor_tensor(out=S[:], in0=S[:], in1=bc(e16[:, 8 + 4 * c:12 + 4 * c].unsqueeze(2), [128, H, 128]), op=ALU.mult),
                      reads=[S, e16], writes=[S])
                P.dve(lambda e: e.tensor_tensor(out=fl(S[:]), in0=fl(S[:]), in1=B6[:], op=ALU.add), reads=[S, B6], writes=[S])
                P.act(lambda e: e.copy(out=Sb[:], in_=S[:]), reads=[S], writes=[Sb])
            yield
            y = yo[t % 2]
            norm_gate(P, B7[:], [B7], tk[:, 0:512], [tk], bc(nwb[:].unsqueeze(1), [128, 4, 128]), [nwb], 4, 128, y, tmp, False)
            P.dma("sp", ymix.t[tok, 1024:1536], y[:], reads=[y], writes=[ymix])
            yield

        load(0)
        load(1)
        pre(0, lambda: None)
        for t in range(NT):
            if t + 2 < NT:
                load(t + 2)
            gen = scan(t)
            adv = (lambda gen=gen: next(gen, None))
            if t + 1 < NT:
                pre(t + 1, adv)
            for _ in gen:
                pass
            bg_tick(P, 2)
        bg_end(P)


NBIG = 30000.0


def t5_bucket_np(rel):
    n = np.maximum(rel, 0)
    exact = 16
    large = exact + (np.log(np.maximum(n, 1).astype(np.float32) / np.float32(exact)) / np.float32(math.log(128 / 16)) * np.float32(32 - exact)).astype(np.int32)
    return np.where(n < exact, n, np.minimum(large, 31)).astype(np.int64)


def nsa_host_tables(rel_bias):
    rb = np.asarray(rel_bias, np.float32)
    ki = np.arange(128)[:, None]
    qi = np.arange(128)[None, :]
    r0 = qi - ki
    r128 = 128 + qi - ki
    qq = np.arange(128)[:, None]
    mm = np.arange(248)[None, :]
    rc = qq - 16 * (mm - 120) - 31
    tab = np.concatenate([
        rb[t5_bucket_np(r0)].transpose(0, 2, 1).reshape(128, 8 * 128),
        rb[t5_bucket_np(r128)].transpose(0, 2, 1).reshape(128, 8 * 128),
        rb[t5_bucket_np(rc)].transpose(0, 2, 1).reshape(128, 8 * 248)], axis=1)
    t31 = np.broadcast_to(rb[31][None, :], (128, 8)).copy()
    return np.ascontiguousarray(tab, np.float32), np.ascontiguousarray(t31, np.float32)


def nsa_host_consts():
    ki = np.arange(128)[:, None]
    qi = np.arange(128)[None, :]
    qq = np.arange(128)[:, None]
    mm = np.arange(248)[None, :]
    rc = qq - 16 * (mm - 120) - 31
    m0 = np.where(qi - ki >= 0, 0.0, -NBIG)
    msk = np.concatenate([
        np.broadcast_to(m0[:, None, :], (128, 8, 128)).reshape(128, -1),
        np.zeros((128, 8 * 128)),
        np.broadcast_to(np.where(rc >= 0, 0.0, -NBIG)[:, None, :], (128, 8, 248)).reshape(128, -1)], axis=1)
    mtri = np.where(ki > qi, 0.0, -NBIG)
    k = np.arange(128)[:, None]
    j = np.arange(32)[None, :]
    ov = ((16 * k <= 64 * j + 63) & (16 * k + 31 >= 64 * j) & (k < 127)).astype(np.float32)
    keep = np.zeros((128, 16, 32)); addc = np.zeros((128, 16, 32))
    for qb in range(16):
        cur = (2 * qb + (np.arange(128) >= 64))[:, None]
        blk = np.arange(32)[None, :]
        forced = (blk == 0) | (blk == cur) | (blk == cur - 1)
        fut = blk > cur
        keep[:, qb, :] = (~forced & ~fut)
        addc[:, qb, :] = np.where(fut, -1e30, np.where(forced, 1e9, 0.0))
    E = np.zeros((128, 2048))
    E[:32] = (np.arange(2048)[None, :] // 64) == np.arange(32)[:, None]
    parts = dict(msk=msk, mtri=mtri, ov=ov, keep=keep.reshape(128, -1), addc=addc.reshape(128, -1), E=E)
    cols = {}
    o = 0
    arrs = []
    for n, a in parts.items():
        cols[n] = (o, a.shape[1]); o += a.shape[1]; arrs.append(a.astype(np.float32))
    return np.concatenate(arrs, axis=1), cols


NSA_CST_NP, NSA_CST_COLS = nsa_host_consts()


def stage_nsa(P, C, projT, projN, ymix, prm, l):
    G, R = 2, 4
    with P.scope():
        cf = [C.f]
        tmp = ng_tmp(P)
        one = tmp["one"]
        Bn0 = P.sbuf("nsa_Bn0", [128, 8, 128], BF16)
        Bn1 = P.sbuf("nsa_Bn1", [128, 8, 128], BF16)
        Mtri = P.sbuf("nsa_Mtri", [128, 4, 128], BF16)
        FT = P.sbuf("nsa_FT", [128, 8, 248])
        ovb = P.sbuf("nsa_ovb", [128, 32], BF16)
        keep = P.sbuf("nsa_keep", [128, 16, 32])
        addc = P.sbuf("nsa_addc", [128, 16, 32])
        Eb = P.sbuf("nsa_Eb", [32, 2048], BF16)
        ncst = prm["nsa_cst"]
        cc = NSA_CST_COLS
        P.dma("sp", keep[:].rearrange("p a b -> p (a b)"), ncst.t[:, cc["keep"][0]:cc["keep"][0] + 512], reads=[ncst], writes=[keep])
        P.dma("sp", addc[:].rearrange("p a b -> p (a b)"), ncst.t[:, cc["addc"][0]:cc["addc"][0] + 512], reads=[ncst], writes=[addc])
        with P.scope():
            tb = P.sbuf("nsa_tb", [128, 4032])
            mk = P.sbuf("nsa_mk", [128, 4032])
            t31 = P.sbuf("nsa_t31", [128, 8])
            st = P.sbuf("nsa_st", [128, 2048])
            P.dma("sp", tb[:], prm["nsa_tab"].t[:, :], reads=[prm["nsa_tab"]], writes=[tb])
            P.dma("sp", mk[:], ncst.t[:, cc["msk"][0]:cc["msk"][0] + 4032], reads=[ncst], writes=[mk])
            P.dma("sp", t31[:], prm["nsa_t31"].t[:, :], reads=[prm["nsa_t31"]], writes=[t31])
            for (o, w, dst) in ((0, 128, Bn0), (1024, 128, Bn1), (2048, 248, FT)):
                v = tb[:, o:o + 8 * w].rearrange("p (h x) -> p h x", h=8)
                P.dve(lambda e, v=v, w=w: e.tensor_tensor(out=v, in0=v, in1=bc(t31[:].unsqueeze(2), [128, 8, w]), op=ALU.subtract), reads=[tb, t31], writes=[tb])
                P.dve(lambda e, v=v, w=w, o=o, dst=dst: e.tensor_tensor(out=dst[:], in0=v, in1=mk[:, o:o + 8 * w].rearrange("p (h x) -> p h x", h=8), op=ALU.add),
                      reads=[tb, mk], writes=[dst])
            P.dma("sp", st[:, 0:128], ncst.t[:, cc["mtri"][0]:cc["mtri"][0] + 128], reads=[ncst], writes=[st])
            P.dve(lambda e: e.tensor_copy(out=Mtri[:], in_=bc(st[:, 0:128].unsqueeze(1), [128, 4, 128])), reads=[st], writes=[Mtri])
            P.dma("sp", st[:, 128:160], ncst.t[:, cc["ov"][0]:cc["ov"][0] + 32], reads=[ncst], writes=[st])
            P.dve(lambda e: e.tensor_copy(out=ovb[:], in_=st[:, 128:160]), reads=[st], writes=[ovb])
            P.dma("sp", st[0:32, :], ncst.t[0:32, cc["E"][0]:cc["E"][0] + 2048], reads=[ncst], writes=[st])
            P.dve(lambda e: e.tensor_copy(out=Eb[:], in_=st[0:32, :]), reads=[st], writes=[Eb])
        dbg(0.1)
        qTb = P.sbuf("nsa_qTb", [64, 8, SEQ], BF16)
        ksT = P.sbuf("nsa_ksT", [64, 2, SEQ], BF16)
        kwT = P.sbuf("nsa_kwT", [64, 2, SEQ], BF16)
        vsb = P.sbuf("nsa_vsb", [128, 16, 2, 65], BF16)
        vwb = P.sbuf("nsa_vwb", [128, 16, 2, 65], BF16)
        gts = P.sbuf("nsa_gts", [128, 16, 24])
        kcT = P.sbuf("nsa_kcT", [64, 2, 128], BF16)
        vcx = P.sbuf("nsa_vcx", [128, 2, 96], BF16)
        with P.scope():
            stg = [P.sbuf(f"nsa_stg{i}", [64, SEQ]) for i in range(2)]
            n = 0
            for h in range(8):
                s_ = stg[n % 2]; n += 1
                P.dma("sp", s_[:], projT.t[PT_NQ + h * 64:PT_NQ + (h + 1) * 64, :], reads=[projT], writes=[s_])
                P.act(lambda e, h=h, s_=s_: e.activation(out=qTb[:, h, :], in_=s_[:], func=AF.Copy, scale=0.125), reads=[s_], writes=[qTb])
            for (r0, dst) in ((PT_NKS, ksT), (PT_NKW, kwT)):
                for g in range(2):
                    s_ = stg[n % 2]; n += 1
                    P.dma("sp", s_[:], projT.t[r0 + g * 64:r0 + (g + 1) * 64, :], reads=[projT], writes=[s_])
                    P.dve(lambda e, g=g, s_=s_, dst=dst: e.tensor_copy(out=dst[:, g, :], in_=s_[:]), reads=[s_], writes=[dst])
            tn = P.sbuf("nsa_tn", [128, 16, 280])
            P.dma("sp", tn[:], projN.t[:, 0:280].rearrange("(t p) c -> p t c", p=128), reads=[projN], writes=[tn])
            P.pool(lambda e: e.memset(vsb[:], 1.0), writes=[vsb])
            P.pool(lambda e: e.memset(vwb[:], 1.0), writes=[vwb])
            P.dve(lambda e: e.tensor_copy(out=vsb[:, :, :, 0:64], in_=tn[:, :, 0:128].rearrange("p t (g d) -> p t g d", g=2)), reads=[tn], writes=[vsb])
            P.dve(lambda e: e.tensor_copy(out=vwb[:, :, :, 0:64], in_=tn[:, :, 128:256].rearrange("p t (g d) -> p t g d", g=2)), reads=[tn], writes=[vwb])
            P.act(lambda e: e.activation(out=gts[:], in_=tn[:, :, 256:280], func=AF.Sigmoid), reads=[tn], writes=[gts])
            dbg(0.2)
            tT = P.sbuf("nsa_tT", [64, 2, SEQ])
            tG = P.sbuf("nsa_tG", [64, 2, 16, 129], BF16)
            P.pool(lambda e: e.memset(tG[:], 0.0), writes=[tG])
            w1f = P.sbuf("nsa_w1f", [64, 32, 64])
            w1b = P.sbuf("nsa_w1b", [64, 32, 64], BF16)
            w2f = P.sbuf("nsa_w2f", [64, 64])
            w2b = P.sbuf("nsa_w2b", [64, 64], BF16)
            posT = P.sbuf("nsa_posT", [64, 32])
            posb = P.sbuf("nsa_posb", [64, 32], BF16)
            cvec = P.sbuf("nsa_cvec", [64, 1])
            hid = P.sbuf("nsa_hid", [64, 2, 128], BF16)
            ps_h = P.psum("nsa_ps_h", [64, 512])
            ps_c = P.psum("nsa_ps_c", [64, 8])
            ps_o = P.psum("nsa_ps_o", [128, 512])
            P.dve(lambda e: e.memset(hid[:], 0.0), writes=[hid])
            for kv in range(2):
                r0 = PT_NKC if kv == 0 else PT_NVC
                for g in range(2):
                    P.dma("sp", tT[:, g, :], projT.t[r0 + g * 64:r0 + (g + 1) * 64, :], reads=[projT], writes=[tT])
                    P.dve(lambda e, g=g: e.tensor_copy(out=tG[:, g, :, 0:128], in_=tT[:, g, :].rearrange("p (n s) -> p s n", s=16)), reads=[tT], writes=[tG])
                w1src = prm["nsa_cmp_w1"].t[l, kv].rearrange("(j d) o -> d j o", d=64)
                P.dma("sp", w1f[:], w1src, reads=[prm["nsa_cmp_w1"]], writes=[w1f])
                P.pool(lambda e: e.tensor_copy(out=w1b[:], in_=w1f[:]), reads=[w1f], writes=[w1b])
                P.dma("sp", w2f[:], prm["nsa_cmp_w2"].t[l, kv], reads=[prm["nsa_cmp_w2"]], writes=[w2f])
                P.dve(lambda e: e.tensor_copy(out=w2b[:], in_=w2f[:]), reads=[w2f], writes=[w2b])
                P.dma("sp", posT[:], prm["nsa_cmp_pos"].t[l, kv], reads=[prm["nsa_cmp_pos"]], writes=[posT])
                P.dve(lambda e: e.tensor_copy(out=posb[:], in_=posT[:]), reads=[posT], writes=[posb])
                for j in range(32):
                    P.pe(lambda e, j=j: e.matmul(ps_c[:, 0:1], lhsT=w1b[:, j, :], rhs=posb[:, j:j + 1], start=(j == 0), stop=(j == 31)), reads=[w1b, posb], writes=[ps_c])
                P.dve(lambda e: e.tensor_copy(out=cvec[:], in_=ps_c[:, 0:1]), reads=[ps_c], writes=[cvec])
                dbg(0.3 + 0.3 * kv)
                for g in range(2):
                    rows = slice(g * 64, (g + 1) * 64)
                    dbg(0.32 + 0.03 * g + 0.3 * kv)
                    for j in range(32):
                        P.pe(lambda e, j=j, g=g, rows=rows: e.matmul(ps_h[:, g * 128:(g + 1) * 128], lhsT=w1b[:, j, :], rhs=tG[:, g, j % 16, (j // 16):(j // 16) + 128],
                                                                    start=(j == 0), stop=(j == 31)), reads=[w1b, tG], writes=[ps_h])
                for g in range(2):
                    P.act(lambda e, g=g: e.activation(out=hid[:, g, :], in_=ps_h[:, g * 128:(g + 1) * 128], func=AF.Silu, bias=cvec[:, 0:1]),
                          reads=[ps_h, cvec], writes=[hid])
                dbg(0.4 + 0.3 * kv)
                if kv == 0:
                    P.pe(lambda e: e.matmul(ps_h[:, 256:512], lhsT=w2b[:], rhs=hid[:].rearrange("o g n -> o (g n)"), start=True, stop=True), reads=[w2b, hid], writes=[ps_h])
                    P.dve(lambda e: e.tensor_copy(out=kcT[:].rearrange("o g n -> o (g n)"), in_=ps_h[:, 256:512]), reads=[ps_h], writes=[kcT])
                else:
                    for g in range(2):
                        P.pe(lambda e, g=g: e.matmul(ps_o[:, g * 64:(g + 1) * 64], lhsT=hid[:, g, :], rhs=w2b[:], start=True, stop=True), reads=[w2b, hid], writes=[ps_o])
                    P.dve(lambda e: e.tensor_copy(out=vcx[:, :, 0:64], in_=ps_o[:, 0:128].rearrange("p (g d) -> p g d", g=2)), reads=[ps_o], writes=[vcx])
                    P.dve(lambda e: e.tensor_copy(out=vcx[:, :, 64:96], in_=bc(ovb[:].unsqueeze(1), [128, 2, 32])), reads=[ovb], writes=[vcx])
        dbg(1)
        ps_sc = [P.psum(f"nsa_ps_sc{i}", [128, 512]) for i in range(2)]
        ps_os = P.psum("nsa_ps_os", [128, 512])
        ps_ow = P.psum("nsa_ps_ow", [128, 512])
        ps_ocs = [P.psum(f"nsa_ps_oc{i}", [128, 512]) for i in range(2)]
        ps_cs = P.psum("nsa_ps_cs", [128, 512])
        ps_tr = P.psum("nsa_ps_tr", [128, 1024], BF16)
        sc = P.sbuf("nsa_sc", [128, 4, 128])
        ssum = P.sbuf("nsa_ssum", [128, 4])
        pnb = P.sbuf("nsa_pnb", [128, 4, 128], BF16)
        pT = P.sbuf("nsa_pT", [128, 4, 128], BF16)
        imp = P.sbuf("nsa_imp", [128, 32])
        mx8 = P.sbuf("nsa_mx8", [128, 8])
        negm = P.sbuf("nsa_negm", [128, 32], BF16)
        negTs = [P.sbuf(f"nsa_negT{i}", [32, 4, 128], BF16) for i in range(2)]
        eT = [P.sbuf(f"nsa_eT{i}", [128, 4, 128], BF16) for i in range(2)]
        cs = P.sbuf("nsa_cs", [128, 3, 4])
        ya = P.sbuf("nsa_ya", [128, 4, 64])
        yb = P.sbuf("nsa_yb", [128, 4, 64])
        yt = [P.sbuf(f"nsa_yt{i}", [128, 512]) for i in range(2)]
        nsc = 0
        bg_begin(P)

        def v3(ap, r=4):
            return ap.rearrange("p (r q) -> p r q", r=r)

        def phaseA(qb, g, slot):
            qtok = slice(qb * 128, (qb + 1) * 128)
            hs = slice(4 * g, 4 * g + 4)
            ps_oc_s = ps_ocs[slot]
            negT_s = negTs[slot]
            for r in range(R):
                P.pe(lambda e, r=r: e.matmul(ps_cs[:, r * 128:(r + 1) * 128], lhsT=qTb[:, 4 * g + r, qtok], rhs=kcT[:, g, :], start=True, stop=True),
                     reads=[qTb, kcT], writes=[ps_cs])
            m0 = 120 - 8 * qb
            P.dve(lambda e: e.tensor_tensor(out=sc[:], in0=v3(ps_cs[:]), in1=FT[:, hs, m0:m0 + 128], op=ALU.add), reads=[ps_cs, FT], writes=[sc])
            P.act(lambda e: e.activation(out=sc[:], in_=sc[:], func=AF.Exp), reads=[sc], writes=[sc])
            P.dve(lambda e: e.tensor_reduce(out=ssum[:], in_=sc[:], axis=AX.X, op=ALU.add), reads=[sc], writes=[ssum])
            P.dve(lambda e: e.tensor_scalar(out=ssum[:], in0=ssum[:], scalar1=1e-30, scalar2=None, op0=ALU.max), reads=[ssum], writes=[ssum])
            P.dve(lambda e: e.reciprocal(out=ssum[:], in_=ssum[:]), reads=[ssum], writes=[ssum])
            P.dve(lambda e: e.tensor_tensor(out=pnb[:], in0=sc[:], in1=bc(ssum[:].unsqueeze(2), [128, 4, 128]), op=ALU.mult), reads=[sc, ssum], writes=[pnb])
            yield
            for r in range(R):
                P.pe(lambda e, r=r: e.transpose(out=ps_tr[:, r * 128:(r + 1) * 128], in_=pnb[:, r, :], identity=C.identb[:]), reads=[pnb, C.identb], writes=[ps_tr])
            P.act(lambda e: e.copy(out=pT[:].rearrange("p r q -> p (r q)"), in_=ps_tr[:, 0:512]), reads=[ps_tr], writes=[pT])
            yield
            for r in range(R):
                P.pe(lambda e, r=r: e.matmul(ps_oc_s[:, r * 96:(r + 1) * 96], lhsT=pT[:, r, :], rhs=vcx[:, g, :], start=True, stop=True), reads=[pT, vcx], writes=[ps_oc_s])
            oc4 = ps_oc_s[:, 0:384].rearrange("p (r x) -> p r x", r=4)
            P.dve(lambda e: e.tensor_reduce(out=imp[:], in_=oc4[:, :, 64:96].rearrange("p r j -> p j r"), axis=AX.X, op=ALU.add), reads=[ps_oc_s], writes=[imp])
            P.dve(lambda e: e.tensor_tensor(out=imp[:], in0=imp[:], in1=keep[:, qb, :], op=ALU.mult), reads=[imp, keep], writes=[imp])
            P.dve(lambda e: e.tensor_tensor(out=imp[:], in0=imp[:], in1=addc[:, qb, :], op=ALU.add), reads=[imp, addc], writes=[imp])
            P.dve(lambda e: e.max(out=mx8[:], in_=imp[:]), reads=[imp], writes=[mx8])
            P.dve(lambda e: e.tensor_scalar(out=imp[:], in0=imp[:], scalar1=mx8[:, 7:8], scalar2=None, op0=ALU.is_ge), reads=[imp, mx8], writes=[imp])
            P.dve(lambda e: e.tensor_scalar(out=negm[:], in0=imp[:], scalar1=-1.0, scalar2=NBIG, op0=ALU.add, op1=ALU.mult), reads=[imp], writes=[negm])
            yield
            P.pe(lambda e: e.transpose(out=ps_tr[0:32, 512:640], in_=negm[:], identity=C.identb[:]), reads=[negm, C.identb], writes=[ps_tr])
            P.dve(lambda e: e.tensor_copy(out=negT_s[:], in_=bc(ps_tr[0:32, 512:640].unsqueeze(1), [32, 4, 128])), reads=[ps_tr], writes=[negT_s])

        def phaseB(qb, g, slot, gen):
            nonlocal nsc
            it = 0
            qtok = slice(qb * 128, (qb + 1) * 128)
            hs = slice(4 * g, 4 * g + 4)
            y = yt[qb % 2]
            ps_oc_s = ps_ocs[slot]
            negT_s = negTs[slot]
            oc4 = ps_oc_s[:, 0:384].rearrange("p (r x) -> p r x", r=4)
            kt0 = max(0, qb - 4)
            iters = [("s", kt) for kt in range(qb + 1)] + [("w", kt) for kt in range(kt0, qb + 1)]
            slots = []

            def scores(ii):
                nonlocal nsc
                br, kt = iters[ii]
                ps = ps_sc[nsc % 2]; et = eT[nsc % 2]; nsc += 1
                slots.append((ps, et))
                ktok = slice(kt * 128, (kt + 1) * 128)
                if br == "s":
                    near = kt >= qb - 1
                    P.pe(lambda e: e.matmul(v3(ps[:]), lhsT=ksT[:, g, ktok], rhs=qTb[:, hs, qtok], start=True, stop=False), reads=[ksT, qTb], writes=[ps])
                    P.pe(lambda e: e.matmul(v3(ps[:]), lhsT=Eb[:, ktok], rhs=negT_s[:], start=False, stop=not near), reads=[Eb, negT_s], writes=[ps])
                    if near:
                        Bn = Bn0 if kt == qb else Bn1
                        P.pe(lambda e: e.matmul(v3(ps[:]), lhsT=C.identb[:], rhs=Bn[:, hs, :], start=False, stop=True), reads=[Bn, C.identb], writes=[ps])
                else:
                    dl = qb - kt
                    extra = {0: Bn0[:, hs, :], 1: Bn1[:, hs, :], 4: Mtri[:]}.get(dl)
                    P.pe(lambda e: e.matmul(v3(ps[:]), lhsT=kwT[:, g, ktok], rhs=qTb[:, hs, qtok], start=True, stop=extra is None), reads=[kwT, qTb], writes=[ps])
                    if extra is not None:
                        P.pe(lambda e: e.matmul(v3(ps[:]), lhsT=C.identb[:], rhs=extra, start=False, stop=True), reads=[Bn0, Bn1, Mtri, C.identb], writes=[ps])

            scores(0)
            for ii, (br, kt) in enumerate(iters):
                if ii + 1 < len(iters):
                    scores(ii + 1)
                ps, et = slots[ii]
                P.act(lambda e: e.activation(out=et[:].rearrange("p r q -> p (r q)"), in_=ps[:], func=AF.Exp), reads=[ps], writes=[et])
                if br == "s":
                    for r in range(R):
                        P.pe(lambda e, r=r: e.matmul(ps_os[:, r * 65:(r + 1) * 65], lhsT=et[:, r, :], rhs=vsb[:, kt, g, :], start=(kt == 0 and r == 0), stop=(kt == qb), skip_group_check=True),
                             reads=[et, vsb], writes=[ps_os])
                else:
                    for r in range(R):
                        P.pe(lambda e, r=r: e.matmul(ps_ow[:, r * 65:(r + 1) * 65], lhsT=et[:, r, :], rhs=vwb[:, kt, g, :], start=(kt == kt0 and r == 0), stop=(kt == qb), skip_group_check=True),
                             reads=[et, vwb], writes=[ps_ow])
                it += 1
                if it % 3 == 2 and gen is not None:
                    next(gen, None)
            if gen is not None:
                for _ in gen:
                    pass
            os4 = ps_os[:, 0:260].rearrange("p (r x) -> p r x", r=4)
            ow4 = ps_ow[:, 0:260].rearrange("p (r x) -> p r x", r=4)
            g3 = gts[:, qb, 12 * g:12 * g + 12].rearrange("p (r b) -> p b r", b=3)
            P.dve(lambda e: e.reciprocal(out=cs[:, 1, :], in_=os4[:, :, 64]), reads=[ps_os], writes=[cs])
            P.dve(lambda e: e.reciprocal(out=cs[:, 2, :], in_=ow4[:, :, 64]), reads=[ps_ow], writes=[cs])
            P.dve(lambda e: e.memset(cs[:, 0, :], 1.0), writes=[cs])
            P.dve(lambda e: e.tensor_tensor(out=cs[:], in0=cs[:], in1=g3, op=ALU.mult), reads=[cs, gts], writes=[cs])
            P.dve(lambda e: e.tensor_tensor(out=ya[:], in0=oc4[:, :, 0:64], in1=bc(cs[:, 0, :].unsqueeze(2), [128, 4, 64]), op=ALU.mult), reads=[ps_oc_s, cs], writes=[ya])
            P.dve(lambda e: e.tensor_tensor(out=yb[:], in0=os4[:, :, 0:64], in1=bc(cs[:, 1, :].unsqueeze(2), [128, 4, 64]), op=ALU.mult), reads=[ps_os, cs], writes=[yb])
            P.pool(lambda e: e.tensor_tensor(out=ya[:], in0=ya[:], in1=yb[:], op=ALU.add), reads=[ya, yb], writes=[ya])
            P.dve(lambda e: e.tensor_tensor(out=yb[:], in0=ow4[:, :, 0:64], in1=bc(cs[:, 2, :].unsqueeze(2), [128, 4, 64]), op=ALU.mult), reads=[ps_ow, cs], writes=[yb])
            P.pool(lambda e: e.tensor_tensor(out=y[:, g * 256:(g + 1) * 256].rearrange("p (r d) -> p r d", r=4), in0=ya[:], in1=yb[:], op=ALU.add), reads=[ya, yb], writes=[y])

        blocks = [(qb, g) for qb in range(NT) for g in range(G)]
        for _ in phaseA(blocks[0][0], blocks[0][1], 0):
            pass
        for bi, (qb, g) in enumerate(blocks):
            gen = phaseA(blocks[bi + 1][0], blocks[bi + 1][1], (bi + 1) % 2) if bi + 1 < len(blocks) else None
            if gen is not None:
                next(gen, None)
            phaseB(qb, g, bi % 2, gen)
            if g == G - 1:
                qtok = slice(qb * 128, (qb + 1) * 128)
                y = yt[qb % 2]
                P.dma("sp", ymix.t[qtok, 0:512], y[:], reads=[y], writes=[ymix])
                bg_tick(P, 4)
                dbg(2 + qb)
        bg_end(P)


WIN_OFF = {}
_o = 0
for (_c0, _n, _r0) in PT_GROUPS:
    WIN_OFF[("T", _c0)] = _o; _o += 16 * _n
for (_c0, _n, _r0) in PN_GROUPS:
    WIN_OFF[("N", _c0)] = _o; _o += 16 * _n
WIN_TOTAL = _o
WOUT_TOTAL = 4 * 16 * 512
W1_TOTAL = 32 * 16 * 256
W2_TOTAL = 4 * 64 * 512


class Background:
    def __init__(self, P, prm, l, wsc):
        self.P = P
        self.jobs = []
        self.loaded = self.cast = self.stored = 0
        self.bufs = None

        def add(src3, srcbuf, nk, nc_, dst, off):
            if nk * nc_ > 4096:
                h = nk // 2
                add(src3[:, 0:h, :], srcbuf, h, nc_, dst, off)
                add(src3[:, h:nk, :], srcbuf, nk - h, nc_, dst, off + h * nc_)
            else:
                self.jobs.append((src3, srcbuf, nk, nc_, dst, off))

        wv = prm["w_in"].t[l].rearrange("(k p) n -> p k n", p=128)
        for (c0, nc_, r0) in PT_GROUPS:
            add(wv[:, :, c0:c0 + nc_], prm["w_in"], 16, nc_, wsc["win"], WIN_OFF[("T", c0)])
        for (c0, nc_, o0) in PN_GROUPS:
            add(wv[:, :, c0:c0 + nc_], prm["w_in"], 16, nc_, wsc["win"], WIN_OFF[("N", c0)])
        wv = prm["w_out"].t[l].rearrange("(k p) n -> p k n", p=128)
        for ct in range(4):
            add(wv[:, :, ct * 512:(ct + 1) * 512], prm["w_out"], 16, 512, wsc["wout"], ct * 8192)
        wv = prm["mlp_w1"].t[l].rearrange("(k p) n -> p k n", p=128)
        for hp in range(32):
            add(wv[:, :, hp * 256:(hp + 1) * 256], prm["mlp_w1"], 16, 256, wsc["w1"], hp * 4096)
        wv = prm["mlp_w2"].t[l].rearrange("(k p) n -> p k n", p=128)
        for ct in range(4):
            for kg in range(8):
                add(wv[:, kg * 8:(kg + 1) * 8, ct * 512:(ct + 1) * 512], prm["mlp_w2"], 8, 512, wsc["w2"], (ct * 8 + kg) * 4096)

    def alloc(self):
        P = self.P
        self.bufs = dict(f=[P.sbuf(f"bg_f{i}", [128, 4096]) for i in range(2)], b=[P.sbuf(f"bg_b{i}", [128, 4096], BF16) for i in range(2)])

    def done(self):
        return self.stored >= len(self.jobs)

    def step(self, load=True):
        if self.bufs is None:
            return
        P = self.P
        f, b_ = self.bufs["f"], self.bufs["b"]
        if self.stored < self.cast:
            j = self.stored
            src3, srcbuf, nk, nc_, dst, off = self.jobs[j]
            tot = nk * nc_
            P.dma("sp", dst.t[:, off:off + tot], b_[j % 2][:, 0:tot], reads=[b_[j % 2]], writes=[dst])
            self.stored += 1
        if self.cast < self.loaded:
            j = self.cast
            tot = self.jobs[j][2] * self.jobs[j][3]
            P.pool(lambda e: e.tensor_copy(out=b_[j % 2][:, 0:tot], in_=f[j % 2][:, 0:tot]), reads=[f[j % 2]], writes=[b_[j % 2]])
            self.cast += 1
        if load and self.loaded < len(self.jobs):
            j = self.loaded
            src3, srcbuf, nk, nc_, dst, off = self.jobs[j]
            P.dma("sp", f[j % 2][:, 0:nk * nc_].rearrange("p (k c) -> p k c", k=nk), src3, reads=[srcbuf], writes=[f[j % 2]])
            self.loaded += 1

    def flush(self):
        while self.stored < self.loaded:
            self.step(load=False)

    def release(self):
        self.flush()
        self.bufs = None


def bg_begin(P):
    if getattr(P, "bg", None) is not None and not P.bg.done():
        P.bg.alloc()


def bg_tick(P, n=1):
    if getattr(P, "bg", None) is not None:
        for _ in range(n):
            P.bg.step()


def bg_end(P):
    if getattr(P, "bg", None) is not None and P.bg.bufs is not None:
        P.bg.release()


def stage_convert_all(P, bg):
    if bg is None or bg.done():
        return
    with P.scope():
        bg.alloc()
        while bg.loaded < len(bg.jobs):
            bg.step()
        bg.release()


def stage_mod(P, C, cT, ada_w, ada_b, modT, gsc, nlayers):
    with P.scope():
        cf = [C.f]
        ca = P.sbuf("mod_ca", [128, 16, BPC])
        P.dma("sp", ca[:], cT.t[:, :, :], reads=[cT], writes=[ca])
        P.act(lambda e: e.activation(out=ca[:], in_=ca[:], func=AF.Silu), reads=[ca], writes=[ca])
        wst = [P.sbuf(f"mod_w{i}", [128, 16, 512]) for i in range(2)]
        brow = [P.sbuf(f"mod_b{i}", [1, 512]) for i in range(2)]
        grow = [P.sbuf(f"mod_g{i}", [BPC, 512]) for i in range(2)]
        ps_f = P.psum("mod_psf", [128, 512])
        ps_g = [P.psum(f"mod_psg{i}", [BPC, 512]) for i in range(2)]
        bg_begin(P)
        n = 0
        for l in range(nlayers):
            wv = ada_w.t[l].rearrange("(k p) n -> p k n", p=128)
            for seg in range(6):
                for ct in range(4):
                    w = wst[n % 2]; br = brow[n % 2]
                    c0 = seg * 2048 + ct * 512
                    P.dma("sp", w[:], wv[:, :, c0:c0 + 512], reads=[ada_w], writes=[w])
                    P.dma("sp", br[:], ada_b.t[l:l + 1, c0:c0 + 512], reads=[ada_b], writes=[br])
                    if seg in (2, 5):
                        pg = ps_g[n % 2]; gr = grow[n % 2]
                        for k in range(16):
                            P.pe(lambda e, k=k, w=w, pg=pg: e.matmul(pg[:], lhsT=ca[:, k, :], rhs=w[:, k, :], start=(k == 0), stop=False), reads=[ca, w], writes=[pg])
                        P.pe(lambda e, br=br, pg=pg: e.matmul(pg[:], lhsT=C.c("ones", 1)[:, 0:BPC], rhs=br[:], start=False, stop=True), reads=[br] + cf, writes=[pg])
                        P.act(lambda e, pg=pg, gr=gr: e.copy(out=gr[:], in_=pg[:]), reads=[pg], writes=[gr])
                        P.dma("sp", gsc.t[l, 0 if seg == 2 else 1, :, ct * 512:(ct + 1) * 512], gr[:], reads=[gr], writes=[gsc])
                    else:
                        si = {0: 0, 1: 1, 3: 2, 4: 3}[seg]
                        pg = ps_g[n % 2]; gr = grow[n % 2]
                        for k in range(16):
                            P.pe(lambda e, k=k, w=w, pg=pg: e.matmul(pg[:], lhsT=ca[:, k, :], rhs=w[:, k, :], start=(k == 0), stop=False), reads=[ca, w], writes=[pg])
                        P.pe(lambda e, br=br, pg=pg: e.matmul(pg[:], lhsT=C.c("ones", 1)[:, 0:BPC], rhs=br[:], start=False, stop=True), reads=[br] + cf, writes=[pg])
                        P.act(lambda e, pg=pg, gr=gr: e.copy(out=gr[:], in_=pg[:]), reads=[pg], writes=[gr])
                        for cc in range(4):
                            col = ((si * 16) + ct * 4 + cc) * BPC
                            P.pe(lambda e, gr=gr, cc=cc, col=col: e.transpose(out=ps_f[:, col:col + BPC], in_=gr[:, cc * 128:(cc + 1) * 128], identity=C.c("ident", BPC)[:, 0:BPC]),
                                 reads=[gr] + cf, writes=[ps_f])
                    n += 1
                    bg_tick(P, 2)
            P.dve(lambda e, l=l: e.tensor_copy(out=modT[:, l].rearrange("p s k b -> p (s k b)"), in_=ps_f[:, 0:4 * 16 * BPC]), reads=[ps_f], writes=[modT])
        bg_end(P)


def to_featmajor(P, C, src, src_ap_fn, ntt, hT, norm, scl=None, shf=None, pools=None):
    xt, xb, ss, ps_tr, eps = pools["xt"], pools["xb"], pools["ss"], pools["ps_tr"], pools["eps"]

    def prep(tt):
        x = xt[tt % 2]; xn = xb[tt % 2]; s1 = ss[tt % 2]
        P.dma("sp", x[:], src_ap_fn(tt), reads=[src], writes=[x])
        if norm:
            P.pool(lambda e: e.memset(s1[:], 0.0), writes=[s1])
            P.act(lambda e: e.activation(out=xn[:], in_=x[:], func=AF.Square, accum_out=s1[:, 0:1]), reads=[x, s1], writes=[xn, s1])
            P.act(lambda e: e.activation(out=s1[:, 0:1], in_=s1[:, 0:1], func=AF.Sqrt, scale=1.0 / D_MODEL, bias=eps[:, 0:1]), reads=[s1, eps], writes=[s1])
            P.dve(lambda e: e.reciprocal(out=s1[:, 0:1], in_=s1[:, 0:1]), reads=[s1], writes=[s1])
            P.dve(lambda e: e.tensor_scalar(out=xn[:], in0=x[:], scalar1=s1[:, 0:1], scalar2=None, op0=ALU.mult), reads=[x, s1], writes=[xn])
        else:
            P.pool(lambda e: e.tensor_copy(out=xn[:], in_=x[:]), reads=[x], writes=[xn])

    def trans(tt):
        xn = xb[tt % 2]
        for half in range(2):
            pt = ps_tr[(2 * tt + half) % len(ps_tr)]
            for kk in range(8):
                k = half * 8 + kk
                P.pe(lambda e, k=k, kk=kk: e.transpose(out=pt[:, kk * 128:(kk + 1) * 128], in_=xn[:, k * 128:(k + 1) * 128], identity=C.identb[:]),
                     reads=[xn, C.identb], writes=[pt])
            dst = hT[:, half * 8:half * 8 + 8, tt * 128:(tt + 1) * 128]
            src3 = pt[:].rearrange("p (k t) -> p k t", k=8)
            if scl is None:
                P.act(lambda e: e.copy(out=dst, in_=src3), reads=[pt], writes=[hT])
            else:
                tm = pools["tm"][(2 * tt + half) % 2]
                P.dve(lambda e: e.tensor_tensor(out=tm[:], in0=src3, in1=bc(scl[:, half * 8:half * 8 + 8].unsqueeze(2), [128, 8, 128]), op=ALU.mult),
                      reads=[pt] + pools["affb"], writes=[tm])
                P.pool(lambda e: e.tensor_tensor(out=dst, in0=tm[:], in1=bc(shf[:, half * 8:half * 8 + 8].unsqueeze(2), [128, 8, 128]), op=ALU.add),
                       reads=[tm] + pools["affb"], writes=[hT])

    prep(0)
    for tt in range(ntt):
        if tt + 1 < ntt:
            prep(tt + 1)
        trans(tt)


def fm_pools(P, affine):
    d = dict(xt=[P.sbuf(f"fm_xt{i}", [128, D_MODEL]) for i in range(2)],
             xb=[P.sbuf(f"fm_xb{i}", [128, D_MODEL], BF16) for i in range(2)],
             ss=[P.sbuf(f"fm_ss{i}", [128, 1]) for i in range(2)],
             ps_tr=[P.psum(f"fm_pst{i}", [128, 1024], BF16) for i in range(2)],
             eps=P.sbuf("fm_eps", [128, 1]))
    P.pool(lambda e: e.memset(d["eps"][:], EPS), writes=[d["eps"]])
    if affine:
        d["tm"] = [P.sbuf(f"fm_tm{i}", [128, 8, 128]) for i in range(2)]
    return d


def affine_vecs(P, modT, l, b, which, nw, scl, shf):
    P.dve(lambda e: e.tensor_scalar(out=scl[:], in0=modT[:, l, 2 * which + 1, :, b], scalar1=1.0, scalar2=None, op0=ALU.add), reads=[modT], writes=[scl])
    P.dve(lambda e: e.tensor_tensor(out=scl[:], in0=scl[:], in1=nw, op=ALU.mult), reads=[scl], writes=[scl])
    P.dve(lambda e: e.tensor_copy(out=shf[:], in_=modT[:, l, 2 * which, :, b]), reads=[modT], writes=[shf])


def stage_inproj(P, C, xsrc, xsrc_fn, modT, nw1T, win, projT, projN, l, b):
    with P.scope():
        hT = P.sbuf("ip_hT", [128, 16, SEQ], BF16)
        scl = P.sbuf("ip_scl", [128, 16]); shf = P.sbuf("ip_shf", [128, 16])
        affine_vecs(P, modT, l, b, 0, nw1T[:, l, :], scl, shf)
        with P.scope():
            pools = fm_pools(P, True)
            pools["affb"] = [scl, shf]
            to_featmajor(P, C, xsrc, xsrc_fn, NT, hT, True, scl[:], shf[:], pools)
        NWB = 3
        wb = [P.sbuf(f"ip_wb{i}", [128, 16, 512], BF16) for i in range(NWB)]
        ev = [P.sbuf(f"ip_ev{i}", [128, 2048]) for i in range(2)]
        ps = [P.psum(f"ip_ps{i}", [128, 512]) for i in range(4)]
        groups = [("T",) + g for g in PT_GROUPS] + [("N",) + g for g in PN_GROUPS]

        def wload(i):
            kind, c0, nc_, _ = groups[i]
            off = WIN_OFF[(kind, c0)]
            wbb = wb[i % NWB]
            P.dma("sp", wbb[:, :, 0:nc_], win.t[:, off:off + 16 * nc_].rearrange("p (k c) -> p k c", k=16), reads=[win], writes=[wbb])

        npp = 0
        nev = 0
        wload(0)
        wload(1)
        for gi, (kind, c0, nc_, dst0) in enumerate(groups):
            if gi + 2 < len(groups):
                wload(gi + 2)
            wbb = wb[gi % NWB]
            if kind == "T":
                e_ = ev[nev % 2]; nev += 1
                for tq in range(4):
                    p_ = ps[npp % 4]; npp += 1
                    for k in range(16):
                        P.pe(lambda e, k=k, p_=p_, wbb=wbb, nc_=nc_, tq=tq: e.matmul(p_[0:nc_, :], lhsT=wbb[:, k, 0:nc_], rhs=hT[:, k, tq * 512:(tq + 1) * 512], start=(k == 0), stop=(k == 15)),
                             reads=[wbb, hT], writes=[p_])
                    if tq % 2 == 0:
                        P.act(lambda e, p_=p_, e_=e_, nc_=nc_, tq=tq: e.copy(out=e_[0:nc_, tq * 512:(tq + 1) * 512], in_=p_[0:nc_, :]), reads=[p_], writes=[e_])
                    else:
                        P.dve(lambda e, p_=p_, e_=e_, nc_=nc_, tq=tq: e.tensor_copy(out=e_[0:nc_, tq * 512:(tq + 1) * 512], in_=p_[0:nc_, :]), reads=[p_], writes=[e_])
                P.dma("sp", projT.t[dst0:dst0 + nc_, :], e_[0:nc_, :], reads=[e_], writes=[projT])
            else:
                for t4 in range(4):
                    e_ = ev[nev % 2]; nev += 1
                    for ti in range(4):
                        tt = t4 * 4 + ti
                        p_ = ps[npp % 4]; npp += 1
                        for k in range(16):
                            P.pe(lambda e, k=k, p_=p_, wbb=wbb, nc_=nc_, tt=tt: e.matmul(p_[:, 0:nc_], lhsT=hT[:, k, tt * 128:(tt + 1) * 128], rhs=wbb[:, k, 0:nc_], start=(k == 0), stop=(k == 15)),
                                 reads=[wbb, hT], writes=[p_])
                        if ti % 2 == 0:
                            P.act(lambda e, p_=p_, e_=e_, nc_=nc_, ti=ti: e.copy(out=e_[:, ti * 512:ti * 512 + nc_], in_=p_[:, 0:nc_]), reads=[p_], writes=[e_])
                        else:
                            P.dve(lambda e, p_=p_, e_=e_, nc_=nc_, ti=ti: e.tensor_copy(out=e_[:, ti * 512:ti * 512 + nc_], in_=p_[:, 0:nc_]), reads=[p_], writes=[e_])
                    P.dma("sp", projN.t[t4 * 512:(t4 + 1) * 512, dst0:dst0 + nc_].rearrange("(i p) c -> p i c", p=128),
                          e_[:].rearrange("p (i c) -> p i c", i=4)[:, :, 0:nc_], reads=[e_], writes=[projN])


def stage_outproj(P, C, ymix, xsrc, xsrc_fn, xdst, xdst_fn, gsc, wout, l, b):
    with P.scope():
        yT = P.sbuf("op_yT", [128, 16, SEQ], BF16)
        with P.scope():
            pools = fm_pools(P, False)
            to_featmajor(P, C, ymix, lambda tt: ymix.t[tt * 128:(tt + 1) * 128, :], NT, yT, False, None, None, pools)
        wob = P.sbuf("op_wob", [128, 16, D_MODEL], BF16)
        gb = P.sbuf("op_gb", [128, D_MODEL])
        P.dma("sp", gb[:], gsc.t[l, 0, b:b + 1, :].partition_broadcast(128), reads=[gsc], writes=[gb])
        for ct in range(4):
            P.dma("sp", wob[:, :, ct * 512:(ct + 1) * 512], wout.t[:, ct * 8192:(ct + 1) * 8192].rearrange("p (k c) -> p k c", k=16), reads=[wout], writes=[wob])
        xt = [P.sbuf(f"op_xt{i}", [128, D_MODEL]) for i in range(2)]
        xo = [P.sbuf(f"op_xo{i}", [128, D_MODEL]) for i in range(2)]
        ps = [P.psum(f"op_ps{i}", [128, 512]) for i in range(8)]
        for tt in range(NT):
            x = xt[tt % 2]; o = xo[tt % 2]
            P.dma("sp", x[:], xsrc_fn(tt), reads=[xsrc], writes=[x])
            for ct in range(4):
                p_ = ps[(tt * 4 + ct) % 8]
                for k in range(16):
                    P.pe(lambda e, k=k, p_=p_, ct=ct, tt=tt: e.matmul(p_[:], lhsT=yT[:, k, tt * 128:(tt + 1) * 128], rhs=wob[:, k, ct * 512:(ct + 1) * 512], start=(k == 0), stop=(k == 15)),
                         reads=[yT, wob], writes=[p_])
                cs = slice(ct * 512, (ct + 1) * 512)
                P.dve(lambda e, p_=p_, o=o, cs=cs: e.tensor_tensor(out=o[:, cs], in0=p_[:], in1=gb[:, cs], op=ALU.mult), reads=[p_, gb], writes=[o])
                P.pool(lambda e, o=o, x=x, cs=cs: e.tensor_tensor(out=o[:, cs], in0=o[:, cs], in1=x[:, cs], op=ALU.add), reads=[o, x], writes=[o])
            P.dma("sp", xdst_fn(tt), o[:], reads=[o], writes=[xdst])


def stage_mlp(P, C, xsrc, xsrc_fn, xdst, xdst_fn, modT, nw2T, gsc, w1s, w2s, l, b):
    HC = 64
    with P.scope():
        scl = P.sbuf("ml_scl", [128, 16]); shf = P.sbuf("ml_shf", [128, 16])
        affine_vecs(P, modT, l, b, 1, nw2T[:, l, :], scl, shf)
        gb = P.sbuf("ml_gb", [128, D_MODEL])
        P.dma("sp", gb[:], gsc.t[l, 1, b:b + 1, :].partition_broadcast(128), reads=[gsc], writes=[gb])
        hTs = [P.sbuf(f"ml_hT{i}", [128, 16, 512], BF16) for i in range(2)]
        uT = P.sbuf("ml_uT", [128, HC, 512], BF16)
        pools = fm_pools(P, True)
        pools["affb"] = [scl, shf]
        NWB = 4
        wbuf = [P.sbuf(f"ml_wb{i}", [128, 4096], BF16) for i in range(NWB)]
        rl = [P.sbuf(f"ml_rl{i}", [128, 512]) for i in range(2)]
        xo = [P.sbuf(f"ml_xo{i}", [128, 1024]) for i in range(2)]
        ps = [P.psum(f"ml_ps{i}", [128, 512]) for i in range(6)]
        NTILE = 64
        total = (SEQ // 512) * NTILE
        state = {"n": 0}

        def wload(j):
            jj = j % NTILE
            wbb = wbuf[j % NWB]
            if jj < 32:
                P.dma("sp", wbb[:], w1s.t[:, jj * 4096:(jj + 1) * 4096], reads=[w1s], writes=[wbb])
            else:
                P.dma("sp", wbb[:], w2s.t[:, (jj - 32) * 4096:(jj - 31) * 4096], reads=[w2s], writes=[wbb])

        PRE = 3
        for j in range(PRE):
            wload(j)
        nps = 0
        j = 0
        NT5 = SEQ // 512
        to_featmajor(P, C, xsrc, lambda tt: xsrc_fn(tt), 4, hTs[0], True, scl[:], shf[:], pools)
        for t5 in range(NT5):
            hT = hTs[t5 % 2]
            for hp in range(32):
                if j + PRE < total:
                    wload(j + PRE)
                wbb = wbuf[j % NWB]; j += 1
                w3 = wbb[:].rearrange("p (k c) -> p k c", k=16)
                for cc in range(2):
                    hc = hp * 2 + cc
                    p_ = ps[nps % 6]; nps += 1
                    r_ = rl[hc % 2]
                    for k in range(16):
                        P.pe(lambda e, k=k, p_=p_, w3=w3, cc=cc: e.matmul(p_[:], lhsT=w3[:, k, cc * 128:(cc + 1) * 128], rhs=hT[:, k, :], start=(k == 0), stop=(k == 15)),
                             reads=[wbb, hT], writes=[p_])
                    P.act(lambda e, p_=p_, r_=r_: e.activation(out=r_[:], in_=p_[:], func=AF.Relu), reads=[p_], writes=[r_])
                    P.dve(lambda e, r_=r_, hc=hc: e.tensor_tensor(out=uT[:, hc, :], in0=r_[:], in1=r_[:], op=ALU.mult), reads=[r_], writes=[uT])
            if t5 + 1 < NT5:
                to_featmajor(P, C, xsrc, lambda tt, t5=t5: xsrc_fn((t5 + 1) * 4 + tt), 4, hTs[(t5 + 1) % 2], True, scl[:], shf[:], pools)
            for ct in range(4):
                pa = [ps[(nps + i) % 6] for i in range(4)]
                nps += 4
                for kg in range(8):
                    if j + PRE < total:
                        wload(j + PRE)
                    wbb = wbuf[j % NWB]; j += 1
                    w3 = wbb[:].rearrange("p (k c) -> p k c", k=8)
                    for kk in range(8):
                        k = kg * 8 + kk
                        for ti in range(4):
                            P.pe(lambda e, k=k, kk=kk, ti=ti, w3=w3, pa=pa: e.matmul(pa[ti][:], lhsT=uT[:, k, ti * 128:(ti + 1) * 128], rhs=w3[:, kk, :], start=(k == 0), stop=(k == HC - 1)),
                                 reads=[uT, wbb], writes=[pa[ti]])
                for ti in range(4):
                    tt = t5 * 4 + ti
                    o = xo[(ct * 4 + ti) % 2]
                    P.dma("sp", o[:, 512:1024], xsrc_fn(tt)[:, ct * 512:(ct + 1) * 512], reads=[xsrc], writes=[o])
                    P.dve(lambda e, o=o, ti=ti, pa=pa, ct=ct: e.tensor_tensor(out=o[:, 0:512], in0=pa[ti][:], in1=gb[:, ct * 512:(ct + 1) * 512], op=ALU.mult),
                          reads=[pa[ti], gb], writes=[o])
                    P.pool(lambda e, o=o: e.tensor_tensor(out=o[:, 0:512], in0=o[:, 0:512], in1=o[:, 512:1024], op=ALU.add), reads=[o], writes=[o])
                    P.dma("sp", xdst_fn(tt)[:, ct * 512:(ct + 1) * 512], o[:, 0:512], reads=[o], writes=[xdst])


def stage_final(P, C, xsrc, xsrc_fn, fnw, out, out_fn, nseq):
    with P.scope():
        nwb = P.sbuf("fn_nwb", [128, D_MODEL])
        P.dma("sp", nwb[:], fnw.t[0:1, :].partition_broadcast(128), reads=[fnw], writes=[nwb])
        eps = P.sbuf("fn_eps", [128, 1])
        P.pool(lambda e: e.memset(eps[:], EPS), writes=[eps])
        xt = [P.sbuf(f"fn_xt{i}", [128, D_MODEL]) for i in range(2)]
        sq = [P.sbuf(f"fn_sq{i}", [128, D_MODEL]) for i in range(2)]
        ss = [P.sbuf(f"fn_ss{i}", [128, 1]) for i in range(2)]
        for i in range(nseq * NT):
            x = xt[i % 2]; q = sq[i % 2]; s1 = ss[i % 2]
            P.dma("sp", x[:], xsrc_fn(i), reads=[xsrc], writes=[x])
            P.pool(lambda e, s1=s1: e.memset(s1[:], 0.0), writes=[s1])
            P.act(lambda e, x=x, q=q, s1=s1: e.activation(out=q[:], in_=x[:], func=AF.Square, accum_out=s1[:, 0:1]), reads=[x, s1], writes=[q, s1])
            P.act(lambda e, s1=s1: e.activation(out=s1[:, 0:1], in_=s1[:, 0:1], func=AF.Sqrt, scale=1.0 / D_MODEL, bias=eps[:, 0:1]), reads=[s1, eps], writes=[s1])
            P.dve(lambda e, s1=s1: e.reciprocal(out=s1[:, 0:1], in_=s1[:, 0:1]), reads=[s1], writes=[s1])
            P.dve(lambda e, x=x, q=q, s1=s1: e.scalar_tensor_tensor(out=q[:], in0=x[:], scalar=s1[:, 0:1], in1=nwb[:], op0=ALU.mult, op1=ALU.mult), reads=[x, s1, nwb], writes=[q])
            P.dma("sp", out_fn(i), q[:], reads=[q], writes=[out])


SMALL_PARAMS = ["gla_gate_w2", "gla_gate_b", "gla_norm_w", "ssd_conv_w", "ssd_conv_b", "ssd_dt_bias", "ssd_a_log", "ssd_d", "ssd_norm_w",
                "gdn_conv_w", "gdn_dt_bias", "gdn_a_log", "gdn_norm_w", "nsa_cmp_pos", "nsa_cmp_w1", "nsa_cmp_w2"]
BIG_PARAMS = ["ada_w", "ada_b", "w_in", "w_out", "mlp_w1", "mlp_w2"]


def build(nlayers=DEPTH, nseq=BPC, shapes=None):
    nc = bass.Bass("TRN2", target_bir_lowering=False)
    st = ExitStack()
    with st:
        P = Prog(nc, st)

        def ext(name, shape):
            return Buf(nc.dram_tensor(name, list(shape), F32, kind="ExternalInput").ap(), name)

        x = ext("x", [nseq, SEQ, D_MODEL])
        cT = ext("cT", [128, 16, BPC])
        prm = {k: ext(k, shapes[k]) for k in SMALL_PARAMS + BIG_PARAMS + ["nw1T", "nw2T", "fnw", "nsa_tab", "nsa_t31", "nsa_cst", "cst"]}
        out = Buf(nc.dram_tensor("out", [nseq, SEQ, D_MODEL], F32, kind="ExternalOutput").ap(), "out")
        xres = P.dram("xres", [nseq, SEQ, D_MODEL])
        projT = P.dram("projT", [PT_ROWS, SEQ])
        projN = P.dram("projN", [SEQ, PN_COLS])
        ymix = P.dram("ymix", [SEQ, D_MODEL])
        gsc = P.dram("gsc", [nlayers, 2, BPC, D_MODEL])
        C = Consts(P, prm["cst"])
        modT = P.sbuf("modT", [128, nlayers, 4, 16, BPC])
        nw1T = P.sbuf("nw1T", [128, shapes["nw1T"][1], 16])
        nw2T = P.sbuf("nw2T", [128, shapes["nw2T"][1], 16])
        P.dma("sp", nw1T[:], prm["nw1T"].t[:, :, :], reads=[prm["nw1T"]], writes=[nw1T])
        P.dma("sp", nw2T[:], prm["nw2T"].t[:, :, :], reads=[prm["nw2T"]], writes=[nw2T])
        P.bg = None
        wsc = dict(win=P.dram("wsc_win", [128, WIN_TOTAL], BF16), wout=P.dram("wsc_wout", [128, WOUT_TOTAL], BF16),
                   w1=P.dram("wsc_w1", [128, W1_TOTAL], BF16), w2=P.dram("wsc_w2", [128, W2_TOTAL], BF16))
        wscs = [wsc, dict(win=P.dram("wsc_win2", [128, WIN_TOTAL], BF16), wout=P.dram("wsc_wout2", [128, WOUT_TOTAL], BF16),
                          w1=P.dram("wsc_w12", [128, W1_TOTAL], BF16), w2=P.dram("wsc_w22", [128, W2_TOTAL], BF16))]
        P.bg = Background(P, prm, 0, wscs[0])
        stage_mod(P, C, cT, prm["ada_w"], prm["ada_b"], modT, gsc, nlayers)
        stage_convert_all(P, P.bg)
        P.bg = None
        for l in range(nlayers):
            wsc = wscs[l % 2]
            for b in range(nseq):
                if l == 0:
                    xs, xs_fn = x, (lambda tt, b=b: x.t[b, tt * 128:(tt + 1) * 128, :])
                else:
                    xs, xs_fn = xres, (lambda tt, b=b: xres.t[b, tt * 128:(tt + 1) * 128, :])
                xr_fn = (lambda tt, b=b: xres.t[b, tt * 128:(tt + 1) * 128, :])
                stage_inproj(P, C, xs, xs_fn, modT, nw1T, wsc["win"], projT, projN, l, b)
                if b == nseq - 1 and l + 1 < nlayers:
                    P.bg = Background(P, prm, l + 1, wscs[(l + 1) % 2])
                stage_nsa(P, C, projT, projN, ymix, prm, l)
                stage_ssd(P, C, projT, projN, ymix, prm, l)
                stage_gdn(P, C, projT, projN, ymix, prm, l)
                stage_gla(P, C, projT, projN, ymix, prm, l)
                if P.bg is not None:
                    stage_convert_all(P, P.bg)
                    P.bg = None
                stage_outproj(P, C, ymix, xs, xs_fn, xres, xr_fn, gsc, wsc["wout"], l, b)
                stage_mlp(P, C, xres, xr_fn, xres, xr_fn, modT, nw2T, gsc, wsc["w1"], wsc["w2"], l, b)
        stage_final(P, C, xres, lambda i: xres.t[i // NT, (i % NT) * 128:(i % NT + 1) * 128, :], prm["fnw"],
                    out, lambda i: out.t[i // NT, (i % NT) * 128:(i % NT + 1) * 128, :], nseq)
        P.finish()
        ninstr = P.ninstr
    return nc, ninstr


def host_inputs(inputs, nlayers=DEPTH):
    d = {}
    for k in SMALL_PARAMS:
        d[k] = host_param(k, inputs[k][:nlayers])
    for k in BIG_PARAMS:
        d[k] = np.ascontiguousarray(np.asarray(inputs[k][:nlayers], np.float32))
    d["nw1T"] = host_param("norm1_w", inputs["norm1_w"][:nlayers])
    d["nw2T"] = host_param("norm2_w", inputs["norm2_w"][:nlayers])
    d["fnw"] = host_param("final_norm_w", inputs["final_norm_w"])
    d["nsa_tab"], d["nsa_t31"] = nsa_host_tables(inputs["rel_bias"])
    d["nsa_cst"] = NSA_CST_NP
    d["cst"] = CST_NP
    return d


def core_inputs(inputs, shared, core, nseq=BPC):
    xs = np.ascontiguousarray(np.asarray(inputs["x"][core * BPC:core * BPC + nseq], np.float32))
    c = np.asarray(inputs["c"][core * BPC:(core + 1) * BPC], np.float32)
    cT = np.ascontiguousarray(c.T.reshape(16, 128, BPC).transpose(1, 0, 2))
    m = dict(shared)
    m["x"] = xs
    m["cT"] = cT
    return m


_CACHE = {}


def kernel(**inputs):
    shared = host_inputs(inputs)
    shapes = {k: v.shape for k, v in shared.items()}
    if "nc" not in _CACHE:
        _CACHE["nc"] = build(DEPTH, BPC, shapes)[0]
    nc = _CACHE["nc"]
    in_maps = [core_inputs(inputs, shared, c) for c in range(NCORES)]
    res = run_bass_kernel_spmd(nc, in_maps, core_ids=list(range(NCORES)))
    out = np.concatenate([r["out"] for r in res.results], axis=0)
    return out.astype(np.float32)
```
